# Optimizing a Trainium2 kernel written in Bass

```python
import math
import jax, jax.numpy as jnp
from jax import lax
import numpy as np

D_MODEL = 2048
BATCH = 8
SEQ = 2048
DEPTH = 1

D_SSM = 1024
SSM_GROUP = 16
N_SSM_GROUPS = D_SSM // SSM_GROUP
SSM_STATE = 64
DT_MIN = 1e-3
DT_MAX = 1e-1
N_HEADS = 8
HEAD_DIM = 128
D_ATT = N_HEADS * HEAD_DIM
Q_BLOCK = 128
N_BRANCHES = 2
D_IN_PROJ = D_SSM + 3 * D_ATT + N_BRANCHES * D_MODEL
N_EXPERT_GROUPS = 4
EXPERTS_PER_GROUP = 8
N_EXPERTS = N_EXPERT_GROUPS * EXPERTS_PER_GROUP
TOP_K_IN_GROUP = 2
D_FF_EXPERT = 512
EPS = 1e-6

kernel_name = "hybrid_s5_stickbreaking_hmoe_block"


def rms_norm(x, g):
    x32 = x.astype(jnp.float32)
    y = x32 * lax.rsqrt(jnp.mean(x32 * x32, axis=-1, keepdims=True) + EPS)
    return (y * g.astype(jnp.float32)).astype(x.dtype)


def _cmul(ar, ai, br, bi):
    return ar * br - ai * bi, ar * bi + ai * br


def s5_mixer(u, lambda_re, lambda_im, log_dt, b_re, b_im, c_re, c_im, d_skip, w_glu):
    bsz, seq_len, _ = u.shape
    u32 = u.astype(jnp.float32).reshape(bsz, seq_len, N_SSM_GROUPS, SSM_GROUP)
    dt = jnp.exp(log_dt.astype(jnp.float32))[:, None]
    lr = lambda_re.astype(jnp.float32)
    li = lambda_im.astype(jnp.float32)
    mag = jnp.exp(lr * dt)
    abar_re, abar_im = mag * jnp.cos(li * dt), mag * jnp.sin(li * dt)
    nr, ni = abar_re - 1.0, abar_im
    den = lr * lr + li * li
    coef_re = (nr * lr + ni * li) / den
    coef_im = (ni * lr - nr * li) / den
    bbar_re, bbar_im = _cmul(coef_re[..., None], coef_im[..., None],
                             b_re.astype(jnp.float32), b_im.astype(jnp.float32))
    bu_re = jnp.einsum('blgh,gph->blgp', u32, bbar_re)
    bu_im = jnp.einsum('blgh,gph->blgp', u32, bbar_im)
    a_re = jnp.broadcast_to(abar_re, (1, seq_len, N_SSM_GROUPS, SSM_STATE))
    a_im = jnp.broadcast_to(abar_im, (1, seq_len, N_SSM_GROUPS, SSM_STATE))

    def combine(left, right):
        a1r, a1i, b1r, b1i = left
        a2r, a2i, b2r, b2i = right
        ar, ai = _cmul(a2r, a2i, a1r, a1i)
        br, bi = _cmul(a2r, a2i, b1r, b1i)
        return ar, ai, br + b2r, bi + b2i

    _, _, xs_re, xs_im = lax.associative_scan(combine, (a_re, a_im, bu_re, bu_im), axis=1)
    y = (jnp.einsum('blgp,ghp->blgh', xs_re, c_re.astype(jnp.float32))
         - jnp.einsum('blgp,ghp->blgh', xs_im, c_im.astype(jnp.float32))
         + d_skip.astype(jnp.float32).reshape(N_SSM_GROUPS, SSM_GROUP) * u32)
    y = y.reshape(bsz, seq_len, D_SSM)
    z = jax.nn.gelu(y)
    out = z * jax.nn.sigmoid(z @ w_glu.astype(jnp.float32))
    return out.astype(u.dtype)


def stick_breaking_attention(q, k, v):
    seq_len = q.shape[1]
    scale = HEAD_DIM ** -0.5
    outs = []
    for blk in range(seq_len // Q_BLOCK):
        q0 = blk * Q_BLOCK
        kl = q0 + Q_BLOCK
        qb, kb, vb = q[:, q0:kl], k[:, :kl], v[:, :kl]
        z = jnp.einsum('bqhd,bkhd->bhqk', qb, kb).astype(jnp.float32) * scale
        q_pos = q0 + jnp.arange(Q_BLOCK)[:, None]
        k_pos = jnp.arange(kl)[None, :]
        causal = k_pos < q_pos
        log_1m_beta = jnp.where(causal, jax.nn.log_sigmoid(-z), 0.0)
        log_tail = lax.cumsum(log_1m_beta, axis=3, reverse=True) - log_1m_beta
        weights = jnp.where(causal, jnp.exp(jax.nn.log_sigmoid(z) + log_tail), 0.0)
        outs.append(jnp.einsum('bhqk,bkhd->bqhd', weights.astype(vb.dtype), vb))
    return jnp.concatenate(outs, axis=1)


def hierarchical_moe(h, rg_w, rg_b, re_w, re_b, w_gate, w_up, w_down):
    bsz, seq_len, d = h.shape
    t = h.reshape(-1, d)
    n_tok = t.shape[0]
    g_prob = jax.nn.softmax((t @ rg_w + rg_b).astype(jnp.float32), axis=-1)
    g_top, g_idx = lax.top_k(g_prob, 1)
    e_logits = (t @ re_w + re_b).astype(jnp.float32).reshape(n_tok, N_EXPERT_GROUPS, EXPERTS_PER_GROUP)
    e_logits = e_logits[jnp.arange(n_tok), g_idx[:, 0]]
    e_prob = jax.nn.softmax(e_logits, axis=-1)
    e_top, e_idx = lax.top_k(e_prob, TOP_K_IN_GROUP)
    e_top = e_top / jnp.sum(e_top, axis=-1, keepdims=True)
    expert_id = g_idx * EXPERTS_PER_GROUP + e_idx
    gate = g_top * e_top
    combine = jnp.sum(jax.nn.one_hot(expert_id, N_EXPERTS, dtype=jnp.float32) * gate[..., None], axis=1)
    combine = combine.astype(t.dtype)
    out = jnp.zeros_like(t)
    for e in range(N_EXPERTS):
        hid = jax.nn.silu(t @ w_gate[e]) * (t @ w_up[e])
        out = out + combine[:, e:e + 1] * (hid @ w_down[e])
    return out.reshape(bsz, seq_len, d)


def setup_inputs(seed: int = 0) -> dict:
    key = jax.random.key(seed)
    ks = jax.random.split(key, 26)
    f32 = jnp.float32
    nrm = lambda k, shape, s: jax.random.normal(k, shape, f32) * s
    G, P, H = N_SSM_GROUPS, SSM_STATE, SSM_GROUP
    x = jax.random.normal(ks[0], (BATCH, SEQ, D_MODEL), f32)
    attn_norm_g = 1.0 + nrm(ks[1], (DEPTH, D_MODEL), 0.02)
    w_in = nrm(ks[2], (DEPTH, D_MODEL, D_IN_PROJ), D_MODEL ** -0.5)
    lambda_re = -0.5 + nrm(ks[3], (DEPTH, G, P), 0.01)
    lambda_im = jnp.pi * jnp.arange(P, dtype=f32)[None, None, :] + nrm(ks[4], (DEPTH, G, P), 0.01)
    log_dt = jax.random.uniform(ks[5], (DEPTH, G), f32, math.log(DT_MIN), math.log(DT_MAX))
    ssm_b_re = nrm(ks[6], (DEPTH, G, P, H), (2.0 * H) ** -0.5)
    ssm_b_im = nrm(ks[7], (DEPTH, G, P, H), (2.0 * H) ** -0.5)
    ssm_c_re = nrm(ks[8], (DEPTH, G, H, P), 0.5 ** 0.5)
    ssm_c_im = nrm(ks[9], (DEPTH, G, H, P), 0.5 ** 0.5)
    ssm_d = nrm(ks[10], (DEPTH, D_SSM), 1.0)
    w_glu = nrm(ks[11], (DEPTH, D_SSM, D_SSM), D_SSM ** -0.5)
    q_norm_g = 1.0 + nrm(ks[12], (DEPTH, HEAD_DIM), 0.02)
    k_norm_g = 1.0 + nrm(ks[13], (DEPTH, HEAD_DIM), 0.02)
    w_branch_ssm = nrm(ks[14], (DEPTH, D_SSM, D_MODEL), D_SSM ** -0.5)
    w_branch_att = nrm(ks[15], (DEPTH, D_ATT, D_MODEL), D_ATT ** -0.5)
    w_out = nrm(ks[16], (DEPTH, D_MODEL, D_MODEL), D_MODEL ** -0.5)
    ffn_norm_g = 1.0 + nrm(ks[17], (DEPTH, D_MODEL), 0.02)
    router_group_w = nrm(ks[18], (DEPTH, D_MODEL, N_EXPERT_GROUPS), D_MODEL ** -0.5)
    router_group_b = nrm(ks[19], (DEPTH, N_EXPERT_GROUPS), 0.01)
    router_expert_w = nrm(ks[20], (DEPTH, D_MODEL, N_EXPERTS), D_MODEL ** -0.5)
    router_expert_b = nrm(ks[21], (DEPTH, N_EXPERTS), 0.01)
    expert_w_gate = nrm(ks[22], (DEPTH, N_EXPERTS, D_MODEL, D_FF_EXPERT), D_MODEL ** -0.5)
    expert_w_up = nrm(ks[23], (DEPTH, N_EXPERTS, D_MODEL, D_FF_EXPERT), D_MODEL ** -0.5)
    expert_w_down = nrm(ks[24], (DEPTH, N_EXPERTS, D_FF_EXPERT, D_MODEL), D_FF_EXPERT ** -0.5)
    return {"x": x, "attn_norm_g": attn_norm_g, "w_in": w_in,
            "lambda_re": lambda_re, "lambda_im": lambda_im, "log_dt": log_dt,
            "ssm_b_re": ssm_b_re, "ssm_b_im": ssm_b_im, "ssm_c_re": ssm_c_re, "ssm_c_im": ssm_c_im,
            "ssm_d": ssm_d, "w_glu": w_glu, "q_norm_g": q_norm_g, "k_norm_g": k_norm_g,
            "w_branch_ssm": w_branch_ssm, "w_branch_att": w_branch_att, "w_out": w_out,
            "ffn_norm_g": ffn_norm_g, "router_group_w": router_group_w, "router_group_b": router_group_b,
            "router_expert_w": router_expert_w, "router_expert_b": router_expert_b,
            "expert_w_gate": expert_w_gate, "expert_w_up": expert_w_up, "expert_w_down": expert_w_down}


def reference(x, attn_norm_g, w_in, lambda_re, lambda_im, log_dt, ssm_b_re, ssm_b_im,
              ssm_c_re, ssm_c_im, ssm_d, w_glu, q_norm_g, k_norm_g, w_branch_ssm,
              w_branch_att, w_out, ffn_norm_g, router_group_w, router_group_b,
              router_expert_w, router_expert_b, expert_w_gate, expert_w_up, expert_w_down):
    bsz, seq_len, _ = x.shape
    splits = [D_SSM, D_SSM + D_ATT, D_SSM + 2 * D_ATT, D_SSM + 3 * D_ATT,
              D_SSM + 3 * D_ATT + D_MODEL]
    for l in range(DEPTH):
        h = rms_norm(x, attn_norm_g[l])
        proj = h @ w_in[l]
        u_ssm, q, k, v, gate_ssm, gate_att = jnp.split(proj, splits, axis=-1)
        y_ssm = s5_mixer(u_ssm, lambda_re[l], lambda_im[l], log_dt[l], ssm_b_re[l], ssm_b_im[l],
                         ssm_c_re[l], ssm_c_im[l], ssm_d[l], w_glu[l])
        q = rms_norm(q.reshape(bsz, seq_len, N_HEADS, HEAD_DIM), q_norm_g[l])
        k = rms_norm(k.reshape(bsz, seq_len, N_HEADS, HEAD_DIM), k_norm_g[l])
        v = v.reshape(bsz, seq_len, N_HEADS, HEAD_DIM)
        y_att = stick_breaking_attention(q, k, v).reshape(bsz, seq_len, D_ATT)
        merged = (jax.nn.sigmoid(gate_ssm) * (y_ssm @ w_branch_ssm[l])
                  + jax.nn.sigmoid(gate_att) * (y_att @ w_branch_att[l]))
        x = x + merged @ w_out[l]
        h2 = rms_norm(x, ffn_norm_g[l])
        x = x + hierarchical_moe(h2, router_group_w[l], router_group_b[l], router_expert_w[l],
                                 router_expert_b[l], expert_w_gate[l], expert_w_up[l], expert_w_down[l])
    return x
```

```python
import math
from contextlib import ExitStack

import numpy as np
import concourse.bass as bass
import concourse.mybir as mybir
from concourse.bass_utils import run_bass_kernel_spmd

F32 = mybir.dt.float32
BF16 = mybir.dt.bfloat16
I32 = mybir.dt.int32
AF = mybir.ActivationFunctionType
ALU = mybir.AluOpType

T = 2048
D = 2048
P = 128
NCK = 256
EPS = 1e-6
NE = 32
DFF = 512


class Sched:
    def __init__(self, nc):
        self.nc = nc
        self.eng = {"pe": nc.tensor, "act": nc.scalar, "dve": nc.vector,
                    "pool": nc.gpsimd, "sp": nc.sync}
        self.sem = {e: nc.alloc_semaphore(name="s_" + e) for e in self.eng}
        self.cnt = {e: 0 for e in self.eng}
        self.waited = {e: {} for e in self.eng}
        self.res = {}
        self.dsem = {}
        self.rr = 0
        self.dead = False
        self.bregs = {}

    def _toks(self, reads, writes):
        toks = []
        for k in reads:
            st = self.res.get(k)
            if st is not None and st[0] is not None:
                toks.append(st[0])
        for k in writes:
            st = self.res.get(k)
            if st is not None:
                if st[0] is not None:
                    toks.append(st[0])
                toks.extend(st[1])
        return toks

    def _wait(self, e, toks):
        need = {}
        for (s, v) in toks:
            if s == e and e in ("pe", "sp"):
                continue
            if v > need.get(s, 0):
                need[s] = v
        for s, v in need.items():
            if self.waited[e].get(s, 0) >= v:
                continue
            h = self.sem[s] if s in self.sem else self.dsem[s][0]
            self.eng[e].wait_ge(h, v)
            self.waited[e][s] = v

    def _mark(self, tok, reads, writes):
        for k in reads:
            st = self.res.setdefault(k, [None, []])
            st[1].append(tok)
            if len(st[1]) > 24:
                mx = {}
                for (s, v) in st[1]:
                    if v > mx.get(s, 0):
                        mx[s] = v
                st[1] = list(mx.items())
        for k in writes:
            self.res[k] = [tok, []]

    def op(self, e, fn, reads=(), writes=(), inc=True):
        if self.dead:
            return None
        self._wait(e, self._toks(reads, writes))
        ins = fn()
        tok = (e, self.cnt[e] + 1)
        if inc:
            ins.then_inc(self.sem[e], 1)
            self.cnt[e] += 1
        self._mark(tok, reads, writes)
        return ins

    def dma(self, q, out, in_, reads=(), writes=(), sem="d", **kw):
        if self.dead:
            return None
        self._wait(q, self._toks(reads, writes))
        if sem not in self.dsem:
            self.dsem[sem] = [self.nc.alloc_semaphore(name="d_" + sem), 0]
        d = self.dsem[sem]
        d[1] += 16
        self.eng[q].dma_start(out=out, in_=in_, **kw).then_inc(d[0], 16)
        self._mark((sem, d[1]), reads, writes)

    def idma(self, out, out_idx, in_, in_idx, reads=(), writes=(), sem="id", bounds=None):
        if self.dead:
            return None
        self._wait("pool", self._toks(reads, writes))
        if sem not in self.dsem:
            self.dsem[sem] = [self.nc.alloc_semaphore(name="d_" + sem), 0]
        d = self.dsem[sem]
        d[1] += 16
        oo = bass.IndirectOffsetOnAxis(ap=out_idx, axis=0) if out_idx is not None else None
        io = bass.IndirectOffsetOnAxis(ap=in_idx, axis=0) if in_idx is not None else None
        kw = {}
        if bounds is not None:
            if bounds not in self.bregs:
                self.bregs[bounds] = self.nc.gpsimd.to_reg(bounds)
            kw = {"bounds_check": self.bregs[bounds], "oob_is_err": False}
        self.nc.gpsimd.indirect_dma_start(out=out, out_offset=oo, in_=in_, in_offset=io, **kw).then_inc(d[0], 16)
        self._mark((sem, d[1]), reads, writes)

    def barrier(self):
        toks = [(e, self.cnt[e]) for e in self.eng if self.cnt[e] > 0]
        toks += [(s, d[1]) for s, d in self.dsem.items() if d[1] > 0]
        for e in self.eng:
            self._wait(e, [t for t in toks if t[0] != e])
        self.res = {}

    def alt(self):
        self.rr ^= 1
        return "act" if self.rr else "dve"


class _Stop(Exception):
    pass


def build(upto="all", taps=()):
    import os
    kgate = int(os.environ.get("KGATE", "0"))

    gate_s = [None]

    def gate(n):
        if kgate == n:
            gate_s[0].dead = True
    nc = bass.Bass("TRN2", target_bir_lowering=False)
    S = Sched(nc)
    gate_s[0] = S
    dram = {}

    def din(name, shape):
        dram[name] = nc.dram_tensor(name, list(shape), F32, kind="ExternalInput").ap()
        return dram[name]

    x = din("x", [T, D])
    attn_norm_g = din("attn_norm_g", [16, 128])
    w_in = din("w_in", [D, 8192])
    lambda_re = din("lambda_re", [32, 128])
    lambda_im = din("lambda_im", [32, 128])
    log_dt = din("log_dt", [32, 2])
    ssm_b_re = din("ssm_b_re", [32, 2048])
    ssm_b_im = din("ssm_b_im", [32, 2048])
    ssm_c_re = din("ssm_c_re", [32, 2048])
    ssm_c_im = din("ssm_c_im", [32, 2048])
    ssm_d = din("ssm_d", [8, 128])
    w_glu = din("w_glu", [1024, 1024])
    q_norm_g = din("q_norm_g", [1, 128])
    k_norm_g = din("k_norm_g", [1, 128])
    w_branch_ssm = din("w_branch_ssm", [1024, 2048])
    w_branch_att = din("w_branch_att", [1024, 2048])
    w_out = din("w_out", [D, D])
    ffn_norm_g = din("ffn_norm_g", [16, 128])
    ffn_norm_g_row = din("ffn_norm_g_row", [1, D])
    router_group_w = din("router_group_w", [D, 4])
    router_group_b = din("router_group_b", [1, 4])
    router_expert_w = din("router_expert_w", [D, 32])
    router_expert_b = din("router_expert_b", [1, 32])
    expert_w_gate = din("expert_w_gate", [NE, D, DFF])
    expert_w_up = din("expert_w_up", [NE, D, DFF])
    expert_w_down = din("expert_w_down", [NE, DFF, D])
    out = nc.dram_tensor("out", [T, D], F32, kind="ExternalOutput").ap()
    x2d = nc.dram_tensor("x2_scratch", [T, D], F32, kind="Internal").ap()
    tapd = {}
    for (nm, shp) in taps:
        tapd[nm] = nc.dram_tensor("tap_" + nm, list(shp), F32, kind="ExternalOutput").ap()

    top = ExitStack()
    with top:
        def sb(es, name, shape, dt=F32):
            return es.enter_context(nc.sbuf_tensor(name, list(shape), dt))

        ps_all = top.enter_context(nc.psum_tensor("ps_all", [P, 4096], F32))
        psb = [ps_all[:, i * 512:(i + 1) * 512] for i in range(8)]
        bank_rr = [0]

        def nbank(lo=0, hi=8):
            b = lo + (bank_rr[0] % (hi - lo))
            bank_rr[0] += 1
            return b

        ident = sb(top, "ident", [P, P])
        ones = sb(top, "ones", [P, P])
        epsc = sb(top, "epsc", [P, 1])
        S.op("dve", lambda: nc.vector.memset(ones[:], 1.0), writes=["ones"])
        S.op("dve", lambda: nc.vector.memset(epsc[:], EPS), writes=["epsc"])
        S.op("pool", lambda: nc.gpsimd.affine_select(
            out=ident[:], in_=ones[:], pattern=[[1, P]], compare_op=ALU.is_equal,
            fill=0.0, base=0, channel_multiplier=-1), reads=["ones"], writes=["ident"])

        def tap(nm, src_ap, key):
            if nm in tapd:
                S.dma("sp", tapd[nm], src_ap, reads=[key], sem="tap")

        tri = sb(top, "tri", [P, P]); lem = sb(top, "lem", [P, P]); cmf = sb(top, "cmf", [P, P])
        cmb = sb(top, "cmb", [P, P], BF16); zer = sb(top, "zer", [P, 512], BF16)
        trib = sb(top, "trib", [P, P], BF16); lemb = sb(top, "lemb", [P, P], BF16)
        gq = sb(top, "gq", [P, 1]); gk = sb(top, "gk", [P, 1])
        g1s = sb(top, "g1s", [16, P])
        g1T = sb(top, "g1T", [P, 16])
        g2s = sb(top, "g2s", [16, P])
        g2T = sb(top, "g2T", [P, 16])
        es_mix = ExitStack()
        hT = sb(es_mix, "hT", [P, 16, T], BF16)
        s5T = sb(es_mix, "s5T", [P, 8, T], BF16)
        S.dma("sp", g1s[:], attn_norm_g, writes=["g1s"], sem="m1")
        b = nbank()
        S.op("pe", lambda: nc.tensor.transpose(out=psb[b][:, 0:16], in_=g1s[:], identity=ident[0:16, 0:16]),
             reads=["g1s", "ident"], writes=["ps%d" % b])
        S.op("dve", lambda: nc.vector.tensor_copy(out=g1T[:], in_=psb[b][:, 0:16]),
             reads=["ps%d" % b], writes=["g1T"])

        def rmsnorm_to_T(es, src_rows, gT, dstT, dst_key, ncols_off=0, ntiles=16, pfx="n1"):
            xt = [sb(es, pfx + "_xt%d" % i, [P, D]) for i in range(2)]
            junk = sb(es, pfx + "_junk", [P, D], BF16)
            ss = sb(es, pfx + "_ss", [P, ntiles])
            rs = sb(es, pfx + "_rs", [P, ntiles])
            for tt in range(ntiles):
                sl = tt % 2
                xk = pfx + "xt%d" % sl
                S.dma("sp", xt[sl][:], src_rows(tt), writes=[xk], sem=pfx + "x%d" % sl)
                S.op("act", lambda: nc.scalar.activation(out=junk[:], in_=xt[sl][:], func=AF.Square,
                                                         accum_out=ss[:, tt:tt + 1]),
                     reads=[xk], writes=[pfx + "junk", (pfx + "ss", tt)])
                S.op("act", lambda: nc.scalar.activation(out=rs[:, tt:tt + 1], in_=ss[:, tt:tt + 1], func=AF.Sqrt,
                                                         bias=epsc[:, 0:1], scale=1.0 / D),
                     reads=[(pfx + "ss", tt), "epsc"], writes=[(pfx + "rs", tt)])
                S.op("dve", lambda: nc.vector.reciprocal(out=rs[:, tt:tt + 1], in_=rs[:, tt:tt + 1]),
                     reads=[(pfx + "rs", tt)], writes=[(pfx + "rs", tt)])
                S.op("dve", lambda: nc.vector.tensor_scalar(out=xt[sl][:], in0=xt[sl][:], scalar1=rs[:, tt:tt + 1],
                                                            scalar2=None, op0=ALU.mult),
                     reads=[xk, (pfx + "rs", tt)], writes=[xk])
                for cb in range(4):
                    bk = nbank()
                    for j in range(4):
                        c = cb * 4 + j
                        S.op("pe", lambda: nc.tensor.transpose(out=psb[bk][:, j * P:(j + 1) * P],
                                                               in_=xt[sl][:, c * P:(c + 1) * P], identity=ident[:]),
                             reads=[xk, "ident"], writes=["ps%d" % bk], inc=(j == 3))
                    for j in range(4):
                        c = cb * 4 + j
                        dst = dstT[:, c, ncols_off + tt * P: ncols_off + (tt + 1) * P]
                        e = S.alt()
                        if e == "dve":
                            S.op("dve", lambda: nc.vector.tensor_scalar(out=dst, in0=psb[bk][:, j * P:(j + 1) * P],
                                                                        scalar1=gT[:, c:c + 1], scalar2=None,
                                                                        op0=ALU.mult),
                                 reads=["ps%d" % bk], writes=[(dst_key, tt)])
                        else:
                            S.op("act", lambda: nc.scalar.activation(out=dst, in_=psb[bk][:, j * P:(j + 1) * P],
                                                                     func=AF.Copy, scale=gT[:, c:c + 1]),
                                 reads=["ps%d" % bk], writes=[(dst_key, tt)])

        with ExitStack() as es:
            rmsnorm_to_T(es, lambda tt: x[tt * P:(tt + 1) * P, :], g1T, hT, "hT")
            S.barrier()
        gate(101)
        if "hT" in tapd:
            with ExitStack() as es:
                tmp = sb(es, "taptmp", [P, 16, T])
                S.op("dve", lambda: nc.vector.tensor_copy(out=tmp[:], in_=hT[:]), writes=["taptmp"])
                S.dma("sp", tapd["hT"].rearrange("p (c t) -> p c t", c=16), tmp[:], reads=["taptmp"], sem="tap")
                S.barrier()


        def kn(ap):
            return ap.name

        def V_tt(out, a, b, op, rk=None, wk=None, e="dve"):
            en = nc.vector if e == "dve" else nc.gpsimd
            return S.op(e, lambda: en.tensor_tensor(out=out, in0=a, in1=b, op=op),
                        reads=rk if rk is not None else [kn(a), kn(b)],
                        writes=wk if wk is not None else [kn(out)])

        def V_ts(out, a, s1, op0, s2=None, op1=None, rk=None, wk=None):
            kw = {}
            if op1 is not None:
                kw["op1"] = op1
            r = rk if rk is not None else [kn(a)] + [kn(s) for s in (s1, s2) if hasattr(s, "name")]
            return S.op("dve", lambda: nc.vector.tensor_scalar(out=out, in0=a, scalar1=s1, scalar2=s2, op0=op0, **kw),
                        reads=r, writes=wk if wk is not None else [kn(out)])

        def V_stt(out, a, s, b, op0, op1, rk=None, wk=None):
            r = rk if rk is not None else [kn(a), kn(b)] + ([kn(s)] if hasattr(s, "name") else [])
            return S.op("dve", lambda: nc.vector.scalar_tensor_tensor(out=out, in0=a, scalar=s, in1=b, op0=op0, op1=op1),
                        reads=r, writes=wk if wk is not None else [kn(out)])

        def V_cp(out, a, rk=None, wk=None, e="dve"):
            if e == "act":
                return S.op("act", lambda: nc.scalar.copy(out=out, in_=a),
                            reads=rk if rk is not None else [kn(a)], writes=wk if wk is not None else [kn(out)])
            en = nc.vector if e == "dve" else nc.gpsimd
            return S.op(e, lambda: en.tensor_copy(out=out, in_=a),
                        reads=rk if rk is not None else [kn(a)], writes=wk if wk is not None else [kn(out)])

        def A_act(out, a, func, scale=1.0, bias=None, rk=None, wk=None, accum_out=None):
            kw = {}
            if bias is not None:
                kw["bias"] = bias
            if accum_out is not None:
                kw["accum_out"] = accum_out
            r = rk if rk is not None else [kn(a)] + [kn(s) for s in (scale, bias) if hasattr(s, "name")]
            return S.op("act", lambda: nc.scalar.activation(out=out, in_=a, func=func, scale=scale, **kw),
                        reads=r, writes=wk if wk is not None else [kn(out)])

        def PE_T(out, in_, n, rk, wk, inc=True):
            return S.op("pe", lambda: nc.tensor.transpose(out=out, in_=in_, identity=ident[0:n, 0:n]),
                        reads=rk, writes=wk, inc=inc)

        def PE_mm(out, lhsT, rhs, start, stop, rk, wk, inc=True, tp=None, sg=False):
            kw = {}
            if sg:
                kw["skip_group_check"] = True
            if tp is not None:
                kw["tile_position"] = tp
            return S.op("pe", lambda: nc.tensor.matmul(out, lhsT=lhsT, rhs=rhs, start=start, stop=stop, **kw),
                        reads=rk, writes=wk, inc=inc)

        def pk(b):
            return "ps%d" % b

        def tap_bf(nm, src, key_list):
            if nm in tapd:
                S.dma("pool", tapd[nm], src, reads=key_list, sem="tap")

        es_ssm = ExitStack()
        APr = sb(es_ssm, "APr", [P, 9, 32]); APi = sb(es_ssm, "APi", [P, 9, 32])
        AKr = sb(es_ssm, "AKr", [P, 8, 32]); AKi = sb(es_ssm, "AKi", [P, 8, 32]); AKn = sb(es_ssm, "AKn", [P, 8, 32])
        BBr = sb(es_ssm, "BBr", [P, 16, 32]); BBi = sb(es_ssm, "BBi", [P, 16, 32])
        CRt = sb(es_ssm, "CRt", [P, 16, 32]); CIt = sb(es_ssm, "CIt", [P, 16, 32])
        Dcol = sb(es_ssm, "Dcol", [P, 8])
        with ExitStack() as es:
            st_lr = sb(es, "st_lr", [32, P]); st_li = sb(es, "st_li", [32, P])
            st_dt = sb(es, "st_dt", [32, 2]); st_dtb = sb(es, "st_dtb", [32, P])
            st_b1 = sb(es, "st_b", [32, 2048]); st_c1 = sb(es, "st_c", [32, 2048])
            st_b21 = sb(es, "st_b2", [32, 16, P]); st_c21 = sb(es, "st_c2", [32, 16, P])
            st_b = [st_b1, st_b1]; st_c = [st_c1, st_c1]; st_b2 = [st_b21, st_b21]; st_c2 = [st_c21, st_c21]
            st_d = sb(es, "st_d", [8, P])
            LLD = sb(es, "LLD", [P, 96])
            BRt = sb(es, "BRt", [P, 16, 32]); BIt = sb(es, "BIt", [P, 16, 32])
            wk_ = [sb(es, "pw%d" % i, [P, 32]) for i in range(12)]
            S.dma("sp", st_lr[:], lambda_re, writes=["st_lr"], sem="m2")
            S.dma("sp", st_li[:], lambda_im, writes=["st_li"], sem="m3")
            S.dma("sp", st_dt[:], log_dt, writes=["st_dt"], sem="m4")
            S.dma("sp", st_d[:], ssm_d, writes=["st_d"], sem="m5")
            for g2 in range(2):
                V_ts(st_dtb[:, g2 * 64:(g2 + 1) * 64], ones[0:32, 0:64], st_dt[:, g2:g2 + 1], ALU.mult,
                     rk=["ones", "st_dt"], wk=["st_dtb"])
            bk = nbank()
            PE_T(psb[bk][:, 0:32], st_lr[:], 32, ["st_lr", "ident"], [pk(bk)], inc=False)
            PE_T(psb[bk][:, 32:64], st_li[:], 32, ["st_li", "ident"], [pk(bk)], inc=False)
            PE_T(psb[bk][:, 64:96], st_dtb[:], 32, ["st_dtb", "ident"], [pk(bk)])
            V_cp(LLD[:], psb[bk][:, 0:96], rk=[pk(bk)], wk=["LLD"])
            bk = nbank()
            PE_T(psb[bk][:, 0:8], st_d[:], 8, ["st_d", "ident"], [pk(bk)])
            V_cp(Dcol[:], psb[bk][:, 0:8], rk=[pk(bk)], wk=["Dcol"])
            for ri in range(2):
                S.dma("sp", st_b[ri][:], (ssm_b_re, ssm_b_im)[ri], writes=["st_b"], sem="stb")
                S.dma("sp", st_c[ri][:], (ssm_c_re, ssm_c_im)[ri], writes=["st_c"], sem="stc")
                V_cp(st_b2[ri][:], st_b[ri][:].rearrange("q (gp h) -> q h gp", h=16), rk=["st_b"], wk=["st_b2"])
                V_cp(st_c2[ri][:].rearrange("q h (g2 p) -> q g2 h p", g2=2),
                     st_c[ri][:].rearrange("q (g2 h p) -> q g2 h p", g2=2, h=16), rk=["st_c"], wk=["st_c2"])
                for (srcs, dst) in ((st_b2[ri], (BRt, BIt)[ri]), (st_c2[ri], (CRt, CIt)[ri])):
                    bk = nbank()
                    for h in range(16):
                        PE_T(psb[bk][:, h * 32:(h + 1) * 32], srcs[:, h, :], 32, [kn(srcs[:]), "ident"], [pk(bk)], inc=(h == 15))
                    V_cp(dst[:].rearrange("p h q -> p (h q)"), psb[bk][:, :], rk=[pk(bk)], wk=[kn(dst[:])])
            LR = LLD[:, 0:32]; LI = LLD[:, 32:64]; LDT = LLD[:, 64:96]
            dtv, lrdt, lidt, mag, cc, sn, t1, t2, t3, cre, cim, den = [w_[:] for w_ in wk_]
            A_act(dtv, LDT, AF.Exp)
            V_tt(lrdt, LR, dtv, ALU.mult)
            V_tt(lidt, LI, dtv, ALU.mult)
            A_act(mag, lrdt, AF.Exp)
            halfpi = sb(es, "halfpi", [P, 1])
            S.op("dve", lambda: nc.vector.memset(halfpi[:], math.pi / 2), writes=["halfpi"])
            A_act(sn, lidt, AF.Sin, scale=1.0 / 32)
            A_act(cc, lidt, AF.Sin, scale=1.0 / 32, bias=halfpi[:, 0:1])
            for _ in range(5):
                V_tt(t1, cc, cc, ALU.mult)
                V_tt(t2, sn, sn, ALU.mult)
                V_tt(t3, cc, sn, ALU.mult)
                V_tt(cc, t1, t2, ALU.subtract)
                V_ts(sn, t3, 2.0, ALU.mult)
            S.op("dve", lambda: nc.vector.memset(APr[:, 0, :], 1.0), writes=["APr"])
            S.op("dve", lambda: nc.vector.memset(APi[:, 0, :], 0.0), writes=["APi"])
            V_tt(APr[:, 1, :], mag, cc, ALU.mult)
            V_tt(APi[:, 1, :], mag, sn, ALU.mult)

            def cmul(o_r, o_i, a_r, a_i, b_r, b_i, tA, tB):
                V_tt(tA, a_r, b_r, ALU.mult)
                V_tt(tB, a_i, b_i, ALU.mult)
                V_tt(o_r, tA, tB, ALU.subtract)
                V_tt(tA, a_r, b_i, ALU.mult)
                V_tt(tB, a_i, b_r, ALU.mult)
                V_tt(o_i, tA, tB, ALU.add)

            for e_ in range(1, 8):
                cmul(APr[:, e_ + 1, :], APi[:, e_ + 1, :], APr[:, e_, :], APi[:, e_, :], APr[:, 1, :], APi[:, 1, :], t1, t2)
            V_cp(AKr[:, 0, :], APr[:, 8, :]); V_cp(AKi[:, 0, :], APi[:, 8, :])
            for k in range(7):
                V_tt(t1, AKr[:, k, :], AKr[:, k, :], ALU.mult)
                V_tt(t2, AKi[:, k, :], AKi[:, k, :], ALU.mult)
                V_tt(t3, AKr[:, k, :], AKi[:, k, :], ALU.mult)
                V_tt(AKr[:, k + 1, :], t1, t2, ALU.subtract)
                V_ts(AKi[:, k + 1, :], t3, 2.0, ALU.mult)
            V_ts(AKn[:], AKi[:], -1.0, ALU.mult)
            V_ts(t1, APr[:, 1, :], -1.0, ALU.add, rk=["APr"])
            V_tt(t2, LR, LR, ALU.mult)
            V_tt(t3, LI, LI, ALU.mult)
            V_tt(den, t2, t3, ALU.add)
            S.op("dve", lambda: nc.vector.reciprocal(out=den, in_=den), reads=[kn(den)], writes=[kn(den)])
            V_tt(t2, t1, LR, ALU.mult)
            V_tt(t3, APi[:, 1, :], LI, ALU.mult)
            V_tt(cre, t2, t3, ALU.add)
            V_tt(cre, cre, den, ALU.mult)
            V_tt(t2, APi[:, 1, :], LR, ALU.mult)
            V_tt(t3, t1, LI, ALU.mult)
            V_tt(cim, t2, t3, ALU.subtract)
            V_tt(cim, cim, den, ALU.mult)
            tb1 = sb(es, "tb1", [P, 16, 32]); tb2 = sb(es, "tb2", [P, 16, 32])
            creb = cre.unsqueeze(1).broadcast_to([P, 16, 32]); cimb = cim.unsqueeze(1).broadcast_to([P, 16, 32])
            cmul(BBr[:], BBi[:], creb, cimb, BRt[:], BIt[:], tb1[:], tb2[:])
            S.barrier()


        uT = sb(es_ssm, "uT", [P, 8, T], BF16)

        def load_w(wbuf, key, srcap, kch, ncols, sem):
            S.dma("pool", wbuf[:, 0:kch, 0:ncols], srcap.rearrange("(c p) f -> p c f", p=P), writes=[key], sem=sem)

        def evac_copy(dst, src_ps, bk, wkeys, e=None):
            e = e or S.alt()
            V_cp(dst, src_ps, rk=[pk(bk)], wk=wkeys, e=e)

        with ExitStack() as es:
            wst = [sb(es, "wst%d" % i, [P, 16, 512], BF16) for i in range(2)]
            for blk in range(2):
                sl = blk % 2
                load_w(wst[sl], "wst%d" % sl, w_in[:, blk * 512:(blk + 1) * 512], 16, 512, "wst%d" % sl)
                for m in range(4):
                    for n in range(4):
                        bk = nbank()
                        for k in range(16):
                            PE_mm(psb[bk][:, :], wst[sl][:, k, m * P:(m + 1) * P], hT[:, k, n * 512:(n + 1) * 512],
                                  k == 0, k == 15, ["wst%d" % sl], [pk(bk)], inc=(k == 15))
                        evac_copy(uT[:, blk * 4 + m, n * 512:(n + 1) * 512], psb[bk][:, :], bk, [("uT", blk * 4 + m, n)])
            S.barrier()
        gate(102)
        tap_bf("uT", uT[:].rearrange("p a t -> p (a t)"), [])

        with ExitStack() as es:
            Xu = sb(es, "Xu", [P, 8, 2, 4, 16])
            XP = sb(es, "XP", [P, 8, 2, 4, 32])
            CAu = sb(es, "CAu", [P, 4, 9, 2, 16])
            tq1 = sb(es, "tq1", [P, 4, 16]); tq2 = sb(es, "tq2", [P, 4, 16])
            WS = [sb(es, "WS%d" % i, [P, 8, 2, P], BF16) for i in range(2)]
            WCp = [sb(es, "WCp%d" % i, [P, 4, 9, 2, 32], BF16) for i in range(2)]
            BPb = [sb(es, "BPb%d" % i, [P, 2, 4, 32], BF16) for i in range(2)]
            BD = [sb(es, "BD%d" % i, [P, 8, P], BF16) for i in range(2)]
            Hb = [[sb(es, "Hb%d%d" % (s_, i), [P, 2, NCK]) for i in range(2)] for s_ in range(2)]
            Hbf = [sb(es, "Hbf%d" % i, [P, 2, NCK], BF16) for i in range(4)]
            y32 = sb(es, "y32", [P, 1024])
            S.op("dve", lambda: nc.vector.memset(XP[:], 0.0), writes=["XP"])
            for i in range(2):
                S.op("pool", lambda: nc.gpsimd.memset(WCp[i][:], 0.0), writes=["WCp%d" % i])
                S.op("pool", lambda: nc.gpsimd.memset(BD[i][:], 0.0), writes=["BD%d" % i])
            XPv = XP[:].rearrange("p i r q (g h) -> p (i r q) g h", g=2)
            for a in range(8):
                par = a % 2
                qs = slice(4 * a, 4 * a + 4)
                for ip in range(8):
                    e_ = 7 - ip
                    arb = APr[:, e_, qs].unsqueeze(2).broadcast_to([P, 4, 16])
                    aib = APi[:, e_, qs].unsqueeze(2).broadcast_to([P, 4, 16])
                    bbr = BBr[:, :, qs].rearrange("p h q -> p q h")
                    bbi = BBi[:, :, qs].rearrange("p h q -> p q h")
                    V_tt(tq1[:], arb, bbr, ALU.mult, rk=[], wk=["tq1"])
                    V_tt(tq2[:], aib, bbi, ALU.mult, rk=[], wk=["tq2"])
                    V_tt(Xu[:, ip, 0, :, :], tq1[:], tq2[:], ALU.subtract, rk=["tq1", "tq2"], wk=["Xu"])
                    V_tt(tq1[:], arb, bbi, ALU.mult, rk=[], wk=["tq1"])
                    V_tt(tq2[:], aib, bbr, ALU.mult, rk=[], wk=["tq2"])
                    V_tt(Xu[:, ip, 1, :, :], tq1[:], tq2[:], ALU.add, rk=["tq1", "tq2"], wk=["Xu"])
                Xuv = Xu[:].rearrange("p i r q h -> p (i r q) h")
                for g2 in range(2):
                    V_cp(XPv[g2 * 64:(g2 + 1) * 64, :, g2, :], Xuv[g2 * 64:(g2 + 1) * 64, :, :], rk=["Xu"], wk=["XP"])
                for e_ in range(9):
                    arb = APr[:, e_, qs].unsqueeze(2).broadcast_to([P, 4, 16])
                    aib = APi[:, e_, qs].unsqueeze(2).broadcast_to([P, 4, 16])
                    crr = CRt[:, :, qs].rearrange("p h q -> p q h")
                    cii = CIt[:, :, qs].rearrange("p h q -> p q h")
                    V_tt(tq1[:], crr, arb, ALU.mult, rk=[], wk=["tq1"])
                    V_tt(tq2[:], cii, aib, ALU.mult, rk=[], wk=["tq2"])
                    V_tt(CAu[:, :, e_, 0, :], tq1[:], tq2[:], ALU.subtract, rk=["tq1", "tq2"], wk=["CAu"])
                    V_tt(tq1[:], cii, arb, ALU.mult, rk=[], wk=["tq1"])
                    V_tt(tq2[:], crr, aib, ALU.mult, rk=[], wk=["tq2"])
                    V_stt(CAu[:, :, e_, 1, :], tq1[:], -1.0, tq2[:], ALU.mult, ALU.subtract, rk=["tq1", "tq2"], wk=["CAu"])
                CAuv = CAu[:].rearrange("p q e r h -> p (q e r) h")
                WCv = WCp[par][:].rearrange("p q e r (g h) -> p (q e r) g h", g=2)
                for g2 in range(2):
                    V_cp(WCv[g2 * 64:(g2 + 1) * 64, :, g2, :], CAuv[g2 * 64:(g2 + 1) * 64, :, :], rk=["CAu"], wk=["WCp%d" % par])
                V_cp(BPb[par][:], XP[:, 7, :, :, :], rk=["XP"], wk=["BPb%d" % par])
                for cb in range(4):
                    bk = nbank(6, 8)
                    for j in range(4):
                        ip, ri = divmod(cb * 4 + j, 2)
                        PE_T(psb[bk][:, j * P:(j + 1) * P], XP[:, ip, ri, :, :].rearrange("p q f -> p (q f)"), P,
                             ["XP", "ident"], [pk(bk)], inc=(j == 3))
                    evac_copy(WS[par][:].rearrange("p i r f -> p (i r f)")[:, cb * 512:(cb + 1) * 512], psb[bk][:, :], bk,
                              ["WS%d" % par])
                bk = nbank(6, 8)
                for qq in range(4):
                    for j in range(8):
                        for ri in range(2):
                            PE_mm(psb[bk][32 * qq:32 * qq + 32, j * 32:(j + 1) * 32], BPb[par][:, ri, qq, :],
                                  WCp[par][:, qq, j, ri, :], ri == 0, ri == 1,
                                  ["BPb%d" % par, "WCp%d" % par], [pk(bk)], inc=(qq == 3 and j == 7 and ri == 1),
                                  tp=(0, 32 * qq), sg=True)
                for qq in range(4):
                    V_cp(BD[par][32 * qq:32 * qq + 32, :, 32 * qq:32 * qq + 32],
                         psb[bk][32 * qq:32 * qq + 32, 0:256].rearrange("p (j f) -> p j f", j=8),
                         rk=[pk(bk)], wk=["BD%d" % par])
                for qq in range(4):
                    q = 4 * a + qq
                    hs = q % 2
                    pb = 32 * qq
                    bk = nbank(4, 6)
                    for ri in range(2):
                        for ip in range(8):
                            PE_mm(psb[bk][:, ri * NCK:(ri + 1) * NCK], WS[par][pb:pb + 32, ip, ri, :],
                                  uT[pb:pb + 32, a, ip::8], ip == 0, ip == 7,
                                  ["WS%d" % par] + [("uT", a, n) for n in range(4)], [pk(bk)],
                                  inc=(ri == 1 and ip == 7), tp=(pb, 0))
                    V_cp(Hb[hs][0][:].rearrange("p r c -> p (r c)"), psb[bk][:, :], rk=[pk(bk)],
                         wk=[("Hb", hs, 0, 0), ("Hb", hs, 0, 1)], e="act")
                    for k in range(8):
                        s_ = 1 << k
                        src_ = Hb[hs][k % 2]; dst_ = Hb[hs][(k + 1) % 2]
                        sp_, dp_ = k % 2, (k + 1) % 2
                        n_ = NCK - s_
                        V_cp(dst_[:, :, 0:s_], src_[:, :, 0:s_], rk=[("Hb", hs, sp_, 0), ("Hb", hs, sp_, 1)],
                             wk=[("Hb", hs, dp_, 0), ("Hb", hs, dp_, 1)], e="pool")
                        akr = AKr[:, k, q:q + 1]; aki = AKi[:, k, q:q + 1]; akn = AKn[:, k, q:q + 1]
                        V_stt(dst_[:, 0, s_:], src_[:, 0, 0:n_], akr, src_[:, 0, s_:], ALU.mult, ALU.add,
                              rk=[("Hb", hs, sp_, 0)], wk=[("Hb", hs, dp_, 0)])
                        V_stt(dst_[:, 1, s_:], src_[:, 1, 0:n_], akr, src_[:, 1, s_:], ALU.mult, ALU.add,
                              rk=[("Hb", hs, sp_, 1)], wk=[("Hb", hs, dp_, 1)])
                        V_stt(dst_[:, 0, s_:], src_[:, 1, 0:n_], akn, dst_[:, 0, s_:], ALU.mult, ALU.add,
                              rk=[("Hb", hs, sp_, 1), ("Hb", hs, dp_, 0)], wk=[("Hb", hs, dp_, 0)])
                        V_stt(dst_[:, 1, s_:], src_[:, 0, 0:n_], aki, dst_[:, 1, s_:], ALU.mult, ALU.add,
                              rk=[("Hb", hs, sp_, 0), ("Hb", hs, dp_, 1)], wk=[("Hb", hs, dp_, 1)])
                    V_cp(Hbf[qq][:], Hb[hs][0][:], rk=[("Hb", hs, 0, 0), ("Hb", hs, 0, 1)], wk=[("Hbf", qq)], e="act")
                for i in range(8):
                    for j in range(i + 1):
                        PE_mm(ps_all[:, i * NCK:(i + 1) * NCK], BD[par][:, j, :], uT[:, a, (i - j)::8],
                              (j == 0 and i % 2 == 0), False,
                              ["BD%d" % par] + [("uT", a, n) for n in range(4)], [pk(i // 2)],
                              inc=(j == i), sg=True)
                for qq in range(4):
                    pb = 32 * qq
                    for i in range(8):
                        for ri in range(2):
                            PE_mm(ps_all[pb:pb + 32, i * NCK + 1:(i + 1) * NCK], WCp[par][:, qq, i + 1, ri, :],
                                  Hbf[qq][:, ri, 0:NCK - 1], False, ri == 1,
                                  ["WCp%d" % par, ("Hbf", qq)], [pk(i // 2)], inc=(ri == 1), tp=(0, pb), sg=True)
                for hf in range(2):
                    Yv = ps_all[:, 0:2048].rearrange("p (i c) -> p c i", i=8)[:, hf * 128:(hf + 1) * 128, :]
                    uv = uT[:, a, hf * 1024:(hf + 1) * 1024].rearrange("p (c i) -> p c i", i=8)
                    V_stt(y32[:].rearrange("p (c i) -> p c i", i=8), uv, Dcol[:, a:a + 1], Yv, ALU.mult, ALU.add,
                          rk=[pk(0), pk(1), pk(2), pk(3)] + [("uT", a, n) for n in range(4)], wk=["y32"])
                    A_act(uT[:, a, hf * 1024:(hf + 1) * 1024], y32[:], AF.Gelu_apprx_tanh, rk=["y32"],
                          wk=[("uT", a, 2 * hf), ("uT", a, 2 * hf + 1)])
            tap_bf("zT", uT[:].rearrange("p a t -> p (a t)"), [("uT", a_, n_) for a_ in range(8) for n_ in range(4)])
            S.barrier()
        with ExitStack() as es:
            wglu = sb(es, "wglu", [P, 8, 1024], BF16)
            load_w(wglu, "wglu", w_glu, 8, 1024, "wglu")
            sg = [sb(es, "sg%d" % i, [P, 512], BF16) for i in range(2)]
            for m in range(8):
                for n in range(4):
                    bk = nbank(4, 8)
                    for k in range(8):
                        PE_mm(psb[bk][:, :], wglu[:, k, m * P:(m + 1) * P], uT[:, k, n * 512:(n + 1) * 512],
                              k == 0, k == 7, ["wglu"] + [("uT", k, n)], [pk(bk)], inc=(k == 7))
                    sl = (m * 4 + n) % 2
                    A_act(sg[sl][:], psb[bk][:, :], AF.Sigmoid, rk=[pk(bk)], wk=["sg%d" % sl])
                    V_tt(s5T[:, m, n * 512:(n + 1) * 512], sg[sl][:], uT[:, m, n * 512:(n + 1) * 512], ALU.mult,
                         rk=["sg%d" % sl, ("uT", m, n)], wk=[("s5T", m, n)])
            S.barrier()
        gate(103)
        tap_bf("s5T", s5T[:].rearrange("p a t -> p (a t)"), [])
        reg = {"APr": APr[:].rearrange("p e q -> p (e q)"), "APi": APi[:].rearrange("p e q -> p (e q)"),
               "AKr": AKr[:].rearrange("p e q -> p (e q)"), "BBr": BBr[:].rearrange("p h q -> p (h q)"),
               "BBi": BBi[:].rearrange("p h q -> p (h q)"), "CRt": CRt[:].rearrange("p h q -> p (h q)"),
               "Dcol": Dcol[:]}
        for nm_, ap_ in reg.items():
            if nm_ in tapd:
                S.dma("pool", tapd[nm_], ap_, sem="tap")
        S.barrier()
        es_ssm.close()


        attT = sb(es_mix, "attT", [P, 8, T], BF16)
        S.op("pool", lambda: nc.gpsimd.affine_select(out=tri[:], in_=ones[:], pattern=[[-1, P]], compare_op=ALU.is_gt,
                                                     fill=0.0, base=0, channel_multiplier=1), reads=["ones"], writes=["tri"])
        S.op("pool", lambda: nc.gpsimd.affine_select(out=lem[:], in_=ones[:], pattern=[[1, P]], compare_op=ALU.is_ge,
                                                     fill=0.0, base=0, channel_multiplier=-1), reads=["ones"], writes=["lem"])
        S.op("pool", lambda: nc.gpsimd.affine_select(out=cmf[:], in_=ones[:], pattern=[[1, P]], compare_op=ALU.is_gt,
                                                     fill=0.0, base=0, channel_multiplier=-1), reads=["ones"], writes=["cmf"])
        V_cp(cmb[:], cmf[:])
        V_cp(trib[:], tri[:])
        V_cp(lemb[:], lem[:])
        S.op("dve", lambda: nc.vector.memset(zer[:], 0.0), writes=["zer"])
        S.dma("sp", gq[:], q_norm_g.rearrange("o d -> d o"), writes=["gq"], sem="m6")
        S.dma("sp", gk[:], k_norm_g.rearrange("o d -> d o"), writes=["gk"], sem="m7")
        V_ts(gq[:], gq[:], 1.0 / math.sqrt(128.0), ALU.mult)
        S.barrier()

        for hg in range(2):
            with ExitStack() as es:
                qT = sb(es, "qT%d" % hg, [P, 4, T], BF16); kT = sb(es, "kT%d" % hg, [P, 4, T], BF16)
                vv = sb(es, "vv%d" % hg, [P, 16, 512], BF16)
                with ExitStack() as es2:
                    wst0 = sb(es2, "wstq0_%d" % hg, [P, 16, 512], BF16)
                    wst = [wst0, wst0]
                    sqf = [sb(es2, "sqf%d_%d" % (i, hg), [P, 512]) for i in range(2)]
                    rsq = [sb(es2, "rsq%d_%d" % (i, hg), [P, 512]) for i in range(2)]
                    cnt_ = 0
                    for which, col0 in (("q", 1024 + 512 * hg), ("k", 2048 + 512 * hg), ("v", 3072 + 512 * hg)):
                        sl = 0
                        load_w(wst[sl], "wstq%d" % sl, w_in[:, col0:col0 + 512], 16, 512, "wstq%d" % sl)
                        if which == "v":
                            for tt in range(16):
                                bk = nbank(0, 4)
                                for k in range(16):
                                    PE_mm(psb[bk][:, :], hT[:, k, tt * P:(tt + 1) * P], wst[sl][:, k, :], k == 0, k == 15,
                                          ["wstq%d" % sl], [pk(bk)], inc=(k == 15))
                                evac_copy(vv[:, tt, :], psb[bk][:, :], bk, [("vv", tt)])
                            continue
                        dstT = qT if which == "q" else kT
                        gcol = gq if which == "q" else gk
                        for m in range(4):
                            for n in range(4):
                                bk = nbank(0, 4)
                                for k in range(16):
                                    PE_mm(psb[bk][:, :], wst[sl][:, k, m * P:(m + 1) * P], hT[:, k, n * 512:(n + 1) * 512],
                                          k == 0, k == 15, ["wstq%d" % sl], [pk(bk)], inc=(k == 15))
                                s2 = (m * 4 + n) % 2
                                A_act(sqf[s2][:], psb[bk][:, :], AF.Square, rk=[pk(bk)], wk=["sqf%d" % s2])
                                b2 = nbank(4, 8)
                                PE_mm(psb[b2][:, :], ones[:], sqf[s2][:], True, True, ["ones", "sqf%d" % s2], [pk(b2)])
                                A_act(rsq[s2][:], psb[b2][:, :], AF.Sqrt, scale=1.0 / 128, bias=epsc[:, 0:1],
                                      rk=[pk(b2)], wk=["rsq%d" % s2])
                                S.op("dve", lambda: nc.vector.reciprocal(out=rsq[s2][:], in_=rsq[s2][:]),
                                     reads=["rsq%d" % s2], writes=["rsq%d" % s2])
                                V_stt(dstT[:, m, n * 512:(n + 1) * 512], psb[bk][:, :], gcol[:, 0:1], rsq[s2][:],
                                      ALU.mult, ALU.mult, rk=[pk(bk), "rsq%d" % s2], wk=[(which, m, n)])
                    S.barrier()
                if hg == 0:
                    tap_bf("qT", qT[:].rearrange("p a t -> p (a t)"), [])
                    tap_bf("kT", kT[:].rearrange("p a t -> p (a t)"), [])
                    tap_bf("vv", vv[:].rearrange("p a t -> p (a t)"), [])
                with ExitStack() as es2:
                    SPb = [sb(es2, "SPb%d_%d" % (i, hg), [P, 1024]) for i in range(1)]
                    SPh = [sb(es2, "SPh%d_%d" % (i, hg), [P, 1024], BF16) for i in range(2)]
                    Ab = sb(es2, "Ab%d" % hg, [P, 1024])
                    Wb_ = [sb(es2, "Wb%d_%d" % (i, hg), [P, 1024], BF16) for i in range(2)]
                    ZB = [ps_all[:, 0:1024], ps_all[:, 1024:2048]]
                    TB = ps_all[:, 2048:3072]
                    OB = ps_all[:, 3072:4096]

                    def bank_ranges(lo):
                        rs_ = []
                        for bh in range(2):
                            a_ = max(lo, 512 * bh); b_ = 512 * (bh + 1)
                            if a_ < b_:
                                rs_.append((bh, a_, b_))
                        return rs_

                    for hl in range(4):
                        h = 4 * hg + hl
                        for qh in range(2):
                            kbs = list(range(8 * qh + 7, -1, -1))
                            N_ = len(kbs)
                            for bh in range(2):
                                PE_mm(TB[:, bh * 512:(bh + 1) * 512], zer[:, 0:P], zer[:, :], True, True, ["zer"], [pk(4 + bh)], inc=False, sg=True)
                                PE_mm(OB[:, bh * 512:(bh + 1) * 512], zer[:, 0:P], zer[:, :], True, True, ["zer"], [pk(6 + bh)], inc=(bh == 1), sg=True)

                            def lo_of(n):
                                return max(0, kbs[n] * P - qh * 1024)

                            def diag(n):
                                return kbs[n] * P >= qh * 1024

                            def S1(n):
                                kb = kbs[n]; lo = lo_of(n); zb = n % 2
                                for (bh, a_, b_) in bank_ranges(lo):
                                    PE_mm(ZB[zb][:, a_:b_], kT[:, hl, kb * P:(kb + 1) * P], qT[:, hl, qh * 1024 + a_: qh * 1024 + b_],
                                          True, True, [], [pk(2 * zb + bh)])
                                zk = [pk(2 * zb), pk(2 * zb + 1)]
                                A_act(SPb[0][:, lo:], ZB[zb][:, lo:], AF.Exp, rk=zk, wk=["SPb0"])
                                A_act(SPh[zb][:, lo:], SPb[0][:, lo:], AF.Ln, bias=ones[:, 0:1], rk=["SPb0"], wk=["SPh%d" % zb])
                                if diag(n):
                                    V_tt(SPh[zb][:, lo:lo + P], SPh[zb][:, lo:lo + P], cmb[:], ALU.mult,
                                         rk=["SPh%d" % zb], wk=["SPh%d" % zb])

                            def S2(n):
                                lo = lo_of(n); zb = n % 2
                                for (bh, a_, b_) in bank_ranges(lo):
                                    PE_mm(TB[:, a_:b_], trib[:], SPh[zb][:, a_:b_], False, False, ["SPh%d" % zb], [pk(4 + bh)], sg=True)

                            def S3a(n):
                                lo = lo_of(n); zb = n % 2
                                zk = [pk(2 * zb), pk(2 * zb + 1)]
                                V_tt(Ab[:, lo:], ZB[zb][:, lo:], SPh[zb][:, lo:], ALU.subtract, rk=zk + ["SPh%d" % zb], wk=["Ab"])
                                V_tt(Ab[:, lo:], Ab[:, lo:], TB[:, lo:], ALU.subtract, rk=["Ab", pk(4), pk(5)], wk=["Ab"])
                                A_act(Wb_[zb][:, lo:], Ab[:, lo:], AF.Exp, rk=["Ab"], wk=["Wb%d" % zb])

                            def S3b(n):
                                lo = lo_of(n); zb = n % 2
                                if diag(n):
                                    V_tt(Wb_[zb][:, lo:lo + P], Wb_[zb][:, lo:lo + P], cmb[:], ALU.mult,
                                         rk=["Wb%d" % zb], wk=["Wb%d" % zb])

                            def S4a(n):
                                lo = lo_of(n); zb = n % 2
                                for (bh, a_, b_) in bank_ranges(lo):
                                    PE_mm(TB[:, a_:b_], lemb[:], SPh[zb][:, a_:b_], False, False, ["SPh%d" % zb], [pk(4 + bh)], sg=True)

                            def S4b(n):
                                kb = kbs[n]; lo = lo_of(n); zb = n % 2
                                for (bh, a_, b_) in bank_ranges(lo):
                                    PE_mm(OB[:, a_:b_], vv[:, kb, hl * P:(hl + 1) * P], Wb_[zb][:, a_:b_], False, n == N_ - 1,
                                          ["Wb%d" % zb], [pk(6 + bh)], sg=True)

                            S1(0)
                            if N_ > 1:
                                S1(1)
                            S2(0)
                            for n in range(N_):
                                S3a(n)
                                S4a(n)
                                if n + 2 < N_:
                                    S1(n + 2)
                                S3b(n)
                                if n + 1 < N_:
                                    S2(n + 1)
                                S4b(n)
                            V_cp(attT[:, h, qh * 1024:(qh + 1) * 1024], OB[:, :], rk=[pk(6), pk(7)], wk=[("attT", h, qh)], e="act")
                    S.barrier()
        gate(104)
        tap_bf("attT", attT[:].rearrange("p a t -> p (a t)"), [])


        for th in range(2):
            with ExitStack() as es:
                mT = sb(es, "mT%d" % th, [P, 16, 1024], BF16)
                with ExitStack() as es2:
                    wbs = [sb(es2, "wbs%d_%d" % (i, th), [P, 8, P], BF16) for i in range(2)]
                    wba = [sb(es2, "wba%d_%d" % (i, th), [P, 8, P], BF16) for i in range(2)]
                    wgs = [sb(es2, "wgs%d_%d" % (i, th), [P, 16, P], BF16) for i in range(2)]
                    wga = [sb(es2, "wga%d_%d" % (i, th), [P, 16, P], BF16) for i in range(2)]
                    sgs = [sb(es2, "sgs%d_%d" % (i, th), [P, 512]) for i in range(2)]
                    sga = [sb(es2, "sga%d_%d" % (i, th), [P, 512]) for i in range(2)]
                    for m in range(16):
                        sl = m % 2
                        cs = slice(m * P, (m + 1) * P)
                        load_w(wbs[sl], "wbs%d" % sl, w_branch_ssm[:, cs], 8, P, "wbs%d" % sl)
                        load_w(wba[sl], "wba%d" % sl, w_branch_att[:, cs], 8, P, "wba%d" % sl)
                        load_w(wgs[sl], "wgs%d" % sl, w_in[:, 4096 + m * P:4096 + (m + 1) * P], 16, P, "wgs%d" % sl)
                        load_w(wga[sl], "wga%d" % sl, w_in[:, 6144 + m * P:6144 + (m + 1) * P], 16, P, "wga%d" % sl)
                        for n in range(2):
                            ts_ = slice(th * 1024 + n * 512, th * 1024 + (n + 1) * 512)
                            b_bs, b_gs, b_ba, b_ga = nbank(), nbank(), nbank(), nbank()
                            for k in range(8):
                                PE_mm(psb[b_bs][:, :], wbs[sl][:, k, :], s5T[:, k, ts_], k == 0, k == 7, ["wbs%d" % sl], [pk(b_bs)], inc=(k == 7))
                            for k in range(16):
                                PE_mm(psb[b_gs][:, :], wgs[sl][:, k, :], hT[:, k, ts_], k == 0, k == 15, ["wgs%d" % sl], [pk(b_gs)], inc=(k == 15))
                            for k in range(8):
                                PE_mm(psb[b_ba][:, :], wba[sl][:, k, :], attT[:, k, ts_], k == 0, k == 7, ["wba%d" % sl], [pk(b_ba)], inc=(k == 7))
                            for k in range(16):
                                PE_mm(psb[b_ga][:, :], wga[sl][:, k, :], hT[:, k, ts_], k == 0, k == 15, ["wga%d" % sl], [pk(b_ga)], inc=(k == 15))
                            s2 = n
                            A_act(sgs[s2][:], psb[b_gs][:, :], AF.Sigmoid, rk=[pk(b_gs)], wk=["sgs%d" % s2])
                            A_act(sga[s2][:], psb[b_ga][:, :], AF.Sigmoid, rk=[pk(b_ga)], wk=["sga%d" % s2])
                            V_tt(sgs[s2][:], sgs[s2][:], psb[b_bs][:, :], ALU.mult, rk=["sgs%d" % s2, pk(b_bs)], wk=["sgs%d" % s2])
                            V_tt(sga[s2][:], sga[s2][:], psb[b_ba][:, :], ALU.mult, rk=["sga%d" % s2, pk(b_ba)], wk=["sga%d" % s2])
                            V_tt(mT[:, m, n * 512:(n + 1) * 512], sgs[s2][:], sga[s2][:], ALU.add,
                                 rk=["sgs%d" % s2, "sga%d" % s2], wk=[("mT", m, n)])
                    S.barrier()
                with ExitStack() as es2:
                    wo = [sb(es2, "wo%d_%d" % (i, th), [P, 16, 512], BF16) for i in range(2)]
                    xin = [sb(es2, "xin%d_%d" % (i, th), [P, 512]) for i in range(2)]
                    xo = [sb(es2, "xo%d_%d" % (i, th), [P, 512]) for i in range(2)]
                    cnt_ = 0
                    for db in range(4):
                        sl = db % 2
                        ds_ = slice(db * 512, (db + 1) * 512)
                        load_w(wo[sl], "wo%d" % sl, w_out[:, ds_], 16, 512, "wo%d" % sl)
                        for tt in range(8):
                            r0 = th * 1024 + tt * P
                            s2 = cnt_ % 2
                            cnt_ += 1
                            S.dma("sp", xin[s2][:], x[r0:r0 + P, ds_], writes=["xin%d" % s2], sem="xin%d" % s2)
                            bk = nbank()
                            for k in range(16):
                                PE_mm(psb[bk][:, :], mT[:, k, tt * P:(tt + 1) * P], wo[sl][:, k, :], k == 0, k == 15,
                                      ["wo%d" % sl], [pk(bk)], inc=(k == 15))
                            V_tt(xo[s2][:], xin[s2][:], psb[bk][:, :], ALU.add, rk=["xin%d" % s2, pk(bk)], wk=["xo%d" % s2])
                            S.dma("sp", x2d[r0:r0 + P, ds_], xo[s2][:], reads=["xo%d" % s2], sem="xo%d" % s2)
                    S.barrier()
        gate(105)
        es_mix.close()
        if "x2" in tapd:
            S.dma("sp", tapd["x2"], x2d, sem="tap")
            S.barrier()

        if upto == "F":
            S.barrier()
            return nc
        TS = 256
        NT = 48
        NS = NT * TS
        h2d = nc.dram_tensor("h2_scratch", [T, D], F32, kind="Internal").ap()
        yd = nc.dram_tensor("y_scratch", [NS, D], F32, kind="Internal").ap()
        stok = nc.dram_tensor("slot_tok", [NS, 16], I32, kind="Internal").ap()
        S.dma("sp", g2s[:], ffn_norm_g, writes=["g2s"], sem="m_g2s")
        bk = nbank()
        PE_T(psb[bk][:, 0:16], g2s[:], 16, ["g2s", "ident"], [pk(bk)])
        V_cp(g2T[:], psb[bk][:, 0:16], rk=[pk(bk)], wk=["g2T"])
        with ExitStack() as es:
            wr = sb(es, "wr", [P, 16, 36])
            rb = sb(es, "rb", [P, 36])
            comb_g1 = sb(es, "comb_g1", [P, 16]); comb_g2 = sb(es, "comb_g2", [P, 16])
            oh1a = sb(es, "oh1a", [P, 16, 32]); oh2a = sb(es, "oh2a", [P, 16, 32])
            selb = sb(es, "selb", [P, 16, 32], BF16)
            s1i = sb(es, "s1i", [P, 16], I32); s2i = sb(es, "s2i", [P, 16], I32)
            widx = sb(es, "widx", [P, NT], I32)
            onesb = sb(es, "onesb", [P, P], BF16)
            V_cp(onesb[:], ones[:])
            with ExitStack() as es1:
                wlg = sb(es1, "wlg", [16, P * 4]); wle = sb(es1, "wle", [16, P * 32])
                S.dma("sp", wlg[:], router_group_w.rearrange("(c p) f -> c (p f)", p=P), writes=["wlg"], sem="m_wlg")
                S.dma("sp", wle[:], router_expert_w.rearrange("(c p) f -> c (p f)", p=P), writes=["wle"], sem="m_wle")
                wlg2 = sb(es1, "wlg2", [16, 4, P]); wle2 = sb(es1, "wle2", [16, 32, P])
                V_cp(wlg2[:], wlg[:].rearrange("c (p f) -> c f p", f=4), rk=["wlg"], wk=["wlg2"])
                V_cp(wle2[:], wle[:].rearrange("c (p f) -> c f p", f=32), rk=["wle"], wk=["wle2"])
                bk = nbank()
                for f in range(4):
                    PE_T(psb[bk][:, f * 16:(f + 1) * 16], wlg2[:, f, :], 16, ["wlg2", "ident"], [pk(bk)], inc=(f == 3))
                V_cp(wr[:, :, 0:4].rearrange("p c f -> p f c"), psb[bk][:, 0:64].rearrange("p (f c) -> p f c", c=16), rk=[pk(bk)], wk=["wr"])
                bk = nbank()
                for f in range(32):
                    PE_T(psb[bk][:, f * 16:(f + 1) * 16], wle2[:, f, :], 16, ["wle2", "ident"], [pk(bk)], inc=(f == 31))
                V_cp(wr[:, :, 4:36].rearrange("p c f -> p f c"), psb[bk][:, 0:512].rearrange("p (f c) -> p f c", c=16), rk=[pk(bk)], wk=["wr"])
                S.barrier()
            S.dma("sp", rb[:, 0:4], router_group_b.to_broadcast([P, 4]), writes=["rb"], sem="m_rb0")
            S.dma("sp", rb[:, 4:36], router_expert_b.to_broadcast([P, 32]), writes=["rb"], sem="m_rb1")
            with ExitStack() as es3:
                gb = sb(es3, "gb", [P, D])
                S.dma("sp", gb[:], ffn_norm_g_row.to_broadcast([P, D]), writes=["gb"], sem="m_gb")
                xt2 = [sb(es3, "xt2_%d" % i, [P, D]) for i in range(2)]
                xn = [sb(es3, "xn_%d" % i, [P, D]) for i in range(2)]
                h2r = [sb(es3, "h2r_%d" % i, [P, D]) for i in range(2)]
                junk = sb(es3, "junk2", [P, D], BF16)
                h32 = sb(es3, "h32", [P, 16, P])
                ss2 = sb(es3, "ss2", [P, 16]); rs2 = sb(es3, "rs2", [P, 16])
                lgA = sb(es3, "lgA", [P, 16, 36])
                gmaxA = sb(es3, "gmaxA", [P, 16]); gexA = sb(es3, "gexA", [P, 16, 4]); gmA = sb(es3, "gmA", [P, 16, 4])
                gsumA = sb(es3, "gsumA", [P, 16]); mlA = sb(es3, "mlA", [P, 16, 32]); ml2A = sb(es3, "ml2A", [P, 16, 32])
                m1A = sb(es3, "m1A", [P, 16]); m2A = sb(es3, "m2A", [P, 16])
                sm = [sb(es3, "sm%d" % i, [P, 1]) for i in range(8)]
                ml = sb(es3, "ml", [P, 32]); ml2 = sb(es3, "ml2", [P, 32])
                gm = sb(es3, "gm", [P, 4]); gex = sb(es3, "gex", [P, 4])
                for tt in range(16):
                    r0 = tt * P
                    sl = tt % 2
                    xk = "xt2_%d" % sl
                    S.dma("sp", xt2[sl][:], x2d[r0:r0 + P, :], writes=[xk], sem="xt2_%d" % sl)
                    A_act(junk[:], xt2[sl][:], AF.Square, rk=[xk], wk=["junk2"], accum_out=ss2[:, tt:tt + 1])
                    A_act(rs2[:, tt:tt + 1], ss2[:, tt:tt + 1], AF.Sqrt, scale=1.0 / D, bias=epsc[:, 0:1],
                          rk=["junk2"], wk=[("rs2", tt)])
                    S.op("dve", lambda: nc.vector.reciprocal(out=rs2[:, tt:tt + 1], in_=rs2[:, tt:tt + 1]),
                         reads=[("rs2", tt)], writes=[("rs2", tt)])
                    V_ts(xn[sl][:], xt2[sl][:], rs2[:, tt:tt + 1], ALU.mult, rk=[xk, ("rs2", tt)], wk=["xn%d" % sl])
                    V_tt(h2r[sl][:], xn[sl][:], gb[:], ALU.mult, rk=["xn%d" % sl, "gb"], wk=["h2r%d" % sl], e="pool")
                    S.dma("sp", h2d[r0:r0 + P, :], h2r[sl][:], reads=["h2r%d" % sl], sem="h2w%d" % sl)
                    for cb in range(4):
                        bk = nbank()
                        for j in range(4):
                            c = cb * 4 + j
                            PE_T(psb[bk][:, j * P:(j + 1) * P], xn[sl][:, c * P:(c + 1) * P], P, ["xn%d" % sl, "ident"], [pk(bk)], inc=(j == 3))
                        e_ = S.alt()
                        for j in range(4):
                            c = cb * 4 + j
                            if e_ == "dve":
                                V_ts(h32[:, c, :], psb[bk][:, j * P:(j + 1) * P], g2T[:, c:c + 1], ALU.mult,
                                     rk=[pk(bk)], wk=[("h32", c)])
                            else:
                                S.op("act", lambda: nc.scalar.activation(out=h32[:, c, :], in_=psb[bk][:, j * P:(j + 1) * P],
                                                                         func=AF.Copy, scale=g2T[:, c:c + 1]),
                                     reads=[pk(bk)], writes=[("h32", c)])
                    bk = nbank()
                    for c in range(16):
                        PE_mm(psb[bk][:, 0:36], h32[:, c, :], wr[:, c, :], c == 0, c == 15,
                              [("h32", c), "wr"], [pk(bk)], inc=(c == 15))
                    V_tt(lgA[:, tt, :], psb[bk][:, 0:36], rb[:], ALU.add, rk=[pk(bk), "rb"], wk=[("lgA", tt)])
                lk = [("lgA", t_) for t_ in range(16)]
                AX = mybir.AxisListType.X
                gl = lgA[:, :, 0:4]
                el4 = lgA[:, :, 4:36].rearrange("p t (g e) -> p t g e", g=4)
                S.op("dve", lambda: nc.vector.tensor_reduce(out=gmaxA[:], in_=gl, axis=AX, op=ALU.max), reads=lk, writes=["gmaxA"])
                V_tt(gexA[:], gl, gmaxA[:].unsqueeze(2).broadcast_to([P, 16, 4]), ALU.subtract, rk=lk + ["gmaxA"], wk=["gexA"])
                V_ts(gmA[:], gexA[:], 0.0, ALU.is_ge, s2=-1.0, op1=ALU.add, rk=["gexA"], wk=["gmA"])
                V_ts(gmA[:], gmA[:], 1e30, ALU.mult, rk=["gmA"], wk=["gmA"])
                A_act(gexA[:], gexA[:], AF.Exp, rk=["gexA"], wk=["gexA"])
                S.op("dve", lambda: nc.vector.tensor_reduce(out=gsumA[:], in_=gexA[:], axis=AX, op=ALU.add), reads=["gexA"], writes=["gsumA"])
                S.op("dve", lambda: nc.vector.reciprocal(out=gsumA[:], in_=gsumA[:]), reads=["gsumA"], writes=["gsumA"])
                V_tt(mlA[:].rearrange("p t (g e) -> p t g e", g=4), el4, gmA[:].unsqueeze(3).broadcast_to([P, 16, 4, 8]), ALU.add,
                     rk=lk + ["gmA"], wk=["mlA"])
                S.op("dve", lambda: nc.vector.tensor_reduce(out=m1A[:], in_=mlA[:], axis=AX, op=ALU.max), reads=["mlA"], writes=["m1A"])
                V_tt(oh1a[:], mlA[:], m1A[:].unsqueeze(2).broadcast_to([P, 16, 32]), ALU.is_equal, rk=["mlA", "m1A"], wk=["oh1a"])
                V_stt(ml2A[:], oh1a[:], -1e30, mlA[:], ALU.mult, ALU.add, rk=["oh1a", "mlA"], wk=["ml2A"])
                S.op("dve", lambda: nc.vector.tensor_reduce(out=m2A[:], in_=ml2A[:], axis=AX, op=ALU.max), reads=["ml2A"], writes=["m2A"])
                V_tt(oh2a[:], ml2A[:], m2A[:].unsqueeze(2).broadcast_to([P, 16, 32]), ALU.is_equal, rk=["ml2A", "m2A"], wk=["oh2a"])
                V_tt(m1A[:], m1A[:], m2A[:], ALU.subtract, rk=["m1A", "m2A"], wk=["m1A"])
                A_act(m1A[:], m1A[:], AF.Sigmoid, rk=["m1A"], wk=["m1A"])
                V_tt(comb_g1[:], gsumA[:], m1A[:], ALU.mult, rk=["gsumA", "m1A"], wk=["comb_g1"])
                V_tt(comb_g2[:], gsumA[:], comb_g1[:], ALU.subtract, rk=["gsumA", "comb_g1"], wk=["comb_g2"])
                V_tt(selb[:], oh1a[:], oh2a[:], ALU.add, rk=["oh1a", "oh2a"], wk=[("selb", t_) for t_ in range(16)])
                S.barrier()
            if "g1" in tapd:
                S.dma("sp", tapd["g1"], comb_g1[:], sem="tap"); S.dma("sp", tapd["oh1"], oh1a[:].rearrange("p t e -> p (t e)"), sem="tap")
                S.barrier()
            gate(106)
            with ExitStack() as es3:
                cntx = sb(es3, "cntx", [P, 16, 32]); tot = sb(es3, "tot", [P, 32])
                ci = sb(es3, "ci", [P, 32], I32); pad = sb(es3, "pad", [P, 32]); incl = sb(es3, "incl", [P, 32])
                base = sb(es3, "base", [P, 32]); zz = sb(es3, "zz", [P, 32])
                slot = sb(es3, "slot", [P, 16, 32]); tmp3 = sb(es3, "tmp3", [P, 16, 32])
                s1f = sb(es3, "s1f", [P, 16]); s2f = sb(es3, "s2f", [P, 16])
                jv = sb(es3, "jv", [P, NT]); pidf = sb(es3, "pidf", [P, 1])
                cmp3 = sb(es3, "cmp3", [P, NT, 32]); ejf = sb(es3, "ejf", [P, NT])
                tid = sb(es3, "tid", [P, 16, 16], I32)
                zi = sb(es3, "zi", [P, NS * 16 // P], I32)
                bk = nbank(); bk2 = nbank()
                for tt in range(16):
                    for t2 in range(tt + 1):
                        PE_mm(psb[bk][:, tt * 32:(tt + 1) * 32], cmb[:] if t2 == tt else onesb[:], selb[:, t2, :],
                              t2 == 0, t2 == tt, [("selb", t2)], [pk(bk)], inc=(t2 == tt), sg=True)
                for tt in range(16):
                    PE_mm(psb[bk2][:, 0:32], onesb[:], selb[:, tt, :], tt == 0, tt == 15, [("selb", tt)], [pk(bk2)], inc=(tt == 15))
                V_cp(cntx[:].rearrange("p t e -> p (t e)"), psb[bk][:, :], rk=[pk(bk)], wk=["cntx"])
                V_cp(tot[:], psb[bk2][:, 0:32], rk=[pk(bk2)], wk=["tot"])
                S.op("dve", lambda: nc.vector.memset(zz[:], 0.0), writes=["zz"])
                V_ts(ci[:], tot[:], float(TS - 1), ALU.add)
                S.op("dve", lambda: nc.vector.tensor_scalar(out=ci[:], in0=ci[:], scalar1=8, scalar2=8,
                                                            op0=ALU.arith_shift_right, op1=ALU.logical_shift_left),
                     reads=["ci"], writes=["ci"])
                V_cp(pad[:], ci[:])
                S.op("dve", lambda: nc.vector.tensor_tensor_scan(out=incl[:], data0=pad[:], data1=zz[:], initial=0.0,
                                                                 op0=ALU.add, op1=ALU.add),
                     reads=["pad", "zz"], writes=["incl"])
                V_tt(base[:], incl[:], pad[:], ALU.subtract)
                V_tt(slot[:], cntx[:], base[:].unsqueeze(1).broadcast_to([P, 16, 32]), ALU.add, rk=["cntx", "base"], wk=["slot"])
                V_tt(tmp3[:], slot[:], oh1a[:], ALU.mult, rk=["slot"], wk=["tmp3"])
                S.op("dve", lambda: nc.vector.tensor_reduce(out=s1f[:], in_=tmp3[:], axis=mybir.AxisListType.X, op=ALU.add),
                     reads=["tmp3"], writes=["s1f"])
                V_cp(s1i[:], s1f[:])
                V_tt(tmp3[:], slot[:], oh2a[:], ALU.mult, rk=["slot", "s1f"], wk=["tmp3"])
                S.op("dve", lambda: nc.vector.tensor_reduce(out=s2f[:], in_=tmp3[:], axis=mybir.AxisListType.X, op=ALU.add),
                     reads=["tmp3"], writes=["s2f"])
                V_cp(s2i[:], s2f[:])
                S.op("pool", lambda: nc.gpsimd.iota(jv[:], pattern=[[TS, NT]], base=0, channel_multiplier=0,
                                                    allow_small_or_imprecise_dtypes=True), writes=["jv"])
                S.op("pool", lambda: nc.gpsimd.iota(pidf[:], pattern=[[0, 1]], base=0, channel_multiplier=1,
                                                    allow_small_or_imprecise_dtypes=True), writes=["pidf"])
                V_tt(cmp3[:], incl[:].unsqueeze(1).broadcast_to([P, NT, 32]), jv[:].unsqueeze(2).broadcast_to([P, NT, 32]),
                     ALU.is_le, rk=["incl", "jv"], wk=["cmp3"])
                S.op("dve", lambda: nc.vector.tensor_reduce(out=ejf[:], in_=cmp3[:], axis=mybir.AxisListType.X, op=ALU.add),
                     reads=["cmp3"], writes=["ejf"])
                emp = sb(es3, "emp", [P, NT])
                V_ts(emp[:], ejf[:], 32.0, ALU.is_ge, s2=65536.0, op1=ALU.mult, rk=["ejf"], wk=["emp"])
                V_ts(ejf[:], ejf[:], 31.0, ALU.min, s2=128.0, op1=ALU.mult)
                V_ts(ejf[:], ejf[:], pidf[:, 0:1], ALU.add)
                V_tt(ejf[:], ejf[:], emp[:], ALU.add)
                V_cp(widx[:], ejf[:])
                S.op("pool", lambda: nc.gpsimd.iota(tid[:], pattern=[[P, 16], [0, 16]], base=0, channel_multiplier=1), writes=["tid"])
                S.op("dve", lambda: nc.vector.memset(zi[:], 1 << 20), writes=["zi"])
                S.dma("sp", stok.rearrange("(p a) f -> p (a f)", p=P), zi[:], reads=["zi"], writes=["stok"], sem="stz")
                for tt in range(16):
                    for (sx, nm_) in ((s1i, "s1i"), (s2i, "s2i")):
                        S.idma(stok, sx[:, tt:tt + 1], tid[:, tt, :], None, reads=[nm_, "tid", "stok"], writes=[("stokw", tt, nm_)], sem="scat")
                S.barrier()
            if "s1i" in tapd:
                S.dma("pool", tapd["s1i"], s1i[:], sem="tap"); S.dma("pool", tapd["widx"], widx[:], sem="tap")
                S.barrier()
            gate(107)
            with ExitStack() as es3:
                tix = [sb(es3, "tix%d" % i, [P, 2], I32) for i in range(2)]
                xg = [sb(es3, "xg%d" % i, [P, 2, D]) for i in range(2)]
                xT = [sb(es3, "xT%d" % i, [P, 16, TS], BF16) for i in range(2)]
                wgb = [sb(es3, "wgb%d" % i, [P, 16, DFF], BF16) for i in range(2)]
                wub = [sb(es3, "wub%d" % i, [P, 16, DFF], BF16) for i in range(2)]
                wdb = [sb(es3, "wdb%d" % i, [P, 4, D], BF16) for i in range(2)]
                hidb = [sb(es3, "hidb%d" % i, [P, 4, TS], BF16) for i in range(2)]
                sgm = [sb(es3, "sgm%d" % i, [P, DFF]) for i in range(2)]
                ysb = [sb(es3, "ysb%d" % i, [P, D]) for i in range(2)]
                for i_ in range(2):
                    S.op("dve", lambda: nc.vector.memset(xg[i_][:], 0.0), writes=[("xg", i_, 0), ("xg", i_, 1)])
                    S.op("dve", lambda: nc.vector.memset(wgb[i_][:], 0.0), writes=["wgb%d" % i_])
                    S.op("dve", lambda: nc.vector.memset(wub[i_][:], 0.0), writes=["wub%d" % i_])
                    S.op("dve", lambda: nc.vector.memset(wdb[i_][:], 0.0), writes=["wdb%d" % i_])
                wgv = expert_w_gate.rearrange("e (p c) f -> (e p) (c f)", p=P)
                wuv = expert_w_up.rearrange("e (p c) f -> (e p) (c f)", p=P)
                wdv = expert_w_down.rearrange("e (p c) d -> (e p) (c d)", p=P)
                ycnt_ = [0]

                def Lt(j, sl):
                    for h in range(2):
                        S.dma("sp", tix[sl][:, h:h + 1], stok[j * TS + h * P:j * TS + (h + 1) * P, 0:1],
                              writes=["tix%d" % sl], sem="tix%d" % sl, allow_slow_non_contiguous=True)
                    for h in range(2):
                        S.idma(xg[sl][:, h, :], None, h2d, tix[sl][:, h:h + 1], reads=["tix%d" % sl], writes=[("xg", sl, h)], sem="xg%d" % sl, bounds=T - 1)
                    S.idma(wgb[sl][:].rearrange("p c f -> p (c f)"), None, wgv, widx[:, j:j + 1], reads=[], writes=["wgb%d" % sl], sem="wgb%d" % sl, bounds=NE * P - 1)
                    S.idma(wub[sl][:].rearrange("p c f -> p (c f)"), None, wuv, widx[:, j:j + 1], reads=[], writes=["wub%d" % sl], sem="wub%d" % sl, bounds=NE * P - 1)
                    S.idma(wdb[sl][:].rearrange("p c f -> p (c f)"), None, wdv, widx[:, j:j + 1], reads=[], writes=["wdb%d" % sl], sem="wdb%d" % sl, bounds=NE * P - 1)

                def Ct(j, sl):
                    for h in range(2):
                        for cb in range(4):
                            bk = nbank(0, 4)
                            for jj in range(4):
                                c = cb * 4 + jj
                                PE_T(psb[bk][:, jj * P:(jj + 1) * P], xg[sl][:, h, c::16], P, [("xg", sl, h), "ident"], [pk(bk)], inc=(jj == 3))
                            evac_copy(xT[sl][:, cb * 4:(cb + 1) * 4, h * P:(h + 1) * P],
                                      psb[bk][:, :].rearrange("p (j s) -> p j s", j=4), bk, [("xT", sl, h, cb)])
                    xk_ = [("xT", sl, h, cb) for h in range(2) for cb in range(4)]
                    for h in range(2):
                        bg, bu = nbank(4, 8), nbank(4, 8)
                        xkh = [("xT", sl, h, cb) for cb in range(4)]
                        for c in range(16):
                            PE_mm(psb[bg][:, :], xT[sl][:, c, h * P:(h + 1) * P], wgb[sl][:, c, :], c == 0, c == 15,
                                  ["wgb%d" % sl] + xkh, [pk(bg)], inc=(c == 15))
                        for c in range(16):
                            PE_mm(psb[bu][:, :], xT[sl][:, c, h * P:(h + 1) * P], wub[sl][:, c, :], c == 0, c == 15,
                                  ["wub%d" % sl] + xkh, [pk(bu)], inc=(c == 15))
                        s2 = h
                        A_act(sgm[s2][:], psb[bg][:, :], AF.Silu, rk=[pk(bg)], wk=["sgm%d" % s2])
                        V_tt(sgm[s2][:], sgm[s2][:], psb[bu][:, :], ALU.mult, rk=["sgm%d" % s2, pk(bu)], wk=["sgm%d" % s2])
                        bt = nbank(0, 4)
                        for fc in range(4):
                            PE_T(psb[bt][:, fc * P:(fc + 1) * P], sgm[s2][:, fc::4], P, ["sgm%d" % s2, "ident"], [pk(bt)], inc=(fc == 3))
                        evac_copy(hidb[sl][:, :, h * P:(h + 1) * P], psb[bt][:, :].rearrange("p (f s) -> p f s", f=4), bt,
                                  [("hidb", sl, fc_, h) for fc_ in range(4)])
                    for h in range(2):
                        ys = ycnt_[0] % 2
                        ycnt_[0] += 1
                        for db in range(4):
                            bk = nbank(0, 4)
                            for fc in range(4):
                                PE_mm(psb[bk][:, :], hidb[sl][:, fc, h * P:(h + 1) * P], wdb[sl][:, fc, db * 512:(db + 1) * 512],
                                      fc == 0, fc == 3, ["wdb%d" % sl, ("hidb", sl, fc, h)], [pk(bk)], inc=(fc == 3))
                            evac_copy(ysb[ys][:, db * 512:(db + 1) * 512], psb[bk][:, :], bk, [("ysb", ys, db)])
                        r0 = j * TS + h * P
                        S.dma("sp", yd[r0:r0 + P, :], ysb[ys][:], reads=[("ysb", ys, db_) for db_ in range(4)], sem="yw%d" % ys)


                NH = 32
                Lt(0, 0); Lt(1, 1)
                for k in range(NH):
                    sl = k % 2
                    Ct(k, sl)
                    if k < NT - NH:
                        Lt(NT - 1 - k, sl); Ct(NT - 1 - k, sl)
                    if k + 2 < NH:
                        Lt(k + 2, sl)
                S.barrier()
            gate(108)
            with ExitStack() as es3:
                xa = [sb(es3, "xa%d" % i, [P, D]) for i in range(2)]
                y1 = [sb(es3, "y1_%d" % i, [P, D]) for i in range(2)]
                y2 = [sb(es3, "y2_%d" % i, [P, D]) for i in range(2)]
                for tt in range(16):
                    sl = tt % 2
                    r0 = tt * P
                    S.dma("sp", xa[sl][:], x2d[r0:r0 + P, :], writes=["xa%d" % sl], sem="xa%d" % sl)
                    S.idma(y1[sl][:], None, yd, s1i[:, tt:tt + 1], reads=[], writes=["y1_%d" % sl], sem="y1_%d" % sl)
                    S.idma(y2[sl][:], None, yd, s2i[:, tt:tt + 1], reads=[], writes=["y2_%d" % sl], sem="y2_%d" % sl)
                    V_stt(xa[sl][:], y1[sl][:], comb_g1[:, tt:tt + 1], xa[sl][:], ALU.mult, ALU.add,
                          rk=["y1_%d" % sl, "xa%d" % sl], wk=["xa%d" % sl])
                    V_stt(xa[sl][:], y2[sl][:], comb_g2[:, tt:tt + 1], xa[sl][:], ALU.mult, ALU.add,
                          rk=["y2_%d" % sl, "xa%d" % sl], wk=["xa%d" % sl])
                    S.dma("sp", out[r0:r0 + P, :], xa[sl][:], reads=["xa%d" % sl], sem="outd%d" % sl)
                S.barrier()

        S.barrier()
        import os
        if os.environ.get("KDEBUG"):
            print("sched counts", S.cnt, {k: v[1] for k, v in S.dsem.items()})
    return nc


_INPUT_LAYOUT = {
    "x": None,
}


def _prep_inputs(inputs, b):
    g = lambda k: np.ascontiguousarray(np.asarray(inputs[k], dtype=np.float32))
    m = {
        "x": g("x")[b],
        "attn_norm_g": g("attn_norm_g")[0].reshape(16, 128),
        "w_in": g("w_in")[0],
        "lambda_re": g("lambda_re")[0].reshape(32, 128),
        "lambda_im": g("lambda_im")[0].reshape(32, 128),
        "log_dt": g("log_dt")[0].reshape(32, 2),
        "ssm_b_re": g("ssm_b_re")[0].reshape(32, 2048),
        "ssm_b_im": g("ssm_b_im")[0].reshape(32, 2048),
        "ssm_c_re": g("ssm_c_re")[0].reshape(32, 2048),
        "ssm_c_im": g("ssm_c_im")[0].reshape(32, 2048),
        "ssm_d": g("ssm_d")[0].reshape(8, 128),
        "w_glu": g("w_glu")[0],
        "q_norm_g": g("q_norm_g")[0].reshape(1, 128),
        "k_norm_g": g("k_norm_g")[0].reshape(1, 128),
        "w_branch_ssm": g("w_branch_ssm")[0],
        "w_branch_att": g("w_branch_att")[0],
        "w_out": g("w_out")[0],
        "ffn_norm_g": g("ffn_norm_g")[0].reshape(16, 128),
        "ffn_norm_g_row": g("ffn_norm_g")[0].reshape(1, D),
        "router_group_w": g("router_group_w")[0],
        "router_group_b": g("router_group_b")[0].reshape(1, 4),
        "router_expert_w": g("router_expert_w")[0],
        "router_expert_b": g("router_expert_b")[0].reshape(1, 32),
        "expert_w_gate": g("expert_w_gate")[0],
        "expert_w_up": g("expert_w_up")[0],
        "expert_w_down": g("expert_w_down")[0],
    }
    return m


def kernel(**inputs):
    nc = build()
    shared = _prep_inputs(inputs, 0)
    xs = np.asarray(inputs["x"], dtype=np.float32)
    in_maps = []
    for b in range(8):
        m = dict(shared)
        m["x"] = np.ascontiguousarray(xs[b])
        in_maps.append(m)
    res = run_bass_kernel_spmd(nc, in_maps, core_ids=list(range(8)))
    return np.stack([np.asarray(r["out"], dtype=np.float32) for r in res.results], axis=0)
```

```python
import math
from contextlib import ExitStack

import numpy as np
import concourse.bass as bass
import concourse.mybir as mybir
from concourse.bass_utils import run_bass_kernel_spmd

F32 = mybir.dt.float32
BF16 = mybir.dt.bfloat16
I32 = mybir.dt.int32
AF = mybir.ActivationFunctionType
ALU = mybir.AluOpType

T = 2048
D = 2048
P = 128
NCK = 256
EPS = 1e-6
NE = 32
DFF = 512


class Sched:
    def __init__(self, nc):
        self.nc = nc
        self.eng = {"pe": nc.tensor, "act": nc.scalar, "dve": nc.vector,
                    "pool": nc.gpsimd, "sp": nc.sync}
        self.sem = {e: nc.alloc_semaphore(name="s_" + e) for e in self.eng}
        self.cnt = {e: 0 for e in self.eng}
        self.waited = {e: {} for e in self.eng}
        self.res = {}
        self.dsem = {}
        self.rr = 0
        self.dead = False
        self.bregs = {}

    def _toks(self, reads, writes):
        toks = []
        for k in reads:
            st = self.res.get(k)
            if st is not None and st[0] is not None:
                toks.append(st[0])
        for k in writes:
            st = self.res.get(k)
            if st is not None:
                if st[0] is not None:
                    toks.append(st[0])
                toks.extend(st[1])
        return toks

    def _wait(self, e, toks):
        need = {}
        for (s, v) in toks:
            if s == e and e in ("pe", "sp"):
                continue
            if v > need.get(s, 0):
                need[s] = v
        for s, v in need.items():
            if self.waited[e].get(s, 0) >= v:
                continue
            h = self.sem[s] if s in self.sem else self.dsem[s][0]
            self.eng[e].wait_ge(h, v)
            self.waited[e][s] = v

    def _mark(self, tok, reads, writes):
        for k in reads:
            st = self.res.setdefault(k, [None, []])
            st[1].append(tok)
            if len(st[1]) > 24:
                mx = {}
                for (s, v) in st[1]:
                    if v > mx.get(s, 0):
                        mx[s] = v
                st[1] = list(mx.items())
        for k in writes:
            self.res[k] = [tok, []]

    def op(self, e, fn, reads=(), writes=(), inc=True):
        if self.dead:
            return None
        self._wait(e, self._toks(reads, writes))
        ins = fn()
        tok = (e, self.cnt[e] + 1)
        if inc:
            ins.then_inc(self.sem[e], 1)
            self.cnt[e] += 1
        self._mark(tok, reads, writes)
        return ins

    def dma(self, q, out, in_, reads=(), writes=(), sem="d", **kw):
        if self.dead:
            return None
        self._wait(q, self._toks(reads, writes))
        if sem not in self.dsem:
            self.dsem[sem] = [self.nc.alloc_semaphore(name="d_" + sem), 0]
        d = self.dsem[sem]
        d[1] += 16
        self.eng[q].dma_start(out=out, in_=in_, **kw).then_inc(d[0], 16)
        self._mark((sem, d[1]), reads, writes)

    def idma(self, out, out_idx, in_, in_idx, reads=(), writes=(), sem="id", bounds=None):
        if self.dead:
            return None
        self._wait("pool", self._toks(reads, writes))
        if sem not in self.dsem:
            self.dsem[sem] = [self.nc.alloc_semaphore(name="d_" + sem), 0]
        d = self.dsem[sem]
        d[1] += 16
        oo = bass.IndirectOffsetOnAxis(ap=out_idx, axis=0) if out_idx is not None else None
        io = bass.IndirectOffsetOnAxis(ap=in_idx, axis=0) if in_idx is not None else None
        kw = {}
        if bounds is not None:
            if bounds not in self.bregs:
                self.bregs[bounds] = self.nc.gpsimd.to_reg(bounds)
            kw = {"bounds_check": self.bregs[bounds], "oob_is_err": False}
        self.nc.gpsimd.indirect_dma_start(out=out, out_offset=oo, in_=in_, in_offset=io, **kw).then_inc(d[0], 16)
        self._mark((sem, d[1]), reads, writes)

    def barrier(self):
        toks = [(e, self.cnt[e]) for e in self.eng if self.cnt[e] > 0]
        toks += [(s, d[1]) for s, d in self.dsem.items() if d[1] > 0]
        for e in self.eng:
            self._wait(e, [t for t in toks if t[0] != e])
        self.res = {}

    def alt(self):
        self.rr ^= 1
        return "act" if self.rr else "dve"


class _Stop(Exception):
    pass


def build(upto="all", taps=()):
    import os
    kgate = int(os.environ.get("KGATE", "0"))

    gate_s = [None]

    def gate(n):
        if kgate == n:
            gate_s[0].dead = True
    nc = bass.Bass("TRN2", target_bir_lowering=False)
    S = Sched(nc)
    gate_s[0] = S
    dram = {}

    def din(name, shape):
        dram[name] = nc.dram_tensor(name, list(shape), F32, kind="ExternalInput").ap()
        return dram[name]

    x = din("x", [T, D])
    attn_norm_g = din("attn_norm_g", [16, 128])
    w_in = din("w_in", [D, 8192])
    lambda_re = din("lambda_re", [32, 128])
    lambda_im = din("lambda_im", [32, 128])
    log_dt = din("log_dt", [32, 2])
    ssm_b_re = din("ssm_b_re", [32, 2048])
    ssm_b_im = din("ssm_b_im", [32, 2048])
    ssm_c_re = din("ssm_c_re", [32, 2048])
    ssm_c_im = din("ssm_c_im", [32, 2048])
    ssm_d = din("ssm_d", [8, 128])
    w_glu = din("w_glu", [1024, 1024])
    q_norm_g = din("q_norm_g", [1, 128])
    k_norm_g = din("k_norm_g", [1, 128])
    w_branch_ssm = din("w_branch_ssm", [1024, 2048])
    w_branch_att = din("w_branch_att", [1024, 2048])
    w_out = din("w_out", [D, D])
    ffn_norm_g = din("ffn_norm_g", [16, 128])
    ffn_norm_g_row = din("ffn_norm_g_row", [1, D])
    router_group_w = din("router_group_w", [D, 4])
    router_group_b = din("router_group_b", [1, 4])
    router_expert_w = din("router_expert_w", [D, 32])
    router_expert_b = din("router_expert_b", [1, 32])
    expert_w_gate = din("expert_w_gate", [NE, D, DFF])
    expert_w_up = din("expert_w_up", [NE, D, DFF])
    expert_w_down = din("expert_w_down", [NE, DFF, D])
    out = nc.dram_tensor("out", [T, D], F32, kind="ExternalOutput").ap()
    x2d = nc.dram_tensor("x2_scratch", [T, D], F32, kind="Internal").ap()
    tapd = {}
    for (nm, shp) in taps:
        tapd[nm] = nc.dram_tensor("tap_" + nm, list(shp), F32, kind="ExternalOutput").ap()

    top = ExitStack()
    with top:
        def sb(es, name, shape, dt=F32):
            return es.enter_context(nc.sbuf_tensor(name, list(shape), dt))

        ps_all = top.enter_context(nc.psum_tensor("ps_all", [P, 4096], F32))
        psb = [ps_all[:, i * 512:(i + 1) * 512] for i in range(8)]
        bank_rr = [0]

        def nbank(lo=0, hi=8):
            b = lo + (bank_rr[0] % (hi - lo))
            bank_rr[0] += 1
            return b

        ident = sb(top, "ident", [P, P])
        ones = sb(top, "ones", [P, P])
        epsc = sb(top, "epsc", [P, 1])
        S.op("dve", lambda: nc.vector.memset(ones[:], 1.0), writes=["ones"])
        S.op("dve", lambda: nc.vector.memset(epsc[:], EPS), writes=["epsc"])
        S.op("pool", lambda: nc.gpsimd.affine_select(
            out=ident[:], in_=ones[:], pattern=[[1, P]], compare_op=ALU.is_equal,
            fill=0.0, base=0, channel_multiplier=-1), reads=["ones"], writes=["ident"])

        def tap(nm, src_ap, key):
            if nm in tapd:
                S.dma("sp", tapd[nm], src_ap, reads=[key], sem="tap")

        tri = sb(top, "tri", [P, P]); lem = sb(top, "lem", [P, P]); cmf = sb(top, "cmf", [P, P])
        cmb = sb(top, "cmb", [P, P], BF16); zer = sb(top, "zer", [P, 512], BF16)
        trib = sb(top, "trib", [P, P], BF16); lemb = sb(top, "lemb", [P, P], BF16)
        gq = sb(top, "gq", [P, 1]); gk = sb(top, "gk", [P, 1])
        g1s = sb(top, "g1s", [16, P])
        g1T = sb(top, "g1T", [P, 16])
        g2s = sb(top, "g2s", [16, P])
        g2T = sb(top, "g2T", [P, 16])
        es_mix = ExitStack()
        hT = sb(es_mix, "hT", [P, 16, T], BF16)
        s5T = sb(es_mix, "s5T", [P, 8, T], BF16)
        S.dma("sp", g1s[:], attn_norm_g, writes=["g1s"], sem="m1")
        b = nbank()
        S.op("pe", lambda: nc.tensor.transpose(out=psb[b][:, 0:16], in_=g1s[:], identity=ident[0:16, 0:16]),
             reads=["g1s", "ident"], writes=["ps%d" % b])
        S.op("dve", lambda: nc.vector.tensor_copy(out=g1T[:], in_=psb[b][:, 0:16]),
             reads=["ps%d" % b], writes=["g1T"])

        def rmsnorm_to_T(es, src_rows, gT, dstT, dst_key, ncols_off=0, ntiles=16, pfx="n1"):
            xt = [sb(es, pfx + "_xt%d" % i, [P, D]) for i in range(2)]
            junk = sb(es, pfx + "_junk", [P, D], BF16)
            ss = sb(es, pfx + "_ss", [P, ntiles])
            rs = sb(es, pfx + "_rs", [P, ntiles])
            for tt in range(ntiles):
                sl = tt % 2
                xk = pfx + "xt%d" % sl
                S.dma("sp", xt[sl][:], src_rows(tt), writes=[xk], sem=pfx + "x%d" % sl)
                S.op("act", lambda: nc.scalar.activation(out=junk[:], in_=xt[sl][:], func=AF.Square,
                                                         accum_out=ss[:, tt:tt + 1]),
                     reads=[xk], writes=[pfx + "junk", (pfx + "ss", tt)])
                S.op("act", lambda: nc.scalar.activation(out=rs[:, tt:tt + 1], in_=ss[:, tt:tt + 1], func=AF.Sqrt,
                                                         bias=epsc[:, 0:1], scale=1.0 / D),
                     reads=[(pfx + "ss", tt), "epsc"], writes=[(pfx + "rs", tt)])
                S.op("dve", lambda: nc.vector.reciprocal(out=rs[:, tt:tt + 1], in_=rs[:, tt:tt + 1]),
                     reads=[(pfx + "rs", tt)], writes=[(pfx + "rs", tt)])
                S.op("dve", lambda: nc.vector.tensor_scalar(out=xt[sl][:], in0=xt[sl][:], scalar1=rs[:, tt:tt + 1],
                                                            scalar2=None, op0=ALU.mult),
                     reads=[xk, (pfx + "rs", tt)], writes=[xk])
                for cb in range(4):
                    bk = nbank()
                    for j in range(4):
                        c = cb * 4 + j
                        S.op("pe", lambda: nc.tensor.transpose(out=psb[bk][:, j * P:(j + 1) * P],
                                                               in_=xt[sl][:, c * P:(c + 1) * P], identity=ident[:]),
                             reads=[xk, "ident"], writes=["ps%d" % bk], inc=(j == 3))
                    for j in range(4):
                        c = cb * 4 + j
                        dst = dstT[:, c, ncols_off + tt * P: ncols_off + (tt + 1) * P]
                        e = S.alt()
                        if e == "dve":
                            S.op("dve", lambda: nc.vector.tensor_scalar(out=dst, in0=psb[bk][:, j * P:(j + 1) * P],
                                                                        scalar1=gT[:, c:c + 1], scalar2=None,
                                                                        op0=ALU.mult),
                                 reads=["ps%d" % bk], writes=[(dst_key, tt)])
                        else:
                            S.op("act", lambda: nc.scalar.activation(out=dst, in_=psb[bk][:, j * P:(j + 1) * P],
                                                                     func=AF.Copy, scale=gT[:, c:c + 1]),
                                 reads=["ps%d" % bk], writes=[(dst_key, tt)])

        with ExitStack() as es:
            rmsnorm_to_T(es, lambda tt: x[tt * P:(tt + 1) * P, :], g1T, hT, "hT")
            S.barrier()
        gate(101)
        if "hT" in tapd:
            with ExitStack() as es:
                tmp = sb(es, "taptmp", [P, 16, T])
                S.op("dve", lambda: nc.vector.tensor_copy(out=tmp[:], in_=hT[:]), writes=["taptmp"])
                S.dma("sp", tapd["hT"].rearrange("p (c t) -> p c t", c=16), tmp[:], reads=["taptmp"], sem="tap")
                S.barrier()


        def kn(ap):
            return ap.name

        def V_tt(out, a, b, op, rk=None, wk=None, e="dve"):
            en = nc.vector if e == "dve" else nc.gpsimd
            return S.op(e, lambda: en.tensor_tensor(out=out, in0=a, in1=b, op=op),
                        reads=rk if rk is not None else [kn(a), kn(b)],
                        writes=wk if wk is not None else [kn(out)])

        def V_ts(out, a, s1, op0, s2=None, op1=None, rk=None, wk=None):
            kw = {}
            if op1 is not None:
                kw["op1"] = op1
            r = rk if rk is not None else [kn(a)] + [kn(s) for s in (s1, s2) if hasattr(s, "name")]
            return S.op("dve", lambda: nc.vector.tensor_scalar(out=out, in0=a, scalar1=s1, scalar2=s2, op0=op0, **kw),
                        reads=r, writes=wk if wk is not None else [kn(out)])

        def V_stt(out, a, s, b, op0, op1, rk=None, wk=None):
            r = rk if rk is not None else [kn(a), kn(b)] + ([kn(s)] if hasattr(s, "name") else [])
            return S.op("dve", lambda: nc.vector.scalar_tensor_tensor(out=out, in0=a, scalar=s, in1=b, op0=op0, op1=op1),
                        reads=r, writes=wk if wk is not None else [kn(out)])

        def V_cp(out, a, rk=None, wk=None, e="dve"):
            if e == "act":
                return S.op("act", lambda: nc.scalar.copy(out=out, in_=a),
                            reads=rk if rk is not None else [kn(a)], writes=wk if wk is not None else [kn(out)])
            en = nc.vector if e == "dve" else nc.gpsimd
            return S.op(e, lambda: en.tensor_copy(out=out, in_=a),
                        reads=rk if rk is not None else [kn(a)], writes=wk if wk is not None else [kn(out)])

        def A_act(out, a, func, scale=1.0, bias=None, rk=None, wk=None, accum_out=None):
            kw = {}
            if bias is not None:
                kw["bias"] = bias
            if accum_out is not None:
                kw["accum_out"] = accum_out
            r = rk if rk is not None else [kn(a)] + [kn(s) for s in (scale, bias) if hasattr(s, "name")]
            return S.op("act", lambda: nc.scalar.activation(out=out, in_=a, func=func, scale=scale, **kw),
                        reads=r, writes=wk if wk is not None else [kn(out)])

        def PE_T(out, in_, n, rk, wk, inc=True):
            return S.op("pe", lambda: nc.tensor.transpose(out=out, in_=in_, identity=ident[0:n, 0:n]),
                        reads=rk, writes=wk, inc=inc)

        def PE_mm(out, lhsT, rhs, start, stop, rk, wk, inc=True, tp=None, sg=False):
            kw = {}
            if sg:
                kw["skip_group_check"] = True
            if tp is not None:
                kw["tile_position"] = tp
            return S.op("pe", lambda: nc.tensor.matmul(out, lhsT=lhsT, rhs=rhs, start=start, stop=stop, **kw),
                        reads=rk, writes=wk, inc=inc)

        def pk(b):
            return "ps%d" % b

        def tap_bf(nm, src, key_list):
            if nm in tapd:
                S.dma("pool", tapd[nm], src, reads=key_list, sem="tap")

        es_ssm = ExitStack()
        APr = sb(es_ssm, "APr", [P, 9, 32]); APi = sb(es_ssm, "APi", [P, 9, 32])
        AKr = sb(es_ssm, "AKr", [P, 8, 32]); AKi = sb(es_ssm, "AKi", [P, 8, 32]); AKn = sb(es_ssm, "AKn", [P, 8, 32])
        BBr = sb(es_ssm, "BBr", [P, 16, 32]); BBi = sb(es_ssm, "BBi", [P, 16, 32])
        CRt = sb(es_ssm, "CRt", [P, 16, 32]); CIt = sb(es_ssm, "CIt", [P, 16, 32])
        Dcol = sb(es_ssm, "Dcol", [P, 8])
        with ExitStack() as es:
            st_lr = sb(es, "st_lr", [32, P]); st_li = sb(es, "st_li", [32, P])
            st_dt = sb(es, "st_dt", [32, 2]); st_dtb = sb(es, "st_dtb", [32, P])
            st_b1 = sb(es, "st_b", [32, 2048]); st_c1 = sb(es, "st_c", [32, 2048])
            st_b21 = sb(es, "st_b2", [32, 16, P]); st_c21 = sb(es, "st_c2", [32, 16, P])
            st_b = [st_b1, st_b1]; st_c = [st_c1, st_c1]; st_b2 = [st_b21, st_b21]; st_c2 = [st_c21, st_c21]
            st_d = sb(es, "st_d", [8, P])
            LLD = sb(es, "LLD", [P, 96])
            BRt = sb(es, "BRt", [P, 16, 32]); BIt = sb(es, "BIt", [P, 16, 32])
            wk_ = [sb(es, "pw%d" % i, [P, 32]) for i in range(12)]
            S.dma("sp", st_lr[:], lambda_re, writes=["st_lr"], sem="m2")
            S.dma("sp", st_li[:], lambda_im, writes=["st_li"], sem="m3")
            S.dma("sp", st_dt[:], log_dt, writes=["st_dt"], sem="m4")
            S.dma("sp", st_d[:], ssm_d, writes=["st_d"], sem="m5")
            for g2 in range(2):
                V_ts(st_dtb[:, g2 * 64:(g2 + 1) * 64], ones[0:32, 0:64], st_dt[:, g2:g2 + 1], ALU.mult,
                     rk=["ones", "st_dt"], wk=["st_dtb"])
            bk = nbank()
            PE_T(psb[bk][:, 0:32], st_lr[:], 32, ["st_lr", "ident"], [pk(bk)], inc=False)
            PE_T(psb[bk][:, 32:64], st_li[:], 32, ["st_li", "ident"], [pk(bk)], inc=False)
            PE_T(psb[bk][:, 64:96], st_dtb[:], 32, ["st_dtb", "ident"], [pk(bk)])
            V_cp(LLD[:], psb[bk][:, 0:96], rk=[pk(bk)], wk=["LLD"])
            bk = nbank()
            PE_T(psb[bk][:, 0:8], st_d[:], 8, ["st_d", "ident"], [pk(bk)])
            V_cp(Dcol[:], psb[bk][:, 0:8], rk=[pk(bk)], wk=["Dcol"])
            for ri in range(2):
                S.dma("sp", st_b[ri][:], (ssm_b_re, ssm_b_im)[ri], writes=["st_b"], sem="stb")
                S.dma("sp", st_c[ri][:], (ssm_c_re, ssm_c_im)[ri], writes=["st_c"], sem="stc")
                V_cp(st_b2[ri][:], st_b[ri][:].rearrange("q (gp h) -> q h gp", h=16), rk=["st_b"], wk=["st_b2"])
                V_cp(st_c2[ri][:].rearrange("q h (g2 p) -> q g2 h p", g2=2),
                     st_c[ri][:].rearrange("q (g2 h p) -> q g2 h p", g2=2, h=16), rk=["st_c"], wk=["st_c2"])
                for (srcs, dst) in ((st_b2[ri], (BRt, BIt)[ri]), (st_c2[ri], (CRt, CIt)[ri])):
                    bk = nbank()
                    for h in range(16):
                        PE_T(psb[bk][:, h * 32:(h + 1) * 32], srcs[:, h, :], 32, [kn(srcs[:]), "ident"], [pk(bk)], inc=(h == 15))
                    V_cp(dst[:].rearrange("p h q -> p (h q)"), psb[bk][:, :], rk=[pk(bk)], wk=[kn(dst[:])])
            LR = LLD[:, 0:32]; LI = LLD[:, 32:64]; LDT = LLD[:, 64:96]
            dtv, lrdt, lidt, mag, cc, sn, t1, t2, t3, cre, cim, den = [w_[:] for w_ in wk_]
            A_act(dtv, LDT, AF.Exp)
            V_tt(lrdt, LR, dtv, ALU.mult)
            V_tt(lidt, LI, dtv, ALU.mult)
            A_act(mag, lrdt, AF.Exp)
            halfpi = sb(es, "halfpi", [P, 1])
            S.op("dve", lambda: nc.vector.memset(halfpi[:], math.pi / 2), writes=["halfpi"])
            A_act(sn, lidt, AF.Sin, scale=1.0 / 32)
            A_act(cc, lidt, AF.Sin, scale=1.0 / 32, bias=halfpi[:, 0:1])
            for _ in range(5):
                V_tt(t1, cc, cc, ALU.mult)
                V_tt(t2, sn, sn, ALU.mult)
                V_tt(t3, cc, sn, ALU.mult)
                V_tt(cc, t1, t2, ALU.subtract)
                V_ts(sn, t3, 2.0, ALU.mult)
            S.op("dve", lambda: nc.vector.memset(APr[:, 0, :], 1.0), writes=["APr"])
            S.op("dve", lambda: nc.vector.memset(APi[:, 0, :], 0.0), writes=["APi"])
            V_tt(APr[:, 1, :], mag, cc, ALU.mult)
            V_tt(APi[:, 1, :], mag, sn, ALU.mult)

            def cmul(o_r, o_i, a_r, a_i, b_r, b_i, tA, tB):
                V_tt(tA, a_r, b_r, ALU.mult)
                V_tt(tB, a_i, b_i, ALU.mult)
                V_tt(o_r, tA, tB, ALU.subtract)
                V_tt(tA, a_r, b_i, ALU.mult)
                V_tt(tB, a_i, b_r, ALU.mult)
                V_tt(o_i, tA, tB, ALU.add)

            for e_ in range(1, 8):
                cmul(APr[:, e_ + 1, :], APi[:, e_ + 1, :], APr[:, e_, :], APi[:, e_, :], APr[:, 1, :], APi[:, 1, :], t1, t2)
            V_cp(AKr[:, 0, :], APr[:, 8, :]); V_cp(AKi[:, 0, :], APi[:, 8, :])
            for k in range(7):
                V_tt(t1, AKr[:, k, :], AKr[:, k, :], ALU.mult)
                V_tt(t2, AKi[:, k, :], AKi[:, k, :], ALU.mult)
                V_tt(t3, AKr[:, k, :], AKi[:, k, :], ALU.mult)
                V_tt(AKr[:, k + 1, :], t1, t2, ALU.subtract)
                V_ts(AKi[:, k + 1, :], t3, 2.0, ALU.mult)
            V_ts(AKn[:], AKi[:], -1.0, ALU.mult)
            V_ts(t1, APr[:, 1, :], -1.0, ALU.add, rk=["APr"])
            V_tt(t2, LR, LR, ALU.mult)
            V_tt(t3, LI, LI, ALU.mult)
            V_tt(den, t2, t3, ALU.add)
            S.op("dve", lambda: nc.vector.reciprocal(out=den, in_=den), reads=[kn(den)], writes=[kn(den)])
            V_tt(t2, t1, LR, ALU.mult)
            V_tt(t3, APi[:, 1, :], LI, ALU.mult)
            V_tt(cre, t2, t3, ALU.add)
            V_tt(cre, cre, den, ALU.mult)
            V_tt(t2, APi[:, 1, :], LR, ALU.mult)
            V_tt(t3, t1, LI, ALU.mult)
            V_tt(cim, t2, t3, ALU.subtract)
            V_tt(cim, cim, den, ALU.mult)
            tb1 = sb(es, "tb1", [P, 16, 32]); tb2 = sb(es, "tb2", [P, 16, 32])
            creb = cre.unsqueeze(1).broadcast_to([P, 16, 32]); cimb = cim.unsqueeze(1).broadcast_to([P, 16, 32])
            cmul(BBr[:], BBi[:], creb, cimb, BRt[:], BIt[:], tb1[:], tb2[:])
            S.barrier()


        uT = sb(es_ssm, "uT", [P, 8, T], BF16)

        def load_w(wbuf, key, srcap, kch, ncols, sem):
            S.dma("pool", wbuf[:, 0:kch, 0:ncols], srcap.rearrange("(c p) f -> p c f", p=P), writes=[key], sem=sem)

        def evac_copy(dst, src_ps, bk, wkeys, e=None):
            e = e or S.alt()
            V_cp(dst, src_ps, rk=[pk(bk)], wk=wkeys, e=e)

        with ExitStack() as es:
            wst = [sb(es, "wst%d" % i, [P, 16, 512], BF16) for i in range(2)]
            for blk in range(2):
                sl = blk % 2
                load_w(wst[sl], "wst%d" % sl, w_in[:, blk * 512:(blk + 1) * 512], 16, 512, "wst%d" % sl)
                for m in range(4):
                    for n in range(4):
                        bk = nbank()
                        for k in range(16):
                            PE_mm(psb[bk][:, :], wst[sl][:, k, m * P:(m + 1) * P], hT[:, k, n * 512:(n + 1) * 512],
                                  k == 0, k == 15, ["wst%d" % sl], [pk(bk)], inc=(k == 15))
                        evac_copy(uT[:, blk * 4 + m, n * 512:(n + 1) * 512], psb[bk][:, :], bk, [("uT", blk * 4 + m, n)])
            S.barrier()
        gate(102)
        tap_bf("uT", uT[:].rearrange("p a t -> p (a t)"), [])

        with ExitStack() as es:
            Xu = sb(es, "Xu", [P, 8, 2, 4, 16])
            XP = sb(es, "XP", [P, 8, 2, 4, 32])
            CAu = sb(es, "CAu", [P, 4, 9, 2, 16])
            tq1 = sb(es, "tq1", [P, 4, 16]); tq2 = sb(es, "tq2", [P, 4, 16])
            WS = [sb(es, "WS%d" % i, [P, 8, 2, P], BF16) for i in range(2)]
            WCp = [sb(es, "WCp%d" % i, [P, 4, 9, 2, 32], BF16) for i in range(2)]
            BPb = [sb(es, "BPb%d" % i, [P, 2, 4, 32], BF16) for i in range(2)]
            BD = [sb(es, "BD%d" % i, [P, 8, P], BF16) for i in range(2)]
            Hb = [[sb(es, "Hb%d%d" % (s_, i), [P, 2, NCK]) for i in range(2)] for s_ in range(2)]
            Hbf = [sb(es, "Hbf%d" % i, [P, 2, NCK], BF16) for i in range(4)]
            y32 = sb(es, "y32", [P, 1024])
            S.op("dve", lambda: nc.vector.memset(XP[:], 0.0), writes=["XP"])
            for i in range(2):
                S.op("pool", lambda: nc.gpsimd.memset(WCp[i][:], 0.0), writes=["WCp%d" % i])
                S.op("pool", lambda: nc.gpsimd.memset(BD[i][:], 0.0), writes=["BD%d" % i])
            XPv = XP[:].rearrange("p i r q (g h) -> p (i r q) g h", g=2)
            for a in range(8):
                par = a % 2
                qs = slice(4 * a, 4 * a + 4)
                for ip in range(8):
                    e_ = 7 - ip
                    arb = APr[:, e_, qs].unsqueeze(2).broadcast_to([P, 4, 16])
                    aib = APi[:, e_, qs].unsqueeze(2).broadcast_to([P, 4, 16])
                    bbr = BBr[:, :, qs].rearrange("p h q -> p q h")
                    bbi = BBi[:, :, qs].rearrange("p h q -> p q h")
                    V_tt(tq1[:], arb, bbr, ALU.mult, rk=[], wk=["tq1"])
                    V_tt(tq2[:], aib, bbi, ALU.mult, rk=[], wk=["tq2"])
                    V_tt(Xu[:, ip, 0, :, :], tq1[:], tq2[:], ALU.subtract, rk=["tq1", "tq2"], wk=["Xu"])
                    V_tt(tq1[:], arb, bbi, ALU.mult, rk=[], wk=["tq1"])
                    V_tt(tq2[:], aib, bbr, ALU.mult, rk=[], wk=["tq2"])
                    V_tt(Xu[:, ip, 1, :, :], tq1[:], tq2[:], ALU.add, rk=["tq1", "tq2"], wk=["Xu"])
                Xuv = Xu[:].rearrange("p i r q h -> p (i r q) h")
                for g2 in range(2):
                    V_cp(XPv[g2 * 64:(g2 + 1) * 64, :, g2, :], Xuv[g2 * 64:(g2 + 1) * 64, :, :], rk=["Xu"], wk=["XP"])
                for e_ in range(9):
                    arb = APr[:, e_, qs].unsqueeze(2).broadcast_to([P, 4, 16])
                    aib = APi[:, e_, qs].unsqueeze(2).broadcast_to([P, 4, 16])
                    crr = CRt[:, :, qs].rearrange("p h q -> p q h")
                    cii = CIt[:, :, qs].rearrange("p h q -> p q h")
                    V_tt(tq1[:], crr, arb, ALU.mult, rk=[], wk=["tq1"])
                    V_tt(tq2[:], cii, aib, ALU.mult, rk=[], wk=["tq2"])
                    V_tt(CAu[:, :, e_, 0, :], tq1[:], tq2[:], ALU.subtract, rk=["tq1", "tq2"], wk=["CAu"])
                    V_tt(tq1[:], cii, arb, ALU.mult, rk=[], wk=["tq1"])
                    V_tt(tq2[:], crr, aib, ALU.mult, rk=[], wk=["tq2"])
                    V_stt(CAu[:, :, e_, 1, :], tq1[:], -1.0, tq2[:], ALU.mult, ALU.subtract, rk=["tq1", "tq2"], wk=["CAu"])
                CAuv = CAu[:].rearrange("p q e r h -> p (q e r) h")
                WCv = WCp[par][:].rearrange("p q e r (g h) -> p (q e r) g h", g=2)
                for g2 in range(2):
                    V_cp(WCv[g2 * 64:(g2 + 1) * 64, :, g2, :], CAuv[g2 * 64:(g2 + 1) * 64, :, :], rk=["CAu"], wk=["WCp%d" % par])
                V_cp(BPb[par][:], XP[:, 7, :, :, :], rk=["XP"], wk=["BPb%d" % par])
                for cb in range(4):
                    bk = nbank(6, 8)
                    for j in range(4):
                        ip, ri = divmod(cb * 4 + j, 2)
                        PE_T(psb[bk][:, j * P:(j + 1) * P], XP[:, ip, ri, :, :].rearrange("p q f -> p (q f)"), P,
                             ["XP", "ident"], [pk(bk)], inc=(j == 3))
                    evac_copy(WS[par][:].rearrange("p i r f -> p (i r f)")[:, cb * 512:(cb + 1) * 512], psb[bk][:, :], bk,
                              ["WS%d" % par])
                bk = nbank(6, 8)
                for qq in range(4):
                    for j in range(8):
                        for ri in range(2):
                            PE_mm(psb[bk][32 * qq:32 * qq + 32, j * 32:(j + 1) * 32], BPb[par][:, ri, qq, :],
                                  WCp[par][:, qq, j, ri, :], ri == 0, ri == 1,
                                  ["BPb%d" % par, "WCp%d" % par], [pk(bk)], inc=(qq == 3 and j == 7 and ri == 1),
                                  tp=(0, 32 * qq), sg=True)
                for qq in range(4):
                    V_cp(BD[par][32 * qq:32 * qq + 32, :, 32 * qq:32 * qq + 32],
                         psb[bk][32 * qq:32 * qq + 32, 0:256].rearrange("p (j f) -> p j f", j=8),
                         rk=[pk(bk)], wk=["BD%d" % par])
                for qq in range(4):
                    q = 4 * a + qq
                    hs = q % 2
                    pb = 32 * qq
                    bk = nbank(4, 6)
                    for ri in range(2):
                        for ip in range(8):
                            PE_mm(psb[bk][:, ri * NCK:(ri + 1) * NCK], WS[par][pb:pb + 32, ip, ri, :],
                                  uT[pb:pb + 32, a, ip::8], ip == 0, ip == 7,
                                  ["WS%d" % par] + [("uT", a, n) for n in range(4)], [pk(bk)],
                                  inc=(ri == 1 and ip == 7), tp=(pb, 0))
                    V_cp(Hb[hs][0][:].rearrange("p r c -> p (r c)"), psb[bk][:, :], rk=[pk(bk)],
                         wk=[("Hb", hs, 0, 0), ("Hb", hs, 0, 1)], e="act")
                    for k in range(8):
                        s_ = 1 << k
                        src_ = Hb[hs][k % 2]; dst_ = Hb[hs][(k + 1) % 2]
                        sp_, dp_ = k % 2, (k + 1) % 2
                        n_ = NCK - s_
                        V_cp(dst_[:, :, 0:s_], src_[:, :, 0:s_], rk=[("Hb", hs, sp_, 0), ("Hb", hs, sp_, 1)],
                             wk=[("Hb", hs, dp_, 0), ("Hb", hs, dp_, 1)], e="pool")
                        akr = AKr[:, k, q:q + 1]; aki = AKi[:, k, q:q + 1]; akn = AKn[:, k, q:q + 1]
                        V_stt(dst_[:, 0, s_:], src_[:, 0, 0:n_], akr, src_[:, 0, s_:], ALU.mult, ALU.add,
                              rk=[("Hb", hs, sp_, 0)], wk=[("Hb", hs, dp_, 0)])
                        V_stt(dst_[:, 1, s_:], src_[:, 1, 0:n_], akr, src_[:, 1, s_:], ALU.mult, ALU.add,
                              rk=[("Hb", hs, sp_, 1)], wk=[("Hb", hs, dp_, 1)])
                        V_stt(dst_[:, 0, s_:], src_[:, 1, 0:n_], akn, dst_[:, 0, s_:], ALU.mult, ALU.add,
                              rk=[("Hb", hs, sp_, 1), ("Hb", hs, dp_, 0)], wk=[("Hb", hs, dp_, 0)])
                        V_stt(dst_[:, 1, s_:], src_[:, 0, 0:n_], aki, dst_[:, 1, s_:], ALU.mult, ALU.add,
                              rk=[("Hb", hs, sp_, 0), ("Hb", hs, dp_, 1)], wk=[("Hb", hs, dp_, 1)])
                    V_cp(Hbf[qq][:], Hb[hs][0][:], rk=[("Hb", hs, 0, 0), ("Hb", hs, 0, 1)], wk=[("Hbf", qq)], e="act")
                for i in range(8):
                    for j in range(i + 1):
                        PE_mm(ps_all[:, i * NCK:(i + 1) * NCK], BD[par][:, j, :], uT[:, a, (i - j)::8],
                              (j == 0 and i % 2 == 0), False,
                              ["BD%d" % par] + [("uT", a, n) for n in range(4)], [pk(i // 2)],
                              inc=(j == i), sg=True)
                for qq in range(4):
                    pb = 32 * qq
                    for i in range(8):
                        for ri in range(2):
                            PE_mm(ps_all[pb:pb + 32, i * NCK + 1:(i + 1) * NCK], WCp[par][:, qq, i + 1, ri, :],
                                  Hbf[qq][:, ri, 0:NCK - 1], False, ri == 1,
                                  ["WCp%d" % par, ("Hbf", qq)], [pk(i // 2)], inc=(ri == 1), tp=(0, pb), sg=True)
                for hf in range(2):
                    Yv = ps_all[:, 0:2048].rearrange("p (i c) -> p c i", i=8)[:, hf * 128:(hf + 1) * 128, :]
                    uv = uT[:, a, hf * 1024:(hf + 1) * 1024].rearrange("p (c i) -> p c i", i=8)
                    V_stt(y32[:].rearrange("p (c i) -> p c i", i=8), uv, Dcol[:, a:a + 1], Yv, ALU.mult, ALU.add,
                          rk=[pk(0), pk(1), pk(2), pk(3)] + [("uT", a, n) for n in range(4)], wk=["y32"])
                    A_act(uT[:, a, hf * 1024:(hf + 1) * 1024], y32[:], AF.Gelu_apprx_tanh, rk=["y32"],
                          wk=[("uT", a, 2 * hf), ("uT", a, 2 * hf + 1)])
            tap_bf("zT", uT[:].rearrange("p a t -> p (a t)"), [("uT", a_, n_) for a_ in range(8) for n_ in range(4)])
            S.barrier()
        with ExitStack() as es:
            wglu = sb(es, "wglu", [P, 8, 1024], BF16)
            load_w(wglu, "wglu", w_glu, 8, 1024, "wglu")
            sg = [sb(es, "sg%d" % i, [P, 512], BF16) for i in range(2)]
            for m in range(8):
                for n in range(4):
                    bk = nbank(4, 8)
                    for k in range(8):
                        PE_mm(psb[bk][:, :], wglu[:, k, m * P:(m + 1) * P], uT[:, k, n * 512:(n + 1) * 512],
                              k == 0, k == 7, ["wglu"] + [("uT", k, n)], [pk(bk)], inc=(k == 7))
                    sl = (m * 4 + n) % 2
                    A_act(sg[sl][:], psb[bk][:, :], AF.Sigmoid, rk=[pk(bk)], wk=["sg%d" % sl])
                    V_tt(s5T[:, m, n * 512:(n + 1) * 512], sg[sl][:], uT[:, m, n * 512:(n + 1) * 512], ALU.mult,
                         rk=["sg%d" % sl, ("uT", m, n)], wk=[("s5T", m, n)])
            S.barrier()
        gate(103)
        tap_bf("s5T", s5T[:].rearrange("p a t -> p (a t)"), [])
        reg = {"APr": APr[:].rearrange("p e q -> p (e q)"), "APi": APi[:].rearrange("p e q -> p (e q)"),
               "AKr": AKr[:].rearrange("p e q -> p (e q)"), "BBr": BBr[:].rearrange("p h q -> p (h q)"),
               "BBi": BBi[:].rearrange("p h q -> p (h q)"), "CRt": CRt[:].rearrange("p h q -> p (h q)"),
               "Dcol": Dcol[:]}
        for nm_, ap_ in reg.items():
            if nm_ in tapd:
                S.dma("pool", tapd[nm_], ap_, sem="tap")
        S.barrier()
        es_ssm.close()


        attT = sb(es_mix, "attT", [P, 8, T], BF16)
        S.op("pool", lambda: nc.gpsimd.affine_select(out=tri[:], in_=ones[:], pattern=[[-1, P]], compare_op=ALU.is_gt,
                                                     fill=0.0, base=0, channel_multiplier=1), reads=["ones"], writes=["tri"])
        S.op("pool", lambda: nc.gpsimd.affine_select(out=lem[:], in_=ones[:], pattern=[[1, P]], compare_op=ALU.is_ge,
                                                     fill=0.0, base=0, channel_multiplier=-1), reads=["ones"], writes=["lem"])
        S.op("pool", lambda: nc.gpsimd.affine_select(out=cmf[:], in_=ones[:], pattern=[[1, P]], compare_op=ALU.is_gt,
                                                     fill=0.0, base=0, channel_multiplier=-1), reads=["ones"], writes=["cmf"])
        V_cp(cmb[:], cmf[:])
        V_cp(trib[:], tri[:])
        V_cp(lemb[:], lem[:])
        S.op("dve", lambda: nc.vector.memset(zer[:], 0.0), writes=["zer"])
        S.dma("sp", gq[:], q_norm_g.rearrange("o d -> d o"), writes=["gq"], sem="m6")
        S.dma("sp", gk[:], k_norm_g.rearrange("o d -> d o"), writes=["gk"], sem="m7")
        V_ts(gq[:], gq[:], 1.0 / math.sqrt(128.0), ALU.mult)
        S.barrier()

        for hg in range(2):
            with ExitStack() as es:
                qT = sb(es, "qT%d" % hg, [P, 4, T], BF16); kT = sb(es, "kT%d" % hg, [P, 4, T], BF16)
                vv = sb(es, "vv%d" % hg, [P, 16, 512], BF16)
                with ExitStack() as es2:
                    wst0 = sb(es2, "wstq0_%d" % hg, [P, 16, 512], BF16)
                    wst = [wst0, wst0]
                    sqf = [sb(es2, "sqf%d_%d" % (i, hg), [P, 512]) for i in range(2)]
                    rsq = [sb(es2, "rsq%d_%d" % (i, hg), [P, 512]) for i in range(2)]
                    cnt_ = 0
                    for which, col0 in (("q", 1024 + 512 * hg), ("k", 2048 + 512 * hg), ("v", 3072 + 512 * hg)):
                        sl = 0
                        load_w(wst[sl], "wstq%d" % sl, w_in[:, col0:col0 + 512], 16, 512, "wstq%d" % sl)
                        if which == "v":
                            for tt in range(16):
                                bk = nbank(0, 4)
                                for k in range(16):
                                    PE_mm(psb[bk][:, :], hT[:, k, tt * P:(tt + 1) * P], wst[sl][:, k, :], k == 0, k == 15,
                                          ["wstq%d" % sl], [pk(bk)], inc=(k == 15))
                                evac_copy(vv[:, tt, :], psb[bk][:, :], bk, [("vv", tt)])
                            continue
                        dstT = qT if which == "q" else kT
                        gcol = gq if which == "q" else gk
                        for m in range(4):
                            for n in range(4):
                                bk = nbank(0, 4)
                                for k in range(16):
                                    PE_mm(psb[bk][:, :], wst[sl][:, k, m * P:(m + 1) * P], hT[:, k, n * 512:(n + 1) * 512],
                                          k == 0, k == 15, ["wstq%d" % sl], [pk(bk)], inc=(k == 15))
                                s2 = (m * 4 + n) % 2
                                A_act(sqf[s2][:], psb[bk][:, :], AF.Square, rk=[pk(bk)], wk=["sqf%d" % s2])
                                b2 = nbank(4, 8)
                                PE_mm(psb[b2][:, :], ones[:], sqf[s2][:], True, True, ["ones", "sqf%d" % s2], [pk(b2)])
                                A_act(rsq[s2][:], psb[b2][:, :], AF.Sqrt, scale=1.0 / 128, bias=epsc[:, 0:1],
                                      rk=[pk(b2)], wk=["rsq%d" % s2])
                                S.op("dve", lambda: nc.vector.reciprocal(out=rsq[s2][:], in_=rsq[s2][:]),
                                     reads=["rsq%d" % s2], writes=["rsq%d" % s2])
                                V_stt(dstT[:, m, n * 512:(n + 1) * 512], psb[bk][:, :], gcol[:, 0:1], rsq[s2][:],
                                      ALU.mult, ALU.mult, rk=[pk(bk), "rsq%d" % s2], wk=[(which, m, n)])
                    S.barrier()
                if hg == 0:
                    tap_bf("qT", qT[:].rearrange("p a t -> p (a t)"), [])
                    tap_bf("kT", kT[:].rearrange("p a t -> p (a t)"), [])
                    tap_bf("vv", vv[:].rearrange("p a t -> p (a t)"), [])
                with ExitStack() as es2:
                    SPb = [sb(es2, "SPb%d_%d" % (i, hg), [P, 1024]) for i in range(1)]
                    SPh = [sb(es2, "SPh%d_%d" % (i, hg), [P, 1024], BF16) for i in range(2)]
                    Ab = sb(es2, "Ab%d" % hg, [P, 1024])
                    Wb_ = [sb(es2, "Wb%d_%d" % (i, hg), [P, 1024], BF16) for i in range(2)]
                    ZB = [ps_all[:, 0:1024], ps_all[:, 1024:2048]]
                    TB = ps_all[:, 2048:3072]
                    OB = ps_all[:, 3072:4096]

                    def bank_ranges(lo):
                        rs_ = []
                        for bh in range(2):
                            a_ = max(lo, 512 * bh); b_ = 512 * (bh + 1)
                            if a_ < b_:
                                rs_.append((bh, a_, b_))
                        return rs_

                    for hl in range(4):
                        h = 4 * hg + hl
                        for qh in range(2):
                            kbs = list(range(8 * qh + 7, -1, -1))
                            N_ = len(kbs)
                            for bh in range(2):
                                PE_mm(TB[:, bh * 512:(bh + 1) * 512], zer[:, 0:P], zer[:, :], True, True, ["zer"], [pk(4 + bh)], inc=False, sg=True)
                                PE_mm(OB[:, bh * 512:(bh + 1) * 512], zer[:, 0:P], zer[:, :], True, True, ["zer"], [pk(6 + bh)], inc=(bh == 1), sg=True)

                            def lo_of(n):
                                return max(0, kbs[n] * P - qh * 1024)

                            def diag(n):
                                return kbs[n] * P >= qh * 1024

                            def S1(n):
                                kb = kbs[n]; lo = lo_of(n); zb = n % 2
                                for (bh, a_, b_) in bank_ranges(lo):
                                    PE_mm(ZB[zb][:, a_:b_], kT[:, hl, kb * P:(kb + 1) * P], qT[:, hl, qh * 1024 + a_: qh * 1024 + b_],
                                          True, True, [], [pk(2 * zb + bh)])
                                zk = [pk(2 * zb), pk(2 * zb + 1)]
                                A_act(SPb[0][:, lo:], ZB[zb][:, lo:], AF.Exp, rk=zk, wk=["SPb0"])
                                A_act(SPh[zb][:, lo:], SPb[0][:, lo:], AF.Ln, bias=ones[:, 0:1], rk=["SPb0"], wk=["SPh%d" % zb])
                                if diag(n):
                                    V_tt(SPh[zb][:, lo:lo + P], SPh[zb][:, lo:lo + P], cmb[:], ALU.mult,
                                         rk=["SPh%d" % zb], wk=["SPh%d" % zb])

                            def S2(n):
                                lo = lo_of(n); zb = n % 2
                                for (bh, a_, b_) in bank_ranges(lo):
                                    PE_mm(TB[:, a_:b_], trib[:], SPh[zb][:, a_:b_], False, False, ["SPh%d" % zb], [pk(4 + bh)], sg=True)

                            def S3a(n):
                                lo = lo_of(n); zb = n % 2
                                zk = [pk(2 * zb), pk(2 * zb + 1)]
                                V_tt(Ab[:, lo:], ZB[zb][:, lo:], SPh[zb][:, lo:], ALU.subtract, rk=zk + ["SPh%d" % zb], wk=["Ab"])
                                V_tt(Ab[:, lo:], Ab[:, lo:], TB[:, lo:], ALU.subtract, rk=["Ab", pk(4), pk(5)], wk=["Ab"])
                                A_act(Wb_[zb][:, lo:], Ab[:, lo:], AF.Exp, rk=["Ab"], wk=["Wb%d" % zb])

                            def S3b(n):
                                lo = lo_of(n); zb = n % 2
                                if diag(n):
                                    V_tt(Wb_[zb][:, lo:lo + P], Wb_[zb][:, lo:lo + P], cmb[:], ALU.mult,
                                         rk=["Wb%d" % zb], wk=["Wb%d" % zb])

                            def S4a(n):
                                lo = lo_of(n); zb = n % 2
                                for (bh, a_, b_) in bank_ranges(lo):
                                    PE_mm(TB[:, a_:b_], lemb[:], SPh[zb][:, a_:b_], False, False, ["SPh%d" % zb], [pk(4 + bh)], sg=True)

                            def S4b(n):
                                kb = kbs[n]; lo = lo_of(n); zb = n % 2
                                for (bh, a_, b_) in bank_ranges(lo):
                                    PE_mm(OB[:, a_:b_], vv[:, kb, hl * P:(hl + 1) * P], Wb_[zb][:, a_:b_], False, n == N_ - 1,
                                          ["Wb%d" % zb], [pk(6 + bh)], sg=True)

                            S1(0)
                            if N_ > 1:
                                S1(1)
                            S2(0)
                            for n in range(N_):
                                S3a(n)
                                S4a(n)
                                if n + 2 < N_:
                                    S1(n + 2)
                                S3b(n)
                                if n + 1 < N_:
                                    S2(n + 1)
                                S4b(n)
                            V_cp(attT[:, h, qh * 1024:(qh + 1) * 1024], OB[:, :], rk=[pk(6), pk(7)], wk=[("attT", h, qh)], e="act")
                    S.barrier()
        gate(104)
        tap_bf("attT", attT[:].rearrange("p a t -> p (a t)"), [])


        for th in range(2):
            with ExitStack() as es:
                mT = sb(es, "mT%d" % th, [P, 16, 1024], BF16)
                with ExitStack() as es2:
                    wbs = [sb(es2, "wbs%d_%d" % (i, th), [P, 8, P], BF16) for i in range(2)]
                    wba = [sb(es2, "wba%d_%d" % (i, th), [P, 8, P], BF16) for i in range(2)]
                    wgs = [sb(es2, "wgs%d_%d" % (i, th), [P, 16, P], BF16) for i in range(2)]
                    wga = [sb(es2, "wga%d_%d" % (i, th), [P, 16, P], BF16) for i in range(2)]
                    sgs = [sb(es2, "sgs%d_%d" % (i, th), [P, 512]) for i in range(2)]
                    sga = [sb(es2, "sga%d_%d" % (i, th), [P, 512]) for i in range(2)]
                    for m in range(16):
                        sl = m % 2
                        cs = slice(m * P, (m + 1) * P)
                        load_w(wbs[sl], "wbs%d" % sl, w_branch_ssm[:, cs], 8, P, "wbs%d" % sl)
                        load_w(wba[sl], "wba%d" % sl, w_branch_att[:, cs], 8, P, "wba%d" % sl)
                        load_w(wgs[sl], "wgs%d" % sl, w_in[:, 4096 + m * P:4096 + (m + 1) * P], 16, P, "wgs%d" % sl)
                        load_w(wga[sl], "wga%d" % sl, w_in[:, 6144 + m * P:6144 + (m + 1) * P], 16, P, "wga%d" % sl)
                        for n in range(2):
                            ts_ = slice(th * 1024 + n * 512, th * 1024 + (n + 1) * 512)
                            b_bs, b_gs, b_ba, b_ga = nbank(), nbank(), nbank(), nbank()
                            for k in range(8):
                                PE_mm(psb[b_bs][:, :], wbs[sl][:, k, :], s5T[:, k, ts_], k == 0, k == 7, ["wbs%d" % sl], [pk(b_bs)], inc=(k == 7))
                            for k in range(16):
                                PE_mm(psb[b_gs][:, :], wgs[sl][:, k, :], hT[:, k, ts_], k == 0, k == 15, ["wgs%d" % sl], [pk(b_gs)], inc=(k == 15))
                            for k in range(8):
                                PE_mm(psb[b_ba][:, :], wba[sl][:, k, :], attT[:, k, ts_], k == 0, k == 7, ["wba%d" % sl], [pk(b_ba)], inc=(k == 7))
                            for k in range(16):
                                PE_mm(psb[b_ga][:, :], wga[sl][:, k, :], hT[:, k, ts_], k == 0, k == 15, ["wga%d" % sl], [pk(b_ga)], inc=(k == 15))
                            s2 = n
                            A_act(sgs[s2][:], psb[b_gs][:, :], AF.Sigmoid, rk=[pk(b_gs)], wk=["sgs%d" % s2])
                            A_act(sga[s2][:], psb[b_ga][:, :], AF.Sigmoid, rk=[pk(b_ga)], wk=["sga%d" % s2])
                            V_tt(sgs[s2][:], sgs[s2][:], psb[b_bs][:, :], ALU.mult, rk=["sgs%d" % s2, pk(b_bs)], wk=["sgs%d" % s2])
                            V_tt(sga[s2][:], sga[s2][:], psb[b_ba][:, :], ALU.mult, rk=["sga%d" % s2, pk(b_ba)], wk=["sga%d" % s2])
                            V_tt(mT[:, m, n * 512:(n + 1) * 512], sgs[s2][:], sga[s2][:], ALU.add,
                                 rk=["sgs%d" % s2, "sga%d" % s2], wk=[("mT", m, n)])
                    S.barrier()
                with ExitStack() as es2:
                    wo = [sb(es2, "wo%d_%d" % (i, th), [P, 16, 512], BF16) for i in range(2)]
                    xin = [sb(es2, "xin%d_%d" % (i, th), [P, 512]) for i in range(2)]
                    xo = [sb(es2, "xo%d_%d" % (i, th), [P, 512]) for i in range(2)]
                    cnt_ = 0
                    for db in range(4):
                        sl = db % 2
                        ds_ = slice(db * 512, (db + 1) * 512)
                        load_w(wo[sl], "wo%d" % sl, w_out[:, ds_], 16, 512, "wo%d" % sl)
                        for tt in range(8):
                            r0 = th * 1024 + tt * P
                            s2 = cnt_ % 2
                            cnt_ += 1
                            S.dma("sp", xin[s2][:], x[r0:r0 + P, ds_], writes=["xin%d" % s2], sem="xin%d" % s2)
                            bk = nbank()
                            for k in range(16):
                                PE_mm(psb[bk][:, :], mT[:, k, tt * P:(tt + 1) * P], wo[sl][:, k, :], k == 0, k == 15,
                                      ["wo%d" % sl], [pk(bk)], inc=(k == 15))
                            V_tt(xo[s2][:], xin[s2][:], psb[bk][:, :], ALU.add, rk=["xin%d" % s2, pk(bk)], wk=["xo%d" % s2])
                            S.dma("sp", x2d[r0:r0 + P, ds_], xo[s2][:], reads=["xo%d" % s2], sem="xo%d" % s2)
                    S.barrier()
        gate(105)
        es_mix.close()
        if "x2" in tapd:
            S.dma("sp", tapd["x2"], x2d, sem="tap")
            S.barrier()

        if upto == "F":
            S.barrier()
            return nc
        TS = 256
        NT = 48
        NS = NT * TS
        h2d = nc.dram_tensor("h2_scratch", [T, D], F32, kind="Internal").ap()
        yd = nc.dram_tensor("y_scratch", [NS, D], F32, kind="Internal").ap()
        stok = nc.dram_tensor("slot_tok", [NS, 16], I32, kind="Internal").ap()
        S.dma("sp", g2s[:], ffn_norm_g, writes=["g2s"], sem="m_g2s")
        bk = nbank()
        PE_T(psb[bk][:, 0:16], g2s[:], 16, ["g2s", "ident"], [pk(bk)])
        V_cp(g2T[:], psb[bk][:, 0:16], rk=[pk(bk)], wk=["g2T"])
        with ExitStack() as es:
            wr = sb(es, "wr", [P, 16, 36])
            rb = sb(es, "rb", [P, 36])
            comb_g1 = sb(es, "comb_g1", [P, 16]); comb_g2 = sb(es, "comb_g2", [P, 16])
            oh1a = sb(es, "oh1a", [P, 16, 32]); oh2a = sb(es, "oh2a", [P, 16, 32])
            selb = sb(es, "selb", [P, 16, 32], BF16)
            s1i = sb(es, "s1i", [P, 16], I32); s2i = sb(es, "s2i", [P, 16], I32)
            widx = sb(es, "widx", [P, NT], I32)
            onesb = sb(es, "onesb", [P, P], BF16)
            V_cp(onesb[:], ones[:])
            with ExitStack() as es1:
                wlg = sb(es1, "wlg", [16, P * 4]); wle = sb(es1, "wle", [16, P * 32])
                S.dma("sp", wlg[:], router_group_w.rearrange("(c p) f -> c (p f)", p=P), writes=["wlg"], sem="m_wlg")
                S.dma("sp", wle[:], router_expert_w.rearrange("(c p) f -> c (p f)", p=P), writes=["wle"], sem="m_wle")
                wlg2 = sb(es1, "wlg2", [16, 4, P]); wle2 = sb(es1, "wle2", [16, 32, P])
                V_cp(wlg2[:], wlg[:].rearrange("c (p f) -> c f p", f=4), rk=["wlg"], wk=["wlg2"])
                V_cp(wle2[:], wle[:].rearrange("c (p f) -> c f p", f=32), rk=["wle"], wk=["wle2"])
                bk = nbank()
                for f in range(4):
                    PE_T(psb[bk][:, f * 16:(f + 1) * 16], wlg2[:, f, :], 16, ["wlg2", "ident"], [pk(bk)], inc=(f == 3))
                V_cp(wr[:, :, 0:4].rearrange("p c f -> p f c"), psb[bk][:, 0:64].rearrange("p (f c) -> p f c", c=16), rk=[pk(bk)], wk=["wr"])
                bk = nbank()
                for f in range(32):
                    PE_T(psb[bk][:, f * 16:(f + 1) * 16], wle2[:, f, :], 16, ["wle2", "ident"], [pk(bk)], inc=(f == 31))
                V_cp(wr[:, :, 4:36].rearrange("p c f -> p f c"), psb[bk][:, 0:512].rearrange("p (f c) -> p f c", c=16), rk=[pk(bk)], wk=["wr"])
                S.barrier()
            S.dma("sp", rb[:, 0:4], router_group_b.to_broadcast([P, 4]), writes=["rb"], sem="m_rb0")
            S.dma("sp", rb[:, 4:36], router_expert_b.to_broadcast([P, 32]), writes=["rb"], sem="m_rb1")
            with ExitStack() as es3:
                gb = sb(es3, "gb", [P, D])
                S.dma("sp", gb[:], ffn_norm_g_row.to_broadcast([P, D]), writes=["gb"], sem="m_gb")
                xt2 = [sb(es3, "xt2_%d" % i, [P, D]) for i in range(2)]
                xn = [sb(es3, "xn_%d" % i, [P, D]) for i in range(2)]
                h2r = [sb(es3, "h2r_%d" % i, [P, D]) for i in range(2)]
                junk = sb(es3, "junk2", [P, D], BF16)
                h32 = sb(es3, "h32", [P, 16, P])
                ss2 = sb(es3, "ss2", [P, 16]); rs2 = sb(es3, "rs2", [P, 16])
                lgA = sb(es3, "lgA", [P, 16, 36])
                gmaxA = sb(es3, "gmaxA", [P, 16]); gexA = sb(es3, "gexA", [P, 16, 4]); gmA = sb(es3, "gmA", [P, 16, 4])
                gsumA = sb(es3, "gsumA", [P, 16]); mlA = sb(es3, "mlA", [P, 16, 32]); ml2A = sb(es3, "ml2A", [P, 16, 32])
                m1A = sb(es3, "m1A", [P, 16]); m2A = sb(es3, "m2A", [P, 16])
                sm = [sb(es3, "sm%d" % i, [P, 1]) for i in range(8)]
                ml = sb(es3, "ml", [P, 32]); ml2 = sb(es3, "ml2", [P, 32])
                gm = sb(es3, "gm", [P, 4]); gex = sb(es3, "gex", [P, 4])
                for tt in range(16):
                    r0 = tt * P
                    sl = tt % 2
                    xk = "xt2_%d" % sl
                    S.dma("sp", xt2[sl][:], x2d[r0:r0 + P, :], writes=[xk], sem="xt2_%d" % sl)
                    A_act(junk[:], xt2[sl][:], AF.Square, rk=[xk], wk=["junk2"], accum_out=ss2[:, tt:tt + 1])
                    A_act(rs2[:, tt:tt + 1], ss2[:, tt:tt + 1], AF.Sqrt, scale=1.0 / D, bias=epsc[:, 0:1],
                          rk=["junk2"], wk=[("rs2", tt)])
                    S.op("dve", lambda: nc.vector.reciprocal(out=rs2[:, tt:tt + 1], in_=rs2[:, tt:tt + 1]),
                         reads=[("rs2", tt)], writes=[("rs2", tt)])
                    V_ts(xn[sl][:], xt2[sl][:], rs2[:, tt:tt + 1], ALU.mult, rk=[xk, ("rs2", tt)], wk=["xn%d" % sl])
                    V_tt(h2r[sl][:], xn[sl][:], gb[:], ALU.mult, rk=["xn%d" % sl, "gb"], wk=["h2r%d" % sl], e="pool")
                    S.dma("sp", h2d[r0:r0 + P, :], h2r[sl][:], reads=["h2r%d" % sl], sem="h2w%d" % sl)
                    for cb in range(4):
                        bk = nbank()
                        for j in range(4):
                            c = cb * 4 + j
                            PE_T(psb[bk][:, j * P:(j + 1) * P], xn[sl][:, c * P:(c + 1) * P], P, ["xn%d" % sl, "ident"], [pk(bk)], inc=(j == 3))
                        e_ = S.alt()
                        for j in range(4):
                            c = cb * 4 + j
                            if e_ == "dve":
                                V_ts(h32[:, c, :], psb[bk][:, j * P:(j + 1) * P], g2T[:, c:c + 1], ALU.mult,
                                     rk=[pk(bk)], wk=[("h32", c)])
                            else:
                                S.op("act", lambda: nc.scalar.activation(out=h32[:, c, :], in_=psb[bk][:, j * P:(j + 1) * P],
                                                                         func=AF.Copy, scale=g2T[:, c:c + 1]),
                                     reads=[pk(bk)], writes=[("h32", c)])
                    bk = nbank()
                    for c in range(16):
                        PE_mm(psb[bk][:, 0:36], h32[:, c, :], wr[:, c, :], c == 0, c == 15,
                              [("h32", c), "wr"], [pk(bk)], inc=(c == 15))
                    V_tt(lgA[:, tt, :], psb[bk][:, 0:36], rb[:], ALU.add, rk=[pk(bk), "rb"], wk=[("lgA", tt)])
                lk = [("lgA", t_) for t_ in range(16)]
                AX = mybir.AxisListType.X
                gl = lgA[:, :, 0:4]
                el4 = lgA[:, :, 4:36].rearrange("p t (g e) -> p t g e", g=4)
                S.op("dve", lambda: nc.vector.tensor_reduce(out=gmaxA[:], in_=gl, axis=AX, op=ALU.max), reads=lk, writes=["gmaxA"])
                V_tt(gexA[:], gl, gmaxA[:].unsqueeze(2).broadcast_to([P, 16, 4]), ALU.subtract, rk=lk + ["gmaxA"], wk=["gexA"])
                V_ts(gmA[:], gexA[:], 0.0, ALU.is_ge, s2=-1.0, op1=ALU.add, rk=["gexA"], wk=["gmA"])
                V_ts(gmA[:], gmA[:], 1e30, ALU.mult, rk=["gmA"], wk=["gmA"])
                A_act(gexA[:], gexA[:], AF.Exp, rk=["gexA"], wk=["gexA"])
                S.op("dve", lambda: nc.vector.tensor_reduce(out=gsumA[:], in_=gexA[:], axis=AX, op=ALU.add), reads=["gexA"], writes=["gsumA"])
                S.op("dve", lambda: nc.vector.reciprocal(out=gsumA[:], in_=gsumA[:]), reads=["gsumA"], writes=["gsumA"])
                V_tt(mlA[:].rearrange("p t (g e) -> p t g e", g=4), el4, gmA[:].unsqueeze(3).broadcast_to([P, 16, 4, 8]), ALU.add,
                     rk=lk + ["gmA"], wk=["mlA"])
                S.op("dve", lambda: nc.vector.tensor_reduce(out=m1A[:], in_=mlA[:], axis=AX, op=ALU.max), reads=["mlA"], writes=["m1A"])
                V_tt(oh1a[:], mlA[:], m1A[:].unsqueeze(2).broadcast_to([P, 16, 32]), ALU.is_equal, rk=["mlA", "m1A"], wk=["oh1a"])
                V_stt(ml2A[:], oh1a[:], -1e30, mlA[:], ALU.mult, ALU.add, rk=["oh1a", "mlA"], wk=["ml2A"])
                S.op("dve", lambda: nc.vector.tensor_reduce(out=m2A[:], in_=ml2A[:], axis=AX, op=ALU.max), reads=["ml2A"], writes=["m2A"])
                V_tt(oh2a[:], ml2A[:], m2A[:].unsqueeze(2).broadcast_to([P, 16, 32]), ALU.is_equal, rk=["ml2A", "m2A"], wk=["oh2a"])
                V_tt(m1A[:], m1A[:], m2A[:], ALU.subtract, rk=["m1A", "m2A"], wk=["m1A"])
                A_act(m1A[:], m1A[:], AF.Sigmoid, rk=["m1A"], wk=["m1A"])
                V_tt(comb_g1[:], gsumA[:], m1A[:], ALU.mult, rk=["gsumA", "m1A"], wk=["comb_g1"])
                V_tt(comb_g2[:], gsumA[:], comb_g1[:], ALU.subtract, rk=["gsumA", "comb_g1"], wk=["comb_g2"])
                V_tt(selb[:], oh1a[:], oh2a[:], ALU.add, rk=["oh1a", "oh2a"], wk=[("selb", t_) for t_ in range(16)])
                S.barrier()
            if "g1" in tapd:
                S.dma("sp", tapd["g1"], comb_g1[:], sem="tap"); S.dma("sp", tapd["oh1"], oh1a[:].rearrange("p t e -> p (t e)"), sem="tap")
                S.barrier()
            gate(106)
            with ExitStack() as es3:
                cntx = sb(es3, "cntx", [P, 16, 32]); tot = sb(es3, "tot", [P, 32])
                ci = sb(es3, "ci", [P, 32], I32); pad = sb(es3, "pad", [P, 32]); incl = sb(es3, "incl", [P, 32])
                base = sb(es3, "base", [P, 32]); zz = sb(es3, "zz", [P, 32])
                slot = sb(es3, "slot", [P, 16, 32]); tmp3 = sb(es3, "tmp3", [P, 16, 32])
                s1f = sb(es3, "s1f", [P, 16]); s2f = sb(es3, "s2f", [P, 16])
                jv = sb(es3, "jv", [P, NT]); pidf = sb(es3, "pidf", [P, 1])
                cmp3 = sb(es3, "cmp3", [P, NT, 32]); ejf = sb(es3, "ejf", [P, NT])
                tid = sb(es3, "tid", [P, 16, 16], I32)
                zi = sb(es3, "zi", [P, NS * 16 // P], I32)
                bk = nbank(); bk2 = nbank()
                for tt in range(16):
                    for t2 in range(tt + 1):
                        PE_mm(psb[bk][:, tt * 32:(tt + 1) * 32], cmb[:] if t2 == tt else onesb[:], selb[:, t2, :],
                              t2 == 0, t2 == tt, [("selb", t2)], [pk(bk)], inc=(t2 == tt), sg=True)
                for tt in range(16):
                    PE_mm(psb[bk2][:, 0:32], onesb[:], selb[:, tt, :], tt == 0, tt == 15, [("selb", tt)], [pk(bk2)], inc=(tt == 15))
                V_cp(cntx[:].rearrange("p t e -> p (t e)"), psb[bk][:, :], rk=[pk(bk)], wk=["cntx"])
                V_cp(tot[:], psb[bk2][:, 0:32], rk=[pk(bk2)], wk=["tot"])
                S.op("dve", lambda: nc.vector.memset(zz[:], 0.0), writes=["zz"])
                V_ts(ci[:], tot[:], float(TS - 1), ALU.add)
                S.op("dve", lambda: nc.vector.tensor_scalar(out=ci[:], in0=ci[:], scalar1=8, scalar2=8,
                                                            op0=ALU.arith_shift_right, op1=ALU.logical_shift_left),
                     reads=["ci"], writes=["ci"])
                V_cp(pad[:], ci[:])
                S.op("dve", lambda: nc.vector.tensor_tensor_scan(out=incl[:], data0=pad[:], data1=zz[:], initial=0.0,
                                                                 op0=ALU.add, op1=ALU.add),
                     reads=["pad", "zz"], writes=["incl"])
                V_tt(base[:], incl[:], pad[:], ALU.subtract)
                V_tt(slot[:], cntx[:], base[:].unsqueeze(1).broadcast_to([P, 16, 32]), ALU.add, rk=["cntx", "base"], wk=["slot"])
                V_tt(tmp3[:], slot[:], oh1a[:], ALU.mult, rk=["slot"], wk=["tmp3"])
                S.op("dve", lambda: nc.vector.tensor_reduce(out=s1f[:], in_=tmp3[:], axis=mybir.AxisListType.X, op=ALU.add),
                     reads=["tmp3"], writes=["s1f"])
                V_cp(s1i[:], s1f[:])
                V_tt(tmp3[:], slot[:], oh2a[:], ALU.mult, rk=["slot", "s1f"], wk=["tmp3"])
                S.op("dve", lambda: nc.vector.tensor_reduce(out=s2f[:], in_=tmp3[:], axis=mybir.AxisListType.X, op=ALU.add),
                     reads=["tmp3"], writes=["s2f"])
                V_cp(s2i[:], s2f[:])
                S.op("pool", lambda: nc.gpsimd.iota(jv[:], pattern=[[TS, NT]], base=0, channel_multiplier=0,
                                                    allow_small_or_imprecise_dtypes=True), writes=["jv"])
                S.op("pool", lambda: nc.gpsimd.iota(pidf[:], pattern=[[0, 1]], base=0, channel_multiplier=1,
                                                    allow_small_or_imprecise_dtypes=True), writes=["pidf"])
                V_tt(cmp3[:], incl[:].unsqueeze(1).broadcast_to([P, NT, 32]), jv[:].unsqueeze(2).broadcast_to([P, NT, 32]),
                     ALU.is_le, rk=["incl", "jv"], wk=["cmp3"])
                S.op("dve", lambda: nc.vector.tensor_reduce(out=ejf[:], in_=cmp3[:], axis=mybir.AxisListType.X, op=ALU.add),
                     reads=["cmp3"], writes=["ejf"])
                emp = sb(es3, "emp", [P, NT])
                V_ts(emp[:], ejf[:], 32.0, ALU.is_ge, s2=65536.0, op1=ALU.mult, rk=["ejf"], wk=["emp"])
                V_ts(ejf[:], ejf[:], 31.0, ALU.min, s2=128.0, op1=ALU.mult)
                V_ts(ejf[:], ejf[:], pidf[:, 0:1], ALU.add)
                V_tt(ejf[:], ejf[:], emp[:], ALU.add)
                V_cp(widx[:], ejf[:])
                S.op("pool", lambda: nc.gpsimd.iota(tid[:], pattern=[[P, 16], [0, 16]], base=0, channel_multiplier=1), writes=["tid"])
                S.op("dve", lambda: nc.vector.memset(zi[:], 4096), writes=["zi"])
                S.dma("sp", stok.rearrange("(p a) f -> p (a f)", p=P), zi[:], reads=["zi"], writes=["stok"], sem="stz")
                for tt in range(16):
                    for (sx, nm_) in ((s1i, "s1i"), (s2i, "s2i")):
                        S.idma(stok, sx[:, tt:tt + 1], tid[:, tt, :], None, reads=[nm_, "tid", "stok"], writes=[("stokw", tt, nm_)], sem="scat")
                S.barrier()
            if "s1i" in tapd:
                S.dma("pool", tapd["s1i"], s1i[:], sem="tap"); S.dma("pool", tapd["widx"], widx[:], sem="tap")
                S.barrier()
            gate(107)
            with ExitStack() as es3:
                tix = [sb(es3, "tix%d" % i, [P, 2], I32) for i in range(2)]
                xg = [sb(es3, "xg%d" % i, [P, 2, D]) for i in range(2)]
                xT = [sb(es3, "xT%d" % i, [P, 16, TS], BF16) for i in range(2)]
                wgb = [sb(es3, "wgb%d" % i, [P, 16, DFF], BF16) for i in range(2)]
                wub = [sb(es3, "wub%d" % i, [P, 16, DFF], BF16) for i in range(2)]
                wdb = [sb(es3, "wdb%d" % i, [P, 4, D], BF16) for i in range(2)]
                hidb = [sb(es3, "hidb%d" % i, [P, 4, TS], BF16) for i in range(2)]
                sgm = [sb(es3, "sgm%d" % i, [P, DFF]) for i in range(2)]
                ysb = [sb(es3, "ysb%d" % i, [P, D]) for i in range(2)]
                for i_ in range(2):
                    S.op("dve", lambda: nc.vector.memset(xg[i_][:], 0.0), writes=[("xg", i_, 0), ("xg", i_, 1)])
                    S.op("dve", lambda: nc.vector.memset(wgb[i_][:], 0.0), writes=["wgb%d" % i_])
                    S.op("dve", lambda: nc.vector.memset(wub[i_][:], 0.0), writes=["wub%d" % i_])
                    S.op("dve", lambda: nc.vector.memset(wdb[i_][:], 0.0), writes=["wdb%d" % i_])
                wgv = expert_w_gate.rearrange("e (p c) f -> (e p) (c f)", p=P)
                wuv = expert_w_up.rearrange("e (p c) f -> (e p) (c f)", p=P)
                wdv = expert_w_down.rearrange("e (p c) d -> (e p) (c d)", p=P)
                ycnt_ = [0]

                def Lt(j, sl):
                    for h in range(2):
                        S.dma("sp", tix[sl][:, h:h + 1], stok[j * TS + h * P:j * TS + (h + 1) * P, 0:1],
                              writes=["tix%d" % sl], sem="tix%d" % sl, allow_slow_non_contiguous=True)
                    for h in range(2):
                        S.idma(xg[sl][:, h, :], None, h2d, tix[sl][:, h:h + 1], reads=["tix%d" % sl], writes=[("xg", sl, h)], sem="xg%d" % sl, bounds=T - 1)
                    S.idma(wgb[sl][:].rearrange("p c f -> p (c f)"), None, wgv, widx[:, j:j + 1], reads=[], writes=["wgb%d" % sl], sem="wgb%d" % sl, bounds=NE * P - 1)
                    S.idma(wub[sl][:].rearrange("p c f -> p (c f)"), None, wuv, widx[:, j:j + 1], reads=[], writes=["wub%d" % sl], sem="wub%d" % sl, bounds=NE * P - 1)
                    S.idma(wdb[sl][:].rearrange("p c f -> p (c f)"), None, wdv, widx[:, j:j + 1], reads=[], writes=["wdb%d" % sl], sem="wdb%d" % sl, bounds=NE * P - 1)

                def Ct(j, sl):
                    for h in range(2):
                        for cb in range(4):
                            bk = nbank(0, 4)
                            for jj in range(4):
                                c = cb * 4 + jj
                                PE_T(psb[bk][:, jj * P:(jj + 1) * P], xg[sl][:, h, c::16], P, [("xg", sl, h), "ident"], [pk(bk)], inc=(jj == 3))
                            evac_copy(xT[sl][:, cb * 4:(cb + 1) * 4, h * P:(h + 1) * P],
                                      psb[bk][:, :].rearrange("p (j s) -> p j s", j=4), bk, [("xT", sl, h, cb)])
                    xk_ = [("xT", sl, h, cb) for h in range(2) for cb in range(4)]
                    for h in range(2):
                        bg, bu = nbank(4, 8), nbank(4, 8)
                        xkh = [("xT", sl, h, cb) for cb in range(4)]
                        for c in range(16):
                            PE_mm(psb[bg][:, :], xT[sl][:, c, h * P:(h + 1) * P], wgb[sl][:, c, :], c == 0, c == 15,
                                  ["wgb%d" % sl] + xkh, [pk(bg)], inc=(c == 15))
                        for c in range(16):
                            PE_mm(psb[bu][:, :], xT[sl][:, c, h * P:(h + 1) * P], wub[sl][:, c, :], c == 0, c == 15,
                                  ["wub%d" % sl] + xkh, [pk(bu)], inc=(c == 15))
                        s2 = h
                        A_act(sgm[s2][:], psb[bg][:, :], AF.Silu, rk=[pk(bg)], wk=["sgm%d" % s2])
                        V_tt(sgm[s2][:], sgm[s2][:], psb[bu][:, :], ALU.mult, rk=["sgm%d" % s2, pk(bu)], wk=["sgm%d" % s2])
                        bt = nbank(0, 4)
                        for fc in range(4):
                            PE_T(psb[bt][:, fc * P:(fc + 1) * P], sgm[s2][:, fc::4], P, ["sgm%d" % s2, "ident"], [pk(bt)], inc=(fc == 3))
                        evac_copy(hidb[sl][:, :, h * P:(h + 1) * P], psb[bt][:, :].rearrange("p (f s) -> p f s", f=4), bt,
                                  [("hidb", sl, fc_, h) for fc_ in range(4)])
                    for h in range(2):
                        ys = ycnt_[0] % 2
                        ycnt_[0] += 1
                        for db in range(4):
                            bk = nbank(0, 4)
                            for fc in range(4):
                                PE_mm(psb[bk][:, :], hidb[sl][:, fc, h * P:(h + 1) * P], wdb[sl][:, fc, db * 512:(db + 1) * 512],
                                      fc == 0, fc == 3, ["wdb%d" % sl, ("hidb", sl, fc, h)], [pk(bk)], inc=(fc == 3))
                            evac_copy(ysb[ys][:, db * 512:(db + 1) * 512], psb[bk][:, :], bk, [("ysb", ys, db)])
                        r0 = j * TS + h * P
                        S.dma("sp", yd[r0:r0 + P, :], ysb[ys][:], reads=[("ysb", ys, db_) for db_ in range(4)], sem="yw%d" % ys)


                NH = 32
                Lt(0, 0); Lt(1, 1)
                for k in range(NH):
                    sl = k % 2
                    Ct(k, sl)
                    if k < NT - NH:
                        Lt(NT - 1 - k, sl); Ct(NT - 1 - k, sl)
                    if k + 2 < NH:
                        Lt(k + 2, sl)
                S.barrier()
            gate(108)
            with ExitStack() as es3:
                xa = [sb(es3, "xa%d" % i, [P, D]) for i in range(2)]
                y1 = [sb(es3, "y1_%d" % i, [P, D]) for i in range(2)]
                y2 = [sb(es3, "y2_%d" % i, [P, D]) for i in range(2)]
                for tt in range(16):
                    sl = tt % 2
                    r0 = tt * P
                    S.dma("sp", xa[sl][:], x2d[r0:r0 + P, :], writes=["xa%d" % sl], sem="xa%d" % sl)
                    S.idma(y1[sl][:], None, yd, s1i[:, tt:tt + 1], reads=[], writes=["y1_%d" % sl], sem="y1_%d" % sl)
                    S.idma(y2[sl][:], None, yd, s2i[:, tt:tt + 1], reads=[], writes=["y2_%d" % sl], sem="y2_%d" % sl)
                    V_stt(xa[sl][:], y1[sl][:], comb_g1[:, tt:tt + 1], xa[sl][:], ALU.mult, ALU.add,
                          rk=["y1_%d" % sl, "xa%d" % sl], wk=["xa%d" % sl])
                    V_stt(xa[sl][:], y2[sl][:], comb_g2[:, tt:tt + 1], xa[sl][:], ALU.mult, ALU.add,
                          rk=["y2_%d" % sl, "xa%d" % sl], wk=["xa%d" % sl])
                    S.dma("sp", out[r0:r0 + P, :], xa[sl][:], reads=["xa%d" % sl], sem="outd%d" % sl)
                S.barrier()

        S.barrier()
        import os
        if os.environ.get("KDEBUG"):
            print("sched counts", S.cnt, {k: v[1] for k, v in S.dsem.items()})
    return nc


_INPUT_LAYOUT = {
    "x": None,
}


def _prep_inputs(inputs, b):
    g = lambda k: np.ascontiguousarray(np.asarray(inputs[k], dtype=np.float32))
    m = {
        "x": g("x")[b],
        "attn_norm_g": g("attn_norm_g")[0].reshape(16, 128),
        "w_in": g("w_in")[0],
        "lambda_re": g("lambda_re")[0].reshape(32, 128),
        "lambda_im": g("lambda_im")[0].reshape(32, 128),
        "log_dt": g("log_dt")[0].reshape(32, 2),
        "ssm_b_re": g("ssm_b_re")[0].reshape(32, 2048),
        "ssm_b_im": g("ssm_b_im")[0].reshape(32, 2048),
        "ssm_c_re": g("ssm_c_re")[0].reshape(32, 2048),
        "ssm_c_im": g("ssm_c_im")[0].reshape(32, 2048),
        "ssm_d": g("ssm_d")[0].reshape(8, 128),
        "w_glu": g("w_glu")[0],
        "q_norm_g": g("q_norm_g")[0].reshape(1, 128),
        "k_norm_g": g("k_norm_g")[0].reshape(1, 128),
        "w_branch_ssm": g("w_branch_ssm")[0],
        "w_branch_att": g("w_branch_att")[0],
        "w_out": g("w_out")[0],
        "ffn_norm_g": g("ffn_norm_g")[0].reshape(16, 128),
        "ffn_norm_g_row": g("ffn_norm_g")[0].reshape(1, D),
        "router_group_w": g("router_group_w")[0],
        "router_group_b": g("router_group_b")[0].reshape(1, 4),
        "router_expert_w": g("router_expert_w")[0],
        "router_expert_b": g("router_expert_b")[0].reshape(1, 32),
        "expert_w_gate": g("expert_w_gate")[0],
        "expert_w_up": g("expert_w_up")[0],
        "expert_w_down": g("expert_w_down")[0],
    }
    return m


def kernel(**inputs):
    nc = build()
    shared = _prep_inputs(inputs, 0)
    xs = np.asarray(inputs["x"], dtype=np.float32)
    in_maps = []
    for b in range(8):
        m = dict(shared)
        m["x"] = np.ascontiguousarray(xs[b])
        in_maps.append(m)
    res = run_bass_kernel_spmd(nc, in_maps, core_ids=list(range(8)))
    return np.stack([np.asarray(r["out"], dtype=np.float32) for r in res.results], axis=0)
```

```python
import math
from contextlib import ExitStack

import numpy as np
import concourse.bass as bass
import concourse.mybir as mybir
from concourse.bass_utils import run_bass_kernel_spmd

F32 = mybir.dt.float32
BF16 = mybir.dt.bfloat16
I32 = mybir.dt.int32
AF = mybir.ActivationFunctionType
ALU = mybir.AluOpType

T = 2048
D = 2048
P = 128
NCK = 256
EPS = 1e-6
NE = 32
DFF = 512


class Sched:
    def __init__(self, nc):
        self.nc = nc
        self.eng = {"pe": nc.tensor, "act": nc.scalar, "dve": nc.vector,
                    "pool": nc.gpsimd, "sp": nc.sync}
        self.sem = {e: nc.alloc_semaphore(name="s_" + e) for e in self.eng}
        self.cnt = {e: 0 for e in self.eng}
        self.waited = {e: {} for e in self.eng}
        self.res = {}
        self.dsem = {}
        self.rr = 0
        self.dead = False
        self.bregs = {}

    def _toks(self, reads, writes):
        toks = []
        for k in reads:
            st = self.res.get(k)
            if st is not None and st[0] is not None:
                toks.append(st[0])
        for k in writes:
            st = self.res.get(k)
            if st is not None:
                if st[0] is not None:
                    toks.append(st[0])
                toks.extend(st[1])
        return toks

    def _wait(self, e, toks):
        need = {}
        for (s, v) in toks:
            if s == e and e in ("pe", "sp"):
                continue
            if v > need.get(s, 0):
                need[s] = v
        for s, v in need.items():
            if self.waited[e].get(s, 0) >= v:
                continue
            h = self.sem[s] if s in self.sem else self.dsem[s][0]
            self.eng[e].wait_ge(h, v)
            self.waited[e][s] = v

    def _mark(self, tok, reads, writes):
        for k in reads:
            st = self.res.setdefault(k, [None, []])
            st[1].append(tok)
            if len(st[1]) > 24:
                mx = {}
                for (s, v) in st[1]:
                    if v > mx.get(s, 0):
                        mx[s] = v
                st[1] = list(mx.items())
        for k in writes:
            self.res[k] = [tok, []]

    def op(self, e, fn, reads=(), writes=(), inc=True):
        if self.dead:
            return None
        self._wait(e, self._toks(reads, writes))
        ins = fn()
        tok = (e, self.cnt[e] + 1)
        if inc:
            ins.then_inc(self.sem[e], 1)
            self.cnt[e] += 1
        self._mark(tok, reads, writes)
        return ins

    def dma(self, q, out, in_, reads=(), writes=(), sem="d", **kw):
        if self.dead:
            return None
        self._wait(q, self._toks(reads, writes))
        if sem not in self.dsem:
            self.dsem[sem] = [self.nc.alloc_semaphore(name="d_" + sem), 0]
        d = self.dsem[sem]
        d[1] += 16
        self.eng[q].dma_start(out=out, in_=in_, **kw).then_inc(d[0], 16)
        self._mark((sem, d[1]), reads, writes)

    def idma(self, out, out_idx, in_, in_idx, reads=(), writes=(), sem="id", bounds=None):
        if self.dead:
            return None
        self._wait("pool", self._toks(reads, writes))
        if sem not in self.dsem:
            self.dsem[sem] = [self.nc.alloc_semaphore(name="d_" + sem), 0]
        d = self.dsem[sem]
        d[1] += 16
        oo = bass.IndirectOffsetOnAxis(ap=out_idx, axis=0) if out_idx is not None else None
        io = bass.IndirectOffsetOnAxis(ap=in_idx, axis=0) if in_idx is not None else None
        kw = {}
        if bounds is not None:
            if bounds not in self.bregs:
                self.bregs[bounds] = self.nc.gpsimd.to_reg(bounds)
            kw = {"bounds_check": self.bregs[bounds], "oob_is_err": False}
        self.nc.gpsimd.indirect_dma_start(out=out, out_offset=oo, in_=in_, in_offset=io, **kw).then_inc(d[0], 16)
        self._mark((sem, d[1]), reads, writes)

    def barrier(self):
        toks = [(e, self.cnt[e]) for e in self.eng if self.cnt[e] > 0]
        toks += [(s, d[1]) for s, d in self.dsem.items() if d[1] > 0]
        for e in self.eng:
            self._wait(e, [t for t in toks if t[0] != e])
        self.res = {}

    def alt(self):
        self.rr ^= 1
        return "act" if self.rr else "dve"


class _Stop(Exception):
    pass


def build(upto="all", taps=()):
    import os
    kgate = int(os.environ.get("KGATE", "0"))

    gate_s = [None]

    def gate(n):
        if kgate == n:
            gate_s[0].dead = True
    nc = bass.Bass("TRN2", target_bir_lowering=False)
    S = Sched(nc)
    gate_s[0] = S
    dram = {}

    def din(name, shape):
        dram[name] = nc.dram_tensor(name, list(shape), F32, kind="ExternalInput").ap()
        return dram[name]

    x = din("x", [T, D])
    attn_norm_g = din("attn_norm_g", [16, 128])
    w_in = din("w_in", [D, 8192])
    lambda_re = din("lambda_re", [32, 128])
    lambda_im = din("lambda_im", [32, 128])
    log_dt = din("log_dt", [32, 2])
    ssm_b_re = din("ssm_b_re", [32, 2048])
    ssm_b_im = din("ssm_b_im", [32, 2048])
    ssm_c_re = din("ssm_c_re", [32, 2048])
    ssm_c_im = din("ssm_c_im", [32, 2048])
    ssm_d = din("ssm_d", [8, 128])
    w_glu = din("w_glu", [1024, 1024])
    q_norm_g = din("q_norm_g", [1, 128])
    k_norm_g = din("k_norm_g", [1, 128])
    w_branch_ssm = din("w_branch_ssm", [1024, 2048])
    w_branch_att = din("w_branch_att", [1024, 2048])
    w_out = din("w_out", [D, D])
    ffn_norm_g = din("ffn_norm_g", [16, 128])
    ffn_norm_g_row = din("ffn_norm_g_row", [1, D])
    router_group_w = din("router_group_w", [D, 4])
    router_group_b = din("router_group_b", [1, 4])
    router_expert_w = din("router_expert_w", [D, 32])
    router_expert_b = din("router_expert_b", [1, 32])
    expert_w_gate = din("expert_w_gate", [NE, D, DFF])
    expert_w_up = din("expert_w_up", [NE, D, DFF])
    expert_w_down = din("expert_w_down", [NE, DFF, D])
    out = nc.dram_tensor("out", [T, D], F32, kind="ExternalOutput").ap()
    x2d = nc.dram_tensor("x2_scratch", [T, D], F32, kind="Internal").ap()
    tapd = {}
    for (nm, shp) in taps:
        tapd[nm] = nc.dram_tensor("tap_" + nm, list(shp), F32, kind="ExternalOutput").ap()

    top = ExitStack()
    with top:
        def sb(es, name, shape, dt=F32):
            return es.enter_context(nc.sbuf_tensor(name, list(shape), dt))

        ps_all = top.enter_context(nc.psum_tensor("ps_all", [P, 4096], F32))
        psb = [ps_all[:, i * 512:(i + 1) * 512] for i in range(8)]
        bank_rr = [0]

        def nbank(lo=0, hi=8):
            b = lo + (bank_rr[0] % (hi - lo))
            bank_rr[0] += 1
            return b

        ident = sb(top, "ident", [P, P])
        ones = sb(top, "ones", [P, P])
        epsc = sb(top, "epsc", [P, 1])
        S.op("dve", lambda: nc.vector.memset(ones[:], 1.0), writes=["ones"])
        S.op("dve", lambda: nc.vector.memset(epsc[:], EPS), writes=["epsc"])
        S.op("pool", lambda: nc.gpsimd.affine_select(
            out=ident[:], in_=ones[:], pattern=[[1, P]], compare_op=ALU.is_equal,
            fill=0.0, base=0, channel_multiplier=-1), reads=["ones"], writes=["ident"])

        def tap(nm, src_ap, key):
            if nm in tapd:
                S.dma("sp", tapd[nm], src_ap, reads=[key], sem="tap")

        tri = sb(top, "tri", [P, P]); lem = sb(top, "lem", [P, P]); cmf = sb(top, "cmf", [P, P])
        cmb = sb(top, "cmb", [P, P], BF16); zer = sb(top, "zer", [P, 512], BF16)
        trib = sb(top, "trib", [P, P], BF16); lemb = sb(top, "lemb", [P, P], BF16)
        gq = sb(top, "gq", [P, 1]); gk = sb(top, "gk", [P, 1])
        g1s = sb(top, "g1s", [16, P])
        g1T = sb(top, "g1T", [P, 16])
        g2s = sb(top, "g2s", [16, P])
        g2T = sb(top, "g2T", [P, 16])
        es_mix = ExitStack()
        hT = sb(es_mix, "hT", [P, 16, T], BF16)
        s5T = sb(es_mix, "s5T", [P, 8, T], BF16)
        S.dma("sp", g1s[:], attn_norm_g, writes=["g1s"], sem="m1")
        b = nbank()
        S.op("pe", lambda: nc.tensor.transpose(out=psb[b][:, 0:16], in_=g1s[:], identity=ident[0:16, 0:16]),
             reads=["g1s", "ident"], writes=["ps%d" % b])
        S.op("dve", lambda: nc.vector.tensor_copy(out=g1T[:], in_=psb[b][:, 0:16]),
             reads=["ps%d" % b], writes=["g1T"])

        def rmsnorm_to_T(es, src_rows, gT, dstT, dst_key, ncols_off=0, ntiles=16, pfx="n1"):
            xt = [sb(es, pfx + "_xt%d" % i, [P, D]) for i in range(2)]
            junk = sb(es, pfx + "_junk", [P, D], BF16)
            ss = sb(es, pfx + "_ss", [P, ntiles])
            rs = sb(es, pfx + "_rs", [P, ntiles])
            for tt in range(ntiles):
                sl = tt % 2
                xk = pfx + "xt%d" % sl
                S.dma("sp", xt[sl][:], src_rows(tt), writes=[xk], sem=pfx + "x%d" % sl)
                S.op("act", lambda: nc.scalar.activation(out=junk[:], in_=xt[sl][:], func=AF.Square,
                                                         accum_out=ss[:, tt:tt + 1]),
                     reads=[xk], writes=[pfx + "junk", (pfx + "ss", tt)])
                S.op("act", lambda: nc.scalar.activation(out=rs[:, tt:tt + 1], in_=ss[:, tt:tt + 1], func=AF.Sqrt,
                                                         bias=epsc[:, 0:1], scale=1.0 / D),
                     reads=[(pfx + "ss", tt), "epsc"], writes=[(pfx + "rs", tt)])
                S.op("dve", lambda: nc.vector.reciprocal(out=rs[:, tt:tt + 1], in_=rs[:, tt:tt + 1]),
                     reads=[(pfx + "rs", tt)], writes=[(pfx + "rs", tt)])
                S.op("dve", lambda: nc.vector.tensor_scalar(out=xt[sl][:], in0=xt[sl][:], scalar1=rs[:, tt:tt + 1],
                                                            scalar2=None, op0=ALU.mult),
                     reads=[xk, (pfx + "rs", tt)], writes=[xk])
                for cb in range(4):
                    bk = nbank()
                    for j in range(4):
                        c = cb * 4 + j
                        S.op("pe", lambda: nc.tensor.transpose(out=psb[bk][:, j * P:(j + 1) * P],
                                                               in_=xt[sl][:, c * P:(c + 1) * P], identity=ident[:]),
                             reads=[xk, "ident"], writes=["ps%d" % bk], inc=(j == 3))
                    for j in range(4):
                        c = cb * 4 + j
                        dst = dstT[:, c, ncols_off + tt * P: ncols_off + (tt + 1) * P]
                        e = S.alt()
                        if e == "dve":
                            S.op("dve", lambda: nc.vector.tensor_scalar(out=dst, in0=psb[bk][:, j * P:(j + 1) * P],
                                                                        scalar1=gT[:, c:c + 1], scalar2=None,
                                                                        op0=ALU.mult),
                                 reads=["ps%d" % bk], writes=[(dst_key, tt)])
                        else:
                            S.op("act", lambda: nc.scalar.activation(out=dst, in_=psb[bk][:, j * P:(j + 1) * P],
                                                                     func=AF.Copy, scale=gT[:, c:c + 1]),
                                 reads=["ps%d" % bk], writes=[(dst_key, tt)])

        with ExitStack() as es:
            rmsnorm_to_T(es, lambda tt: x[tt * P:(tt + 1) * P, :], g1T, hT, "hT")
            S.barrier()
        gate(101)
        if "hT" in tapd:
            with ExitStack() as es:
                tmp = sb(es, "taptmp", [P, 16, T])
                S.op("dve", lambda: nc.vector.tensor_copy(out=tmp[:], in_=hT[:]), writes=["taptmp"])
                S.dma("sp", tapd["hT"].rearrange("p (c t) -> p c t", c=16), tmp[:], reads=["taptmp"], sem="tap")
                S.barrier()


        def kn(ap):
            return ap.name

        def V_tt(out, a, b, op, rk=None, wk=None, e="dve"):
            en = nc.vector if e == "dve" else nc.gpsimd
            return S.op(e, lambda: en.tensor_tensor(out=out, in0=a, in1=b, op=op),
                        reads=rk if rk is not None else [kn(a), kn(b)],
                        writes=wk if wk is not None else [kn(out)])

        def V_ts(out, a, s1, op0, s2=None, op1=None, rk=None, wk=None):
            kw = {}
            if op1 is not None:
                kw["op1"] = op1
            r = rk if rk is not None else [kn(a)] + [kn(s) for s in (s1, s2) if hasattr(s, "name")]
            return S.op("dve", lambda: nc.vector.tensor_scalar(out=out, in0=a, scalar1=s1, scalar2=s2, op0=op0, **kw),
                        reads=r, writes=wk if wk is not None else [kn(out)])

        def V_stt(out, a, s, b, op0, op1, rk=None, wk=None):
            r = rk if rk is not None else [kn(a), kn(b)] + ([kn(s)] if hasattr(s, "name") else [])
            return S.op("dve", lambda: nc.vector.scalar_tensor_tensor(out=out, in0=a, scalar=s, in1=b, op0=op0, op1=op1),
                        reads=r, writes=wk if wk is not None else [kn(out)])

        def V_cp(out, a, rk=None, wk=None, e="dve"):
            if e == "act":
                return S.op("act", lambda: nc.scalar.copy(out=out, in_=a),
                            reads=rk if rk is not None else [kn(a)], writes=wk if wk is not None else [kn(out)])
            en = nc.vector if e == "dve" else nc.gpsimd
            return S.op(e, lambda: en.tensor_copy(out=out, in_=a),
                        reads=rk if rk is not None else [kn(a)], writes=wk if wk is not None else [kn(out)])

        def A_act(out, a, func, scale=1.0, bias=None, rk=None, wk=None, accum_out=None):
            kw = {}
            if bias is not None:
                kw["bias"] = bias
            if accum_out is not None:
                kw["accum_out"] = accum_out
            r = rk if rk is not None else [kn(a)] + [kn(s) for s in (scale, bias) if hasattr(s, "name")]
            return S.op("act", lambda: nc.scalar.activation(out=out, in_=a, func=func, scale=scale, **kw),
                        reads=r, writes=wk if wk is not None else [kn(out)])

        def PE_T(out, in_, n, rk, wk, inc=True):
            return S.op("pe", lambda: nc.tensor.transpose(out=out, in_=in_, identity=ident[0:n, 0:n]),
                        reads=rk, writes=wk, inc=inc)

        def PE_mm(out, lhsT, rhs, start, stop, rk, wk, inc=True, tp=None, sg=False):
            kw = {}
            if sg:
                kw["skip_group_check"] = True
            if tp is not None:
                kw["tile_position"] = tp
            return S.op("pe", lambda: nc.tensor.matmul(out, lhsT=lhsT, rhs=rhs, start=start, stop=stop, **kw),
                        reads=rk, writes=wk, inc=inc)

        def pk(b):
            return "ps%d" % b

        def tap_bf(nm, src, key_list):
            if nm in tapd:
                S.dma("pool", tapd[nm], src, reads=key_list, sem="tap")

        es_ssm = ExitStack()
        APr = sb(es_ssm, "APr", [P, 9, 32]); APi = sb(es_ssm, "APi", [P, 9, 32])
        AKr = sb(es_ssm, "AKr", [P, 8, 32]); AKi = sb(es_ssm, "AKi", [P, 8, 32]); AKn = sb(es_ssm, "AKn", [P, 8, 32])
        BBr = sb(es_ssm, "BBr", [P, 16, 32]); BBi = sb(es_ssm, "BBi", [P, 16, 32])
        CRt = sb(es_ssm, "CRt", [P, 16, 32]); CIt = sb(es_ssm, "CIt", [P, 16, 32])
        Dcol = sb(es_ssm, "Dcol", [P, 8])
        with ExitStack() as es:
            st_lr = sb(es, "st_lr", [32, P]); st_li = sb(es, "st_li", [32, P])
            st_dt = sb(es, "st_dt", [32, 2]); st_dtb = sb(es, "st_dtb", [32, P])
            st_b1 = sb(es, "st_b", [32, 2048]); st_c1 = sb(es, "st_c", [32, 2048])
            st_b21 = sb(es, "st_b2", [32, 16, P]); st_c21 = sb(es, "st_c2", [32, 16, P])
            st_b = [st_b1, st_b1]; st_c = [st_c1, st_c1]; st_b2 = [st_b21, st_b21]; st_c2 = [st_c21, st_c21]
            st_d = sb(es, "st_d", [8, P])
            LLD = sb(es, "LLD", [P, 96])
            BRt = sb(es, "BRt", [P, 16, 32]); BIt = sb(es, "BIt", [P, 16, 32])
            wk_ = [sb(es, "pw%d" % i, [P, 32]) for i in range(12)]
            S.dma("sp", st_lr[:], lambda_re, writes=["st_lr"], sem="m2")
            S.dma("sp", st_li[:], lambda_im, writes=["st_li"], sem="m3")
            S.dma("sp", st_dt[:], log_dt, writes=["st_dt"], sem="m4")
            S.dma("sp", st_d[:], ssm_d, writes=["st_d"], sem="m5")
            for g2 in range(2):
                V_ts(st_dtb[:, g2 * 64:(g2 + 1) * 64], ones[0:32, 0:64], st_dt[:, g2:g2 + 1], ALU.mult,
                     rk=["ones", "st_dt"], wk=["st_dtb"])
            bk = nbank()
            PE_T(psb[bk][:, 0:32], st_lr[:], 32, ["st_lr", "ident"], [pk(bk)], inc=False)
            PE_T(psb[bk][:, 32:64], st_li[:], 32, ["st_li", "ident"], [pk(bk)], inc=False)
            PE_T(psb[bk][:, 64:96], st_dtb[:], 32, ["st_dtb", "ident"], [pk(bk)])
            V_cp(LLD[:], psb[bk][:, 0:96], rk=[pk(bk)], wk=["LLD"])
            bk = nbank()
            PE_T(psb[bk][:, 0:8], st_d[:], 8, ["st_d", "ident"], [pk(bk)])
            V_cp(Dcol[:], psb[bk][:, 0:8], rk=[pk(bk)], wk=["Dcol"])
            for ri in range(2):
                S.dma("sp", st_b[ri][:], (ssm_b_re, ssm_b_im)[ri], writes=["st_b"], sem="stb")
                S.dma("sp", st_c[ri][:], (ssm_c_re, ssm_c_im)[ri], writes=["st_c"], sem="stc")
                V_cp(st_b2[ri][:], st_b[ri][:].rearrange("q (gp h) -> q h gp", h=16), rk=["st_b"], wk=["st_b2"])
                V_cp(st_c2[ri][:].rearrange("q h (g2 p) -> q g2 h p", g2=2),
                     st_c[ri][:].rearrange("q (g2 h p) -> q g2 h p", g2=2, h=16), rk=["st_c"], wk=["st_c2"])
                for (srcs, dst) in ((st_b2[ri], (BRt, BIt)[ri]), (st_c2[ri], (CRt, CIt)[ri])):
                    bk = nbank()
                    for h in range(16):
                        PE_T(psb[bk][:, h * 32:(h + 1) * 32], srcs[:, h, :], 32, [kn(srcs[:]), "ident"], [pk(bk)], inc=(h == 15))
                    V_cp(dst[:].rearrange("p h q -> p (h q)"), psb[bk][:, :], rk=[pk(bk)], wk=[kn(dst[:])])
            LR = LLD[:, 0:32]; LI = LLD[:, 32:64]; LDT = LLD[:, 64:96]
            dtv, lrdt, lidt, mag, cc, sn, t1, t2, t3, cre, cim, den = [w_[:] for w_ in wk_]
            A_act(dtv, LDT, AF.Exp)
            V_tt(lrdt, LR, dtv, ALU.mult)
            V_tt(lidt, LI, dtv, ALU.mult)
            A_act(mag, lrdt, AF.Exp)
            halfpi = sb(es, "halfpi", [P, 1])
            S.op("dve", lambda: nc.vector.memset(halfpi[:], math.pi / 2), writes=["halfpi"])
            A_act(sn, lidt, AF.Sin, scale=1.0 / 32)
            A_act(cc, lidt, AF.Sin, scale=1.0 / 32, bias=halfpi[:, 0:1])
            for _ in range(5):
                V_tt(t1, cc, cc, ALU.mult)
                V_tt(t2, sn, sn, ALU.mult)
                V_tt(t3, cc, sn, ALU.mult)
                V_tt(cc, t1, t2, ALU.subtract)
                V_ts(sn, t3, 2.0, ALU.mult)
            S.op("dve", lambda: nc.vector.memset(APr[:, 0, :], 1.0), writes=["APr"])
            S.op("dve", lambda: nc.vector.memset(APi[:, 0, :], 0.0), writes=["APi"])
            V_tt(APr[:, 1, :], mag, cc, ALU.mult)
            V_tt(APi[:, 1, :], mag, sn, ALU.mult)

            def cmul(o_r, o_i, a_r, a_i, b_r, b_i, tA, tB):
                V_tt(tA, a_r, b_r, ALU.mult)
                V_tt(tB, a_i, b_i, ALU.mult)
                V_tt(o_r, tA, tB, ALU.subtract)
                V_tt(tA, a_r, b_i, ALU.mult)
                V_tt(tB, a_i, b_r, ALU.mult)
                V_tt(o_i, tA, tB, ALU.add)

            for e_ in range(1, 8):
                cmul(APr[:, e_ + 1, :], APi[:, e_ + 1, :], APr[:, e_, :], APi[:, e_, :], APr[:, 1, :], APi[:, 1, :], t1, t2)
            V_cp(AKr[:, 0, :], APr[:, 8, :]); V_cp(AKi[:, 0, :], APi[:, 8, :])
            for k in range(7):
                V_tt(t1, AKr[:, k, :], AKr[:, k, :], ALU.mult)
                V_tt(t2, AKi[:, k, :], AKi[:, k, :], ALU.mult)
                V_tt(t3, AKr[:, k, :], AKi[:, k, :], ALU.mult)
                V_tt(AKr[:, k + 1, :], t1, t2, ALU.subtract)
                V_ts(AKi[:, k + 1, :], t3, 2.0, ALU.mult)
            V_ts(AKn[:], AKi[:], -1.0, ALU.mult)
            V_ts(t1, APr[:, 1, :], -1.0, ALU.add, rk=["APr"])
            V_tt(t2, LR, LR, ALU.mult)
            V_tt(t3, LI, LI, ALU.mult)
            V_tt(den, t2, t3, ALU.add)
            S.op("dve", lambda: nc.vector.reciprocal(out=den, in_=den), reads=[kn(den)], writes=[kn(den)])
            V_tt(t2, t1, LR, ALU.mult)
            V_tt(t3, APi[:, 1, :], LI, ALU.mult)
            V_tt(cre, t2, t3, ALU.add)
            V_tt(cre, cre, den, ALU.mult)
            V_tt(t2, APi[:, 1, :], LR, ALU.mult)
            V_tt(t3, t1, LI, ALU.mult)
            V_tt(cim, t2, t3, ALU.subtract)
            V_tt(cim, cim, den, ALU.mult)
            tb1 = sb(es, "tb1", [P, 16, 32]); tb2 = sb(es, "tb2", [P, 16, 32])
            creb = cre.unsqueeze(1).broadcast_to([P, 16, 32]); cimb = cim.unsqueeze(1).broadcast_to([P, 16, 32])
            cmul(BBr[:], BBi[:], creb, cimb, BRt[:], BIt[:], tb1[:], tb2[:])
            S.barrier()


        uT = sb(es_ssm, "uT", [P, 8, T], BF16)

        def load_w(wbuf, key, srcap, kch, ncols, sem):
            S.dma("pool", wbuf[:, 0:kch, 0:ncols], srcap.rearrange("(c p) f -> p c f", p=P), writes=[key], sem=sem)

        def evac_copy(dst, src_ps, bk, wkeys, e=None):
            e = e or S.alt()
            V_cp(dst, src_ps, rk=[pk(bk)], wk=wkeys, e=e)

        with ExitStack() as es:
            wst = [sb(es, "wst%d" % i, [P, 16, 512], BF16) for i in range(2)]
            for blk in range(2):
                sl = blk % 2
                load_w(wst[sl], "wst%d" % sl, w_in[:, blk * 512:(blk + 1) * 512], 16, 512, "wst%d" % sl)
                for m in range(4):
                    for n in range(4):
                        bk = nbank()
                        for k in range(16):
                            PE_mm(psb[bk][:, :], wst[sl][:, k, m * P:(m + 1) * P], hT[:, k, n * 512:(n + 1) * 512],
                                  k == 0, k == 15, ["wst%d" % sl], [pk(bk)], inc=(k == 15))
                        evac_copy(uT[:, blk * 4 + m, n * 512:(n + 1) * 512], psb[bk][:, :], bk, [("uT", blk * 4 + m, n)])
            S.barrier()
        gate(102)
        tap_bf("uT", uT[:].rearrange("p a t -> p (a t)"), [])

        with ExitStack() as es:
            Xu = sb(es, "Xu", [P, 8, 2, 4, 16])
            XP = sb(es, "XP", [P, 8, 2, 4, 32])
            CAu = sb(es, "CAu", [P, 4, 9, 2, 16])
            tq1 = sb(es, "tq1", [P, 4, 16]); tq2 = sb(es, "tq2", [P, 4, 16])
            WS = [sb(es, "WS%d" % i, [P, 8, 2, P], BF16) for i in range(2)]
            WCp = [sb(es, "WCp%d" % i, [P, 4, 9, 2, 32], BF16) for i in range(2)]
            BPb = [sb(es, "BPb%d" % i, [P, 2, 4, 32], BF16) for i in range(2)]
            BD = [sb(es, "BD%d" % i, [P, 8, P], BF16) for i in range(2)]
            Hb = [[sb(es, "Hb%d%d" % (s_, i), [P, 2, NCK]) for i in range(2)] for s_ in range(2)]
            Hbf = [sb(es, "Hbf%d" % i, [P, 2, NCK], BF16) for i in range(4)]
            y32 = sb(es, "y32", [P, 1024])
            S.op("dve", lambda: nc.vector.memset(XP[:], 0.0), writes=["XP"])
            for i in range(2):
                S.op("pool", lambda: nc.gpsimd.memset(WCp[i][:], 0.0), writes=["WCp%d" % i])
                S.op("pool", lambda: nc.gpsimd.memset(BD[i][:], 0.0), writes=["BD%d" % i])
            XPv = XP[:].rearrange("p i r q (g h) -> p (i r q) g h", g=2)
            for a in range(8):
                par = a % 2
                qs = slice(4 * a, 4 * a + 4)
                for ip in range(8):
                    e_ = 7 - ip
                    arb = APr[:, e_, qs].unsqueeze(2).broadcast_to([P, 4, 16])
                    aib = APi[:, e_, qs].unsqueeze(2).broadcast_to([P, 4, 16])
                    bbr = BBr[:, :, qs].rearrange("p h q -> p q h")
                    bbi = BBi[:, :, qs].rearrange("p h q -> p q h")
                    V_tt(tq1[:], arb, bbr, ALU.mult, rk=[], wk=["tq1"])
                    V_tt(tq2[:], aib, bbi, ALU.mult, rk=[], wk=["tq2"])
                    V_tt(Xu[:, ip, 0, :, :], tq1[:], tq2[:], ALU.subtract, rk=["tq1", "tq2"], wk=["Xu"])
                    V_tt(tq1[:], arb, bbi, ALU.mult, rk=[], wk=["tq1"])
                    V_tt(tq2[:], aib, bbr, ALU.mult, rk=[], wk=["tq2"])
                    V_tt(Xu[:, ip, 1, :, :], tq1[:], tq2[:], ALU.add, rk=["tq1", "tq2"], wk=["Xu"])
                Xuv = Xu[:].rearrange("p i r q h -> p (i r q) h")
                for g2 in range(2):
                    V_cp(XPv[g2 * 64:(g2 + 1) * 64, :, g2, :], Xuv[g2 * 64:(g2 + 1) * 64, :, :], rk=["Xu"], wk=["XP"])
                for e_ in range(9):
                    arb = APr[:, e_, qs].unsqueeze(2).broadcast_to([P, 4, 16])
                    aib = APi[:, e_, qs].unsqueeze(2).broadcast_to([P, 4, 16])
                    crr = CRt[:, :, qs].rearrange("p h q -> p q h")
                    cii = CIt[:, :, qs].rearrange("p h q -> p q h")
                    V_tt(tq1[:], crr, arb, ALU.mult, rk=[], wk=["tq1"])
                    V_tt(tq2[:], cii, aib, ALU.mult, rk=[], wk=["tq2"])
                    V_tt(CAu[:, :, e_, 0, :], tq1[:], tq2[:], ALU.subtract, rk=["tq1", "tq2"], wk=["CAu"])
                    V_tt(tq1[:], cii, arb, ALU.mult, rk=[], wk=["tq1"])
                    V_tt(tq2[:], crr, aib, ALU.mult, rk=[], wk=["tq2"])
                    V_stt(CAu[:, :, e_, 1, :], tq1[:], -1.0, tq2[:], ALU.mult, ALU.subtract, rk=["tq1", "tq2"], wk=["CAu"])
                CAuv = CAu[:].rearrange("p q e r h -> p (q e r) h")
                WCv = WCp[par][:].rearrange("p q e r (g h) -> p (q e r) g h", g=2)
                for g2 in range(2):
                    V_cp(WCv[g2 * 64:(g2 + 1) * 64, :, g2, :], CAuv[g2 * 64:(g2 + 1) * 64, :, :], rk=["CAu"], wk=["WCp%d" % par])
                V_cp(BPb[par][:], XP[:, 7, :, :, :], rk=["XP"], wk=["BPb%d" % par])
                for cb in range(4):
                    bk = nbank(6, 8)
                    for j in range(4):
                        ip, ri = divmod(cb * 4 + j, 2)
                        PE_T(psb[bk][:, j * P:(j + 1) * P], XP[:, ip, ri, :, :].rearrange("p q f -> p (q f)"), P,
                             ["XP", "ident"], [pk(bk)], inc=(j == 3))
                    evac_copy(WS[par][:].rearrange("p i r f -> p (i r f)")[:, cb * 512:(cb + 1) * 512], psb[bk][:, :], bk,
                              ["WS%d" % par])
                bk = nbank(6, 8)
                for qq in range(4):
                    for j in range(8):
                        for ri in range(2):
                            PE_mm(psb[bk][32 * qq:32 * qq + 32, j * 32:(j + 1) * 32], BPb[par][:, ri, qq, :],
                                  WCp[par][:, qq, j, ri, :], ri == 0, ri == 1,
                                  ["BPb%d" % par, "WCp%d" % par], [pk(bk)], inc=(qq == 3 and j == 7 and ri == 1),
                                  tp=(0, 32 * qq), sg=True)
                for qq in range(4):
                    V_cp(BD[par][32 * qq:32 * qq + 32, :, 32 * qq:32 * qq + 32],
                         psb[bk][32 * qq:32 * qq + 32, 0:256].rearrange("p (j f) -> p j f", j=8),
                         rk=[pk(bk)], wk=["BD%d" % par])
                for qq in range(4):
                    q = 4 * a + qq
                    hs = q % 2
                    pb = 32 * qq
                    bk = nbank(4, 6)
                    for ri in range(2):
                        for ip in range(8):
                            PE_mm(psb[bk][:, ri * NCK:(ri + 1) * NCK], WS[par][pb:pb + 32, ip, ri, :],
                                  uT[pb:pb + 32, a, ip::8], ip == 0, ip == 7,
                                  ["WS%d" % par] + [("uT", a, n) for n in range(4)], [pk(bk)],
                                  inc=(ri == 1 and ip == 7), tp=(pb, 0))
                    V_cp(Hb[hs][0][:].rearrange("p r c -> p (r c)"), psb[bk][:, :], rk=[pk(bk)],
                         wk=[("Hb", hs, 0, 0), ("Hb", hs, 0, 1)], e="act")
                    for k in range(8):
                        s_ = 1 << k
                        src_ = Hb[hs][k % 2]; dst_ = Hb[hs][(k + 1) % 2]
                        sp_, dp_ = k % 2, (k + 1) % 2
                        n_ = NCK - s_
                        V_cp(dst_[:, :, 0:s_], src_[:, :, 0:s_], rk=[("Hb", hs, sp_, 0), ("Hb", hs, sp_, 1)],
                             wk=[("Hb", hs, dp_, 0), ("Hb", hs, dp_, 1)], e="pool")
                        akr = AKr[:, k, q:q + 1]; aki = AKi[:, k, q:q + 1]; akn = AKn[:, k, q:q + 1]
                        V_stt(dst_[:, 0, s_:], src_[:, 0, 0:n_], akr, src_[:, 0, s_:], ALU.mult, ALU.add,
                              rk=[("Hb", hs, sp_, 0)], wk=[("Hb", hs, dp_, 0)])
                        V_stt(dst_[:, 1, s_:], src_[:, 1, 0:n_], akr, src_[:, 1, s_:], ALU.mult, ALU.add,
                              rk=[("Hb", hs, sp_, 1)], wk=[("Hb", hs, dp_, 1)])
                        V_stt(dst_[:, 0, s_:], src_[:, 1, 0:n_], akn, dst_[:, 0, s_:], ALU.mult, ALU.add,
                              rk=[("Hb", hs, sp_, 1), ("Hb", hs, dp_, 0)], wk=[("Hb", hs, dp_, 0)])
                        V_stt(dst_[:, 1, s_:], src_[:, 0, 0:n_], aki, dst_[:, 1, s_:], ALU.mult, ALU.add,
                              rk=[("Hb", hs, sp_, 0), ("Hb", hs, dp_, 1)], wk=[("Hb", hs, dp_, 1)])
                    V_cp(Hbf[qq][:], Hb[hs][0][:], rk=[("Hb", hs, 0, 0), ("Hb", hs, 0, 1)], wk=[("Hbf", qq)], e="act")
                for i in range(8):
                    for j in range(i + 1):
                        PE_mm(ps_all[:, i * NCK:(i + 1) * NCK], BD[par][:, j, :], uT[:, a, (i - j)::8],
                              (j == 0 and i % 2 == 0), False,
                              ["BD%d" % par] + [("uT", a, n) for n in range(4)], [pk(i // 2)],
                              inc=(j == i), sg=True)
                for qq in range(4):
                    pb = 32 * qq
                    for i in range(8):
                        for ri in range(2):
                            PE_mm(ps_all[pb:pb + 32, i * NCK + 1:(i + 1) * NCK], WCp[par][:, qq, i + 1, ri, :],
                                  Hbf[qq][:, ri, 0:NCK - 1], False, ri == 1,
                                  ["WCp%d" % par, ("Hbf", qq)], [pk(i // 2)], inc=(ri == 1), tp=(0, pb), sg=True)
                for hf in range(2):
                    Yv = ps_all[:, 0:2048].rearrange("p (i c) -> p c i", i=8)[:, hf * 128:(hf + 1) * 128, :]
                    uv = uT[:, a, hf * 1024:(hf + 1) * 1024].rearrange("p (c i) -> p c i", i=8)
                    V_stt(y32[:].rearrange("p (c i) -> p c i", i=8), uv, Dcol[:, a:a + 1], Yv, ALU.mult, ALU.add,
                          rk=[pk(0), pk(1), pk(2), pk(3)] + [("uT", a, n) for n in range(4)], wk=["y32"])
                    A_act(uT[:, a, hf * 1024:(hf + 1) * 1024], y32[:], AF.Gelu_apprx_tanh, rk=["y32"],
                          wk=[("uT", a, 2 * hf), ("uT", a, 2 * hf + 1)])
            tap_bf("zT", uT[:].rearrange("p a t -> p (a t)"), [("uT", a_, n_) for a_ in range(8) for n_ in range(4)])
            S.barrier()
        with ExitStack() as es:
            wglu = sb(es, "wglu", [P, 8, 1024], BF16)
            load_w(wglu, "wglu", w_glu, 8, 1024, "wglu")
            sg = [sb(es, "sg%d" % i, [P, 512], BF16) for i in range(2)]
            for m in range(8):
                for n in range(4):
                    bk = nbank(4, 8)
                    for k in range(8):
                        PE_mm(psb[bk][:, :], wglu[:, k, m * P:(m + 1) * P], uT[:, k, n * 512:(n + 1) * 512],
                              k == 0, k == 7, ["wglu"] + [("uT", k, n)], [pk(bk)], inc=(k == 7))
                    sl = (m * 4 + n) % 2
                    A_act(sg[sl][:], psb[bk][:, :], AF.Sigmoid, rk=[pk(bk)], wk=["sg%d" % sl])
                    V_tt(s5T[:, m, n * 512:(n + 1) * 512], sg[sl][:], uT[:, m, n * 512:(n + 1) * 512], ALU.mult,
                         rk=["sg%d" % sl, ("uT", m, n)], wk=[("s5T", m, n)])
            S.barrier()
        gate(103)
        tap_bf("s5T", s5T[:].rearrange("p a t -> p (a t)"), [])
        reg = {"APr": APr[:].rearrange("p e q -> p (e q)"), "APi": APi[:].rearrange("p e q -> p (e q)"),
               "AKr": AKr[:].rearrange("p e q -> p (e q)"), "BBr": BBr[:].rearrange("p h q -> p (h q)"),
               "BBi": BBi[:].rearrange("p h q -> p (h q)"), "CRt": CRt[:].rearrange("p h q -> p (h q)"),
               "Dcol": Dcol[:]}
        for nm_, ap_ in reg.items():
            if nm_ in tapd:
                S.dma("pool", tapd[nm_], ap_, sem="tap")
        S.barrier()
        es_ssm.close()


        attT = sb(es_mix, "attT", [P, 8, T], BF16)
        S.op("pool", lambda: nc.gpsimd.affine_select(out=tri[:], in_=ones[:], pattern=[[-1, P]], compare_op=ALU.is_gt,
                                                     fill=0.0, base=0, channel_multiplier=1), reads=["ones"], writes=["tri"])
        S.op("pool", lambda: nc.gpsimd.affine_select(out=lem[:], in_=ones[:], pattern=[[1, P]], compare_op=ALU.is_ge,
                                                     fill=0.0, base=0, channel_multiplier=-1), reads=["ones"], writes=["lem"])
        S.op("pool", lambda: nc.gpsimd.affine_select(out=cmf[:], in_=ones[:], pattern=[[1, P]], compare_op=ALU.is_gt,
                                                     fill=0.0, base=0, channel_multiplier=-1), reads=["ones"], writes=["cmf"])
        V_cp(cmb[:], cmf[:])
        V_cp(trib[:], tri[:])
        V_cp(lemb[:], lem[:])
        S.op("dve", lambda: nc.vector.memset(zer[:], 0.0), writes=["zer"])
        S.dma("sp", gq[:], q_norm_g.rearrange("o d -> d o"), writes=["gq"], sem="m6")
        S.dma("sp", gk[:], k_norm_g.rearrange("o d -> d o"), writes=["gk"], sem="m7")
        V_ts(gq[:], gq[:], 1.0 / math.sqrt(128.0), ALU.mult)
        S.barrier()

        for hg in range(2):
            with ExitStack() as es:
                qT = sb(es, "qT%d" % hg, [P, 4, T], BF16); kT = sb(es, "kT%d" % hg, [P, 4, T], BF16)
                vv = sb(es, "vv%d" % hg, [P, 16, 512], BF16)
                with ExitStack() as es2:
                    wst0 = sb(es2, "wstq0_%d" % hg, [P, 16, 512], BF16)
                    wst = [wst0, wst0]
                    sqf = [sb(es2, "sqf%d_%d" % (i, hg), [P, 512]) for i in range(2)]
                    rsq = [sb(es2, "rsq%d_%d" % (i, hg), [P, 512]) for i in range(2)]
                    cnt_ = 0
                    for which, col0 in (("q", 1024 + 512 * hg), ("k", 2048 + 512 * hg), ("v", 3072 + 512 * hg)):
                        sl = 0
                        load_w(wst[sl], "wstq%d" % sl, w_in[:, col0:col0 + 512], 16, 512, "wstq%d" % sl)
                        if which == "v":
                            for tt in range(16):
                                bk = nbank(0, 4)
                                for k in range(16):
                                    PE_mm(psb[bk][:, :], hT[:, k, tt * P:(tt + 1) * P], wst[sl][:, k, :], k == 0, k == 15,
                                          ["wstq%d" % sl], [pk(bk)], inc=(k == 15))
                                evac_copy(vv[:, tt, :], psb[bk][:, :], bk, [("vv", tt)])
                            continue
                        dstT = qT if which == "q" else kT
                        gcol = gq if which == "q" else gk
                        for m in range(4):
                            for n in range(4):
                                bk = nbank(0, 4)
                                for k in range(16):
                                    PE_mm(psb[bk][:, :], wst[sl][:, k, m * P:(m + 1) * P], hT[:, k, n * 512:(n + 1) * 512],
                                          k == 0, k == 15, ["wstq%d" % sl], [pk(bk)], inc=(k == 15))
                                s2 = (m * 4 + n) % 2
                                A_act(sqf[s2][:], psb[bk][:, :], AF.Square, rk=[pk(bk)], wk=["sqf%d" % s2])
                                b2 = nbank(4, 8)
                                PE_mm(psb[b2][:, :], ones[:], sqf[s2][:], True, True, ["ones", "sqf%d" % s2], [pk(b2)])
                                A_act(rsq[s2][:], psb[b2][:, :], AF.Sqrt, scale=1.0 / 128, bias=epsc[:, 0:1],
                                      rk=[pk(b2)], wk=["rsq%d" % s2])
                                S.op("dve", lambda: nc.vector.reciprocal(out=rsq[s2][:], in_=rsq[s2][:]),
                                     reads=["rsq%d" % s2], writes=["rsq%d" % s2])
                                V_stt(dstT[:, m, n * 512:(n + 1) * 512], psb[bk][:, :], gcol[:, 0:1], rsq[s2][:],
                                      ALU.mult, ALU.mult, rk=[pk(bk), "rsq%d" % s2], wk=[(which, m, n)])
                    S.barrier()
                if hg == 0:
                    tap_bf("qT", qT[:].rearrange("p a t -> p (a t)"), [])
                    tap_bf("kT", kT[:].rearrange("p a t -> p (a t)"), [])
                    tap_bf("vv", vv[:].rearrange("p a t -> p (a t)"), [])
                with ExitStack() as es2:
                    SPb = [sb(es2, "SPb%d_%d" % (i, hg), [P, 1024]) for i in range(1)]
                    SPh = [sb(es2, "SPh%d_%d" % (i, hg), [P, 1024], BF16) for i in range(2)]
                    Ab = sb(es2, "Ab%d" % hg, [P, 1024])
                    Wb_ = [sb(es2, "Wb%d_%d" % (i, hg), [P, 1024], BF16) for i in range(2)]
                    ZB = [ps_all[:, 0:1024], ps_all[:, 1024:2048]]
                    TB = ps_all[:, 2048:3072]
                    OB = ps_all[:, 3072:4096]

                    def bank_ranges(lo):
                        rs_ = []
                        for bh in range(2):
                            a_ = max(lo, 512 * bh); b_ = 512 * (bh + 1)
                            if a_ < b_:
                                rs_.append((bh, a_, b_))
                        return rs_

                    for hl in range(4):
                        h = 4 * hg + hl
                        for qh in range(2):
                            kbs = list(range(8 * qh + 7, -1, -1))
                            N_ = len(kbs)
                            for bh in range(2):
                                PE_mm(TB[:, bh * 512:(bh + 1) * 512], zer[:, 0:P], zer[:, :], True, True, ["zer"], [pk(4 + bh)], inc=False, sg=True)
                                PE_mm(OB[:, bh * 512:(bh + 1) * 512], zer[:, 0:P], zer[:, :], True, True, ["zer"], [pk(6 + bh)], inc=(bh == 1), sg=True)

                            def lo_of(n):
                                return max(0, kbs[n] * P - qh * 1024)

                            def diag(n):
                                return kbs[n] * P >= qh * 1024

                            def S1(n):
                                kb = kbs[n]; lo = lo_of(n); zb = n % 2
                                for (bh, a_, b_) in bank_ranges(lo):
                                    PE_mm(ZB[zb][:, a_:b_], kT[:, hl, kb * P:(kb + 1) * P], qT[:, hl, qh * 1024 + a_: qh * 1024 + b_],
                                          True, True, [], [pk(2 * zb + bh)])
                                zk = [pk(2 * zb), pk(2 * zb + 1)]
                                A_act(SPb[0][:, lo:], ZB[zb][:, lo:], AF.Exp, rk=zk, wk=["SPb0"])
                                A_act(SPh[zb][:, lo:], SPb[0][:, lo:], AF.Ln, bias=ones[:, 0:1], rk=["SPb0"], wk=["SPh%d" % zb])
                                if diag(n):
                                    V_tt(SPh[zb][:, lo:lo + P], SPh[zb][:, lo:lo + P], cmb[:], ALU.mult,
                                         rk=["SPh%d" % zb], wk=["SPh%d" % zb])

                            def S2(n):
                                lo = lo_of(n); zb = n % 2
                                for (bh, a_, b_) in bank_ranges(lo):
                                    PE_mm(TB[:, a_:b_], trib[:], SPh[zb][:, a_:b_], False, False, ["SPh%d" % zb], [pk(4 + bh)], sg=True)

                            def S3a(n):
                                lo = lo_of(n); zb = n % 2
                                zk = [pk(2 * zb), pk(2 * zb + 1)]
                                V_tt(Ab[:, lo:], ZB[zb][:, lo:], SPh[zb][:, lo:], ALU.subtract, rk=zk + ["SPh%d" % zb], wk=["Ab"])
                                V_tt(Ab[:, lo:], Ab[:, lo:], TB[:, lo:], ALU.subtract, rk=["Ab", pk(4), pk(5)], wk=["Ab"])
                                A_act(Wb_[zb][:, lo:], Ab[:, lo:], AF.Exp, rk=["Ab"], wk=["Wb%d" % zb])

                            def S3b(n):
                                lo = lo_of(n); zb = n % 2
                                if diag(n):
                                    V_tt(Wb_[zb][:, lo:lo + P], Wb_[zb][:, lo:lo + P], cmb[:], ALU.mult,
                                         rk=["Wb%d" % zb], wk=["Wb%d" % zb])

                            def S4a(n):
                                lo = lo_of(n); zb = n % 2
                                for (bh, a_, b_) in bank_ranges(lo):
                                    PE_mm(TB[:, a_:b_], lemb[:], SPh[zb][:, a_:b_], False, False, ["SPh%d" % zb], [pk(4 + bh)], sg=True)

                            def S4b(n):
                                kb = kbs[n]; lo = lo_of(n); zb = n % 2
                                for (bh, a_, b_) in bank_ranges(lo):
                                    PE_mm(OB[:, a_:b_], vv[:, kb, hl * P:(hl + 1) * P], Wb_[zb][:, a_:b_], False, n == N_ - 1,
                                          ["Wb%d" % zb], [pk(6 + bh)], sg=True)

                            S1(0)
                            if N_ > 1:
                                S1(1)
                            S2(0)
                            for n in range(N_):
                                S3a(n)
                                S4a(n)
                                if n + 2 < N_:
                                    S1(n + 2)
                                S3b(n)
                                if n + 1 < N_:
                                    S2(n + 1)
                                S4b(n)
                            V_cp(attT[:, h, qh * 1024:(qh + 1) * 1024], OB[:, :], rk=[pk(6), pk(7)], wk=[("attT", h, qh)], e="act")
                    S.barrier()
        gate(104)
        tap_bf("attT", attT[:].rearrange("p a t -> p (a t)"), [])


        for th in range(2):
            with ExitStack() as es:
                mT = sb(es, "mT%d" % th, [P, 16, 1024], BF16)
                with ExitStack() as es2:
                    wbs = [sb(es2, "wbs%d_%d" % (i, th), [P, 8, P], BF16) for i in range(2)]
                    wba = [sb(es2, "wba%d_%d" % (i, th), [P, 8, P], BF16) for i in range(2)]
                    wgs = [sb(es2, "wgs%d_%d" % (i, th), [P, 16, P], BF16) for i in range(2)]
                    wga = [sb(es2, "wga%d_%d" % (i, th), [P, 16, P], BF16) for i in range(2)]
                    sgs = [sb(es2, "sgs%d_%d" % (i, th), [P, 512]) for i in range(2)]
                    sga = [sb(es2, "sga%d_%d" % (i, th), [P, 512]) for i in range(2)]
                    for m in range(16):
                        sl = m % 2
                        cs = slice(m * P, (m + 1) * P)
                        load_w(wbs[sl], "wbs%d" % sl, w_branch_ssm[:, cs], 8, P, "wbs%d" % sl)
                        load_w(wba[sl], "wba%d" % sl, w_branch_att[:, cs], 8, P, "wba%d" % sl)
                        load_w(wgs[sl], "wgs%d" % sl, w_in[:, 4096 + m * P:4096 + (m + 1) * P], 16, P, "wgs%d" % sl)
                        load_w(wga[sl], "wga%d" % sl, w_in[:, 6144 + m * P:6144 + (m + 1) * P], 16, P, "wga%d" % sl)
                        for n in range(2):
                            ts_ = slice(th * 1024 + n * 512, th * 1024 + (n + 1) * 512)
                            b_bs, b_gs, b_ba, b_ga = nbank(), nbank(), nbank(), nbank()
                            for k in range(8):
                                PE_mm(psb[b_bs][:, :], wbs[sl][:, k, :], s5T[:, k, ts_], k == 0, k == 7, ["wbs%d" % sl], [pk(b_bs)], inc=(k == 7))
                            for k in range(16):
                                PE_mm(psb[b_gs][:, :], wgs[sl][:, k, :], hT[:, k, ts_], k == 0, k == 15, ["wgs%d" % sl], [pk(b_gs)], inc=(k == 15))
                            for k in range(8):
                                PE_mm(psb[b_ba][:, :], wba[sl][:, k, :], attT[:, k, ts_], k == 0, k == 7, ["wba%d" % sl], [pk(b_ba)], inc=(k == 7))
                            for k in range(16):
                                PE_mm(psb[b_ga][:, :], wga[sl][:, k, :], hT[:, k, ts_], k == 0, k == 15, ["wga%d" % sl], [pk(b_ga)], inc=(k == 15))
                            s2 = n
                            A_act(sgs[s2][:], psb[b_gs][:, :], AF.Sigmoid, rk=[pk(b_gs)], wk=["sgs%d" % s2])
                            A_act(sga[s2][:], psb[b_ga][:, :], AF.Sigmoid, rk=[pk(b_ga)], wk=["sga%d" % s2])
                            V_tt(sgs[s2][:], sgs[s2][:], psb[b_bs][:, :], ALU.mult, rk=["sgs%d" % s2, pk(b_bs)], wk=["sgs%d" % s2])
                            V_tt(sga[s2][:], sga[s2][:], psb[b_ba][:, :], ALU.mult, rk=["sga%d" % s2, pk(b_ba)], wk=["sga%d" % s2])
                            V_tt(mT[:, m, n * 512:(n + 1) * 512], sgs[s2][:], sga[s2][:], ALU.add,
                                 rk=["sgs%d" % s2, "sga%d" % s2], wk=[("mT", m, n)])
                    S.barrier()
                with ExitStack() as es2:
                    wo = [sb(es2, "wo%d_%d" % (i, th), [P, 16, 512], BF16) for i in range(2)]
                    xin = [sb(es2, "xin%d_%d" % (i, th), [P, 512]) for i in range(2)]
                    xo = [sb(es2, "xo%d_%d" % (i, th), [P, 512]) for i in range(2)]
                    cnt_ = 0
                    for db in range(4):
                        sl = db % 2
                        ds_ = slice(db * 512, (db + 1) * 512)
                        load_w(wo[sl], "wo%d" % sl, w_out[:, ds_], 16, 512, "wo%d" % sl)
                        for tt in range(8):
                            r0 = th * 1024 + tt * P
                            s2 = cnt_ % 2
                            cnt_ += 1
                            S.dma("sp", xin[s2][:], x[r0:r0 + P, ds_], writes=["xin%d" % s2], sem="xin%d" % s2)
                            bk = nbank()
                            for k in range(16):
                                PE_mm(psb[bk][:, :], mT[:, k, tt * P:(tt + 1) * P], wo[sl][:, k, :], k == 0, k == 15,
                                      ["wo%d" % sl], [pk(bk)], inc=(k == 15))
                            V_tt(xo[s2][:], xin[s2][:], psb[bk][:, :], ALU.add, rk=["xin%d" % s2, pk(bk)], wk=["xo%d" % s2])
                            S.dma("sp", x2d[r0:r0 + P, ds_], xo[s2][:], reads=["xo%d" % s2], sem="xo%d" % s2)
                    S.barrier()
        gate(105)
        es_mix.close()
        if "x2" in tapd:
            S.dma("sp", tapd["x2"], x2d, sem="tap")
            S.barrier()

        if upto == "F":
            S.barrier()
            return nc
        TS = 256
        NT = 48
        NS = NT * TS
        h2d = nc.dram_tensor("h2_scratch", [T, D], F32, kind="Internal").ap()
        yd = nc.dram_tensor("y_scratch", [NS, D], F32, kind="Internal").ap()
        stok = nc.dram_tensor("slot_tok", [NS, 16], I32, kind="Internal").ap()
        S.dma("sp", g2s[:], ffn_norm_g, writes=["g2s"], sem="m_g2s")
        bk = nbank()
        PE_T(psb[bk][:, 0:16], g2s[:], 16, ["g2s", "ident"], [pk(bk)])
        V_cp(g2T[:], psb[bk][:, 0:16], rk=[pk(bk)], wk=["g2T"])
        with ExitStack() as es:
            wr = sb(es, "wr", [P, 16, 36])
            rb = sb(es, "rb", [P, 36])
            comb_g1 = sb(es, "comb_g1", [P, 16]); comb_g2 = sb(es, "comb_g2", [P, 16])
            oh1a = sb(es, "oh1a", [P, 16, 32]); oh2a = sb(es, "oh2a", [P, 16, 32])
            selb = sb(es, "selb", [P, 16, 32], BF16)
            s1i = sb(es, "s1i", [P, 16], I32); s2i = sb(es, "s2i", [P, 16], I32)
            widx = sb(es, "widx", [P, NT], I32)
            yix = sb(es, "yix", [P, NT * 2], I32)
            onesb = sb(es, "onesb", [P, P], BF16)
            V_cp(onesb[:], ones[:])
            with ExitStack() as es1:
                wlg = sb(es1, "wlg", [16, P * 4]); wle = sb(es1, "wle", [16, P * 32])
                S.dma("sp", wlg[:], router_group_w.rearrange("(c p) f -> c (p f)", p=P), writes=["wlg"], sem="m_wlg")
                S.dma("sp", wle[:], router_expert_w.rearrange("(c p) f -> c (p f)", p=P), writes=["wle"], sem="m_wle")
                wlg2 = sb(es1, "wlg2", [16, 4, P]); wle2 = sb(es1, "wle2", [16, 32, P])
                V_cp(wlg2[:], wlg[:].rearrange("c (p f) -> c f p", f=4), rk=["wlg"], wk=["wlg2"])
                V_cp(wle2[:], wle[:].rearrange("c (p f) -> c f p", f=32), rk=["wle"], wk=["wle2"])
                bk = nbank()
                for f in range(4):
                    PE_T(psb[bk][:, f * 16:(f + 1) * 16], wlg2[:, f, :], 16, ["wlg2", "ident"], [pk(bk)], inc=(f == 3))
                V_cp(wr[:, :, 0:4].rearrange("p c f -> p f c"), psb[bk][:, 0:64].rearrange("p (f c) -> p f c", c=16), rk=[pk(bk)], wk=["wr"])
                bk = nbank()
                for f in range(32):
                    PE_T(psb[bk][:, f * 16:(f + 1) * 16], wle2[:, f, :], 16, ["wle2", "ident"], [pk(bk)], inc=(f == 31))
                V_cp(wr[:, :, 4:36].rearrange("p c f -> p f c"), psb[bk][:, 0:512].rearrange("p (f c) -> p f c", c=16), rk=[pk(bk)], wk=["wr"])
                S.barrier()
            S.dma("sp", rb[:, 0:4], router_group_b.to_broadcast([P, 4]), writes=["rb"], sem="m_rb0")
            S.dma("sp", rb[:, 4:36], router_expert_b.to_broadcast([P, 32]), writes=["rb"], sem="m_rb1")
            with ExitStack() as es3:
                gb = sb(es3, "gb", [P, D])
                S.dma("sp", gb[:], ffn_norm_g_row.to_broadcast([P, D]), writes=["gb"], sem="m_gb")
                xt2 = [sb(es3, "xt2_%d" % i, [P, D]) for i in range(2)]
                xn = [sb(es3, "xn_%d" % i, [P, D]) for i in range(2)]
                h2r = [sb(es3, "h2r_%d" % i, [P, D]) for i in range(2)]
                junk = sb(es3, "junk2", [P, D], BF16)
                h32 = sb(es3, "h32", [P, 16, P])
                ss2 = sb(es3, "ss2", [P, 16]); rs2 = sb(es3, "rs2", [P, 16])
                lgA = sb(es3, "lgA", [P, 16, 36])
                gmaxA = sb(es3, "gmaxA", [P, 16]); gexA = sb(es3, "gexA", [P, 16, 4]); gmA = sb(es3, "gmA", [P, 16, 4])
                gsumA = sb(es3, "gsumA", [P, 16]); mlA = sb(es3, "mlA", [P, 16, 32]); ml2A = sb(es3, "ml2A", [P, 16, 32])
                m1A = sb(es3, "m1A", [P, 16]); m2A = sb(es3, "m2A", [P, 16])
                sm = [sb(es3, "sm%d" % i, [P, 1]) for i in range(8)]
                ml = sb(es3, "ml", [P, 32]); ml2 = sb(es3, "ml2", [P, 32])
                gm = sb(es3, "gm", [P, 4]); gex = sb(es3, "gex", [P, 4])
                for tt in range(16):
                    r0 = tt * P
                    sl = tt % 2
                    xk = "xt2_%d" % sl
                    S.dma("sp", xt2[sl][:], x2d[r0:r0 + P, :], writes=[xk], sem="xt2_%d" % sl)
                    A_act(junk[:], xt2[sl][:], AF.Square, rk=[xk], wk=["junk2"], accum_out=ss2[:, tt:tt + 1])
                    A_act(rs2[:, tt:tt + 1], ss2[:, tt:tt + 1], AF.Sqrt, scale=1.0 / D, bias=epsc[:, 0:1],
                          rk=["junk2"], wk=[("rs2", tt)])
                    S.op("dve", lambda: nc.vector.reciprocal(out=rs2[:, tt:tt + 1], in_=rs2[:, tt:tt + 1]),
                         reads=[("rs2", tt)], writes=[("rs2", tt)])
                    V_ts(xn[sl][:], xt2[sl][:], rs2[:, tt:tt + 1], ALU.mult, rk=[xk, ("rs2", tt)], wk=["xn%d" % sl])
                    V_tt(h2r[sl][:], xn[sl][:], gb[:], ALU.mult, rk=["xn%d" % sl, "gb"], wk=["h2r%d" % sl], e="pool")
                    S.dma("sp", h2d[r0:r0 + P, :], h2r[sl][:], reads=["h2r%d" % sl], sem="h2w%d" % sl)
                    for cb in range(4):
                        bk = nbank()
                        for j in range(4):
                            c = cb * 4 + j
                            PE_T(psb[bk][:, j * P:(j + 1) * P], xn[sl][:, c * P:(c + 1) * P], P, ["xn%d" % sl, "ident"], [pk(bk)], inc=(j == 3))
                        e_ = S.alt()
                        for j in range(4):
                            c = cb * 4 + j
                            if e_ == "dve":
                                V_ts(h32[:, c, :], psb[bk][:, j * P:(j + 1) * P], g2T[:, c:c + 1], ALU.mult,
                                     rk=[pk(bk)], wk=[("h32", c)])
                            else:
                                S.op("act", lambda: nc.scalar.activation(out=h32[:, c, :], in_=psb[bk][:, j * P:(j + 1) * P],
                                                                         func=AF.Copy, scale=g2T[:, c:c + 1]),
                                     reads=[pk(bk)], writes=[("h32", c)])
                    bk = nbank()
                    for c in range(16):
                        PE_mm(psb[bk][:, 0:36], h32[:, c, :], wr[:, c, :], c == 0, c == 15,
                              [("h32", c), "wr"], [pk(bk)], inc=(c == 15))
                    V_tt(lgA[:, tt, :], psb[bk][:, 0:36], rb[:], ALU.add, rk=[pk(bk), "rb"], wk=[("lgA", tt)])
                lk = [("lgA", t_) for t_ in range(16)]
                AX = mybir.AxisListType.X
                gl = lgA[:, :, 0:4]
                el4 = lgA[:, :, 4:36].rearrange("p t (g e) -> p t g e", g=4)
                S.op("dve", lambda: nc.vector.tensor_reduce(out=gmaxA[:], in_=gl, axis=AX, op=ALU.max), reads=lk, writes=["gmaxA"])
                V_tt(gexA[:], gl, gmaxA[:].unsqueeze(2).broadcast_to([P, 16, 4]), ALU.subtract, rk=lk + ["gmaxA"], wk=["gexA"])
                V_ts(gmA[:], gexA[:], 0.0, ALU.is_ge, s2=-1.0, op1=ALU.add, rk=["gexA"], wk=["gmA"])
                V_ts(gmA[:], gmA[:], 1e30, ALU.mult, rk=["gmA"], wk=["gmA"])
                A_act(gexA[:], gexA[:], AF.Exp, rk=["gexA"], wk=["gexA"])
                S.op("dve", lambda: nc.vector.tensor_reduce(out=gsumA[:], in_=gexA[:], axis=AX, op=ALU.add), reads=["gexA"], writes=["gsumA"])
                S.op("dve", lambda: nc.vector.reciprocal(out=gsumA[:], in_=gsumA[:]), reads=["gsumA"], writes=["gsumA"])
                V_tt(mlA[:].rearrange("p t (g e) -> p t g e", g=4), el4, gmA[:].unsqueeze(3).broadcast_to([P, 16, 4, 8]), ALU.add,
                     rk=lk + ["gmA"], wk=["mlA"])
                S.op("dve", lambda: nc.vector.tensor_reduce(out=m1A[:], in_=mlA[:], axis=AX, op=ALU.max), reads=["mlA"], writes=["m1A"])
                V_tt(oh1a[:], mlA[:], m1A[:].unsqueeze(2).broadcast_to([P, 16, 32]), ALU.is_equal, rk=["mlA", "m1A"], wk=["oh1a"])
                V_stt(ml2A[:], oh1a[:], -1e30, mlA[:], ALU.mult, ALU.add, rk=["oh1a", "mlA"], wk=["ml2A"])
                S.op("dve", lambda: nc.vector.tensor_reduce(out=m2A[:], in_=ml2A[:], axis=AX, op=ALU.max), reads=["ml2A"], writes=["m2A"])
                V_tt(oh2a[:], ml2A[:], m2A[:].unsqueeze(2).broadcast_to([P, 16, 32]), ALU.is_equal, rk=["ml2A", "m2A"], wk=["oh2a"])
                V_tt(m1A[:], m1A[:], m2A[:], ALU.subtract, rk=["m1A", "m2A"], wk=["m1A"])
                A_act(m1A[:], m1A[:], AF.Sigmoid, rk=["m1A"], wk=["m1A"])
                V_tt(comb_g1[:], gsumA[:], m1A[:], ALU.mult, rk=["gsumA", "m1A"], wk=["comb_g1"])
                V_tt(comb_g2[:], gsumA[:], comb_g1[:], ALU.subtract, rk=["gsumA", "comb_g1"], wk=["comb_g2"])
                V_tt(selb[:], oh1a[:], oh2a[:], ALU.add, rk=["oh1a", "oh2a"], wk=[("selb", t_) for t_ in range(16)])
                S.barrier()
            if "g1" in tapd:
                S.dma("sp", tapd["g1"], comb_g1[:], sem="tap"); S.dma("sp", tapd["oh1"], oh1a[:].rearrange("p t e -> p (t e)"), sem="tap")
                S.barrier()
            gate(106)
            with ExitStack() as es3:
                cntx = sb(es3, "cntx", [P, 16, 32]); tot = sb(es3, "tot", [P, 32])
                ci = sb(es3, "ci", [P, 32], I32); pad = sb(es3, "pad", [P, 32]); incl = sb(es3, "incl", [P, 32])
                base = sb(es3, "base", [P, 32]); zz = sb(es3, "zz", [P, 32])
                slot = sb(es3, "slot", [P, 16, 32]); tmp3 = sb(es3, "tmp3", [P, 16, 32])
                s1f = sb(es3, "s1f", [P, 16]); s2f = sb(es3, "s2f", [P, 16])
                jv = sb(es3, "jv", [P, NT]); pidf = sb(es3, "pidf", [P, 1])
                cmp3 = sb(es3, "cmp3", [P, NT, 32]); ejf = sb(es3, "ejf", [P, NT])
                tid = sb(es3, "tid", [P, 16, 16], I32)
                zi = sb(es3, "zi", [P, NS * 16 // P], I32)
                bk = nbank(); bk2 = nbank()
                for tt in range(16):
                    for t2 in range(tt + 1):
                        PE_mm(psb[bk][:, tt * 32:(tt + 1) * 32], cmb[:] if t2 == tt else onesb[:], selb[:, t2, :],
                              t2 == 0, t2 == tt, [("selb", t2)], [pk(bk)], inc=(t2 == tt), sg=True)
                for tt in range(16):
                    PE_mm(psb[bk2][:, 0:32], onesb[:], selb[:, tt, :], tt == 0, tt == 15, [("selb", tt)], [pk(bk2)], inc=(tt == 15))
                V_cp(cntx[:].rearrange("p t e -> p (t e)"), psb[bk][:, :], rk=[pk(bk)], wk=["cntx"])
                V_cp(tot[:], psb[bk2][:, 0:32], rk=[pk(bk2)], wk=["tot"])
                S.op("dve", lambda: nc.vector.memset(zz[:], 0.0), writes=["zz"])
                V_ts(ci[:], tot[:], float(TS - 1), ALU.add)
                S.op("dve", lambda: nc.vector.tensor_scalar(out=ci[:], in0=ci[:], scalar1=8, scalar2=8,
                                                            op0=ALU.arith_shift_right, op1=ALU.logical_shift_left),
                     reads=["ci"], writes=["ci"])
                V_cp(pad[:], ci[:])
                S.op("dve", lambda: nc.vector.tensor_tensor_scan(out=incl[:], data0=pad[:], data1=zz[:], initial=0.0,
                                                                 op0=ALU.add, op1=ALU.add),
                     reads=["pad", "zz"], writes=["incl"])
                V_tt(base[:], incl[:], pad[:], ALU.subtract)
                V_tt(slot[:], cntx[:], base[:].unsqueeze(1).broadcast_to([P, 16, 32]), ALU.add, rk=["cntx", "base"], wk=["slot"])
                V_tt(tmp3[:], slot[:], oh1a[:], ALU.mult, rk=["slot"], wk=["tmp3"])
                S.op("dve", lambda: nc.vector.tensor_reduce(out=s1f[:], in_=tmp3[:], axis=mybir.AxisListType.X, op=ALU.add),
                     reads=["tmp3"], writes=["s1f"])
                V_cp(s1i[:], s1f[:])
                V_tt(tmp3[:], slot[:], oh2a[:], ALU.mult, rk=["slot", "s1f"], wk=["tmp3"])
                S.op("dve", lambda: nc.vector.tensor_reduce(out=s2f[:], in_=tmp3[:], axis=mybir.AxisListType.X, op=ALU.add),
                     reads=["tmp3"], writes=["s2f"])
                V_cp(s2i[:], s2f[:])
                S.op("pool", lambda: nc.gpsimd.iota(jv[:], pattern=[[TS, NT]], base=0, channel_multiplier=0,
                                                    allow_small_or_imprecise_dtypes=True), writes=["jv"])
                S.op("pool", lambda: nc.gpsimd.iota(pidf[:], pattern=[[0, 1]], base=0, channel_multiplier=1,
                                                    allow_small_or_imprecise_dtypes=True), writes=["pidf"])
                V_tt(cmp3[:], incl[:].unsqueeze(1).broadcast_to([P, NT, 32]), jv[:].unsqueeze(2).broadcast_to([P, NT, 32]),
                     ALU.is_le, rk=["incl", "jv"], wk=["cmp3"])
                S.op("dve", lambda: nc.vector.tensor_reduce(out=ejf[:], in_=cmp3[:], axis=mybir.AxisListType.X, op=ALU.add),
                     reads=["cmp3"], writes=["ejf"])
                emp = sb(es3, "emp", [P, NT])
                V_ts(emp[:], ejf[:], 32.0, ALU.is_ge, s2=65536.0, op1=ALU.mult, rk=["ejf"], wk=["emp"])
                V_ts(ejf[:], ejf[:], 31.0, ALU.min, s2=128.0, op1=ALU.mult)
                V_ts(ejf[:], ejf[:], pidf[:, 0:1], ALU.add)
                V_tt(ejf[:], ejf[:], emp[:], ALU.add)
                V_cp(widx[:], ejf[:])
                rowf = sb(es3, "rowf", [P, NT * 2]); endv = sb(es3, "endv", [P, 32])
                cAB = sb(es3, "cAB", [P, NT * 2, 32]); nA = sb(es3, "nA", [P, NT * 2]); nB = sb(es3, "nB", [P, NT * 2])
                S.op("pool", lambda: nc.gpsimd.iota(rowf[:], pattern=[[P, NT * 2]], base=0, channel_multiplier=1,
                                                    allow_small_or_imprecise_dtypes=True), writes=["rowf"])
                V_tt(endv[:], base[:], tot[:], ALU.add, rk=["base", "tot"], wk=["endv"])
                V_tt(cAB[:], base[:].unsqueeze(1).broadcast_to([P, NT * 2, 32]), rowf[:].unsqueeze(2).broadcast_to([P, NT * 2, 32]),
                     ALU.is_le, rk=["base", "rowf"], wk=["cAB"])
                S.op("dve", lambda: nc.vector.tensor_reduce(out=nA[:], in_=cAB[:], axis=mybir.AxisListType.X, op=ALU.add),
                     reads=["cAB"], writes=["nA"])
                V_tt(cAB[:], endv[:].unsqueeze(1).broadcast_to([P, NT * 2, 32]), rowf[:].unsqueeze(2).broadcast_to([P, NT * 2, 32]),
                     ALU.is_le, rk=["endv", "rowf", "nA"], wk=["cAB"])
                S.op("dve", lambda: nc.vector.tensor_reduce(out=nB[:], in_=cAB[:], axis=mybir.AxisListType.X, op=ALU.add),
                     reads=["cAB"], writes=["nB"])
                V_tt(nA[:], nA[:], nB[:], ALU.subtract, rk=["nA", "nB"], wk=["nA"])
                V_ts(nA[:], nA[:], -65536.0, ALU.mult, s2=65536.0, op1=ALU.add, rk=["nA"], wk=["nA"])
                V_tt(nA[:], nA[:], rowf[:], ALU.add, rk=["nA", "rowf"], wk=["nA"])
                V_cp(yix[:], nA[:], rk=["nA"], wk=["yix"])
                S.op("pool", lambda: nc.gpsimd.iota(tid[:], pattern=[[P, 16], [0, 16]], base=0, channel_multiplier=1), writes=["tid"])
                S.op("dve", lambda: nc.vector.memset(zi[:], 4096), writes=["zi"])
                S.dma("sp", stok.rearrange("(p a) f -> p (a f)", p=P), zi[:], reads=["zi"], writes=["stok"], sem="stz")
                for tt in range(16):
                    for (sx, nm_) in ((s1i, "s1i"), (s2i, "s2i")):
                        S.idma(stok, sx[:, tt:tt + 1], tid[:, tt, :], None, reads=[nm_, "tid", "stok"], writes=[("stokw", tt, nm_)], sem="scat")
                S.barrier()
            if "s1i" in tapd:
                S.dma("pool", tapd["s1i"], s1i[:], sem="tap"); S.dma("pool", tapd["widx"], widx[:], sem="tap")
                S.barrier()
            gate(107)
            with ExitStack() as es3:
                tix = [sb(es3, "tix%d" % i, [P, 2], I32) for i in range(2)]
                xg = [sb(es3, "xg%d" % i, [P, 2, D]) for i in range(2)]
                xT = [sb(es3, "xT%d" % i, [P, 16, TS], BF16) for i in range(2)]
                wgb = [sb(es3, "wgb%d" % i, [P, 16, DFF], BF16) for i in range(2)]
                wub = [sb(es3, "wub%d" % i, [P, 16, DFF], BF16) for i in range(2)]
                wdb = [sb(es3, "wdb%d" % i, [P, 4, D], BF16) for i in range(2)]
                hidb = [sb(es3, "hidb%d" % i, [P, 4, TS], BF16) for i in range(2)]
                sgm = [sb(es3, "sgm%d" % i, [P, DFF]) for i in range(2)]
                ysb = [sb(es3, "ysb%d" % i, [P, D]) for i in range(2)]
                for i_ in range(2):
                    S.op("dve", lambda: nc.vector.memset(xg[i_][:], 0.0), writes=[("xg", i_, 0), ("xg", i_, 1)])
                    S.op("dve", lambda: nc.vector.memset(wgb[i_][:], 0.0), writes=["wgb%d" % i_])
                    S.op("dve", lambda: nc.vector.memset(wub[i_][:], 0.0), writes=["wub%d" % i_])
                    S.op("dve", lambda: nc.vector.memset(wdb[i_][:], 0.0), writes=["wdb%d" % i_])
                wgv = expert_w_gate.rearrange("e (p c) f -> (e p) (c f)", p=P)
                wuv = expert_w_up.rearrange("e (p c) f -> (e p) (c f)", p=P)
                wdv = expert_w_down.rearrange("e (p c) d -> (e p) (c d)", p=P)
                ycnt_ = [0]

                def Lt(j, sl):
                    for h in range(2):
                        S.dma("sp", tix[sl][:, h:h + 1], stok[j * TS + h * P:j * TS + (h + 1) * P, 0:1],
                              writes=["tix%d" % sl], sem="tix%d" % sl, allow_slow_non_contiguous=True)
                    for h in range(2):
                        S.idma(xg[sl][:, h, :], None, h2d, tix[sl][:, h:h + 1], reads=["tix%d" % sl], writes=[("xg", sl, h)], sem="xg%d" % sl, bounds=T - 1)
                    S.idma(wgb[sl][:].rearrange("p c f -> p (c f)"), None, wgv, widx[:, j:j + 1], reads=[], writes=["wgb%d" % sl], sem="wgb%d" % sl, bounds=NE * P - 1)
                    S.idma(wub[sl][:].rearrange("p c f -> p (c f)"), None, wuv, widx[:, j:j + 1], reads=[], writes=["wub%d" % sl], sem="wub%d" % sl, bounds=NE * P - 1)
                    S.idma(wdb[sl][:].rearrange("p c f -> p (c f)"), None, wdv, widx[:, j:j + 1], reads=[], writes=["wdb%d" % sl], sem="wdb%d" % sl, bounds=NE * P - 1)

                def Ct(j, sl):
                    for h in range(2):
                        for cb in range(4):
                            bk = nbank(0, 4)
                            for jj in range(4):
                                c = cb * 4 + jj
                                PE_T(psb[bk][:, jj * P:(jj + 1) * P], xg[sl][:, h, c::16], P, [("xg", sl, h), "ident"], [pk(bk)], inc=(jj == 3))
                            evac_copy(xT[sl][:, cb * 4:(cb + 1) * 4, h * P:(h + 1) * P],
                                      psb[bk][:, :].rearrange("p (j s) -> p j s", j=4), bk, [("xT", sl, h, cb)])
                    xk_ = [("xT", sl, h, cb) for h in range(2) for cb in range(4)]
                    for h in range(2):
                        bg, bu = nbank(4, 8), nbank(4, 8)
                        xkh = [("xT", sl, h, cb) for cb in range(4)]
                        for c in range(16):
                            PE_mm(psb[bg][:, :], xT[sl][:, c, h * P:(h + 1) * P], wgb[sl][:, c, :], c == 0, c == 15,
                                  ["wgb%d" % sl] + xkh, [pk(bg)], inc=(c == 15))
                        for c in range(16):
                            PE_mm(psb[bu][:, :], xT[sl][:, c, h * P:(h + 1) * P], wub[sl][:, c, :], c == 0, c == 15,
                                  ["wub%d" % sl] + xkh, [pk(bu)], inc=(c == 15))
                        s2 = h
                        A_act(sgm[s2][:], psb[bg][:, :], AF.Silu, rk=[pk(bg)], wk=["sgm%d" % s2])
                        V_tt(sgm[s2][:], sgm[s2][:], psb[bu][:, :], ALU.mult, rk=["sgm%d" % s2, pk(bu)], wk=["sgm%d" % s2])
                        bt = nbank(0, 4)
                        for fc in range(4):
                            PE_T(psb[bt][:, fc * P:(fc + 1) * P], sgm[s2][:, fc::4], P, ["sgm%d" % s2, "ident"], [pk(bt)], inc=(fc == 3))
                        evac_copy(hidb[sl][:, :, h * P:(h + 1) * P], psb[bt][:, :].rearrange("p (f s) -> p f s", f=4), bt,
                                  [("hidb", sl, fc_, h) for fc_ in range(4)])
                    for h in range(2):
                        ys = ycnt_[0] % 2
                        ycnt_[0] += 1
                        for db in range(4):
                            bk = nbank(0, 4)
                            for fc in range(4):
                                PE_mm(psb[bk][:, :], hidb[sl][:, fc, h * P:(h + 1) * P], wdb[sl][:, fc, db * 512:(db + 1) * 512],
                                      fc == 0, fc == 3, ["wdb%d" % sl, ("hidb", sl, fc, h)], [pk(bk)], inc=(fc == 3))
                            evac_copy(ysb[ys][:, db * 512:(db + 1) * 512], psb[bk][:, :], bk, [("ysb", ys, db)])
                        r0 = j * TS + h * P
                        S.idma(yd, yix[:, 2 * j + h:2 * j + h + 1], ysb[ys][:], None, reads=[("ysb", ys, db_) for db_ in range(4)],
                               sem="yw%d" % ys, bounds=NS - 1)


                NH = 32
                Lt(0, 0); Lt(1, 1)
                for k in range(NH):
                    sl = k % 2
                    Ct(k, sl)
                    if k < NT - NH:
                        Lt(NT - 1 - k, sl); Ct(NT - 1 - k, sl)
                    if k + 2 < NH:
                        Lt(k + 2, sl)
                S.barrier()
            gate(108)
            with ExitStack() as es3:
                xa = [sb(es3, "xa%d" % i, [P, D]) for i in range(2)]
                y1 = [sb(es3, "y1_%d" % i, [P, D]) for i in range(2)]
                y2 = [sb(es3, "y2_%d" % i, [P, D]) for i in range(2)]
                for tt in range(16):
                    sl = tt % 2
                    r0 = tt * P
                    S.dma("sp", xa[sl][:], x2d[r0:r0 + P, :], writes=["xa%d" % sl], sem="xa%d" % sl)
                    S.idma(y1[sl][:], None, yd, s1i[:, tt:tt + 1], reads=[], writes=["y1_%d" % sl], sem="y1_%d" % sl)
                    S.idma(y2[sl][:], None, yd, s2i[:, tt:tt + 1], reads=[], writes=["y2_%d" % sl], sem="y2_%d" % sl)
                    V_stt(xa[sl][:], y1[sl][:], comb_g1[:, tt:tt + 1], xa[sl][:], ALU.mult, ALU.add,
                          rk=["y1_%d" % sl, "xa%d" % sl], wk=["xa%d" % sl])
                    V_stt(xa[sl][:], y2[sl][:], comb_g2[:, tt:tt + 1], xa[sl][:], ALU.mult, ALU.add,
                          rk=["y2_%d" % sl, "xa%d" % sl], wk=["xa%d" % sl])
                    S.dma("sp", out[r0:r0 + P, :], xa[sl][:], reads=["xa%d" % sl], sem="outd%d" % sl)
                S.barrier()

        S.barrier()
        import os
        if os.environ.get("KDEBUG"):
            print("sched counts", S.cnt, {k: v[1] for k, v in S.dsem.items()})
    return nc


_INPUT_LAYOUT = {
    "x": None,
}


def _prep_inputs(inputs, b):
    g = lambda k: np.ascontiguousarray(np.asarray(inputs[k], dtype=np.float32))
    m = {
        "x": g("x")[b],
        "attn_norm_g": g("attn_norm_g")[0].reshape(16, 128),
        "w_in": g("w_in")[0],
        "lambda_re": g("lambda_re")[0].reshape(32, 128),
        "lambda_im": g("lambda_im")[0].reshape(32, 128),
        "log_dt": g("log_dt")[0].reshape(32, 2),
        "ssm_b_re": g("ssm_b_re")[0].reshape(32, 2048),
        "ssm_b_im": g("ssm_b_im")[0].reshape(32, 2048),
        "ssm_c_re": g("ssm_c_re")[0].reshape(32, 2048),
        "ssm_c_im": g("ssm_c_im")[0].reshape(32, 2048),
        "ssm_d": g("ssm_d")[0].reshape(8, 128),
        "w_glu": g("w_glu")[0],
        "q_norm_g": g("q_norm_g")[0].reshape(1, 128),
        "k_norm_g": g("k_norm_g")[0].reshape(1, 128),
        "w_branch_ssm": g("w_branch_ssm")[0],
        "w_branch_att": g("w_branch_att")[0],
        "w_out": g("w_out")[0],
        "ffn_norm_g": g("ffn_norm_g")[0].reshape(16, 128),
        "ffn_norm_g_row": g("ffn_norm_g")[0].reshape(1, D),
        "router_group_w": g("router_group_w")[0],
        "router_group_b": g("router_group_b")[0].reshape(1, 4),
        "router_expert_w": g("router_expert_w")[0],
        "router_expert_b": g("router_expert_b")[0].reshape(1, 32),
        "expert_w_gate": g("expert_w_gate")[0],
        "expert_w_up": g("expert_w_up")[0],
        "expert_w_down": g("expert_w_down")[0],
    }
    return m


def kernel(**inputs):
    nc = build()
    shared = _prep_inputs(inputs, 0)
    xs = np.asarray(inputs["x"], dtype=np.float32)
    in_maps = []
    for b in range(8):
        m = dict(shared)
        m["x"] = np.ascontiguousarray(xs[b])
        in_maps.append(m)
    res = run_bass_kernel_spmd(nc, in_maps, core_ids=list(range(8)))
    return np.stack([np.asarray(r["out"], dtype=np.float32) for r in res.results], axis=0)
```

```python
import math
from contextlib import ExitStack

import numpy as np
import concourse.bass as bass
import concourse.mybir as mybir
from concourse.bass_utils import run_bass_kernel_spmd

F32 = mybir.dt.float32
BF16 = mybir.dt.bfloat16
I32 = mybir.dt.int32
AF = mybir.ActivationFunctionType
ALU = mybir.AluOpType

T = 2048
D = 2048
P = 128
NCK = 256
EPS = 1e-6
NE = 32
DFF = 512


class Sched:
    def __init__(self, nc):
        self.nc = nc
        self.eng = {"pe": nc.tensor, "act": nc.scalar, "dve": nc.vector,
                    "pool": nc.gpsimd, "sp": nc.sync}
        self.sem = {e: nc.alloc_semaphore(name="s_" + e) for e in self.eng}
        self.cnt = {e: 0 for e in self.eng}
        self.waited = {e: {} for e in self.eng}
        self.res = {}
        self.dsem = {}
        self.rr = 0
        self.dead = False
        self.bregs = {}

    def _toks(self, reads, writes):
        toks = []
        for k in reads:
            st = self.res.get(k)
            if st is not None and st[0] is not None:
                toks.append(st[0])
        for k in writes:
            st = self.res.get(k)
            if st is not None:
                if st[0] is not None:
                    toks.append(st[0])
                toks.extend(st[1])
        return toks

    def _wait(self, e, toks):
        need = {}
        for (s, v) in toks:
            if s == e and e in ("pe", "sp"):
                continue
            if v > need.get(s, 0):
                need[s] = v
        for s, v in need.items():
            if self.waited[e].get(s, 0) >= v:
                continue
            h = self.sem[s] if s in self.sem else self.dsem[s][0]
            self.eng[e].wait_ge(h, v)
            self.waited[e][s] = v

    def _mark(self, tok, reads, writes):
        for k in reads:
            st = self.res.setdefault(k, [None, []])
            st[1].append(tok)
            if len(st[1]) > 24:
                mx = {}
                for (s, v) in st[1]:
                    if v > mx.get(s, 0):
                        mx[s] = v
                st[1] = list(mx.items())
        for k in writes:
            self.res[k] = [tok, []]

    def op(self, e, fn, reads=(), writes=(), inc=True):
        if self.dead:
            return None
        self._wait(e, self._toks(reads, writes))
        ins = fn()
        tok = (e, self.cnt[e] + 1)
        if inc:
            ins.then_inc(self.sem[e], 1)
            self.cnt[e] += 1
        self._mark(tok, reads, writes)
        return ins

    def dma(self, q, out, in_, reads=(), writes=(), sem="d", **kw):
        if self.dead:
            return None
        self._wait(q, self._toks(reads, writes))
        if sem not in self.dsem:
            self.dsem[sem] = [self.nc.alloc_semaphore(name="d_" + sem), 0]
        d = self.dsem[sem]
        d[1] += 16
        self.eng[q].dma_start(out=out, in_=in_, **kw).then_inc(d[0], 16)
        self._mark((sem, d[1]), reads, writes)

    def idma(self, out, out_idx, in_, in_idx, reads=(), writes=(), sem="id", bounds=None):
        if self.dead:
            return None
        self._wait("pool", self._toks(reads, writes))
        if sem not in self.dsem:
            self.dsem[sem] = [self.nc.alloc_semaphore(name="d_" + sem), 0]
        d = self.dsem[sem]
        d[1] += 16
        oo = bass.IndirectOffsetOnAxis(ap=out_idx, axis=0) if out_idx is not None else None
        io = bass.IndirectOffsetOnAxis(ap=in_idx, axis=0) if in_idx is not None else None
        kw = {}
        if bounds is not None:
            if bounds not in self.bregs:
                self.bregs[bounds] = self.nc.gpsimd.to_reg(bounds)
            kw = {"bounds_check": self.bregs[bounds], "oob_is_err": False}
        self.nc.gpsimd.indirect_dma_start(out=out, out_offset=oo, in_=in_, in_offset=io, **kw).then_inc(d[0], 16)
        self._mark((sem, d[1]), reads, writes)

    def barrier(self):
        toks = [(e, self.cnt[e]) for e in self.eng if self.cnt[e] > 0]
        toks += [(s, d[1]) for s, d in self.dsem.items() if d[1] > 0]
        for e in self.eng:
            self._wait(e, [t for t in toks if t[0] != e])
        self.res = {}

    def alt(self):
        self.rr ^= 1
        return "act" if self.rr else "dve"


class _Stop(Exception):
    pass


def build(upto="all", taps=()):
    import os
    kgate = int(os.environ.get("KGATE", "0"))

    gate_s = [None]

    def gate(n):
        if kgate == n:
            gate_s[0].dead = True
    nc = bass.Bass("TRN2", target_bir_lowering=False)
    S = Sched(nc)
    gate_s[0] = S
    dram = {}

    def din(name, shape):
        dram[name] = nc.dram_tensor(name, list(shape), F32, kind="ExternalInput").ap()
        return dram[name]

    x = din("x", [T, D])
    attn_norm_g = din("attn_norm_g", [16, 128])
    w_in = din("w_in", [D, 8192])
    lambda_re = din("lambda_re", [32, 128])
    lambda_im = din("lambda_im", [32, 128])
    log_dt = din("log_dt", [32, 2])
    ssm_b_re = din("ssm_b_re", [32, 2048])
    ssm_b_im = din("ssm_b_im", [32, 2048])
    ssm_c_re = din("ssm_c_re", [32, 2048])
    ssm_c_im = din("ssm_c_im", [32, 2048])
    ssm_d = din("ssm_d", [8, 128])
    w_glu = din("w_glu", [1024, 1024])
    q_norm_g = din("q_norm_g", [1, 128])
    k_norm_g = din("k_norm_g", [1, 128])
    w_branch_ssm = din("w_branch_ssm", [1024, 2048])
    w_branch_att = din("w_branch_att", [1024, 2048])
    w_out = din("w_out", [D, D])
    ffn_norm_g = din("ffn_norm_g", [16, 128])
    ffn_norm_g_row = din("ffn_norm_g_row", [1, D])
    router_group_w = din("router_group_w", [D, 4])
    router_group_b = din("router_group_b", [1, 4])
    router_expert_w = din("router_expert_w", [D, 32])
    router_expert_b = din("router_expert_b", [1, 32])
    expert_w_gate = din("expert_w_gate", [NE, D, DFF])
    expert_w_up = din("expert_w_up", [NE, D, DFF])
    expert_w_down = din("expert_w_down", [NE, DFF, D])
    out = nc.dram_tensor("out", [T, D], F32, kind="ExternalOutput").ap()
    x2d = nc.dram_tensor("x2_scratch", [T, D], F32, kind="Internal").ap()
    tapd = {}
    for (nm, shp) in taps:
        tapd[nm] = nc.dram_tensor("tap_" + nm, list(shp), F32, kind="ExternalOutput").ap()

    top = ExitStack()
    with top:
        def sb(es, name, shape, dt=F32):
            return es.enter_context(nc.sbuf_tensor(name, list(shape), dt))

        ps_all = top.enter_context(nc.psum_tensor("ps_all", [P, 4096], F32))
        psb = [ps_all[:, i * 512:(i + 1) * 512] for i in range(8)]
        bank_rr = [0]

        def nbank(lo=0, hi=8):
            b = lo + (bank_rr[0] % (hi - lo))
            bank_rr[0] += 1
            return b

        ident = sb(top, "ident", [P, P])
        ones = sb(top, "ones", [P, P])
        epsc = sb(top, "epsc", [P, 1])
        S.op("dve", lambda: nc.vector.memset(ones[:], 1.0), writes=["ones"])
        S.op("dve", lambda: nc.vector.memset(epsc[:], EPS), writes=["epsc"])
        S.op("pool", lambda: nc.gpsimd.affine_select(
            out=ident[:], in_=ones[:], pattern=[[1, P]], compare_op=ALU.is_equal,
            fill=0.0, base=0, channel_multiplier=-1), reads=["ones"], writes=["ident"])

        def tap(nm, src_ap, key):
            if nm in tapd:
                S.dma("sp", tapd[nm], src_ap, reads=[key], sem="tap")

        tri = sb(top, "tri", [P, P]); lem = sb(top, "lem", [P, P]); cmf = sb(top, "cmf", [P, P])
        cmb = sb(top, "cmb", [P, P], BF16); zer = sb(top, "zer", [P, 512], BF16)
        trib = sb(top, "trib", [P, P], BF16); lemb = sb(top, "lemb", [P, P], BF16)
        gq = sb(top, "gq", [P, 1]); gk = sb(top, "gk", [P, 1])
        g1s = sb(top, "g1s", [16, P])
        g1T = sb(top, "g1T", [P, 16])
        g2s = sb(top, "g2s", [16, P])
        g2T = sb(top, "g2T", [P, 16])
        es_mix = ExitStack()
        hT = sb(es_mix, "hT", [P, 16, T], BF16)
        s5T = sb(es_mix, "s5T", [P, 8, T], BF16)
        S.dma("sp", g1s[:], attn_norm_g, writes=["g1s"], sem="m1")
        b = nbank()
        S.op("pe", lambda: nc.tensor.transpose(out=psb[b][:, 0:16], in_=g1s[:], identity=ident[0:16, 0:16]),
             reads=["g1s", "ident"], writes=["ps%d" % b])
        S.op("dve", lambda: nc.vector.tensor_copy(out=g1T[:], in_=psb[b][:, 0:16]),
             reads=["ps%d" % b], writes=["g1T"])

        def rmsnorm_to_T(es, src_rows, gT, dstT, dst_key, ncols_off=0, ntiles=16, pfx="n1"):
            xt = [sb(es, pfx + "_xt%d" % i, [P, D]) for i in range(2)]
            junk = sb(es, pfx + "_junk", [P, D], BF16)
            ss = sb(es, pfx + "_ss", [P, ntiles])
            rs = sb(es, pfx + "_rs", [P, ntiles])
            for tt in range(ntiles):
                sl = tt % 2
                xk = pfx + "xt%d" % sl
                S.dma("sp", xt[sl][:], src_rows(tt), writes=[xk], sem=pfx + "x%d" % sl)
                S.op("act", lambda: nc.scalar.activation(out=junk[:], in_=xt[sl][:], func=AF.Square,
                                                         accum_out=ss[:, tt:tt + 1]),
                     reads=[xk], writes=[pfx + "junk", (pfx + "ss", tt)])
                S.op("act", lambda: nc.scalar.activation(out=rs[:, tt:tt + 1], in_=ss[:, tt:tt + 1], func=AF.Sqrt,
                                                         bias=epsc[:, 0:1], scale=1.0 / D),
                     reads=[(pfx + "ss", tt), "epsc"], writes=[(pfx + "rs", tt)])
                S.op("dve", lambda: nc.vector.reciprocal(out=rs[:, tt:tt + 1], in_=rs[:, tt:tt + 1]),
                     reads=[(pfx + "rs", tt)], writes=[(pfx + "rs", tt)])
                S.op("dve", lambda: nc.vector.tensor_scalar(out=xt[sl][:], in0=xt[sl][:], scalar1=rs[:, tt:tt + 1],
                                                            scalar2=None, op0=ALU.mult),
                     reads=[xk, (pfx + "rs", tt)], writes=[xk])
                for cb in range(4):
                    bk = nbank()
                    for j in range(4):
                        c = cb * 4 + j
                        S.op("pe", lambda: nc.tensor.transpose(out=psb[bk][:, j * P:(j + 1) * P],
                                                               in_=xt[sl][:, c * P:(c + 1) * P], identity=ident[:]),
                             reads=[xk, "ident"], writes=["ps%d" % bk], inc=(j == 3))
                    for j in range(4):
                        c = cb * 4 + j
                        dst = dstT[:, c, ncols_off + tt * P: ncols_off + (tt + 1) * P]
                        e = S.alt()
                        if e == "dve":
                            S.op("dve", lambda: nc.vector.tensor_scalar(out=dst, in0=psb[bk][:, j * P:(j + 1) * P],
                                                                        scalar1=gT[:, c:c + 1], scalar2=None,
                                                                        op0=ALU.mult),
                                 reads=["ps%d" % bk], writes=[(dst_key, tt)])
                        else:
                            S.op("act", lambda: nc.scalar.activation(out=dst, in_=psb[bk][:, j * P:(j + 1) * P],
                                                                     func=AF.Copy, scale=gT[:, c:c + 1]),
                                 reads=["ps%d" % bk], writes=[(dst_key, tt)])

        with ExitStack() as es:
            rmsnorm_to_T(es, lambda tt: x[tt * P:(tt + 1) * P, :], g1T, hT, "hT")
            S.barrier()
        gate(101)
        if "hT" in tapd:
            with ExitStack() as es:
                tmp = sb(es, "taptmp", [P, 16, T])
                S.op("dve", lambda: nc.vector.tensor_copy(out=tmp[:], in_=hT[:]), writes=["taptmp"])
                S.dma("sp", tapd["hT"].rearrange("p (c t) -> p c t", c=16), tmp[:], reads=["taptmp"], sem="tap")
                S.barrier()


        def kn(ap):
            return ap.name

        def V_tt(out, a, b, op, rk=None, wk=None, e="dve"):
            en = nc.vector if e == "dve" else nc.gpsimd
            return S.op(e, lambda: en.tensor_tensor(out=out, in0=a, in1=b, op=op),
                        reads=rk if rk is not None else [kn(a), kn(b)],
                        writes=wk if wk is not None else [kn(out)])

        def V_ts(out, a, s1, op0, s2=None, op1=None, rk=None, wk=None):
            kw = {}
            if op1 is not None:
                kw["op1"] = op1
            r = rk if rk is not None else [kn(a)] + [kn(s) for s in (s1, s2) if hasattr(s, "name")]
            return S.op("dve", lambda: nc.vector.tensor_scalar(out=out, in0=a, scalar1=s1, scalar2=s2, op0=op0, **kw),
                        reads=r, writes=wk if wk is not None else [kn(out)])

        def V_stt(out, a, s, b, op0, op1, rk=None, wk=None):
            r = rk if rk is not None else [kn(a), kn(b)] + ([kn(s)] if hasattr(s, "name") else [])
            return S.op("dve", lambda: nc.vector.scalar_tensor_tensor(out=out, in0=a, scalar=s, in1=b, op0=op0, op1=op1),
                        reads=r, writes=wk if wk is not None else [kn(out)])

        def V_cp(out, a, rk=None, wk=None, e="dve"):
            if e == "act":
                return S.op("act", lambda: nc.scalar.copy(out=out, in_=a),
                            reads=rk if rk is not None else [kn(a)], writes=wk if wk is not None else [kn(out)])
            en = nc.vector if e == "dve" else nc.gpsimd
            return S.op(e, lambda: en.tensor_copy(out=out, in_=a),
                        reads=rk if rk is not None else [kn(a)], writes=wk if wk is not None else [kn(out)])

        def A_act(out, a, func, scale=1.0, bias=None, rk=None, wk=None, accum_out=None):
            kw = {}
            if bias is not None:
                kw["bias"] = bias
            if accum_out is not None:
                kw["accum_out"] = accum_out
            r = rk if rk is not None else [kn(a)] + [kn(s) for s in (scale, bias) if hasattr(s, "name")]
            return S.op("act", lambda: nc.scalar.activation(out=out, in_=a, func=func, scale=scale, **kw),
                        reads=r, writes=wk if wk is not None else [kn(out)])

        def PE_T(out, in_, n, rk, wk, inc=True):
            return S.op("pe", lambda: nc.tensor.transpose(out=out, in_=in_, identity=ident[0:n, 0:n]),
                        reads=rk, writes=wk, inc=inc)

        def PE_mm(out, lhsT, rhs, start, stop, rk, wk, inc=True, tp=None, sg=False):
            kw = {}
            if sg:
                kw["skip_group_check"] = True
            if tp is not None:
                kw["tile_position"] = tp
            return S.op("pe", lambda: nc.tensor.matmul(out, lhsT=lhsT, rhs=rhs, start=start, stop=stop, **kw),
                        reads=rk, writes=wk, inc=inc)

        def pk(b):
            return "ps%d" % b

        def tap_bf(nm, src, key_list):
            if nm in tapd:
                S.dma("pool", tapd[nm], src, reads=key_list, sem="tap")

        es_ssm = ExitStack()
        APr = sb(es_ssm, "APr", [P, 9, 32]); APi = sb(es_ssm, "APi", [P, 9, 32])
        AKr = sb(es_ssm, "AKr", [P, 8, 32]); AKi = sb(es_ssm, "AKi", [P, 8, 32]); AKn = sb(es_ssm, "AKn", [P, 8, 32])
        BBr = sb(es_ssm, "BBr", [P, 16, 32]); BBi = sb(es_ssm, "BBi", [P, 16, 32])
        CRt = sb(es_ssm, "CRt", [P, 16, 32]); CIt = sb(es_ssm, "CIt", [P, 16, 32])
        Dcol = sb(es_ssm, "Dcol", [P, 8])
        with ExitStack() as es:
            st_lr = sb(es, "st_lr", [32, P]); st_li = sb(es, "st_li", [32, P])
            st_dt = sb(es, "st_dt", [32, 2]); st_dtb = sb(es, "st_dtb", [32, P])
            st_b1 = sb(es, "st_b", [32, 2048]); st_c1 = sb(es, "st_c", [32, 2048])
            st_b21 = sb(es, "st_b2", [32, 16, P]); st_c21 = sb(es, "st_c2", [32, 16, P])
            st_b = [st_b1, st_b1]; st_c = [st_c1, st_c1]; st_b2 = [st_b21, st_b21]; st_c2 = [st_c21, st_c21]
            st_d = sb(es, "st_d", [8, P])
            LLD = sb(es, "LLD", [P, 96])
            BRt = sb(es, "BRt", [P, 16, 32]); BIt = sb(es, "BIt", [P, 16, 32])
            wk_ = [sb(es, "pw%d" % i, [P, 32]) for i in range(12)]
            S.dma("sp", st_lr[:], lambda_re, writes=["st_lr"], sem="m2")
            S.dma("sp", st_li[:], lambda_im, writes=["st_li"], sem="m3")
            S.dma("sp", st_dt[:], log_dt, writes=["st_dt"], sem="m4")
            S.dma("sp", st_d[:], ssm_d, writes=["st_d"], sem="m5")
            for g2 in range(2):
                V_ts(st_dtb[:, g2 * 64:(g2 + 1) * 64], ones[0:32, 0:64], st_dt[:, g2:g2 + 1], ALU.mult,
                     rk=["ones", "st_dt"], wk=["st_dtb"])
            bk = nbank()
            PE_T(psb[bk][:, 0:32], st_lr[:], 32, ["st_lr", "ident"], [pk(bk)], inc=False)
            PE_T(psb[bk][:, 32:64], st_li[:], 32, ["st_li", "ident"], [pk(bk)], inc=False)
            PE_T(psb[bk][:, 64:96], st_dtb[:], 32, ["st_dtb", "ident"], [pk(bk)])
            V_cp(LLD[:], psb[bk][:, 0:96], rk=[pk(bk)], wk=["LLD"])
            bk = nbank()
            PE_T(psb[bk][:, 0:8], st_d[:], 8, ["st_d", "ident"], [pk(bk)])
            V_cp(Dcol[:], psb[bk][:, 0:8], rk=[pk(bk)], wk=["Dcol"])
            for ri in range(2):
                S.dma("sp", st_b[ri][:], (ssm_b_re, ssm_b_im)[ri], writes=["st_b"], sem="stb")
                S.dma("sp", st_c[ri][:], (ssm_c_re, ssm_c_im)[ri], writes=["st_c"], sem="stc")
                V_cp(st_b2[ri][:], st_b[ri][:].rearrange("q (gp h) -> q h gp", h=16), rk=["st_b"], wk=["st_b2"])
                V_cp(st_c2[ri][:].rearrange("q h (g2 p) -> q g2 h p", g2=2),
                     st_c[ri][:].rearrange("q (g2 h p) -> q g2 h p", g2=2, h=16), rk=["st_c"], wk=["st_c2"])
                for (srcs, dst) in ((st_b2[ri], (BRt, BIt)[ri]), (st_c2[ri], (CRt, CIt)[ri])):
                    bk = nbank()
                    for h in range(16):
                        PE_T(psb[bk][:, h * 32:(h + 1) * 32], srcs[:, h, :], 32, [kn(srcs[:]), "ident"], [pk(bk)], inc=(h == 15))
                    V_cp(dst[:].rearrange("p h q -> p (h q)"), psb[bk][:, :], rk=[pk(bk)], wk=[kn(dst[:])])
            LR = LLD[:, 0:32]; LI = LLD[:, 32:64]; LDT = LLD[:, 64:96]
            dtv, lrdt, lidt, mag, cc, sn, t1, t2, t3, cre, cim, den = [w_[:] for w_ in wk_]
            A_act(dtv, LDT, AF.Exp)
            V_tt(lrdt, LR, dtv, ALU.mult)
            V_tt(lidt, LI, dtv, ALU.mult)
            A_act(mag, lrdt, AF.Exp)
            halfpi = sb(es, "halfpi", [P, 1])
            S.op("dve", lambda: nc.vector.memset(halfpi[:], math.pi / 2), writes=["halfpi"])
            A_act(sn, lidt, AF.Sin, scale=1.0 / 32)
            A_act(cc, lidt, AF.Sin, scale=1.0 / 32, bias=halfpi[:, 0:1])
            for _ in range(5):
                V_tt(t1, cc, cc, ALU.mult)
                V_tt(t2, sn, sn, ALU.mult)
                V_tt(t3, cc, sn, ALU.mult)
                V_tt(cc, t1, t2, ALU.subtract)
                V_ts(sn, t3, 2.0, ALU.mult)
            S.op("dve", lambda: nc.vector.memset(APr[:, 0, :], 1.0), writes=["APr"])
            S.op("dve", lambda: nc.vector.memset(APi[:, 0, :], 0.0), writes=["APi"])
            V_tt(APr[:, 1, :], mag, cc, ALU.mult)
            V_tt(APi[:, 1, :], mag, sn, ALU.mult)

            def cmul(o_r, o_i, a_r, a_i, b_r, b_i, tA, tB):
                V_tt(tA, a_r, b_r, ALU.mult)
                V_tt(tB, a_i, b_i, ALU.mult)
                V_tt(o_r, tA, tB, ALU.subtract)
                V_tt(tA, a_r, b_i, ALU.mult)
                V_tt(tB, a_i, b_r, ALU.mult)
                V_tt(o_i, tA, tB, ALU.add)

            for e_ in range(1, 8):
                cmul(APr[:, e_ + 1, :], APi[:, e_ + 1, :], APr[:, e_, :], APi[:, e_, :], APr[:, 1, :], APi[:, 1, :], t1, t2)
            V_cp(AKr[:, 0, :], APr[:, 8, :]); V_cp(AKi[:, 0, :], APi[:, 8, :])
            for k in range(7):
                V_tt(t1, AKr[:, k, :], AKr[:, k, :], ALU.mult)
                V_tt(t2, AKi[:, k, :], AKi[:, k, :], ALU.mult)
                V_tt(t3, AKr[:, k, :], AKi[:, k, :], ALU.mult)
                V_tt(AKr[:, k + 1, :], t1, t2, ALU.subtract)
                V_ts(AKi[:, k + 1, :], t3, 2.0, ALU.mult)
            V_ts(AKn[:], AKi[:], -1.0, ALU.mult)
            V_ts(t1, APr[:, 1, :], -1.0, ALU.add, rk=["APr"])
            V_tt(t2, LR, LR, ALU.mult)
            V_tt(t3, LI, LI, ALU.mult)
            V_tt(den, t2, t3, ALU.add)
            S.op("dve", lambda: nc.vector.reciprocal(out=den, in_=den), reads=[kn(den)], writes=[kn(den)])
            V_tt(t2, t1, LR, ALU.mult)
            V_tt(t3, APi[:, 1, :], LI, ALU.mult)
            V_tt(cre, t2, t3, ALU.add)
            V_tt(cre, cre, den, ALU.mult)
            V_tt(t2, APi[:, 1, :], LR, ALU.mult)
            V_tt(t3, t1, LI, ALU.mult)
            V_tt(cim, t2, t3, ALU.subtract)
            V_tt(cim, cim, den, ALU.mult)
            tb1 = sb(es, "tb1", [P, 16, 32]); tb2 = sb(es, "tb2", [P, 16, 32])
            creb = cre.unsqueeze(1).broadcast_to([P, 16, 32]); cimb = cim.unsqueeze(1).broadcast_to([P, 16, 32])
            cmul(BBr[:], BBi[:], creb, cimb, BRt[:], BIt[:], tb1[:], tb2[:])
            S.barrier()


        uT = sb(es_ssm, "uT", [P, 8, T], BF16)

        def load_w(wbuf, key, srcap, kch, ncols, sem):
            S.dma("pool", wbuf[:, 0:kch, 0:ncols], srcap.rearrange("(c p) f -> p c f", p=P), writes=[key], sem=sem)

        def evac_copy(dst, src_ps, bk, wkeys, e=None):
            e = e or S.alt()
            V_cp(dst, src_ps, rk=[pk(bk)], wk=wkeys, e=e)

        with ExitStack() as es:
            wst = [sb(es, "wst%d" % i, [P, 16, 512], BF16) for i in range(2)]
            for blk in range(2):
                sl = blk % 2
                load_w(wst[sl], "wst%d" % sl, w_in[:, blk * 512:(blk + 1) * 512], 16, 512, "wst%d" % sl)
                for m in range(4):
                    for n in range(4):
                        bk = nbank()
                        for k in range(16):
                            PE_mm(psb[bk][:, :], wst[sl][:, k, m * P:(m + 1) * P], hT[:, k, n * 512:(n + 1) * 512],
                                  k == 0, k == 15, ["wst%d" % sl], [pk(bk)], inc=(k == 15))
                        evac_copy(uT[:, blk * 4 + m, n * 512:(n + 1) * 512], psb[bk][:, :], bk, [("uT", blk * 4 + m, n)])
            S.barrier()
        gate(102)
        tap_bf("uT", uT[:].rearrange("p a t -> p (a t)"), [])

        with ExitStack() as es:
            Xu = sb(es, "Xu", [P, 8, 2, 4, 16])
            XP = sb(es, "XP", [P, 8, 2, 4, 32])
            CAu = sb(es, "CAu", [P, 4, 9, 2, 16])
            tq1 = sb(es, "tq1", [P, 4, 16]); tq2 = sb(es, "tq2", [P, 4, 16])
            WS = [sb(es, "WS%d" % i, [P, 8, 2, P], BF16) for i in range(2)]
            WCp = [sb(es, "WCp%d" % i, [P, 4, 9, 2, 32], BF16) for i in range(2)]
            BPb = [sb(es, "BPb%d" % i, [P, 2, 4, 32], BF16) for i in range(2)]
            BD = [sb(es, "BD%d" % i, [P, 8, P], BF16) for i in range(2)]
            Hb = [[sb(es, "Hb%d%d" % (s_, i), [P, 2, NCK]) for i in range(2)] for s_ in range(2)]
            Hbf = [sb(es, "Hbf%d" % i, [P, 2, NCK], BF16) for i in range(4)]
            y32 = sb(es, "y32", [P, 1024])
            S.op("dve", lambda: nc.vector.memset(XP[:], 0.0), writes=["XP"])
            for i in range(2):
                S.op("pool", lambda: nc.gpsimd.memset(WCp[i][:], 0.0), writes=["WCp%d" % i])
                S.op("pool", lambda: nc.gpsimd.memset(BD[i][:], 0.0), writes=["BD%d" % i])
            XPv = XP[:].rearrange("p i r q (g h) -> p (i r q) g h", g=2)
            for a in range(8):
                par = a % 2
                qs = slice(4 * a, 4 * a + 4)
                for ip in range(8):
                    e_ = 7 - ip
                    arb = APr[:, e_, qs].unsqueeze(2).broadcast_to([P, 4, 16])
                    aib = APi[:, e_, qs].unsqueeze(2).broadcast_to([P, 4, 16])
                    bbr = BBr[:, :, qs].rearrange("p h q -> p q h")
                    bbi = BBi[:, :, qs].rearrange("p h q -> p q h")
                    V_tt(tq1[:], arb, bbr, ALU.mult, rk=[], wk=["tq1"])
                    V_tt(tq2[:], aib, bbi, ALU.mult, rk=[], wk=["tq2"])
                    V_tt(Xu[:, ip, 0, :, :], tq1[:], tq2[:], ALU.subtract, rk=["tq1", "tq2"], wk=["Xu"])
                    V_tt(tq1[:], arb, bbi, ALU.mult, rk=[], wk=["tq1"])
                    V_tt(tq2[:], aib, bbr, ALU.mult, rk=[], wk=["tq2"])
                    V_tt(Xu[:, ip, 1, :, :], tq1[:], tq2[:], ALU.add, rk=["tq1", "tq2"], wk=["Xu"])
                Xuv = Xu[:].rearrange("p i r q h -> p (i r q) h")
                for g2 in range(2):
                    V_cp(XPv[g2 * 64:(g2 + 1) * 64, :, g2, :], Xuv[g2 * 64:(g2 + 1) * 64, :, :], rk=["Xu"], wk=["XP"])
                for e_ in range(9):
                    arb = APr[:, e_, qs].unsqueeze(2).broadcast_to([P, 4, 16])
                    aib = APi[:, e_, qs].unsqueeze(2).broadcast_to([P, 4, 16])
                    crr = CRt[:, :, qs].rearrange("p h q -> p q h")
                    cii = CIt[:, :, qs].rearrange("p h q -> p q h")
                    V_tt(tq1[:], crr, arb, ALU.mult, rk=[], wk=["tq1"])
                    V_tt(tq2[:], cii, aib, ALU.mult, rk=[], wk=["tq2"])
                    V_tt(CAu[:, :, e_, 0, :], tq1[:], tq2[:], ALU.subtract, rk=["tq1", "tq2"], wk=["CAu"])
                    V_tt(tq1[:], cii, arb, ALU.mult, rk=[], wk=["tq1"])
                    V_tt(tq2[:], crr, aib, ALU.mult, rk=[], wk=["tq2"])
                    V_stt(CAu[:, :, e_, 1, :], tq1[:], -1.0, tq2[:], ALU.mult, ALU.subtract, rk=["tq1", "tq2"], wk=["CAu"])
                CAuv = CAu[:].rearrange("p q e r h -> p (q e r) h")
                WCv = WCp[par][:].rearrange("p q e r (g h) -> p (q e r) g h", g=2)
                for g2 in range(2):
                    V_cp(WCv[g2 * 64:(g2 + 1) * 64, :, g2, :], CAuv[g2 * 64:(g2 + 1) * 64, :, :], rk=["CAu"], wk=["WCp%d" % par])
                V_cp(BPb[par][:], XP[:, 7, :, :, :], rk=["XP"], wk=["BPb%d" % par])
                for cb in range(4):
                    bk = nbank(6, 8)
                    for j in range(4):
                        ip, ri = divmod(cb * 4 + j, 2)
                        PE_T(psb[bk][:, j * P:(j + 1) * P], XP[:, ip, ri, :, :].rearrange("p q f -> p (q f)"), P,
                             ["XP", "ident"], [pk(bk)], inc=(j == 3))
                    evac_copy(WS[par][:].rearrange("p i r f -> p (i r f)")[:, cb * 512:(cb + 1) * 512], psb[bk][:, :], bk,
                              ["WS%d" % par])
                bk = nbank(6, 8)
                for qq in range(4):
                    for j in range(8):
                        for ri in range(2):
                            PE_mm(psb[bk][32 * qq:32 * qq + 32, j * 32:(j + 1) * 32], BPb[par][:, ri, qq, :],
                                  WCp[par][:, qq, j, ri, :], ri == 0, ri == 1,
                                  ["BPb%d" % par, "WCp%d" % par], [pk(bk)], inc=(qq == 3 and j == 7 and ri == 1),
                                  tp=(0, 32 * qq), sg=True)
                for qq in range(4):
                    V_cp(BD[par][32 * qq:32 * qq + 32, :, 32 * qq:32 * qq + 32],
                         psb[bk][32 * qq:32 * qq + 32, 0:256].rearrange("p (j f) -> p j f", j=8),
                         rk=[pk(bk)], wk=["BD%d" % par])
                for qq in range(4):
                    q = 4 * a + qq
                    hs = q % 2
                    pb = 32 * qq
                    bk = nbank(4, 6)
                    for ri in range(2):
                        for ip in range(8):
                            PE_mm(psb[bk][:, ri * NCK:(ri + 1) * NCK], WS[par][pb:pb + 32, ip, ri, :],
                                  uT[pb:pb + 32, a, ip::8], ip == 0, ip == 7,
                                  ["WS%d" % par] + [("uT", a, n) for n in range(4)], [pk(bk)],
                                  inc=(ri == 1 and ip == 7), tp=(pb, 0))
                    V_cp(Hb[hs][0][:].rearrange("p r c -> p (r c)"), psb[bk][:, :], rk=[pk(bk)],
                         wk=[("Hb", hs, 0, 0), ("Hb", hs, 0, 1)], e="act")
                    for k in range(8):
                        s_ = 1 << k
                        src_ = Hb[hs][k % 2]; dst_ = Hb[hs][(k + 1) % 2]
                        sp_, dp_ = k % 2, (k + 1) % 2
                        n_ = NCK - s_
                        V_cp(dst_[:, :, 0:s_], src_[:, :, 0:s_], rk=[("Hb", hs, sp_, 0), ("Hb", hs, sp_, 1)],
                             wk=[("Hb", hs, dp_, 0), ("Hb", hs, dp_, 1)], e="pool")
                        akr = AKr[:, k, q:q + 1]; aki = AKi[:, k, q:q + 1]; akn = AKn[:, k, q:q + 1]
                        V_stt(dst_[:, 0, s_:], src_[:, 0, 0:n_], akr, src_[:, 0, s_:], ALU.mult, ALU.add,
                              rk=[("Hb", hs, sp_, 0)], wk=[("Hb", hs, dp_, 0)])
                        V_stt(dst_[:, 1, s_:], src_[:, 1, 0:n_], akr, src_[:, 1, s_:], ALU.mult, ALU.add,
                              rk=[("Hb", hs, sp_, 1)], wk=[("Hb", hs, dp_, 1)])
                        V_stt(dst_[:, 0, s_:], src_[:, 1, 0:n_], akn, dst_[:, 0, s_:], ALU.mult, ALU.add,
                              rk=[("Hb", hs, sp_, 1), ("Hb", hs, dp_, 0)], wk=[("Hb", hs, dp_, 0)])
                        V_stt(dst_[:, 1, s_:], src_[:, 0, 0:n_], aki, dst_[:, 1, s_:], ALU.mult, ALU.add,
                              rk=[("Hb", hs, sp_, 0), ("Hb", hs, dp_, 1)], wk=[("Hb", hs, dp_, 1)])
                    V_cp(Hbf[qq][:], Hb[hs][0][:], rk=[("Hb", hs, 0, 0), ("Hb", hs, 0, 1)], wk=[("Hbf", qq)], e="act")
                for i in range(8):
                    for j in range(i + 1):
                        PE_mm(ps_all[:, i * NCK:(i + 1) * NCK], BD[par][:, j, :], uT[:, a, (i - j)::8],
                              (j == 0 and i % 2 == 0), False,
                              ["BD%d" % par] + [("uT", a, n) for n in range(4)], [pk(i // 2)],
                              inc=(j == i), sg=True)
                for qq in range(4):
                    pb = 32 * qq
                    for i in range(8):
                        for ri in range(2):
                            PE_mm(ps_all[pb:pb + 32, i * NCK + 1:(i + 1) * NCK], WCp[par][:, qq, i + 1, ri, :],
                                  Hbf[qq][:, ri, 0:NCK - 1], False, ri == 1,
                                  ["WCp%d" % par, ("Hbf", qq)], [pk(i // 2)], inc=(ri == 1), tp=(0, pb), sg=True)
                for hf in range(2):
                    Yv = ps_all[:, 0:2048].rearrange("p (i c) -> p c i", i=8)[:, hf * 128:(hf + 1) * 128, :]
                    uv = uT[:, a, hf * 1024:(hf + 1) * 1024].rearrange("p (c i) -> p c i", i=8)
                    V_stt(y32[:].rearrange("p (c i) -> p c i", i=8), uv, Dcol[:, a:a + 1], Yv, ALU.mult, ALU.add,
                          rk=[pk(0), pk(1), pk(2), pk(3)] + [("uT", a, n) for n in range(4)], wk=["y32"])
                    A_act(uT[:, a, hf * 1024:(hf + 1) * 1024], y32[:], AF.Gelu_apprx_tanh, rk=["y32"],
                          wk=[("uT", a, 2 * hf), ("uT", a, 2 * hf + 1)])
            tap_bf("zT", uT[:].rearrange("p a t -> p (a t)"), [("uT", a_, n_) for a_ in range(8) for n_ in range(4)])
            S.barrier()
        with ExitStack() as es:
            wglu = sb(es, "wglu", [P, 8, 1024], BF16)
            load_w(wglu, "wglu", w_glu, 8, 1024, "wglu")
            sg = [sb(es, "sg%d" % i, [P, 512], BF16) for i in range(2)]
            for m in range(8):
                for n in range(4):
                    bk = nbank(4, 8)
                    for k in range(8):
                        PE_mm(psb[bk][:, :], wglu[:, k, m * P:(m + 1) * P], uT[:, k, n * 512:(n + 1) * 512],
                              k == 0, k == 7, ["wglu"] + [("uT", k, n)], [pk(bk)], inc=(k == 7))
                    sl = (m * 4 + n) % 2
                    A_act(sg[sl][:], psb[bk][:, :], AF.Sigmoid, rk=[pk(bk)], wk=["sg%d" % sl])
                    V_tt(s5T[:, m, n * 512:(n + 1) * 512], sg[sl][:], uT[:, m, n * 512:(n + 1) * 512], ALU.mult,
                         rk=["sg%d" % sl, ("uT", m, n)], wk=[("s5T", m, n)])
            S.barrier()
        gate(103)
        tap_bf("s5T", s5T[:].rearrange("p a t -> p (a t)"), [])
        reg = {"APr": APr[:].rearrange("p e q -> p (e q)"), "APi": APi[:].rearrange("p e q -> p (e q)"),
               "AKr": AKr[:].rearrange("p e q -> p (e q)"), "BBr": BBr[:].rearrange("p h q -> p (h q)"),
               "BBi": BBi[:].rearrange("p h q -> p (h q)"), "CRt": CRt[:].rearrange("p h q -> p (h q)"),
               "Dcol": Dcol[:]}
        for nm_, ap_ in reg.items():
            if nm_ in tapd:
                S.dma("pool", tapd[nm_], ap_, sem="tap")
        S.barrier()
        es_ssm.close()


        attT = sb(es_mix, "attT", [P, 8, T], BF16)
        S.op("pool", lambda: nc.gpsimd.affine_select(out=tri[:], in_=ones[:], pattern=[[-1, P]], compare_op=ALU.is_gt,
                                                     fill=0.0, base=0, channel_multiplier=1), reads=["ones"], writes=["tri"])
        S.op("pool", lambda: nc.gpsimd.affine_select(out=lem[:], in_=ones[:], pattern=[[1, P]], compare_op=ALU.is_ge,
                                                     fill=0.0, base=0, channel_multiplier=-1), reads=["ones"], writes=["lem"])
        S.op("pool", lambda: nc.gpsimd.affine_select(out=cmf[:], in_=ones[:], pattern=[[1, P]], compare_op=ALU.is_gt,
                                                     fill=0.0, base=0, channel_multiplier=-1), reads=["ones"], writes=["cmf"])
        V_cp(cmb[:], cmf[:])
        V_cp(trib[:], tri[:])
        V_cp(lemb[:], lem[:])
        S.op("dve", lambda: nc.vector.memset(zer[:], 0.0), writes=["zer"])
        S.dma("sp", gq[:], q_norm_g.rearrange("o d -> d o"), writes=["gq"], sem="m6")
        S.dma("sp", gk[:], k_norm_g.rearrange("o d -> d o"), writes=["gk"], sem="m7")
        V_ts(gq[:], gq[:], 1.0 / math.sqrt(128.0), ALU.mult)
        S.barrier()

        for hg in range(2):
            with ExitStack() as es:
                qT = sb(es, "qT%d" % hg, [P, 4, T], BF16); kT = sb(es, "kT%d" % hg, [P, 4, T], BF16)
                vv = sb(es, "vv%d" % hg, [P, 16, 512], BF16)
                with ExitStack() as es2:
                    wst0 = sb(es2, "wstq0_%d" % hg, [P, 16, 512], BF16)
                    wst = [wst0, wst0]
                    sqf = [sb(es2, "sqf%d_%d" % (i, hg), [P, 512]) for i in range(2)]
                    rsq = [sb(es2, "rsq%d_%d" % (i, hg), [P, 512]) for i in range(2)]
                    cnt_ = 0
                    for which, col0 in (("q", 1024 + 512 * hg), ("k", 2048 + 512 * hg), ("v", 3072 + 512 * hg)):
                        sl = 0
                        load_w(wst[sl], "wstq%d" % sl, w_in[:, col0:col0 + 512], 16, 512, "wstq%d" % sl)
                        if which == "v":
                            for tt in range(16):
                                bk = nbank(0, 4)
                                for k in range(16):
                                    PE_mm(psb[bk][:, :], hT[:, k, tt * P:(tt + 1) * P], wst[sl][:, k, :], k == 0, k == 15,
                                          ["wstq%d" % sl], [pk(bk)], inc=(k == 15))
                                evac_copy(vv[:, tt, :], psb[bk][:, :], bk, [("vv", tt)])
                            continue
                        dstT = qT if which == "q" else kT
                        gcol = gq if which == "q" else gk
                        for m in range(4):
                            for n in range(4):
                                bk = nbank(0, 4)
                                for k in range(16):
                                    PE_mm(psb[bk][:, :], wst[sl][:, k, m * P:(m + 1) * P], hT[:, k, n * 512:(n + 1) * 512],
                                          k == 0, k == 15, ["wstq%d" % sl], [pk(bk)], inc=(k == 15))
                                s2 = (m * 4 + n) % 2
                                A_act(sqf[s2][:], psb[bk][:, :], AF.Square, rk=[pk(bk)], wk=["sqf%d" % s2])
                                b2 = nbank(4, 8)
                                PE_mm(psb[b2][:, :], ones[:], sqf[s2][:], True, True, ["ones", "sqf%d" % s2], [pk(b2)])
                                A_act(rsq[s2][:], psb[b2][:, :], AF.Sqrt, scale=1.0 / 128, bias=epsc[:, 0:1],
                                      rk=[pk(b2)], wk=["rsq%d" % s2])
                                S.op("dve", lambda: nc.vector.reciprocal(out=rsq[s2][:], in_=rsq[s2][:]),
                                     reads=["rsq%d" % s2], writes=["rsq%d" % s2])
                                V_stt(dstT[:, m, n * 512:(n + 1) * 512], psb[bk][:, :], gcol[:, 0:1], rsq[s2][:],
                                      ALU.mult, ALU.mult, rk=[pk(bk), "rsq%d" % s2], wk=[(which, m, n)])
                    S.barrier()
                if hg == 0:
                    tap_bf("qT", qT[:].rearrange("p a t -> p (a t)"), [])
                    tap_bf("kT", kT[:].rearrange("p a t -> p (a t)"), [])
                    tap_bf("vv", vv[:].rearrange("p a t -> p (a t)"), [])
                with ExitStack() as es2:
                    SPb = [sb(es2, "SPb%d_%d" % (i, hg), [P, 1024]) for i in range(1)]
                    SPh = [sb(es2, "SPh%d_%d" % (i, hg), [P, 1024], BF16) for i in range(2)]
                    Ab = sb(es2, "Ab%d" % hg, [P, 1024])
                    Wb_ = [sb(es2, "Wb%d_%d" % (i, hg), [P, 1024], BF16) for i in range(2)]
                    ZB = [ps_all[:, 0:1024], ps_all[:, 1024:2048]]
                    TB = ps_all[:, 2048:3072]
                    OB = ps_all[:, 3072:4096]

                    def bank_ranges(lo):
                        rs_ = []
                        for bh in range(2):
                            a_ = max(lo, 512 * bh); b_ = 512 * (bh + 1)
                            if a_ < b_:
                                rs_.append((bh, a_, b_))
                        return rs_

                    for hl in range(4):
                        h = 4 * hg + hl
                        for qh in range(2):
                            kbs = list(range(8 * qh + 7, -1, -1))
                            N_ = len(kbs)
                            for bh in range(2):
                                PE_mm(TB[:, bh * 512:(bh + 1) * 512], zer[:, 0:P], zer[:, :], True, True, ["zer"], [pk(4 + bh)], inc=False, sg=True)
                                PE_mm(OB[:, bh * 512:(bh + 1) * 512], zer[:, 0:P], zer[:, :], True, True, ["zer"], [pk(6 + bh)], inc=(bh == 1), sg=True)

                            def lo_of(n):
                                return max(0, kbs[n] * P - qh * 1024)

                            def diag(n):
                                return kbs[n] * P >= qh * 1024

                            def S1(n):
                                kb = kbs[n]; lo = lo_of(n); zb = n % 2
                                for (bh, a_, b_) in bank_ranges(lo):
                                    PE_mm(ZB[zb][:, a_:b_], kT[:, hl, kb * P:(kb + 1) * P], qT[:, hl, qh * 1024 + a_: qh * 1024 + b_],
                                          True, True, [], [pk(2 * zb + bh)])
                                zk = [pk(2 * zb), pk(2 * zb + 1)]
                                A_act(SPb[0][:, lo:], ZB[zb][:, lo:], AF.Exp, rk=zk, wk=["SPb0"])
                                A_act(SPh[zb][:, lo:], SPb[0][:, lo:], AF.Ln, bias=ones[:, 0:1], rk=["SPb0"], wk=["SPh%d" % zb])
                                if diag(n):
                                    V_tt(SPh[zb][:, lo:lo + P], SPh[zb][:, lo:lo + P], cmb[:], ALU.mult,
                                         rk=["SPh%d" % zb], wk=["SPh%d" % zb])

                            def S2(n):
                                lo = lo_of(n); zb = n % 2
                                for (bh, a_, b_) in bank_ranges(lo):
                                    PE_mm(TB[:, a_:b_], trib[:], SPh[zb][:, a_:b_], False, False, ["SPh%d" % zb], [pk(4 + bh)], sg=True)

                            def S3a(n):
                                lo = lo_of(n); zb = n % 2
                                zk = [pk(2 * zb), pk(2 * zb + 1)]
                                V_tt(Ab[:, lo:], ZB[zb][:, lo:], SPh[zb][:, lo:], ALU.subtract, rk=zk + ["SPh%d" % zb], wk=["Ab"])
                                V_tt(Ab[:, lo:], Ab[:, lo:], TB[:, lo:], ALU.subtract, rk=["Ab", pk(4), pk(5)], wk=["Ab"])
                                A_act(Wb_[zb][:, lo:], Ab[:, lo:], AF.Exp, rk=["Ab"], wk=["Wb%d" % zb])

                            def S3b(n):
                                lo = lo_of(n); zb = n % 2
                                if diag(n):
                                    V_tt(Wb_[zb][:, lo:lo + P], Wb_[zb][:, lo:lo + P], cmb[:], ALU.mult,
                                         rk=["Wb%d" % zb], wk=["Wb%d" % zb])

                            def S4a(n):
                                lo = lo_of(n); zb = n % 2
                                for (bh, a_, b_) in bank_ranges(lo):
                                    PE_mm(TB[:, a_:b_], lemb[:], SPh[zb][:, a_:b_], False, False, ["SPh%d" % zb], [pk(4 + bh)], sg=True)

                            def S4b(n):
                                kb = kbs[n]; lo = lo_of(n); zb = n % 2
                                for (bh, a_, b_) in bank_ranges(lo):
                                    PE_mm(OB[:, a_:b_], vv[:, kb, hl * P:(hl + 1) * P], Wb_[zb][:, a_:b_], False, n == N_ - 1,
                                          ["Wb%d" % zb], [pk(6 + bh)], sg=True)

                            S1(0)
                            if N_ > 1:
                                S1(1)
                            S2(0)
                            for n in range(N_):
                                S3a(n)
                                S4a(n)
                                if n + 2 < N_:
                                    S1(n + 2)
                                S3b(n)
                                if n + 1 < N_:
                                    S2(n + 1)
                                S4b(n)
                            V_cp(attT[:, h, qh * 1024:(qh + 1) * 1024], OB[:, :], rk=[pk(6), pk(7)], wk=[("attT", h, qh)], e="act")
                    S.barrier()
        gate(104)
        tap_bf("attT", attT[:].rearrange("p a t -> p (a t)"), [])


        for th in range(2):
            with ExitStack() as es:
                mT = sb(es, "mT%d" % th, [P, 16, 1024], BF16)
                with ExitStack() as es2:
                    wbs = [sb(es2, "wbs%d_%d" % (i, th), [P, 8, P], BF16) for i in range(2)]
                    wba = [sb(es2, "wba%d_%d" % (i, th), [P, 8, P], BF16) for i in range(2)]
                    wgs = [sb(es2, "wgs%d_%d" % (i, th), [P, 16, P], BF16) for i in range(2)]
                    wga = [sb(es2, "wga%d_%d" % (i, th), [P, 16, P], BF16) for i in range(2)]
                    sgs = [sb(es2, "sgs%d_%d" % (i, th), [P, 512]) for i in range(2)]
                    sga = [sb(es2, "sga%d_%d" % (i, th), [P, 512]) for i in range(2)]
                    for m in range(16):
                        sl = m % 2
                        cs = slice(m * P, (m + 1) * P)
                        load_w(wbs[sl], "wbs%d" % sl, w_branch_ssm[:, cs], 8, P, "wbs%d" % sl)
                        load_w(wba[sl], "wba%d" % sl, w_branch_att[:, cs], 8, P, "wba%d" % sl)
                        load_w(wgs[sl], "wgs%d" % sl, w_in[:, 4096 + m * P:4096 + (m + 1) * P], 16, P, "wgs%d" % sl)
                        load_w(wga[sl], "wga%d" % sl, w_in[:, 6144 + m * P:6144 + (m + 1) * P], 16, P, "wga%d" % sl)
                        for n in range(2):
                            ts_ = slice(th * 1024 + n * 512, th * 1024 + (n + 1) * 512)
                            b_bs, b_gs, b_ba, b_ga = nbank(), nbank(), nbank(), nbank()
                            for k in range(8):
                                PE_mm(psb[b_bs][:, :], wbs[sl][:, k, :], s5T[:, k, ts_], k == 0, k == 7, ["wbs%d" % sl], [pk(b_bs)], inc=(k == 7))
                            for k in range(16):
                                PE_mm(psb[b_gs][:, :], wgs[sl][:, k, :], hT[:, k, ts_], k == 0, k == 15, ["wgs%d" % sl], [pk(b_gs)], inc=(k == 15))
                            for k in range(8):
                                PE_mm(psb[b_ba][:, :], wba[sl][:, k, :], attT[:, k, ts_], k == 0, k == 7, ["wba%d" % sl], [pk(b_ba)], inc=(k == 7))
                            for k in range(16):
                                PE_mm(psb[b_ga][:, :], wga[sl][:, k, :], hT[:, k, ts_], k == 0, k == 15, ["wga%d" % sl], [pk(b_ga)], inc=(k == 15))
                            s2 = n
                            A_act(sgs[s2][:], psb[b_gs][:, :], AF.Sigmoid, rk=[pk(b_gs)], wk=["sgs%d" % s2])
                            A_act(sga[s2][:], psb[b_ga][:, :], AF.Sigmoid, rk=[pk(b_ga)], wk=["sga%d" % s2])
                            V_tt(sgs[s2][:], sgs[s2][:], psb[b_bs][:, :], ALU.mult, rk=["sgs%d" % s2, pk(b_bs)], wk=["sgs%d" % s2])
                            V_tt(sga[s2][:], sga[s2][:], psb[b_ba][:, :], ALU.mult, rk=["sga%d" % s2, pk(b_ba)], wk=["sga%d" % s2])
                            V_tt(mT[:, m, n * 512:(n + 1) * 512], sgs[s2][:], sga[s2][:], ALU.add,
                                 rk=["sgs%d" % s2, "sga%d" % s2], wk=[("mT", m, n)])
                    S.barrier()
                with ExitStack() as es2:
                    wo = [sb(es2, "wo%d_%d" % (i, th), [P, 16, 512], BF16) for i in range(2)]
                    xin = [sb(es2, "xin%d_%d" % (i, th), [P, 512]) for i in range(2)]
                    xo = [sb(es2, "xo%d_%d" % (i, th), [P, 512]) for i in range(2)]
                    cnt_ = 0
                    for db in range(4):
                        sl = db % 2
                        ds_ = slice(db * 512, (db + 1) * 512)
                        load_w(wo[sl], "wo%d" % sl, w_out[:, ds_], 16, 512, "wo%d" % sl)
                        for tt in range(8):
                            r0 = th * 1024 + tt * P
                            s2 = cnt_ % 2
                            cnt_ += 1
                            S.dma("sp", xin[s2][:], x[r0:r0 + P, ds_], writes=["xin%d" % s2], sem="xin%d" % s2)
                            bk = nbank()
                            for k in range(16):
                                PE_mm(psb[bk][:, :], mT[:, k, tt * P:(tt + 1) * P], wo[sl][:, k, :], k == 0, k == 15,
                                      ["wo%d" % sl], [pk(bk)], inc=(k == 15))
                            V_tt(xo[s2][:], xin[s2][:], psb[bk][:, :], ALU.add, rk=["xin%d" % s2, pk(bk)], wk=["xo%d" % s2])
                            S.dma("sp", x2d[r0:r0 + P, ds_], xo[s2][:], reads=["xo%d" % s2], sem="xo%d" % s2)
                    S.barrier()
        gate(105)
        es_mix.close()
        if "x2" in tapd:
            S.dma("sp", tapd["x2"], x2d, sem="tap")
            S.barrier()

        if upto == "F":
            S.barrier()
            return nc
        TS = 256
        NT = 48
        NS = NT * TS
        h2d = nc.dram_tensor("h2_scratch", [T, D], F32, kind="Internal").ap()
        yd = nc.dram_tensor("y_scratch", [NS, D], F32, kind="Internal").ap()
        stok = nc.dram_tensor("slot_tok", [NS, 16], I32, kind="Internal").ap()
        S.dma("sp", g2s[:], ffn_norm_g, writes=["g2s"], sem="m_g2s")
        bk = nbank()
        PE_T(psb[bk][:, 0:16], g2s[:], 16, ["g2s", "ident"], [pk(bk)])
        V_cp(g2T[:], psb[bk][:, 0:16], rk=[pk(bk)], wk=["g2T"])
        with ExitStack() as es:
            wr = sb(es, "wr", [P, 16, 36])
            rb = sb(es, "rb", [P, 36])
            comb_g1 = sb(es, "comb_g1", [P, 16]); comb_g2 = sb(es, "comb_g2", [P, 16])
            oh1a = sb(es, "oh1a", [P, 16, 32]); oh2a = sb(es, "oh2a", [P, 16, 32])
            selb = sb(es, "selb", [P, 16, 32], BF16)
            s1i = sb(es, "s1i", [P, 16], I32); s2i = sb(es, "s2i", [P, 16], I32)
            widx = sb(es, "widx", [P, NT], I32)
            yix = sb(es, "yix", [P, NT * 2], I32)
            onesb = sb(es, "onesb", [P, P], BF16)
            V_cp(onesb[:], ones[:])
            with ExitStack() as es1:
                wlg = sb(es1, "wlg", [16, P * 4]); wle = sb(es1, "wle", [16, P * 32])
                S.dma("sp", wlg[:], router_group_w.rearrange("(c p) f -> c (p f)", p=P), writes=["wlg"], sem="m_wlg")
                S.dma("sp", wle[:], router_expert_w.rearrange("(c p) f -> c (p f)", p=P), writes=["wle"], sem="m_wle")
                wlg2 = sb(es1, "wlg2", [16, 4, P]); wle2 = sb(es1, "wle2", [16, 32, P])
                V_cp(wlg2[:], wlg[:].rearrange("c (p f) -> c f p", f=4), rk=["wlg"], wk=["wlg2"])
                V_cp(wle2[:], wle[:].rearrange("c (p f) -> c f p", f=32), rk=["wle"], wk=["wle2"])
                bk = nbank()
                for f in range(4):
                    PE_T(psb[bk][:, f * 16:(f + 1) * 16], wlg2[:, f, :], 16, ["wlg2", "ident"], [pk(bk)], inc=(f == 3))
                V_cp(wr[:, :, 0:4].rearrange("p c f -> p f c"), psb[bk][:, 0:64].rearrange("p (f c) -> p f c", c=16), rk=[pk(bk)], wk=["wr"])
                bk = nbank()
                for f in range(32):
                    PE_T(psb[bk][:, f * 16:(f + 1) * 16], wle2[:, f, :], 16, ["wle2", "ident"], [pk(bk)], inc=(f == 31))
                V_cp(wr[:, :, 4:36].rearrange("p c f -> p f c"), psb[bk][:, 0:512].rearrange("p (f c) -> p f c", c=16), rk=[pk(bk)], wk=["wr"])
                S.barrier()
            S.dma("sp", rb[:, 0:4], router_group_b.to_broadcast([P, 4]), writes=["rb"], sem="m_rb0")
            S.dma("sp", rb[:, 4:36], router_expert_b.to_broadcast([P, 32]), writes=["rb"], sem="m_rb1")
            with ExitStack() as es3:
                gb = sb(es3, "gb", [P, D])
                S.dma("sp", gb[:], ffn_norm_g_row.to_broadcast([P, D]), writes=["gb"], sem="m_gb")
                xt2 = [sb(es3, "xt2_%d" % i, [P, D]) for i in range(2)]
                xn = [sb(es3, "xn_%d" % i, [P, D]) for i in range(2)]
                h2r = [sb(es3, "h2r_%d" % i, [P, D]) for i in range(2)]
                junk = sb(es3, "junk2", [P, D], BF16)
                h32 = sb(es3, "h32", [P, 16, P])
                ss2 = sb(es3, "ss2", [P, 16]); rs2 = sb(es3, "rs2", [P, 16])
                lgA = sb(es3, "lgA", [P, 16, 36])
                gmaxA = sb(es3, "gmaxA", [P, 16]); gexA = sb(es3, "gexA", [P, 16, 4]); gmA = sb(es3, "gmA", [P, 16, 4])
                gsumA = sb(es3, "gsumA", [P, 16]); mlA = sb(es3, "mlA", [P, 16, 32]); ml2A = sb(es3, "ml2A", [P, 16, 32])
                m1A = sb(es3, "m1A", [P, 16]); m2A = sb(es3, "m2A", [P, 16])
                sm = [sb(es3, "sm%d" % i, [P, 1]) for i in range(8)]
                ml = sb(es3, "ml", [P, 32]); ml2 = sb(es3, "ml2", [P, 32])
                gm = sb(es3, "gm", [P, 4]); gex = sb(es3, "gex", [P, 4])
                for tt in range(16):
                    r0 = tt * P
                    sl = tt % 2
                    xk = "xt2_%d" % sl
                    S.dma("sp", xt2[sl][:], x2d[r0:r0 + P, :], writes=[xk], sem="xt2_%d" % sl)
                    A_act(junk[:], xt2[sl][:], AF.Square, rk=[xk], wk=["junk2"], accum_out=ss2[:, tt:tt + 1])
                    A_act(rs2[:, tt:tt + 1], ss2[:, tt:tt + 1], AF.Sqrt, scale=1.0 / D, bias=epsc[:, 0:1],
                          rk=["junk2"], wk=[("rs2", tt)])
                    S.op("dve", lambda: nc.vector.reciprocal(out=rs2[:, tt:tt + 1], in_=rs2[:, tt:tt + 1]),
                         reads=[("rs2", tt)], writes=[("rs2", tt)])
                    V_ts(xn[sl][:], xt2[sl][:], rs2[:, tt:tt + 1], ALU.mult, rk=[xk, ("rs2", tt)], wk=["xn%d" % sl])
                    V_tt(h2r[sl][:], xn[sl][:], gb[:], ALU.mult, rk=["xn%d" % sl, "gb"], wk=["h2r%d" % sl], e="pool")
                    S.dma("sp", h2d[r0:r0 + P, :], h2r[sl][:], reads=["h2r%d" % sl], sem="h2w%d" % sl)
                    for cb in range(4):
                        bk = nbank()
                        for j in range(4):
                            c = cb * 4 + j
                            PE_T(psb[bk][:, j * P:(j + 1) * P], xn[sl][:, c * P:(c + 1) * P], P, ["xn%d" % sl, "ident"], [pk(bk)], inc=(j == 3))
                        e_ = S.alt()
                        for j in range(4):
                            c = cb * 4 + j
                            if e_ == "dve":
                                V_ts(h32[:, c, :], psb[bk][:, j * P:(j + 1) * P], g2T[:, c:c + 1], ALU.mult,
                                     rk=[pk(bk)], wk=[("h32", c)])
                            else:
                                S.op("act", lambda: nc.scalar.activation(out=h32[:, c, :], in_=psb[bk][:, j * P:(j + 1) * P],
                                                                         func=AF.Copy, scale=g2T[:, c:c + 1]),
                                     reads=[pk(bk)], writes=[("h32", c)])
                    bk = nbank()
                    for c in range(16):
                        PE_mm(psb[bk][:, 0:36], h32[:, c, :], wr[:, c, :], c == 0, c == 15,
                              [("h32", c), "wr"], [pk(bk)], inc=(c == 15))
                    V_tt(lgA[:, tt, :], psb[bk][:, 0:36], rb[:], ALU.add, rk=[pk(bk), "rb"], wk=[("lgA", tt)])
                lk = [("lgA", t_) for t_ in range(16)]
                AX = mybir.AxisListType.X
                gl = lgA[:, :, 0:4]
                el4 = lgA[:, :, 4:36].rearrange("p t (g e) -> p t g e", g=4)
                S.op("dve", lambda: nc.vector.tensor_reduce(out=gmaxA[:], in_=gl, axis=AX, op=ALU.max), reads=lk, writes=["gmaxA"])
                V_tt(gexA[:], gl, gmaxA[:].unsqueeze(2).broadcast_to([P, 16, 4]), ALU.subtract, rk=lk + ["gmaxA"], wk=["gexA"])
                V_ts(gmA[:], gexA[:], 0.0, ALU.is_ge, s2=-1.0, op1=ALU.add, rk=["gexA"], wk=["gmA"])
                V_ts(gmA[:], gmA[:], 1e30, ALU.mult, rk=["gmA"], wk=["gmA"])
                A_act(gexA[:], gexA[:], AF.Exp, rk=["gexA"], wk=["gexA"])
                S.op("dve", lambda: nc.vector.tensor_reduce(out=gsumA[:], in_=gexA[:], axis=AX, op=ALU.add), reads=["gexA"], writes=["gsumA"])
                S.op("dve", lambda: nc.vector.reciprocal(out=gsumA[:], in_=gsumA[:]), reads=["gsumA"], writes=["gsumA"])
                V_tt(mlA[:].rearrange("p t (g e) -> p t g e", g=4), el4, gmA[:].unsqueeze(3).broadcast_to([P, 16, 4, 8]), ALU.add,
                     rk=lk + ["gmA"], wk=["mlA"])
                S.op("dve", lambda: nc.vector.tensor_reduce(out=m1A[:], in_=mlA[:], axis=AX, op=ALU.max), reads=["mlA"], writes=["m1A"])
                V_tt(oh1a[:], mlA[:], m1A[:].unsqueeze(2).broadcast_to([P, 16, 32]), ALU.is_equal, rk=["mlA", "m1A"], wk=["oh1a"])
                V_stt(ml2A[:], oh1a[:], -1e30, mlA[:], ALU.mult, ALU.add, rk=["oh1a", "mlA"], wk=["ml2A"])
                S.op("dve", lambda: nc.vector.tensor_reduce(out=m2A[:], in_=ml2A[:], axis=AX, op=ALU.max), reads=["ml2A"], writes=["m2A"])
                V_tt(oh2a[:], ml2A[:], m2A[:].unsqueeze(2).broadcast_to([P, 16, 32]), ALU.is_equal, rk=["ml2A", "m2A"], wk=["oh2a"])
                V_tt(m1A[:], m1A[:], m2A[:], ALU.subtract, rk=["m1A", "m2A"], wk=["m1A"])
                A_act(m1A[:], m1A[:], AF.Sigmoid, rk=["m1A"], wk=["m1A"])
                V_tt(comb_g1[:], gsumA[:], m1A[:], ALU.mult, rk=["gsumA", "m1A"], wk=["comb_g1"])
                V_tt(comb_g2[:], gsumA[:], comb_g1[:], ALU.subtract, rk=["gsumA", "comb_g1"], wk=["comb_g2"])
                V_tt(selb[:], oh1a[:], oh2a[:], ALU.add, rk=["oh1a", "oh2a"], wk=[("selb", t_) for t_ in range(16)])
                S.barrier()
            if "g1" in tapd:
                S.dma("sp", tapd["g1"], comb_g1[:], sem="tap"); S.dma("sp", tapd["oh1"], oh1a[:].rearrange("p t e -> p (t e)"), sem="tap")
                S.barrier()
            gate(106)
            with ExitStack() as es3:
                cntx = sb(es3, "cntx", [P, 16, 32]); tot = sb(es3, "tot", [P, 32])
                ci = sb(es3, "ci", [P, 32], I32); pad = sb(es3, "pad", [P, 32]); incl = sb(es3, "incl", [P, 32])
                base = sb(es3, "base", [P, 32]); zz = sb(es3, "zz", [P, 32])
                slot = sb(es3, "slot", [P, 16, 32]); tmp3 = sb(es3, "tmp3", [P, 16, 32])
                s1f = sb(es3, "s1f", [P, 16]); s2f = sb(es3, "s2f", [P, 16])
                jv = sb(es3, "jv", [P, NT]); pidf = sb(es3, "pidf", [P, 1])
                cmp3 = sb(es3, "cmp3", [P, NT, 32]); ejf = sb(es3, "ejf", [P, NT])
                tid = sb(es3, "tid", [P, 16, 16], I32)
                zi = sb(es3, "zi", [P, NS * 16 // P], I32)
                bk = nbank(); bk2 = nbank()
                for tt in range(16):
                    for t2 in range(tt + 1):
                        PE_mm(psb[bk][:, tt * 32:(tt + 1) * 32], cmb[:] if t2 == tt else onesb[:], selb[:, t2, :],
                              t2 == 0, t2 == tt, [("selb", t2)], [pk(bk)], inc=(t2 == tt), sg=True)
                for tt in range(16):
                    PE_mm(psb[bk2][:, 0:32], onesb[:], selb[:, tt, :], tt == 0, tt == 15, [("selb", tt)], [pk(bk2)], inc=(tt == 15))
                V_cp(cntx[:].rearrange("p t e -> p (t e)"), psb[bk][:, :], rk=[pk(bk)], wk=["cntx"])
                V_cp(tot[:], psb[bk2][:, 0:32], rk=[pk(bk2)], wk=["tot"])
                S.op("dve", lambda: nc.vector.memset(zz[:], 0.0), writes=["zz"])
                V_ts(ci[:], tot[:], float(TS - 1), ALU.add)
                S.op("dve", lambda: nc.vector.tensor_scalar(out=ci[:], in0=ci[:], scalar1=8, scalar2=8,
                                                            op0=ALU.arith_shift_right, op1=ALU.logical_shift_left),
                     reads=["ci"], writes=["ci"])
                V_cp(pad[:], ci[:])
                S.op("dve", lambda: nc.vector.tensor_tensor_scan(out=incl[:], data0=pad[:], data1=zz[:], initial=0.0,
                                                                 op0=ALU.add, op1=ALU.add),
                     reads=["pad", "zz"], writes=["incl"])
                V_tt(base[:], incl[:], pad[:], ALU.subtract)
                V_tt(slot[:], cntx[:], base[:].unsqueeze(1).broadcast_to([P, 16, 32]), ALU.add, rk=["cntx", "base"], wk=["slot"])
                V_tt(tmp3[:], slot[:], oh1a[:], ALU.mult, rk=["slot"], wk=["tmp3"])
                S.op("dve", lambda: nc.vector.tensor_reduce(out=s1f[:], in_=tmp3[:], axis=mybir.AxisListType.X, op=ALU.add),
                     reads=["tmp3"], writes=["s1f"])
                V_cp(s1i[:], s1f[:])
                V_tt(tmp3[:], slot[:], oh2a[:], ALU.mult, rk=["slot", "s1f"], wk=["tmp3"])
                S.op("dve", lambda: nc.vector.tensor_reduce(out=s2f[:], in_=tmp3[:], axis=mybir.AxisListType.X, op=ALU.add),
                     reads=["tmp3"], writes=["s2f"])
                V_cp(s2i[:], s2f[:])
                S.op("pool", lambda: nc.gpsimd.iota(jv[:], pattern=[[TS, NT]], base=0, channel_multiplier=0,
                                                    allow_small_or_imprecise_dtypes=True), writes=["jv"])
                S.op("pool", lambda: nc.gpsimd.iota(pidf[:], pattern=[[0, 1]], base=0, channel_multiplier=1,
                                                    allow_small_or_imprecise_dtypes=True), writes=["pidf"])
                V_tt(cmp3[:], incl[:].unsqueeze(1).broadcast_to([P, NT, 32]), jv[:].unsqueeze(2).broadcast_to([P, NT, 32]),
                     ALU.is_le, rk=["incl", "jv"], wk=["cmp3"])
                S.op("dve", lambda: nc.vector.tensor_reduce(out=ejf[:], in_=cmp3[:], axis=mybir.AxisListType.X, op=ALU.add),
                     reads=["cmp3"], writes=["ejf"])
                emp = sb(es3, "emp", [P, NT])
                V_ts(emp[:], ejf[:], 32.0, ALU.is_ge, s2=65536.0, op1=ALU.mult, rk=["ejf"], wk=["emp"])
                V_ts(ejf[:], ejf[:], 31.0, ALU.min, s2=128.0, op1=ALU.mult)
                V_ts(ejf[:], ejf[:], pidf[:, 0:1], ALU.add)
                V_tt(ejf[:], ejf[:], emp[:], ALU.add)
                V_cp(widx[:], ejf[:])
                rowf = sb(es3, "rowf", [P, NT * 2]); endv = sb(es3, "endv", [P, 32])
                cAB = sb(es3, "cAB", [P, NT * 2, 32]); nA = sb(es3, "nA", [P, NT * 2]); nB = sb(es3, "nB", [P, NT * 2])
                S.op("pool", lambda: nc.gpsimd.iota(rowf[:], pattern=[[P, NT * 2]], base=0, channel_multiplier=1,
                                                    allow_small_or_imprecise_dtypes=True), writes=["rowf"])
                V_tt(endv[:], base[:], tot[:], ALU.add, rk=["base", "tot"], wk=["endv"])
                V_tt(cAB[:], base[:].unsqueeze(1).broadcast_to([P, NT * 2, 32]), rowf[:].unsqueeze(2).broadcast_to([P, NT * 2, 32]),
                     ALU.is_le, rk=["base", "rowf"], wk=["cAB"])
                S.op("dve", lambda: nc.vector.tensor_reduce(out=nA[:], in_=cAB[:], axis=mybir.AxisListType.X, op=ALU.add),
                     reads=["cAB"], writes=["nA"])
                V_tt(cAB[:], endv[:].unsqueeze(1).broadcast_to([P, NT * 2, 32]), rowf[:].unsqueeze(2).broadcast_to([P, NT * 2, 32]),
                     ALU.is_le, rk=["endv", "rowf", "nA"], wk=["cAB"])
                S.op("dve", lambda: nc.vector.tensor_reduce(out=nB[:], in_=cAB[:], axis=mybir.AxisListType.X, op=ALU.add),
                     reads=["cAB"], writes=["nB"])
                V_tt(nA[:], nA[:], nB[:], ALU.subtract, rk=["nA", "nB"], wk=["nA"])
                V_ts(nA[:], nA[:], -65536.0, ALU.mult, s2=65536.0, op1=ALU.add, rk=["nA"], wk=["nA"])
                V_tt(nA[:], nA[:], rowf[:], ALU.add, rk=["nA", "rowf"], wk=["nA"])
                V_cp(yix[:], nA[:], rk=["nA"], wk=["yix"])
                S.op("pool", lambda: nc.gpsimd.iota(tid[:], pattern=[[P, 16], [0, 16]], base=0, channel_multiplier=1), writes=["tid"])
                S.op("dve", lambda: nc.vector.memset(zi[:], 4096), writes=["zi"])
                S.dma("sp", stok.rearrange("(p a) f -> p (a f)", p=P), zi[:], reads=["zi"], writes=["stok"], sem="stz")
                for tt in range(16):
                    for (sx, nm_) in ((s1i, "s1i"), (s2i, "s2i")):
                        S.idma(stok, sx[:, tt:tt + 1], tid[:, tt, :], None, reads=[nm_, "tid", "stok"], writes=[("stokw", tt, nm_)], sem="scat")
                S.barrier()
            if "s1i" in tapd:
                S.dma("pool", tapd["s1i"], s1i[:], sem="tap"); S.dma("pool", tapd["widx"], widx[:], sem="tap")
                S.barrier()
            gate(107)
            with ExitStack() as es3:
                tix = [sb(es3, "tix%d" % i, [P, 2], I32) for i in range(2)]
                xg = [sb(es3, "xg%d" % i, [P, 2, D]) for i in range(2)]
                xT = [sb(es3, "xT%d" % i, [P, 16, TS], BF16) for i in range(2)]
                wgb = [sb(es3, "wgb%d" % i, [P, 16, DFF], BF16) for i in range(2)]
                wub = [sb(es3, "wub%d" % i, [P, 16, DFF], BF16) for i in range(2)]
                wdb = [sb(es3, "wdb%d" % i, [P, 4, D], BF16) for i in range(2)]
                hidb = [sb(es3, "hidb%d" % i, [P, 4, TS], BF16) for i in range(2)]
                sgm = [sb(es3, "sgm%d" % i, [P, DFF]) for i in range(2)]
                ysb = [sb(es3, "ysb%d" % i, [P, D]) for i in range(2)]
                for i_ in range(2):
                    S.op("dve", lambda: nc.vector.memset(xg[i_][:], 0.0), writes=[("xg", i_, 0), ("xg", i_, 1)])
                    S.op("dve", lambda: nc.vector.memset(wgb[i_][:], 0.0), writes=["wgb%d" % i_])
                    S.op("dve", lambda: nc.vector.memset(wub[i_][:], 0.0), writes=["wub%d" % i_])
                    S.op("dve", lambda: nc.vector.memset(wdb[i_][:], 0.0), writes=["wdb%d" % i_])
                wgv = expert_w_gate.rearrange("e (p c) f -> (e p) (c f)", p=P)
                wuv = expert_w_up.rearrange("e (p c) f -> (e p) (c f)", p=P)
                wdv = expert_w_down.rearrange("e (p c) d -> (e p) (c d)", p=P)
                ycnt_ = [0]

                def Lx(j, sl):
                    for h in range(2):
                        S.dma("sp", tix[sl][:, h:h + 1], stok[j * TS + h * P:j * TS + (h + 1) * P, 0:1],
                              writes=["tix%d" % sl], sem="tix%d" % sl, allow_slow_non_contiguous=True)
                    for h in range(2):
                        S.idma(xg[sl][:, h, :], None, h2d, tix[sl][:, h:h + 1], reads=["tix%d" % sl], writes=[("xg", sl, h)], sem="xg%d" % sl, bounds=T - 1)

                def Lwg(j, sl):
                    S.idma(wgb[sl][:].rearrange("p c f -> p (c f)"), None, wgv, widx[:, j:j + 1], reads=[], writes=["wgb%d" % sl], sem="wgb%d" % sl, bounds=NE * P - 1)

                def Lwu(j, sl):
                    S.idma(wub[sl][:].rearrange("p c f -> p (c f)"), None, wuv, widx[:, j:j + 1], reads=[], writes=["wub%d" % sl], sem="wub%d" % sl, bounds=NE * P - 1)

                def Lwd(j, sl):
                    S.idma(wdb[sl][:].rearrange("p c f -> p (c f)"), None, wdv, widx[:, j:j + 1], reads=[], writes=["wdb%d" % sl], sem="wdb%d" % sl, bounds=NE * P - 1)

                def Lt(j, sl):
                    Lx(j, sl); Lwg(j, sl); Lwu(j, sl); Lwd(j, sl)

                def Ct(j, sl, nxt=None):
                    for h in range(2):
                        for cb in range(4):
                            bk = nbank(0, 4)
                            for jj in range(4):
                                c = cb * 4 + jj
                                PE_T(psb[bk][:, jj * P:(jj + 1) * P], xg[sl][:, h, c::16], P, [("xg", sl, h), "ident"], [pk(bk)], inc=(jj == 3))
                            evac_copy(xT[sl][:, cb * 4:(cb + 1) * 4, h * P:(h + 1) * P],
                                      psb[bk][:, :].rearrange("p (j s) -> p j s", j=4), bk, [("xT", sl, h, cb)])
                    xk_ = [("xT", sl, h, cb) for h in range(2) for cb in range(4)]
                    if nxt is not None:
                        Lx(nxt, sl)
                    bgs = [nbank(4, 8) for _ in range(2)]
                    bus = [nbank(4, 8) for _ in range(2)]
                    for h in range(2):
                        xkh = [("xT", sl, h, cb) for cb in range(4)]
                        for c in range(16):
                            PE_mm(psb[bgs[h]][:, :], xT[sl][:, c, h * P:(h + 1) * P], wgb[sl][:, c, :], c == 0, c == 15,
                                  ["wgb%d" % sl] + xkh, [pk(bgs[h])], inc=(c == 15))
                    if nxt is not None:
                        Lwg(nxt, sl)
                    for h in range(2):
                        xkh = [("xT", sl, h, cb) for cb in range(4)]
                        for c in range(16):
                            PE_mm(psb[bus[h]][:, :], xT[sl][:, c, h * P:(h + 1) * P], wub[sl][:, c, :], c == 0, c == 15,
                                  ["wub%d" % sl] + xkh, [pk(bus[h])], inc=(c == 15))
                    if nxt is not None:
                        Lwu(nxt, sl)
                    for h in range(2):
                        bg, bu = bgs[h], bus[h]
                        s2 = h
                        A_act(sgm[s2][:], psb[bg][:, :], AF.Silu, rk=[pk(bg)], wk=["sgm%d" % s2])
                        V_tt(sgm[s2][:], sgm[s2][:], psb[bu][:, :], ALU.mult, rk=["sgm%d" % s2, pk(bu)], wk=["sgm%d" % s2])
                        bt = nbank(0, 4)
                        for fc in range(4):
                            PE_T(psb[bt][:, fc * P:(fc + 1) * P], sgm[s2][:, fc::4], P, ["sgm%d" % s2, "ident"], [pk(bt)], inc=(fc == 3))
                        evac_copy(hidb[sl][:, :, h * P:(h + 1) * P], psb[bt][:, :].rearrange("p (f s) -> p f s", f=4), bt,
                                  [("hidb", sl, fc_, h) for fc_ in range(4)])
                    for h in range(2):
                        ys = ycnt_[0] % 2
                        ycnt_[0] += 1
                        for db in range(4):
                            bk = nbank(0, 4)
                            for fc in range(4):
                                PE_mm(psb[bk][:, :], hidb[sl][:, fc, h * P:(h + 1) * P], wdb[sl][:, fc, db * 512:(db + 1) * 512],
                                      fc == 0, fc == 3, ["wdb%d" % sl, ("hidb", sl, fc, h)], [pk(bk)], inc=(fc == 3))
                            evac_copy(ysb[ys][:, db * 512:(db + 1) * 512], psb[bk][:, :], bk, [("ysb", ys, db)])
                        r0 = j * TS + h * P
                        if h == 1 and nxt is not None:
                            Lwd(nxt, sl)
                        S.idma(yd, yix[:, 2 * j + h:2 * j + h + 1], ysb[ys][:], None, reads=[("ysb", ys, db_) for db_ in range(4)],
                               sem="yw%d" % ys, bounds=NS - 1)


                NH = 32
                order = []
                for k in range(NH):
                    order.append((k, k % 2))
                    if k < NT - NH:
                        order.append((NT - 1 - k, k % 2))
                Lt(0, 0); Lt(1, 1)
                for i_, (j_, sl) in enumerate(order):
                    nx = [jj for (jj, s_) in order[i_ + 1:] if s_ == sl]
                    Ct(j_, sl, nx[0] if nx else None)
                S.barrier()
            gate(108)
            with ExitStack() as es3:
                xa = [sb(es3, "xa%d" % i, [P, D]) for i in range(2)]
                y1 = [sb(es3, "y1_%d" % i, [P, D]) for i in range(2)]
                y2 = [sb(es3, "y2_%d" % i, [P, D]) for i in range(2)]
                for tt in range(16):
                    sl = tt % 2
                    r0 = tt * P
                    S.dma("sp", xa[sl][:], x2d[r0:r0 + P, :], writes=["xa%d" % sl], sem="xa%d" % sl)
                    S.idma(y1[sl][:], None, yd, s1i[:, tt:tt + 1], reads=[], writes=["y1_%d" % sl], sem="y1_%d" % sl)
                    S.idma(y2[sl][:], None, yd, s2i[:, tt:tt + 1], reads=[], writes=["y2_%d" % sl], sem="y2_%d" % sl)
                    V_stt(xa[sl][:], y1[sl][:], comb_g1[:, tt:tt + 1], xa[sl][:], ALU.mult, ALU.add,
                          rk=["y1_%d" % sl, "xa%d" % sl], wk=["xa%d" % sl])
                    V_stt(xa[sl][:], y2[sl][:], comb_g2[:, tt:tt + 1], xa[sl][:], ALU.mult, ALU.add,
                          rk=["y2_%d" % sl, "xa%d" % sl], wk=["xa%d" % sl])
                    S.dma("sp", out[r0:r0 + P, :], xa[sl][:], reads=["xa%d" % sl], sem="outd%d" % sl)
                S.barrier()

        S.barrier()
        import os
        if os.environ.get("KDEBUG"):
            print("sched counts", S.cnt, {k: v[1] for k, v in S.dsem.items()})
    return nc


_INPUT_LAYOUT = {
    "x": None,
}


def _prep_inputs(inputs, b):
    g = lambda k: np.ascontiguousarray(np.asarray(inputs[k], dtype=np.float32))
    m = {
        "x": g("x")[b],
        "attn_norm_g": g("attn_norm_g")[0].reshape(16, 128),
        "w_in": g("w_in")[0],
        "lambda_re": g("lambda_re")[0].reshape(32, 128),
        "lambda_im": g("lambda_im")[0].reshape(32, 128),
        "log_dt": g("log_dt")[0].reshape(32, 2),
        "ssm_b_re": g("ssm_b_re")[0].reshape(32, 2048),
        "ssm_b_im": g("ssm_b_im")[0].reshape(32, 2048),
        "ssm_c_re": g("ssm_c_re")[0].reshape(32, 2048),
        "ssm_c_im": g("ssm_c_im")[0].reshape(32, 2048),
        "ssm_d": g("ssm_d")[0].reshape(8, 128),
        "w_glu": g("w_glu")[0],
        "q_norm_g": g("q_norm_g")[0].reshape(1, 128),
        "k_norm_g": g("k_norm_g")[0].reshape(1, 128),
        "w_branch_ssm": g("w_branch_ssm")[0],
        "w_branch_att": g("w_branch_att")[0],
        "w_out": g("w_out")[0],
        "ffn_norm_g": g("ffn_norm_g")[0].reshape(16, 128),
        "ffn_norm_g_row": g("ffn_norm_g")[0].reshape(1, D),
        "router_group_w": g("router_group_w")[0],
        "router_group_b": g("router_group_b")[0].reshape(1, 4),
        "router_expert_w": g("router_expert_w")[0],
        "router_expert_b": g("router_expert_b")[0].reshape(1, 32),
        "expert_w_gate": g("expert_w_gate")[0],
        "expert_w_up": g("expert_w_up")[0],
        "expert_w_down": g("expert_w_down")[0],
    }
    return m


def kernel(**inputs):
    nc = build()
    shared = _prep_inputs(inputs, 0)
    xs = np.asarray(inputs["x"], dtype=np.float32)
    in_maps = []
    for b in range(8):
        m = dict(shared)
        m["x"] = np.ascontiguousarray(xs[b])
        in_maps.append(m)
    res = run_bass_kernel_spmd(nc, in_maps, core_ids=list(range(8)))
    return np.stack([np.asarray(r["out"], dtype=np.float32) for r in res.results], axis=0)
```

```python
import math
from contextlib import ExitStack

import numpy as np
import concourse.bass as bass
import concourse.mybir as mybir
from concourse.bass_utils import run_bass_kernel_spmd

F32 = mybir.dt.float32
BF16 = mybir.dt.bfloat16
I32 = mybir.dt.int32
AF = mybir.ActivationFunctionType
ALU = mybir.AluOpType

T = 2048
D = 2048
P = 128
NCK = 256
EPS = 1e-6
NE = 32
DFF = 512


class Sched:
    def __init__(self, nc):
        self.nc = nc
        self.eng = {"pe": nc.tensor, "act": nc.scalar, "dve": nc.vector,
                    "pool": nc.gpsimd, "sp": nc.sync}
        self.sem = {e: nc.alloc_semaphore(name="s_" + e) for e in self.eng}
        self.cnt = {e: 0 for e in self.eng}
        self.waited = {e: {} for e in self.eng}
        self.res = {}
        self.dsem = {}
        self.rr = 0
        self.dead = False
        self.bregs = {}

    def _toks(self, reads, writes):
        toks = []
        for k in reads:
            st = self.res.get(k)
            if st is not None and st[0] is not None:
                toks.append(st[0])
        for k in writes:
            st = self.res.get(k)
            if st is not None:
                if st[0] is not None:
                    toks.append(st[0])
                toks.extend(st[1])
        return toks

    def _wait(self, e, toks):
        need = {}
        for (s, v) in toks:
            if s == e and e in ("pe", "sp"):
                continue
            if v > need.get(s, 0):
                need[s] = v
        for s, v in need.items():
            if self.waited[e].get(s, 0) >= v:
                continue
            h = self.sem[s] if s in self.sem else self.dsem[s][0]
            self.eng[e].wait_ge(h, v)
            self.waited[e][s] = v

    def _mark(self, tok, reads, writes):
        for k in reads:
            st = self.res.setdefault(k, [None, []])
            st[1].append(tok)
            if len(st[1]) > 24:
                mx = {}
                for (s, v) in st[1]:
                    if v > mx.get(s, 0):
                        mx[s] = v
                st[1] = list(mx.items())
        for k in writes:
            self.res[k] = [tok, []]

    def op(self, e, fn, reads=(), writes=(), inc=True):
        if self.dead:
            return None
        self._wait(e, self._toks(reads, writes))
        ins = fn()
        tok = (e, self.cnt[e] + 1)
        if inc:
            ins.then_inc(self.sem[e], 1)
            self.cnt[e] += 1
        self._mark(tok, reads, writes)
        return ins

    def dma(self, q, out, in_, reads=(), writes=(), sem="d", **kw):
        if self.dead:
            return None
        self._wait(q, self._toks(reads, writes))
        if sem not in self.dsem:
            self.dsem[sem] = [self.nc.alloc_semaphore(name="d_" + sem), 0]
        d = self.dsem[sem]
        d[1] += 16
        self.eng[q].dma_start(out=out, in_=in_, **kw).then_inc(d[0], 16)
        self._mark((sem, d[1]), reads, writes)

    def idma(self, out, out_idx, in_, in_idx, reads=(), writes=(), sem="id", bounds=None):
        if self.dead:
            return None
        self._wait("pool", self._toks(reads, writes))
        if sem not in self.dsem:
            self.dsem[sem] = [self.nc.alloc_semaphore(name="d_" + sem), 0]
        d = self.dsem[sem]
        d[1] += 16
        oo = bass.IndirectOffsetOnAxis(ap=out_idx, axis=0) if out_idx is not None else None
        io = bass.IndirectOffsetOnAxis(ap=in_idx, axis=0) if in_idx is not None else None
        kw = {}
        if bounds is not None:
            if bounds not in self.bregs:
                self.bregs[bounds] = self.nc.gpsimd.to_reg(bounds)
            kw = {"bounds_check": self.bregs[bounds], "oob_is_err": False}
        self.nc.gpsimd.indirect_dma_start(out=out, out_offset=oo, in_=in_, in_offset=io, **kw).then_inc(d[0], 16)
        self._mark((sem, d[1]), reads, writes)

    def barrier(self):
        toks = [(e, self.cnt[e]) for e in self.eng if self.cnt[e] > 0]
        toks += [(s, d[1]) for s, d in self.dsem.items() if d[1] > 0]
        for e in self.eng:
            self._wait(e, [t for t in toks if t[0] != e])
        self.res = {}

    def alt(self):
        self.rr ^= 1
        return "act" if self.rr else "dve"


class _Stop(Exception):
    pass


def build(upto="all", taps=()):
    import os
    kgate = int(os.environ.get("KGATE", "0"))

    gate_s = [None]

    def gate(n):
        if kgate == n:
            gate_s[0].dead = True
    nc = bass.Bass("TRN2", target_bir_lowering=False)
    S = Sched(nc)
    gate_s[0] = S
    dram = {}

    def din(name, shape):
        dram[name] = nc.dram_tensor(name, list(shape), F32, kind="ExternalInput").ap()
        return dram[name]

    x = din("x", [T, D])
    attn_norm_g = din("attn_norm_g", [16, 128])
    w_in = din("w_in", [D, 8192])
    lambda_re = din("lambda_re", [32, 128])
    lambda_im = din("lambda_im", [32, 128])
    log_dt = din("log_dt", [32, 2])
    ssm_b_re = din("ssm_b_re", [32, 2048])
    ssm_b_im = din("ssm_b_im", [32, 2048])
    ssm_c_re = din("ssm_c_re", [32, 2048])
    ssm_c_im = din("ssm_c_im", [32, 2048])
    ssm_d = din("ssm_d", [8, 128])
    w_glu = din("w_glu", [1024, 1024])
    q_norm_g = din("q_norm_g", [1, 128])
    k_norm_g = din("k_norm_g", [1, 128])
    w_branch_ssm = din("w_branch_ssm", [1024, 2048])
    w_branch_att = din("w_branch_att", [1024, 2048])
    w_out = din("w_out", [D, D])
    ffn_norm_g = din("ffn_norm_g", [16, 128])
    ffn_norm_g_row = din("ffn_norm_g_row", [1, D])
    router_group_w = din("router_group_w", [D, 4])
    router_group_b = din("router_group_b", [1, 4])
    router_expert_w = din("router_expert_w", [D, 32])
    router_expert_b = din("router_expert_b", [1, 32])
    expert_w_gate = din("expert_w_gate", [NE, D, DFF])
    expert_w_up = din("expert_w_up", [NE, D, DFF])
    expert_w_down = din("expert_w_down", [NE, DFF, D])
    out = nc.dram_tensor("out", [T, D], F32, kind="ExternalOutput").ap()
    x2d = nc.dram_tensor("x2_scratch", [T, D], F32, kind="Internal").ap()
    tapd = {}
    for (nm, shp) in taps:
        tapd[nm] = nc.dram_tensor("tap_" + nm, list(shp), F32, kind="ExternalOutput").ap()

    top = ExitStack()
    with top:
        def sb(es, name, shape, dt=F32):
            return es.enter_context(nc.sbuf_tensor(name, list(shape), dt))

        ps_all = top.enter_context(nc.psum_tensor("ps_all", [P, 4096], F32))
        psb = [ps_all[:, i * 512:(i + 1) * 512] for i in range(8)]
        bank_rr = [0]

        def nbank(lo=0, hi=8):
            b = lo + (bank_rr[0] % (hi - lo))
            bank_rr[0] += 1
            return b

        ident = sb(top, "ident", [P, P])
        ones = sb(top, "ones", [P, P])
        epsc = sb(top, "epsc", [P, 1])
        S.op("dve", lambda: nc.vector.memset(ones[:], 1.0), writes=["ones"])
        S.op("dve", lambda: nc.vector.memset(epsc[:], EPS), writes=["epsc"])
        S.op("pool", lambda: nc.gpsimd.affine_select(
            out=ident[:], in_=ones[:], pattern=[[1, P]], compare_op=ALU.is_equal,
            fill=0.0, base=0, channel_multiplier=-1), reads=["ones"], writes=["ident"])

        def tap(nm, src_ap, key):
            if nm in tapd:
                S.dma("sp", tapd[nm], src_ap, reads=[key], sem="tap")

        tri = sb(top, "tri", [P, P]); lem = sb(top, "lem", [P, P]); cmf = sb(top, "cmf", [P, P])
        cmb = sb(top, "cmb", [P, P], BF16); zer = sb(top, "zer", [P, 512], BF16)
        trib = sb(top, "trib", [P, P], BF16); lemb = sb(top, "lemb", [P, P], BF16)
        gq = sb(top, "gq", [P, 1]); gk = sb(top, "gk", [P, 1])
        g1s = sb(top, "g1s", [16, P])
        g1T = sb(top, "g1T", [P, 16])
        g2s = sb(top, "g2s", [16, P])
        g2T = sb(top, "g2T", [P, 16])
        es_mix = ExitStack()
        hT = sb(es_mix, "hT", [P, 16, T], BF16)
        s5T = sb(es_mix, "s5T", [P, 8, T], BF16)
        S.dma("sp", g1s[:], attn_norm_g, writes=["g1s"], sem="m1")
        b = nbank()
        S.op("pe", lambda: nc.tensor.transpose(out=psb[b][:, 0:16], in_=g1s[:], identity=ident[0:16, 0:16]),
             reads=["g1s", "ident"], writes=["ps%d" % b])
        S.op("dve", lambda: nc.vector.tensor_copy(out=g1T[:], in_=psb[b][:, 0:16]),
             reads=["ps%d" % b], writes=["g1T"])

        def rmsnorm_to_T(es, src_rows, gT, dstT, dst_key, ncols_off=0, ntiles=16, pfx="n1"):
            xt = [sb(es, pfx + "_xt%d" % i, [P, D]) for i in range(2)]
            junk = sb(es, pfx + "_junk", [P, D], BF16)
            ss = sb(es, pfx + "_ss", [P, ntiles])
            rs = sb(es, pfx + "_rs", [P, ntiles])
            for tt in range(ntiles):
                sl = tt % 2
                xk = pfx + "xt%d" % sl
                S.dma("sp", xt[sl][:], src_rows(tt), writes=[xk], sem=pfx + "x%d" % sl)
                S.op("act", lambda: nc.scalar.activation(out=junk[:], in_=xt[sl][:], func=AF.Square,
                                                         accum_out=ss[:, tt:tt + 1]),
                     reads=[xk], writes=[pfx + "junk", (pfx + "ss", tt)])
                S.op("act", lambda: nc.scalar.activation(out=rs[:, tt:tt + 1], in_=ss[:, tt:tt + 1], func=AF.Sqrt,
                                                         bias=epsc[:, 0:1], scale=1.0 / D),
                     reads=[(pfx + "ss", tt), "epsc"], writes=[(pfx + "rs", tt)])
                S.op("dve", lambda: nc.vector.reciprocal(out=rs[:, tt:tt + 1], in_=rs[:, tt:tt + 1]),
                     reads=[(pfx + "rs", tt)], writes=[(pfx + "rs", tt)])
                S.op("dve", lambda: nc.vector.tensor_scalar(out=xt[sl][:], in0=xt[sl][:], scalar1=rs[:, tt:tt + 1],
                                                            scalar2=None, op0=ALU.mult),
                     reads=[xk, (pfx + "rs", tt)], writes=[xk])
                for cb in range(4):
                    bk = nbank()
                    for j in range(4):
                        c = cb * 4 + j
                        S.op("pe", lambda: nc.tensor.transpose(out=psb[bk][:, j * P:(j + 1) * P],
                                                               in_=xt[sl][:, c * P:(c + 1) * P], identity=ident[:]),
                             reads=[xk, "ident"], writes=["ps%d" % bk], inc=(j == 3))
                    for j in range(4):
                        c = cb * 4 + j
                        dst = dstT[:, c, ncols_off + tt * P: ncols_off + (tt + 1) * P]
                        e = S.alt()
                        if e == "dve":
                            S.op("dve", lambda: nc.vector.tensor_scalar(out=dst, in0=psb[bk][:, j * P:(j + 1) * P],
                                                                        scalar1=gT[:, c:c + 1], scalar2=None,
                                                                        op0=ALU.mult),
                                 reads=["ps%d" % bk], writes=[(dst_key, tt)])
                        else:
                            S.op("act", lambda: nc.scalar.activation(out=dst, in_=psb[bk][:, j * P:(j + 1) * P],
                                                                     func=AF.Copy, scale=gT[:, c:c + 1]),
                                 reads=["ps%d" % bk], writes=[(dst_key, tt)])

        with ExitStack() as es:
            rmsnorm_to_T(es, lambda tt: x[tt * P:(tt + 1) * P, :], g1T, hT, "hT")
            S.barrier()
        gate(101)
        if "hT" in tapd:
            with ExitStack() as es:
                tmp = sb(es, "taptmp", [P, 16, T])
                S.op("dve", lambda: nc.vector.tensor_copy(out=tmp[:], in_=hT[:]), writes=["taptmp"])
                S.dma("sp", tapd["hT"].rearrange("p (c t) -> p c t", c=16), tmp[:], reads=["taptmp"], sem="tap")
                S.barrier()


        def kn(ap):
            return ap.name

        def V_tt(out, a, b, op, rk=None, wk=None, e="dve"):
            en = nc.vector if e == "dve" else nc.gpsimd
            return S.op(e, lambda: en.tensor_tensor(out=out, in0=a, in1=b, op=op),
                        reads=rk if rk is not None else [kn(a), kn(b)],
                        writes=wk if wk is not None else [kn(out)])

        def V_ts(out, a, s1, op0, s2=None, op1=None, rk=None, wk=None):
            kw = {}
            if op1 is not None:
                kw["op1"] = op1
            r = rk if rk is not None else [kn(a)] + [kn(s) for s in (s1, s2) if hasattr(s, "name")]
            return S.op("dve", lambda: nc.vector.tensor_scalar(out=out, in0=a, scalar1=s1, scalar2=s2, op0=op0, **kw),
                        reads=r, writes=wk if wk is not None else [kn(out)])

        def V_stt(out, a, s, b, op0, op1, rk=None, wk=None):
            r = rk if rk is not None else [kn(a), kn(b)] + ([kn(s)] if hasattr(s, "name") else [])
            return S.op("dve", lambda: nc.vector.scalar_tensor_tensor(out=out, in0=a, scalar=s, in1=b, op0=op0, op1=op1),
                        reads=r, writes=wk if wk is not None else [kn(out)])

        def V_cp(out, a, rk=None, wk=None, e="dve"):
            if e == "act":
                return S.op("act", lambda: nc.scalar.copy(out=out, in_=a),
                            reads=rk if rk is not None else [kn(a)], writes=wk if wk is not None else [kn(out)])
            en = nc.vector if e == "dve" else nc.gpsimd
            return S.op(e, lambda: en.tensor_copy(out=out, in_=a),
                        reads=rk if rk is not None else [kn(a)], writes=wk if wk is not None else [kn(out)])

        def A_act(out, a, func, scale=1.0, bias=None, rk=None, wk=None, accum_out=None):
            kw = {}
            if bias is not None:
                kw["bias"] = bias
            if accum_out is not None:
                kw["accum_out"] = accum_out
            r = rk if rk is not None else [kn(a)] + [kn(s) for s in (scale, bias) if hasattr(s, "name")]
            return S.op("act", lambda: nc.scalar.activation(out=out, in_=a, func=func, scale=scale, **kw),
                        reads=r, writes=wk if wk is not None else [kn(out)])

        def PE_T(out, in_, n, rk, wk, inc=True):
            return S.op("pe", lambda: nc.tensor.transpose(out=out, in_=in_, identity=ident[0:n, 0:n]),
                        reads=rk, writes=wk, inc=inc)

        def PE_mm(out, lhsT, rhs, start, stop, rk, wk, inc=True, tp=None, sg=False):
            kw = {}
            if sg:
                kw["skip_group_check"] = True
            if tp is not None:
                kw["tile_position"] = tp
            return S.op("pe", lambda: nc.tensor.matmul(out, lhsT=lhsT, rhs=rhs, start=start, stop=stop, **kw),
                        reads=rk, writes=wk, inc=inc)

        def pk(b):
            return "ps%d" % b

        def tap_bf(nm, src, key_list):
            if nm in tapd:
                S.dma("pool", tapd[nm], src, reads=key_list, sem="tap")

        es_ssm = ExitStack()
        APr = sb(es_ssm, "APr", [P, 9, 32]); APi = sb(es_ssm, "APi", [P, 9, 32])
        AKr = sb(es_ssm, "AKr", [P, 8, 32]); AKi = sb(es_ssm, "AKi", [P, 8, 32]); AKn = sb(es_ssm, "AKn", [P, 8, 32])
        BBr = sb(es_ssm, "BBr", [P, 16, 32]); BBi = sb(es_ssm, "BBi", [P, 16, 32])
        CRt = sb(es_ssm, "CRt", [P, 16, 32]); CIt = sb(es_ssm, "CIt", [P, 16, 32])
        Dcol = sb(es_ssm, "Dcol", [P, 8])
        with ExitStack() as es:
            st_lr = sb(es, "st_lr", [32, P]); st_li = sb(es, "st_li", [32, P])
            st_dt = sb(es, "st_dt", [32, 2]); st_dtb = sb(es, "st_dtb", [32, P])
            st_b1 = sb(es, "st_b", [32, 2048]); st_c1 = sb(es, "st_c", [32, 2048])
            st_b21 = sb(es, "st_b2", [32, 16, P]); st_c21 = sb(es, "st_c2", [32, 16, P])
            st_b = [st_b1, st_b1]; st_c = [st_c1, st_c1]; st_b2 = [st_b21, st_b21]; st_c2 = [st_c21, st_c21]
            st_d = sb(es, "st_d", [8, P])
            LLD = sb(es, "LLD", [P, 96])
            BRt = sb(es, "BRt", [P, 16, 32]); BIt = sb(es, "BIt", [P, 16, 32])
            wk_ = [sb(es, "pw%d" % i, [P, 32]) for i in range(12)]
            S.dma("sp", st_lr[:], lambda_re, writes=["st_lr"], sem="m2")
            S.dma("sp", st_li[:], lambda_im, writes=["st_li"], sem="m3")
            S.dma("sp", st_dt[:], log_dt, writes=["st_dt"], sem="m4")
            S.dma("sp", st_d[:], ssm_d, writes=["st_d"], sem="m5")
            for g2 in range(2):
                V_ts(st_dtb[:, g2 * 64:(g2 + 1) * 64], ones[0:32, 0:64], st_dt[:, g2:g2 + 1], ALU.mult,
                     rk=["ones", "st_dt"], wk=["st_dtb"])
            bk = nbank()
            PE_T(psb[bk][:, 0:32], st_lr[:], 32, ["st_lr", "ident"], [pk(bk)], inc=False)
            PE_T(psb[bk][:, 32:64], st_li[:], 32, ["st_li", "ident"], [pk(bk)], inc=False)
            PE_T(psb[bk][:, 64:96], st_dtb[:], 32, ["st_dtb", "ident"], [pk(bk)])
            V_cp(LLD[:], psb[bk][:, 0:96], rk=[pk(bk)], wk=["LLD"])
            bk = nbank()
            PE_T(psb[bk][:, 0:8], st_d[:], 8, ["st_d", "ident"], [pk(bk)])
            V_cp(Dcol[:], psb[bk][:, 0:8], rk=[pk(bk)], wk=["Dcol"])
            for ri in range(2):
                S.dma("sp", st_b[ri][:], (ssm_b_re, ssm_b_im)[ri], writes=["st_b"], sem="stb")
                S.dma("sp", st_c[ri][:], (ssm_c_re, ssm_c_im)[ri], writes=["st_c"], sem="stc")
                V_cp(st_b2[ri][:], st_b[ri][:].rearrange("q (gp h) -> q h gp", h=16), rk=["st_b"], wk=["st_b2"])
                V_cp(st_c2[ri][:].rearrange("q h (g2 p) -> q g2 h p", g2=2),
                     st_c[ri][:].rearrange("q (g2 h p) -> q g2 h p", g2=2, h=16), rk=["st_c"], wk=["st_c2"])
                for (srcs, dst) in ((st_b2[ri], (BRt, BIt)[ri]), (st_c2[ri], (CRt, CIt)[ri])):
                    bk = nbank()
                    for h in range(16):
                        PE_T(psb[bk][:, h * 32:(h + 1) * 32], srcs[:, h, :], 32, [kn(srcs[:]), "ident"], [pk(bk)], inc=(h == 15))
                    V_cp(dst[:].rearrange("p h q -> p (h q)"), psb[bk][:, :], rk=[pk(bk)], wk=[kn(dst[:])])
            LR = LLD[:, 0:32]; LI = LLD[:, 32:64]; LDT = LLD[:, 64:96]
            dtv, lrdt, lidt, mag, cc, sn, t1, t2, t3, cre, cim, den = [w_[:] for w_ in wk_]
            A_act(dtv, LDT, AF.Exp)
            V_tt(lrdt, LR, dtv, ALU.mult)
            V_tt(lidt, LI, dtv, ALU.mult)
            A_act(mag, lrdt, AF.Exp)
            halfpi = sb(es, "halfpi", [P, 1])
            S.op("dve", lambda: nc.vector.memset(halfpi[:], math.pi / 2), writes=["halfpi"])
            A_act(sn, lidt, AF.Sin, scale=1.0 / 32)
            A_act(cc, lidt, AF.Sin, scale=1.0 / 32, bias=halfpi[:, 0:1])
            for _ in range(5):
                V_tt(t1, cc, cc, ALU.mult)
                V_tt(t2, sn, sn, ALU.mult)
                V_tt(t3, cc, sn, ALU.mult)
                V_tt(cc, t1, t2, ALU.subtract)
                V_ts(sn, t3, 2.0, ALU.mult)
            S.op("dve", lambda: nc.vector.memset(APr[:, 0, :], 1.0), writes=["APr"])
            S.op("dve", lambda: nc.vector.memset(APi[:, 0, :], 0.0), writes=["APi"])
            V_tt(APr[:, 1, :], mag, cc, ALU.mult)
            V_tt(APi[:, 1, :], mag, sn, ALU.mult)

            def cmul(o_r, o_i, a_r, a_i, b_r, b_i, tA, tB):
                V_tt(tA, a_r, b_r, ALU.mult)
                V_tt(tB, a_i, b_i, ALU.mult)
                V_tt(o_r, tA, tB, ALU.subtract)
                V_tt(tA, a_r, b_i, ALU.mult)
                V_tt(tB, a_i, b_r, ALU.mult)
                V_tt(o_i, tA, tB, ALU.add)

            for e_ in range(1, 8):
                cmul(APr[:, e_ + 1, :], APi[:, e_ + 1, :], APr[:, e_, :], APi[:, e_, :], APr[:, 1, :], APi[:, 1, :], t1, t2)
            V_cp(AKr[:, 0, :], APr[:, 8, :]); V_cp(AKi[:, 0, :], APi[:, 8, :])
            for k in range(7):
                V_tt(t1, AKr[:, k, :], AKr[:, k, :], ALU.mult)
                V_tt(t2, AKi[:, k, :], AKi[:, k, :], ALU.mult)
                V_tt(t3, AKr[:, k, :], AKi[:, k, :], ALU.mult)
                V_tt(AKr[:, k + 1, :], t1, t2, ALU.subtract)
                V_ts(AKi[:, k + 1, :], t3, 2.0, ALU.mult)
            V_ts(AKn[:], AKi[:], -1.0, ALU.mult)
            V_ts(t1, APr[:, 1, :], -1.0, ALU.add, rk=["APr"])
            V_tt(t2, LR, LR, ALU.mult)
            V_tt(t3, LI, LI, ALU.mult)
            V_tt(den, t2, t3, ALU.add)
            S.op("dve", lambda: nc.vector.reciprocal(out=den, in_=den), reads=[kn(den)], writes=[kn(den)])
            V_tt(t2, t1, LR, ALU.mult)
            V_tt(t3, APi[:, 1, :], LI, ALU.mult)
            V_tt(cre, t2, t3, ALU.add)
            V_tt(cre, cre, den, ALU.mult)
            V_tt(t2, APi[:, 1, :], LR, ALU.mult)
            V_tt(t3, t1, LI, ALU.mult)
            V_tt(cim, t2, t3, ALU.subtract)
            V_tt(cim, cim, den, ALU.mult)
            tb1 = sb(es, "tb1", [P, 16, 32]); tb2 = sb(es, "tb2", [P, 16, 32])
            creb = cre.unsqueeze(1).broadcast_to([P, 16, 32]); cimb = cim.unsqueeze(1).broadcast_to([P, 16, 32])
            cmul(BBr[:], BBi[:], creb, cimb, BRt[:], BIt[:], tb1[:], tb2[:])
            S.barrier()


        uT = sb(es_ssm, "uT", [P, 8, T], BF16)

        def load_w(wbuf, key, srcap, kch, ncols, sem):
            S.dma("pool", wbuf[:, 0:kch, 0:ncols], srcap.rearrange("(c p) f -> p c f", p=P), writes=[key], sem=sem)

        def evac_copy(dst, src_ps, bk, wkeys, e=None):
            e = e or S.alt()
            V_cp(dst, src_ps, rk=[pk(bk)], wk=wkeys, e=e)

        with ExitStack() as es:
            wst = [sb(es, "wst%d" % i, [P, 16, 512], BF16) for i in range(2)]
            for blk in range(2):
                sl = blk % 2
                load_w(wst[sl], "wst%d" % sl, w_in[:, blk * 512:(blk + 1) * 512], 16, 512, "wst%d" % sl)
                for m in range(4):
                    for n in range(4):
                        bk = nbank()
                        for k in range(16):
                            PE_mm(psb[bk][:, :], wst[sl][:, k, m * P:(m + 1) * P], hT[:, k, n * 512:(n + 1) * 512],
                                  k == 0, k == 15, ["wst%d" % sl], [pk(bk)], inc=(k == 15))
                        evac_copy(uT[:, blk * 4 + m, n * 512:(n + 1) * 512], psb[bk][:, :], bk, [("uT", blk * 4 + m, n)])
            S.barrier()
        gate(102)
        tap_bf("uT", uT[:].rearrange("p a t -> p (a t)"), [])

        with ExitStack() as es:
            Xu = sb(es, "Xu", [P, 8, 2, 4, 16])
            XP = sb(es, "XP", [P, 8, 2, 4, 32])
            CAu = sb(es, "CAu", [P, 4, 9, 2, 16])
            tq1 = sb(es, "tq1", [P, 4, 16]); tq2 = sb(es, "tq2", [P, 4, 16])
            WS = [sb(es, "WS%d" % i, [P, 8, 2, P], BF16) for i in range(2)]
            WCp = [sb(es, "WCp%d" % i, [P, 4, 9, 2, 32], BF16) for i in range(2)]
            BPb = [sb(es, "BPb%d" % i, [P, 2, 4, 32], BF16) for i in range(2)]
            BD = [sb(es, "BD%d" % i, [P, 8, P], BF16) for i in range(2)]
            Hb = [[sb(es, "Hb%d%d" % (s_, i), [P, 2, NCK]) for i in range(2)] for s_ in range(2)]
            Hbf = [sb(es, "Hbf%d" % i, [P, 2, NCK], BF16) for i in range(4)]
            y32 = sb(es, "y32", [P, 1024])
            S.op("dve", lambda: nc.vector.memset(XP[:], 0.0), writes=["XP"])
            for i in range(2):
                S.op("pool", lambda: nc.gpsimd.memset(WCp[i][:], 0.0), writes=["WCp%d" % i])
                S.op("pool", lambda: nc.gpsimd.memset(BD[i][:], 0.0), writes=["BD%d" % i])
            XPv = XP[:].rearrange("p i r q (g h) -> p (i r q) g h", g=2)
            for a in range(8):
                par = a % 2
                qs = slice(4 * a, 4 * a + 4)
                for ip in range(8):
                    e_ = 7 - ip
                    arb = APr[:, e_, qs].unsqueeze(2).broadcast_to([P, 4, 16])
                    aib = APi[:, e_, qs].unsqueeze(2).broadcast_to([P, 4, 16])
                    bbr = BBr[:, :, qs].rearrange("p h q -> p q h")
                    bbi = BBi[:, :, qs].rearrange("p h q -> p q h")
                    V_tt(tq1[:], arb, bbr, ALU.mult, rk=[], wk=["tq1"])
                    V_tt(tq2[:], aib, bbi, ALU.mult, rk=[], wk=["tq2"])
                    V_tt(Xu[:, ip, 0, :, :], tq1[:], tq2[:], ALU.subtract, rk=["tq1", "tq2"], wk=["Xu"])
                    V_tt(tq1[:], arb, bbi, ALU.mult, rk=[], wk=["tq1"])
                    V_tt(tq2[:], aib, bbr, ALU.mult, rk=[], wk=["tq2"])
                    V_tt(Xu[:, ip, 1, :, :], tq1[:], tq2[:], ALU.add, rk=["tq1", "tq2"], wk=["Xu"])
                Xuv = Xu[:].rearrange("p i r q h -> p (i r q) h")
                for g2 in range(2):
                    V_cp(XPv[g2 * 64:(g2 + 1) * 64, :, g2, :], Xuv[g2 * 64:(g2 + 1) * 64, :, :], rk=["Xu"], wk=["XP"])
                for e_ in range(9):
                    arb = APr[:, e_, qs].unsqueeze(2).broadcast_to([P, 4, 16])
                    aib = APi[:, e_, qs].unsqueeze(2).broadcast_to([P, 4, 16])
                    crr = CRt[:, :, qs].rearrange("p h q -> p q h")
                    cii = CIt[:, :, qs].rearrange("p h q -> p q h")
                    V_tt(tq1[:], crr, arb, ALU.mult, rk=[], wk=["tq1"])
                    V_tt(tq2[:], cii, aib, ALU.mult, rk=[], wk=["tq2"])
                    V_tt(CAu[:, :, e_, 0, :], tq1[:], tq2[:], ALU.subtract, rk=["tq1", "tq2"], wk=["CAu"])
                    V_tt(tq1[:], cii, arb, ALU.mult, rk=[], wk=["tq1"])
                    V_tt(tq2[:], crr, aib, ALU.mult, rk=[], wk=["tq2"])
                    V_stt(CAu[:, :, e_, 1, :], tq1[:], -1.0, tq2[:], ALU.mult, ALU.subtract, rk=["tq1", "tq2"], wk=["CAu"])
                CAuv = CAu[:].rearrange("p q e r h -> p (q e r) h")
                WCv = WCp[par][:].rearrange("p q e r (g h) -> p (q e r) g h", g=2)
                for g2 in range(2):
                    V_cp(WCv[g2 * 64:(g2 + 1) * 64, :, g2, :], CAuv[g2 * 64:(g2 + 1) * 64, :, :], rk=["CAu"], wk=["WCp%d" % par])
                V_cp(BPb[par][:], XP[:, 7, :, :, :], rk=["XP"], wk=["BPb%d" % par])
                for cb in range(4):
                    bk = nbank(6, 8)
                    for j in range(4):
                        ip, ri = divmod(cb * 4 + j, 2)
                        PE_T(psb[bk][:, j * P:(j + 1) * P], XP[:, ip, ri, :, :].rearrange("p q f -> p (q f)"), P,
                             ["XP", "ident"], [pk(bk)], inc=(j == 3))
                    evac_copy(WS[par][:].rearrange("p i r f -> p (i r f)")[:, cb * 512:(cb + 1) * 512], psb[bk][:, :], bk,
                              ["WS%d" % par])
                bk = nbank(6, 8)
                for qq in range(4):
                    for j in range(8):
                        for ri in range(2):
                            PE_mm(psb[bk][32 * qq:32 * qq + 32, j * 32:(j + 1) * 32], BPb[par][:, ri, qq, :],
                                  WCp[par][:, qq, j, ri, :], ri == 0, ri == 1,
                                  ["BPb%d" % par, "WCp%d" % par], [pk(bk)], inc=(qq == 3 and j == 7 and ri == 1),
                                  tp=(0, 32 * qq), sg=True)
                for qq in range(4):
                    V_cp(BD[par][32 * qq:32 * qq + 32, :, 32 * qq:32 * qq + 32],
                         psb[bk][32 * qq:32 * qq + 32, 0:256].rearrange("p (j f) -> p j f", j=8),
                         rk=[pk(bk)], wk=["BD%d" % par])
                for qq in range(4):
                    q = 4 * a + qq
                    hs = q % 2
                    pb = 32 * qq
                    bk = nbank(4, 6)
                    for ri in range(2):
                        for ip in range(8):
                            PE_mm(psb[bk][:, ri * NCK:(ri + 1) * NCK], WS[par][pb:pb + 32, ip, ri, :],
                                  uT[pb:pb + 32, a, ip::8], ip == 0, ip == 7,
                                  ["WS%d" % par] + [("uT", a, n) for n in range(4)], [pk(bk)],
                                  inc=(ri == 1 and ip == 7), tp=(pb, 0))
                    V_cp(Hb[hs][0][:].rearrange("p r c -> p (r c)"), psb[bk][:, :], rk=[pk(bk)],
                         wk=[("Hb", hs, 0, 0), ("Hb", hs, 0, 1)], e="act")
                    for k in range(8):
                        s_ = 1 << k
                        src_ = Hb[hs][k % 2]; dst_ = Hb[hs][(k + 1) % 2]
                        sp_, dp_ = k % 2, (k + 1) % 2
                        n_ = NCK - s_
                        V_cp(dst_[:, :, 0:s_], src_[:, :, 0:s_], rk=[("Hb", hs, sp_, 0), ("Hb", hs, sp_, 1)],
                             wk=[("Hb", hs, dp_, 0), ("Hb", hs, dp_, 1)], e="pool")
                        akr = AKr[:, k, q:q + 1]; aki = AKi[:, k, q:q + 1]; akn = AKn[:, k, q:q + 1]
                        V_stt(dst_[:, 0, s_:], src_[:, 0, 0:n_], akr, src_[:, 0, s_:], ALU.mult, ALU.add,
                              rk=[("Hb", hs, sp_, 0)], wk=[("Hb", hs, dp_, 0)])
                        V_stt(dst_[:, 1, s_:], src_[:, 1, 0:n_], akr, src_[:, 1, s_:], ALU.mult, ALU.add,
                              rk=[("Hb", hs, sp_, 1)], wk=[("Hb", hs, dp_, 1)])
                        V_stt(dst_[:, 0, s_:], src_[:, 1, 0:n_], akn, dst_[:, 0, s_:], ALU.mult, ALU.add,
                              rk=[("Hb", hs, sp_, 1), ("Hb", hs, dp_, 0)], wk=[("Hb", hs, dp_, 0)])
                        V_stt(dst_[:, 1, s_:], src_[:, 0, 0:n_], aki, dst_[:, 1, s_:], ALU.mult, ALU.add,
                              rk=[("Hb", hs, sp_, 0), ("Hb", hs, dp_, 1)], wk=[("Hb", hs, dp_, 1)])
                    V_cp(Hbf[qq][:], Hb[hs][0][:], rk=[("Hb", hs, 0, 0), ("Hb", hs, 0, 1)], wk=[("Hbf", qq)], e="act")
                for i in range(8):
                    for j in range(i + 1):
                        PE_mm(ps_all[:, i * NCK:(i + 1) * NCK], BD[par][:, j, :], uT[:, a, (i - j)::8],
                              (j == 0 and i % 2 == 0), False,
                              ["BD%d" % par] + [("uT", a, n) for n in range(4)], [pk(i // 2)],
                              inc=(j == i), sg=True)
                for qq in range(4):
                    pb = 32 * qq
                    for i in range(8):
                        for ri in range(2):
                            PE_mm(ps_all[pb:pb + 32, i * NCK + 1:(i + 1) * NCK], WCp[par][:, qq, i + 1, ri, :],
                                  Hbf[qq][:, ri, 0:NCK - 1], False, ri == 1,
                                  ["WCp%d" % par, ("Hbf", qq)], [pk(i // 2)], inc=(ri == 1), tp=(0, pb), sg=True)
                for hf in range(2):
                    Yv = ps_all[:, 0:2048].rearrange("p (i c) -> p c i", i=8)[:, hf * 128:(hf + 1) * 128, :]
                    uv = uT[:, a, hf * 1024:(hf + 1) * 1024].rearrange("p (c i) -> p c i", i=8)
                    V_stt(y32[:].rearrange("p (c i) -> p c i", i=8), uv, Dcol[:, a:a + 1], Yv, ALU.mult, ALU.add,
                          rk=[pk(0), pk(1), pk(2), pk(3)] + [("uT", a, n) for n in range(4)], wk=["y32"])
                    A_act(uT[:, a, hf * 1024:(hf + 1) * 1024], y32[:], AF.Gelu_apprx_tanh, rk=["y32"],
                          wk=[("uT", a, 2 * hf), ("uT", a, 2 * hf + 1)])
            tap_bf("zT", uT[:].rearrange("p a t -> p (a t)"), [("uT", a_, n_) for a_ in range(8) for n_ in range(4)])
            S.barrier()
        with ExitStack() as es:
            wglu = sb(es, "wglu", [P, 8, 1024], BF16)
            load_w(wglu, "wglu", w_glu, 8, 1024, "wglu")
            sg = [sb(es, "sg%d" % i, [P, 512], BF16) for i in range(2)]
            for m in range(8):
                for n in range(4):
                    bk = nbank(4, 8)
                    for k in range(8):
                        PE_mm(psb[bk][:, :], wglu[:, k, m * P:(m + 1) * P], uT[:, k, n * 512:(n + 1) * 512],
                              k == 0, k == 7, ["wglu"] + [("uT", k, n)], [pk(bk)], inc=(k == 7))
                    sl = (m * 4 + n) % 2
                    A_act(sg[sl][:], psb[bk][:, :], AF.Sigmoid, rk=[pk(bk)], wk=["sg%d" % sl])
                    V_tt(s5T[:, m, n * 512:(n + 1) * 512], sg[sl][:], uT[:, m, n * 512:(n + 1) * 512], ALU.mult,
                         rk=["sg%d" % sl, ("uT", m, n)], wk=[("s5T", m, n)])
            S.barrier()
        gate(103)
        tap_bf("s5T", s5T[:].rearrange("p a t -> p (a t)"), [])
        reg = {"APr": APr[:].rearrange("p e q -> p (e q)"), "APi": APi[:].rearrange("p e q -> p (e q)"),
               "AKr": AKr[:].rearrange("p e q -> p (e q)"), "BBr": BBr[:].rearrange("p h q -> p (h q)"),
               "BBi": BBi[:].rearrange("p h q -> p (h q)"), "CRt": CRt[:].rearrange("p h q -> p (h q)"),
               "Dcol": Dcol[:]}
        for nm_, ap_ in reg.items():
            if nm_ in tapd:
                S.dma("pool", tapd[nm_], ap_, sem="tap")
        S.barrier()
        es_ssm.close()


        attT = sb(es_mix, "attT", [P, 8, T], BF16)
        S.op("pool", lambda: nc.gpsimd.affine_select(out=tri[:], in_=ones[:], pattern=[[-1, P]], compare_op=ALU.is_gt,
                                                     fill=0.0, base=0, channel_multiplier=1), reads=["ones"], writes=["tri"])
        S.op("pool", lambda: nc.gpsimd.affine_select(out=lem[:], in_=ones[:], pattern=[[1, P]], compare_op=ALU.is_ge,
                                                     fill=0.0, base=0, channel_multiplier=-1), reads=["ones"], writes=["lem"])
        S.op("pool", lambda: nc.gpsimd.affine_select(out=cmf[:], in_=ones[:], pattern=[[1, P]], compare_op=ALU.is_gt,
                                                     fill=0.0, base=0, channel_multiplier=-1), reads=["ones"], writes=["cmf"])
        V_cp(cmb[:], cmf[:])
        V_cp(trib[:], tri[:])
        V_cp(lemb[:], lem[:])
        S.op("dve", lambda: nc.vector.memset(zer[:], 0.0), writes=["zer"])
        S.dma("sp", gq[:], q_norm_g.rearrange("o d -> d o"), writes=["gq"], sem="m6")
        S.dma("sp", gk[:], k_norm_g.rearrange("o d -> d o"), writes=["gk"], sem="m7")
        V_ts(gq[:], gq[:], 1.0 / math.sqrt(128.0), ALU.mult)
        S.barrier()

        for hg in range(2):
            with ExitStack() as es:
                qT = sb(es, "qT%d" % hg, [P, 4, T], BF16); kT = sb(es, "kT%d" % hg, [P, 4, T], BF16)
                vv = sb(es, "vv%d" % hg, [P, 16, 512], BF16)
                with ExitStack() as es2:
                    wst0 = sb(es2, "wstq0_%d" % hg, [P, 16, 512], BF16)
                    wst = [wst0, wst0]
                    sqf = [sb(es2, "sqf%d_%d" % (i, hg), [P, 512]) for i in range(2)]
                    rsq = [sb(es2, "rsq%d_%d" % (i, hg), [P, 512]) for i in range(2)]
                    cnt_ = 0
                    for which, col0 in (("q", 1024 + 512 * hg), ("k", 2048 + 512 * hg), ("v", 3072 + 512 * hg)):
                        sl = 0
                        load_w(wst[sl], "wstq%d" % sl, w_in[:, col0:col0 + 512], 16, 512, "wstq%d" % sl)
                        if which == "v":
                            for tt in range(16):
                                bk = nbank(0, 4)
                                for k in range(16):
                                    PE_mm(psb[bk][:, :], hT[:, k, tt * P:(tt + 1) * P], wst[sl][:, k, :], k == 0, k == 15,
                                          ["wstq%d" % sl], [pk(bk)], inc=(k == 15))
                                evac_copy(vv[:, tt, :], psb[bk][:, :], bk, [("vv", tt)])
                            continue
                        dstT = qT if which == "q" else kT
                        gcol = gq if which == "q" else gk
                        for m in range(4):
                            for n in range(4):
                                bk = nbank(0, 4)
                                for k in range(16):
                                    PE_mm(psb[bk][:, :], wst[sl][:, k, m * P:(m + 1) * P], hT[:, k, n * 512:(n + 1) * 512],
                                          k == 0, k == 15, ["wstq%d" % sl], [pk(bk)], inc=(k == 15))
                                s2 = (m * 4 + n) % 2
                                A_act(sqf[s2][:], psb[bk][:, :], AF.Square, rk=[pk(bk)], wk=["sqf%d" % s2])
                                b2 = nbank(4, 8)
                                PE_mm(psb[b2][:, :], ones[:], sqf[s2][:], True, True, ["ones", "sqf%d" % s2], [pk(b2)])
                                A_act(rsq[s2][:], psb[b2][:, :], AF.Sqrt, scale=1.0 / 128, bias=epsc[:, 0:1],
                                      rk=[pk(b2)], wk=["rsq%d" % s2])
                                S.op("dve", lambda: nc.vector.reciprocal(out=rsq[s2][:], in_=rsq[s2][:]),
                                     reads=["rsq%d" % s2], writes=["rsq%d" % s2])
                                V_stt(dstT[:, m, n * 512:(n + 1) * 512], psb[bk][:, :], gcol[:, 0:1], rsq[s2][:],
                                      ALU.mult, ALU.mult, rk=[pk(bk), "rsq%d" % s2], wk=[(which, m, n)])
                    S.barrier()
                if hg == 0:
                    tap_bf("qT", qT[:].rearrange("p a t -> p (a t)"), [])
                    tap_bf("kT", kT[:].rearrange("p a t -> p (a t)"), [])
                    tap_bf("vv", vv[:].rearrange("p a t -> p (a t)"), [])
                with ExitStack() as es2:
                    SPb = [sb(es2, "SPb%d_%d" % (i, hg), [P, 1024]) for i in range(1)]
                    SPh = [sb(es2, "SPh%d_%d" % (i, hg), [P, 1024], BF16) for i in range(2)]
                    Ab = sb(es2, "Ab%d" % hg, [P, 1024])
                    Wb_ = [sb(es2, "Wb%d_%d" % (i, hg), [P, 1024], BF16) for i in range(2)]
                    ZB = [ps_all[:, 0:1024], ps_all[:, 1024:2048]]
                    TB = ps_all[:, 2048:3072]
                    OB = ps_all[:, 3072:4096]

                    def bank_ranges(lo):
                        rs_ = []
                        for bh in range(2):
                            a_ = max(lo, 512 * bh); b_ = 512 * (bh + 1)
                            if a_ < b_:
                                rs_.append((bh, a_, b_))
                        return rs_

                    for hl in range(4):
                        h = 4 * hg + hl
                        for qh in range(2):
                            kbs = list(range(8 * qh + 7, -1, -1))
                            N_ = len(kbs)
                            for bh in range(2):
                                PE_mm(TB[:, bh * 512:(bh + 1) * 512], zer[:, 0:P], zer[:, :], True, True, ["zer"], [pk(4 + bh)], inc=False, sg=True)
                                PE_mm(OB[:, bh * 512:(bh + 1) * 512], zer[:, 0:P], zer[:, :], True, True, ["zer"], [pk(6 + bh)], inc=(bh == 1), sg=True)

                            def lo_of(n):
                                return max(0, kbs[n] * P - qh * 1024)

                            def diag(n):
                                return kbs[n] * P >= qh * 1024

                            def S1(n):
                                kb = kbs[n]; lo = lo_of(n); zb = n % 2
                                for (bh, a_, b_) in bank_ranges(lo):
                                    PE_mm(ZB[zb][:, a_:b_], kT[:, hl, kb * P:(kb + 1) * P], qT[:, hl, qh * 1024 + a_: qh * 1024 + b_],
                                          True, True, [], [pk(2 * zb + bh)])
                                zk = [pk(2 * zb), pk(2 * zb + 1)]
                                A_act(SPb[0][:, lo:], ZB[zb][:, lo:], AF.Exp, rk=zk, wk=["SPb0"])
                                A_act(SPh[zb][:, lo:], SPb[0][:, lo:], AF.Ln, bias=ones[:, 0:1], rk=["SPb0"], wk=["SPh%d" % zb])
                                if diag(n):
                                    V_tt(SPh[zb][:, lo:lo + P], SPh[zb][:, lo:lo + P], cmb[:], ALU.mult,
                                         rk=["SPh%d" % zb], wk=["SPh%d" % zb])

                            def S2(n):
                                lo = lo_of(n); zb = n % 2
                                for (bh, a_, b_) in bank_ranges(lo):
                                    PE_mm(TB[:, a_:b_], trib[:], SPh[zb][:, a_:b_], False, False, ["SPh%d" % zb], [pk(4 + bh)], sg=True)

                            def S3a(n):
                                lo = lo_of(n); zb = n % 2
                                zk = [pk(2 * zb), pk(2 * zb + 1)]
                                V_tt(Ab[:, lo:], ZB[zb][:, lo:], SPh[zb][:, lo:], ALU.subtract, rk=zk + ["SPh%d" % zb], wk=["Ab"])
                                V_tt(Ab[:, lo:], Ab[:, lo:], TB[:, lo:], ALU.subtract, rk=["Ab", pk(4), pk(5)], wk=["Ab"])
                                A_act(Wb_[zb][:, lo:], Ab[:, lo:], AF.Exp, rk=["Ab"], wk=["Wb%d" % zb])

                            def S3b(n):
                                lo = lo_of(n); zb = n % 2
                                if diag(n):
                                    V_tt(Wb_[zb][:, lo:lo + P], Wb_[zb][:, lo:lo + P], cmb[:], ALU.mult,
                                         rk=["Wb%d" % zb], wk=["Wb%d" % zb])

                            def S4a(n):
                                lo = lo_of(n); zb = n % 2
                                for (bh, a_, b_) in bank_ranges(lo):
                                    PE_mm(TB[:, a_:b_], lemb[:], SPh[zb][:, a_:b_], False, False, ["SPh%d" % zb], [pk(4 + bh)], sg=True)

                            def S4b(n):
                                kb = kbs[n]; lo = lo_of(n); zb = n % 2
                                for (bh, a_, b_) in bank_ranges(lo):
                                    PE_mm(OB[:, a_:b_], vv[:, kb, hl * P:(hl + 1) * P], Wb_[zb][:, a_:b_], False, n == N_ - 1,
                                          ["Wb%d" % zb], [pk(6 + bh)], sg=True)

                            S1(0)
                            if N_ > 1:
                                S1(1)
                            S2(0)
                            for n in range(N_):
                                S3a(n)
                                S4a(n)
                                if n + 2 < N_:
                                    S1(n + 2)
                                S3b(n)
                                if n + 1 < N_:
                                    S2(n + 1)
                                S4b(n)
                            V_cp(attT[:, h, qh * 1024:(qh + 1) * 1024], OB[:, :], rk=[pk(6), pk(7)], wk=[("attT", h, qh)], e="act")
                    S.barrier()
        gate(104)
        tap_bf("attT", attT[:].rearrange("p a t -> p (a t)"), [])


        for th in range(2):
            with ExitStack() as es:
                mT = sb(es, "mT%d" % th, [P, 16, 1024], BF16)
                with ExitStack() as es2:
                    wbs = [sb(es2, "wbs%d_%d" % (i, th), [P, 8, P], BF16) for i in range(2)]
                    wba = [sb(es2, "wba%d_%d" % (i, th), [P, 8, P], BF16) for i in range(2)]
                    wgs = [sb(es2, "wgs%d_%d" % (i, th), [P, 16, P], BF16) for i in range(2)]
                    wga = [sb(es2, "wga%d_%d" % (i, th), [P, 16, P], BF16) for i in range(2)]
                    sgs = [sb(es2, "sgs%d_%d" % (i, th), [P, 512]) for i in range(2)]
                    sga = [sb(es2, "sga%d_%d" % (i, th), [P, 512]) for i in range(2)]
                    for m in range(16):
                        sl = m % 2
                        cs = slice(m * P, (m + 1) * P)
                        load_w(wbs[sl], "wbs%d" % sl, w_branch_ssm[:, cs], 8, P, "wbs%d" % sl)
                        load_w(wba[sl], "wba%d" % sl, w_branch_att[:, cs], 8, P, "wba%d" % sl)
                        load_w(wgs[sl], "wgs%d" % sl, w_in[:, 4096 + m * P:4096 + (m + 1) * P], 16, P, "wgs%d" % sl)
                        load_w(wga[sl], "wga%d" % sl, w_in[:, 6144 + m * P:6144 + (m + 1) * P], 16, P, "wga%d" % sl)
                        for n in range(2):
                            ts_ = slice(th * 1024 + n * 512, th * 1024 + (n + 1) * 512)
                            b_bs, b_gs, b_ba, b_ga = nbank(), nbank(), nbank(), nbank()
                            for k in range(8):
                                PE_mm(psb[b_bs][:, :], wbs[sl][:, k, :], s5T[:, k, ts_], k == 0, k == 7, ["wbs%d" % sl], [pk(b_bs)], inc=(k == 7))
                            for k in range(16):
                                PE_mm(psb[b_gs][:, :], wgs[sl][:, k, :], hT[:, k, ts_], k == 0, k == 15, ["wgs%d" % sl], [pk(b_gs)], inc=(k == 15))
                            for k in range(8):
                                PE_mm(psb[b_ba][:, :], wba[sl][:, k, :], attT[:, k, ts_], k == 0, k == 7, ["wba%d" % sl], [pk(b_ba)], inc=(k == 7))
                            for k in range(16):
                                PE_mm(psb[b_ga][:, :], wga[sl][:, k, :], hT[:, k, ts_], k == 0, k == 15, ["wga%d" % sl], [pk(b_ga)], inc=(k == 15))
                            s2 = n
                            A_act(sgs[s2][:], psb[b_gs][:, :], AF.Sigmoid, rk=[pk(b_gs)], wk=["sgs%d" % s2])
                            A_act(sga[s2][:], psb[b_ga][:, :], AF.Sigmoid, rk=[pk(b_ga)], wk=["sga%d" % s2])
                            V_tt(sgs[s2][:], sgs[s2][:], psb[b_bs][:, :], ALU.mult, rk=["sgs%d" % s2, pk(b_bs)], wk=["sgs%d" % s2])
                            V_tt(sga[s2][:], sga[s2][:], psb[b_ba][:, :], ALU.mult, rk=["sga%d" % s2, pk(b_ba)], wk=["sga%d" % s2])
                            V_tt(mT[:, m, n * 512:(n + 1) * 512], sgs[s2][:], sga[s2][:], ALU.add,
                                 rk=["sgs%d" % s2, "sga%d" % s2], wk=[("mT", m, n)])
                    S.barrier()
                with ExitStack() as es2:
                    wo = [sb(es2, "wo%d_%d" % (i, th), [P, 16, 512], BF16) for i in range(2)]
                    xin = [sb(es2, "xin%d_%d" % (i, th), [P, 512]) for i in range(2)]
                    xo = [sb(es2, "xo%d_%d" % (i, th), [P, 512]) for i in range(2)]
                    cnt_ = 0
                    for db in range(4):
                        sl = db % 2
                        ds_ = slice(db * 512, (db + 1) * 512)
                        load_w(wo[sl], "wo%d" % sl, w_out[:, ds_], 16, 512, "wo%d" % sl)
                        for tt in range(8):
                            r0 = th * 1024 + tt * P
                            s2 = cnt_ % 2
                            cnt_ += 1
                            S.dma("sp", xin[s2][:], x[r0:r0 + P, ds_], writes=["xin%d" % s2], sem="xin%d" % s2)
                            bk = nbank()
                            for k in range(16):
                                PE_mm(psb[bk][:, :], mT[:, k, tt * P:(tt + 1) * P], wo[sl][:, k, :], k == 0, k == 15,
                                      ["wo%d" % sl], [pk(bk)], inc=(k == 15))
                            V_tt(xo[s2][:], xin[s2][:], psb[bk][:, :], ALU.add, rk=["xin%d" % s2, pk(bk)], wk=["xo%d" % s2])
                            S.dma("sp", x2d[r0:r0 + P, ds_], xo[s2][:], reads=["xo%d" % s2], sem="xo%d" % s2)
                    S.barrier()
        gate(105)
        es_mix.close()
        if "x2" in tapd:
            S.dma("sp", tapd["x2"], x2d, sem="tap")
            S.barrier()

        if upto == "F":
            S.barrier()
            return nc
        TS = 256
        NT = 48
        NS = NT * TS
        h2d = nc.dram_tensor("h2_scratch", [T, D], F32, kind="Internal").ap()
        yd = nc.dram_tensor("y_scratch", [NS, D], F32, kind="Internal").ap()
        stok = nc.dram_tensor("slot_tok", [NS, 16], I32, kind="Internal").ap()
        S.dma("sp", g2s[:], ffn_norm_g, writes=["g2s"], sem="m_g2s")
        bk = nbank()
        PE_T(psb[bk][:, 0:16], g2s[:], 16, ["g2s", "ident"], [pk(bk)])
        V_cp(g2T[:], psb[bk][:, 0:16], rk=[pk(bk)], wk=["g2T"])
        with ExitStack() as es:
            wr = sb(es, "wr", [P, 16, 36])
            rb = sb(es, "rb", [P, 36])
            comb_g1 = sb(es, "comb_g1", [P, 16]); comb_g2 = sb(es, "comb_g2", [P, 16])
            oh1a = sb(es, "oh1a", [P, 16, 32]); oh2a = sb(es, "oh2a", [P, 16, 32])
            selb = sb(es, "selb", [P, 16, 32], BF16)
            s1i = sb(es, "s1i", [P, 16], I32); s2i = sb(es, "s2i", [P, 16], I32)
            widx = sb(es, "widx", [P, NT], I32)
            yix = sb(es, "yix", [P, NT * 2], I32)
            onesb = sb(es, "onesb", [P, P], BF16)
            V_cp(onesb[:], ones[:])
            with ExitStack() as es1:
                wlg = sb(es1, "wlg", [16, P * 4]); wle = sb(es1, "wle", [16, P * 32])
                S.dma("sp", wlg[:], router_group_w.rearrange("(c p) f -> c (p f)", p=P), writes=["wlg"], sem="m_wlg")
                S.dma("sp", wle[:], router_expert_w.rearrange("(c p) f -> c (p f)", p=P), writes=["wle"], sem="m_wle")
                wlg2 = sb(es1, "wlg2", [16, 4, P]); wle2 = sb(es1, "wle2", [16, 32, P])
                V_cp(wlg2[:], wlg[:].rearrange("c (p f) -> c f p", f=4), rk=["wlg"], wk=["wlg2"])
                V_cp(wle2[:], wle[:].rearrange("c (p f) -> c f p", f=32), rk=["wle"], wk=["wle2"])
                bk = nbank()
                for f in range(4):
                    PE_T(psb[bk][:, f * 16:(f + 1) * 16], wlg2[:, f, :], 16, ["wlg2", "ident"], [pk(bk)], inc=(f == 3))
                V_cp(wr[:, :, 0:4].rearrange("p c f -> p f c"), psb[bk][:, 0:64].rearrange("p (f c) -> p f c", c=16), rk=[pk(bk)], wk=["wr"])
                bk = nbank()
                for f in range(32):
                    PE_T(psb[bk][:, f * 16:(f + 1) * 16], wle2[:, f, :], 16, ["wle2", "ident"], [pk(bk)], inc=(f == 31))
                V_cp(wr[:, :, 4:36].rearrange("p c f -> p f c"), psb[bk][:, 0:512].rearrange("p (f c) -> p f c", c=16), rk=[pk(bk)], wk=["wr"])
                S.barrier()
            S.dma("sp", rb[:, 0:4], router_group_b.to_broadcast([P, 4]), writes=["rb"], sem="m_rb0")
            S.dma("sp", rb[:, 4:36], router_expert_b.to_broadcast([P, 32]), writes=["rb"], sem="m_rb1")
            with ExitStack() as es3:
                gb = sb(es3, "gb", [P, D])
                S.dma("sp", gb[:], ffn_norm_g_row.to_broadcast([P, D]), writes=["gb"], sem="m_gb")
                xt2 = [sb(es3, "xt2_%d" % i, [P, D]) for i in range(2)]
                xn = [sb(es3, "xn_%d" % i, [P, D]) for i in range(2)]
                h2r = [sb(es3, "h2r_%d" % i, [P, D]) for i in range(2)]
                junk = sb(es3, "junk2", [P, D], BF16)
                h32 = sb(es3, "h32", [P, 16, P])
                ss2 = sb(es3, "ss2", [P, 16]); rs2 = sb(es3, "rs2", [P, 16])
                lgA = sb(es3, "lgA", [P, 16, 36])
                gmaxA = sb(es3, "gmaxA", [P, 16]); gexA = sb(es3, "gexA", [P, 16, 4]); gmA = sb(es3, "gmA", [P, 16, 4])
                gsumA = sb(es3, "gsumA", [P, 16]); mlA = sb(es3, "mlA", [P, 16, 32]); ml2A = sb(es3, "ml2A", [P, 16, 32])
                m1A = sb(es3, "m1A", [P, 16]); m2A = sb(es3, "m2A", [P, 16])
                sm = [sb(es3, "sm%d" % i, [P, 1]) for i in range(8)]
                ml = sb(es3, "ml", [P, 32]); ml2 = sb(es3, "ml2", [P, 32])
                gm = sb(es3, "gm", [P, 4]); gex = sb(es3, "gex", [P, 4])
                for tt in range(16):
                    r0 = tt * P
                    sl = tt % 2
                    xk = "xt2_%d" % sl
                    S.dma("sp", xt2[sl][:], x2d[r0:r0 + P, :], writes=[xk], sem="xt2_%d" % sl)
                    A_act(junk[:], xt2[sl][:], AF.Square, rk=[xk], wk=["junk2"], accum_out=ss2[:, tt:tt + 1])
                    A_act(rs2[:, tt:tt + 1], ss2[:, tt:tt + 1], AF.Sqrt, scale=1.0 / D, bias=epsc[:, 0:1],
                          rk=["junk2"], wk=[("rs2", tt)])
                    S.op("dve", lambda: nc.vector.reciprocal(out=rs2[:, tt:tt + 1], in_=rs2[:, tt:tt + 1]),
                         reads=[("rs2", tt)], writes=[("rs2", tt)])
                    V_ts(xn[sl][:], xt2[sl][:], rs2[:, tt:tt + 1], ALU.mult, rk=[xk, ("rs2", tt)], wk=["xn%d" % sl])
                    V_tt(h2r[sl][:], xn[sl][:], gb[:], ALU.mult, rk=["xn%d" % sl, "gb"], wk=["h2r%d" % sl], e="pool")
                    S.dma("sp", h2d[r0:r0 + P, :], h2r[sl][:], reads=["h2r%d" % sl], sem="h2w%d" % sl)
                    for cb in range(4):
                        bk = nbank()
                        for j in range(4):
                            c = cb * 4 + j
                            PE_T(psb[bk][:, j * P:(j + 1) * P], xn[sl][:, c * P:(c + 1) * P], P, ["xn%d" % sl, "ident"], [pk(bk)], inc=(j == 3))
                        e_ = S.alt()
                        for j in range(4):
                            c = cb * 4 + j
                            if e_ == "dve":
                                V_ts(h32[:, c, :], psb[bk][:, j * P:(j + 1) * P], g2T[:, c:c + 1], ALU.mult,
                                     rk=[pk(bk)], wk=[("h32", c)])
                            else:
                                S.op("act", lambda: nc.scalar.activation(out=h32[:, c, :], in_=psb[bk][:, j * P:(j + 1) * P],
                                                                         func=AF.Copy, scale=g2T[:, c:c + 1]),
                                     reads=[pk(bk)], writes=[("h32", c)])
                    bk = nbank()
                    for c in range(16):
                        PE_mm(psb[bk][:, 0:36], h32[:, c, :], wr[:, c, :], c == 0, c == 15,
                              [("h32", c), "wr"], [pk(bk)], inc=(c == 15))
                    V_tt(lgA[:, tt, :], psb[bk][:, 0:36], rb[:], ALU.add, rk=[pk(bk), "rb"], wk=[("lgA", tt)])
                lk = [("lgA", t_) for t_ in range(16)]
                AX = mybir.AxisListType.X
                gl = lgA[:, :, 0:4]
                el4 = lgA[:, :, 4:36].rearrange("p t (g e) -> p t g e", g=4)
                S.op("dve", lambda: nc.vector.tensor_reduce(out=gmaxA[:], in_=gl, axis=AX, op=ALU.max), reads=lk, writes=["gmaxA"])
                V_tt(gexA[:], gl, gmaxA[:].unsqueeze(2).broadcast_to([P, 16, 4]), ALU.subtract, rk=lk + ["gmaxA"], wk=["gexA"])
                V_ts(gmA[:], gexA[:], 0.0, ALU.is_ge, s2=-1.0, op1=ALU.add, rk=["gexA"], wk=["gmA"])
                V_ts(gmA[:], gmA[:], 1e30, ALU.mult, rk=["gmA"], wk=["gmA"])
                A_act(gexA[:], gexA[:], AF.Exp, rk=["gexA"], wk=["gexA"])
                S.op("dve", lambda: nc.vector.tensor_reduce(out=gsumA[:], in_=gexA[:], axis=AX, op=ALU.add), reads=["gexA"], writes=["gsumA"])
                S.op("dve", lambda: nc.vector.reciprocal(out=gsumA[:], in_=gsumA[:]), reads=["gsumA"], writes=["gsumA"])
                V_tt(mlA[:].rearrange("p t (g e) -> p t g e", g=4), el4, gmA[:].unsqueeze(3).broadcast_to([P, 16, 4, 8]), ALU.add,
                     rk=lk + ["gmA"], wk=["mlA"])
                S.op("dve", lambda: nc.vector.tensor_reduce(out=m1A[:], in_=mlA[:], axis=AX, op=ALU.max), reads=["mlA"], writes=["m1A"])
                V_tt(oh1a[:], mlA[:], m1A[:].unsqueeze(2).broadcast_to([P, 16, 32]), ALU.is_equal, rk=["mlA", "m1A"], wk=["oh1a"])
                V_stt(ml2A[:], oh1a[:], -1e30, mlA[:], ALU.mult, ALU.add, rk=["oh1a", "mlA"], wk=["ml2A"])
                S.op("dve", lambda: nc.vector.tensor_reduce(out=m2A[:], in_=ml2A[:], axis=AX, op=ALU.max), reads=["ml2A"], writes=["m2A"])
                V_tt(oh2a[:], ml2A[:], m2A[:].unsqueeze(2).broadcast_to([P, 16, 32]), ALU.is_equal, rk=["ml2A", "m2A"], wk=["oh2a"])
                V_tt(m1A[:], m1A[:], m2A[:], ALU.subtract, rk=["m1A", "m2A"], wk=["m1A"])
                A_act(m1A[:], m1A[:], AF.Sigmoid, rk=["m1A"], wk=["m1A"])
                V_tt(comb_g1[:], gsumA[:], m1A[:], ALU.mult, rk=["gsumA", "m1A"], wk=["comb_g1"])
                V_tt(comb_g2[:], gsumA[:], comb_g1[:], ALU.subtract, rk=["gsumA", "comb_g1"], wk=["comb_g2"])
                V_tt(selb[:], oh1a[:], oh2a[:], ALU.add, rk=["oh1a", "oh2a"], wk=[("selb", t_) for t_ in range(16)])
                S.barrier()
            if "g1" in tapd:
                S.dma("sp", tapd["g1"], comb_g1[:], sem="tap"); S.dma("sp", tapd["oh1"], oh1a[:].rearrange("p t e -> p (t e)"), sem="tap")
                S.barrier()
            gate(106)
            with ExitStack() as es3:
                cntx = sb(es3, "cntx", [P, 16, 32]); tot = sb(es3, "tot", [P, 32])
                ci = sb(es3, "ci", [P, 32], I32); pad = sb(es3, "pad", [P, 32]); incl = sb(es3, "incl", [P, 32])
                base = sb(es3, "base", [P, 32]); zz = sb(es3, "zz", [P, 32])
                slot = sb(es3, "slot", [P, 16, 32]); tmp3 = sb(es3, "tmp3", [P, 16, 32])
                s1f = sb(es3, "s1f", [P, 16]); s2f = sb(es3, "s2f", [P, 16])
                jv = sb(es3, "jv", [P, NT]); pidf = sb(es3, "pidf", [P, 1])
                cmp3 = sb(es3, "cmp3", [P, NT, 32]); ejf = sb(es3, "ejf", [P, NT])
                tid = sb(es3, "tid", [P, 16, 16], I32)
                zi = sb(es3, "zi", [P, NS * 16 // P], I32)
                bk = nbank(); bk2 = nbank()
                for tt in range(16):
                    for t2 in range(tt + 1):
                        PE_mm(psb[bk][:, tt * 32:(tt + 1) * 32], cmb[:] if t2 == tt else onesb[:], selb[:, t2, :],
                              t2 == 0, t2 == tt, [("selb", t2)], [pk(bk)], inc=(t2 == tt), sg=True)
                for tt in range(16):
                    PE_mm(psb[bk2][:, 0:32], onesb[:], selb[:, tt, :], tt == 0, tt == 15, [("selb", tt)], [pk(bk2)], inc=(tt == 15))
                V_cp(cntx[:].rearrange("p t e -> p (t e)"), psb[bk][:, :], rk=[pk(bk)], wk=["cntx"])
                V_cp(tot[:], psb[bk2][:, 0:32], rk=[pk(bk2)], wk=["tot"])
                S.op("dve", lambda: nc.vector.memset(zz[:], 0.0), writes=["zz"])
                V_ts(ci[:], tot[:], float(TS - 1), ALU.add)
                S.op("dve", lambda: nc.vector.tensor_scalar(out=ci[:], in0=ci[:], scalar1=8, scalar2=8,
                                                            op0=ALU.arith_shift_right, op1=ALU.logical_shift_left),
                     reads=["ci"], writes=["ci"])
                V_cp(pad[:], ci[:])
                S.op("dve", lambda: nc.vector.tensor_tensor_scan(out=incl[:], data0=pad[:], data1=zz[:], initial=0.0,
                                                                 op0=ALU.add, op1=ALU.add),
                     reads=["pad", "zz"], writes=["incl"])
                V_tt(base[:], incl[:], pad[:], ALU.subtract)
                V_tt(slot[:], cntx[:], base[:].unsqueeze(1).broadcast_to([P, 16, 32]), ALU.add, rk=["cntx", "base"], wk=["slot"])
                V_tt(tmp3[:], slot[:], oh1a[:], ALU.mult, rk=["slot"], wk=["tmp3"])
                S.op("dve", lambda: nc.vector.tensor_reduce(out=s1f[:], in_=tmp3[:], axis=mybir.AxisListType.X, op=ALU.add),
                     reads=["tmp3"], writes=["s1f"])
                V_cp(s1i[:], s1f[:])
                V_tt(tmp3[:], slot[:], oh2a[:], ALU.mult, rk=["slot", "s1f"], wk=["tmp3"])
                S.op("dve", lambda: nc.vector.tensor_reduce(out=s2f[:], in_=tmp3[:], axis=mybir.AxisListType.X, op=ALU.add),
                     reads=["tmp3"], writes=["s2f"])
                V_cp(s2i[:], s2f[:])
                S.op("pool", lambda: nc.gpsimd.iota(jv[:], pattern=[[TS, NT]], base=0, channel_multiplier=0,
                                                    allow_small_or_imprecise_dtypes=True), writes=["jv"])
                S.op("pool", lambda: nc.gpsimd.iota(pidf[:], pattern=[[0, 1]], base=0, channel_multiplier=1,
                                                    allow_small_or_imprecise_dtypes=True), writes=["pidf"])
                V_tt(cmp3[:], incl[:].unsqueeze(1).broadcast_to([P, NT, 32]), jv[:].unsqueeze(2).broadcast_to([P, NT, 32]),
                     ALU.is_le, rk=["incl", "jv"], wk=["cmp3"])
                S.op("dve", lambda: nc.vector.tensor_reduce(out=ejf[:], in_=cmp3[:], axis=mybir.AxisListType.X, op=ALU.add),
                     reads=["cmp3"], writes=["ejf"])
                emp = sb(es3, "emp", [P, NT])
                V_ts(emp[:], ejf[:], 32.0, ALU.is_ge, s2=65536.0, op1=ALU.mult, rk=["ejf"], wk=["emp"])
                V_ts(ejf[:], ejf[:], 31.0, ALU.min, s2=128.0, op1=ALU.mult)
                V_ts(ejf[:], ejf[:], pidf[:, 0:1], ALU.add)
                V_tt(ejf[:], ejf[:], emp[:], ALU.add)
                V_cp(widx[:], ejf[:])
                rowf = sb(es3, "rowf", [P, NT * 2]); endv = sb(es3, "endv", [P, 32])
                cAB = sb(es3, "cAB", [P, NT * 2, 32]); nA = sb(es3, "nA", [P, NT * 2]); nB = sb(es3, "nB", [P, NT * 2])
                S.op("pool", lambda: nc.gpsimd.iota(rowf[:], pattern=[[P, NT * 2]], base=0, channel_multiplier=1,
                                                    allow_small_or_imprecise_dtypes=True), writes=["rowf"])
                V_tt(endv[:], base[:], tot[:], ALU.add, rk=["base", "tot"], wk=["endv"])
                V_tt(cAB[:], base[:].unsqueeze(1).broadcast_to([P, NT * 2, 32]), rowf[:].unsqueeze(2).broadcast_to([P, NT * 2, 32]),
                     ALU.is_le, rk=["base", "rowf"], wk=["cAB"])
                S.op("dve", lambda: nc.vector.tensor_reduce(out=nA[:], in_=cAB[:], axis=mybir.AxisListType.X, op=ALU.add),
                     reads=["cAB"], writes=["nA"])
                V_tt(cAB[:], endv[:].unsqueeze(1).broadcast_to([P, NT * 2, 32]), rowf[:].unsqueeze(2).broadcast_to([P, NT * 2, 32]),
                     ALU.is_le, rk=["endv", "rowf", "nA"], wk=["cAB"])
                S.op("dve", lambda: nc.vector.tensor_reduce(out=nB[:], in_=cAB[:], axis=mybir.AxisListType.X, op=ALU.add),
                     reads=["cAB"], writes=["nB"])
                V_tt(nA[:], nA[:], nB[:], ALU.subtract, rk=["nA", "nB"], wk=["nA"])
                V_ts(nA[:], nA[:], -65536.0, ALU.mult, s2=65536.0, op1=ALU.add, rk=["nA"], wk=["nA"])
                V_tt(nA[:], nA[:], rowf[:], ALU.add, rk=["nA", "rowf"], wk=["nA"])
                V_cp(yix[:], nA[:], rk=["nA"], wk=["yix"])
                S.op("pool", lambda: nc.gpsimd.iota(tid[:], pattern=[[P, 16], [0, 16]], base=0, channel_multiplier=1), writes=["tid"])
                S.op("dve", lambda: nc.vector.memset(zi[:], 4096), writes=["zi"])
                S.dma("sp", stok.rearrange("(p a) f -> p (a f)", p=P), zi[:], reads=["zi"], writes=["stok"], sem="stz")
                for tt in range(16):
                    for (sx, nm_) in ((s1i, "s1i"), (s2i, "s2i")):
                        S.idma(stok, sx[:, tt:tt + 1], tid[:, tt, :], None, reads=[nm_, "tid", "stok"], writes=[("stokw", tt, nm_)], sem="scat")
                S.barrier()
            if "s1i" in tapd:
                S.dma("pool", tapd["s1i"], s1i[:], sem="tap"); S.dma("pool", tapd["widx"], widx[:], sem="tap")
                S.barrier()
            gate(107)
            with ExitStack() as es3:
                tix = [sb(es3, "tix%d" % i, [P, 2], I32) for i in range(2)]
                xg = [sb(es3, "xg%d" % i, [P, 2, D]) for i in range(2)]
                xT = [sb(es3, "xT%d" % i, [P, 16, TS], BF16) for i in range(2)]
                wgb = [sb(es3, "wgb%d" % i, [P, 16, DFF], BF16) for i in range(2)]
                wub = [sb(es3, "wub%d" % i, [P, 16, DFF], BF16) for i in range(2)]
                wdb = [sb(es3, "wdb%d" % i, [P, 4, D], BF16) for i in range(2)]
                hidb = [sb(es3, "hidb%d" % i, [P, 4, TS], BF16) for i in range(2)]
                sgm = [sb(es3, "sgm%d" % i, [P, DFF]) for i in range(2)]
                ysb = [sb(es3, "ysb%d" % i, [P, D]) for i in range(2)]
                for i_ in range(2):
                    S.op("dve", lambda: nc.vector.memset(xg[i_][:], 0.0), writes=[("xg", i_, 0), ("xg", i_, 1)])
                wgv = expert_w_gate.rearrange("e (p c) f -> (e p) (c f)", p=P)
                wuv = expert_w_up.rearrange("e (p c) f -> (e p) (c f)", p=P)
                wdv = expert_w_down.rearrange("e (p c) d -> (e p) (c d)", p=P)
                ycnt_ = [0]

                def Lx(j, sl):
                    for h in range(2):
                        S.dma("sp", tix[sl][:, h:h + 1], stok[j * TS + h * P:j * TS + (h + 1) * P, 0:1],
                              writes=["tix%d" % sl], sem="tix%d" % sl, allow_slow_non_contiguous=True)
                    for h in range(2):
                        S.idma(xg[sl][:, h, :], None, h2d, tix[sl][:, h:h + 1], reads=["tix%d" % sl], writes=[("xg", sl, h)], sem="xg%d_%d" % (sl, h), bounds=T - 1)

                def Lwg(j, sl):
                    S.idma(wgb[sl][:].rearrange("p c f -> p (c f)"), None, wgv, widx[:, j:j + 1], reads=[], writes=["wgb%d" % sl], sem="wgb%d" % sl, bounds=NE * P - 1)

                def Lwu(j, sl):
                    S.idma(wub[sl][:].rearrange("p c f -> p (c f)"), None, wuv, widx[:, j:j + 1], reads=[], writes=["wub%d" % sl], sem="wub%d" % sl, bounds=NE * P - 1)

                def Lwd(j, sl):
                    S.idma(wdb[sl][:].rearrange("p c f -> p (c f)"), None, wdv, widx[:, j:j + 1], reads=[], writes=["wdb%d" % sl], sem="wdb%d" % sl, bounds=NE * P - 1)

                def Lt(j, sl):
                    Lx(j, sl); Lwg(j, sl); Lwu(j, sl); Lwd(j, sl)

                def Ct(j, sl, nxt=None):
                    for h in range(2):
                        for cb in range(4):
                            bk = nbank(0, 4)
                            for jj in range(4):
                                c = cb * 4 + jj
                                PE_T(psb[bk][:, jj * P:(jj + 1) * P], xg[sl][:, h, c::16], P, [("xg", sl, h), "ident"], [pk(bk)], inc=(jj == 3))
                            evac_copy(xT[sl][:, cb * 4:(cb + 1) * 4, h * P:(h + 1) * P],
                                      psb[bk][:, :].rearrange("p (j s) -> p j s", j=4), bk, [("xT", sl, h, cb)])
                    xk_ = [("xT", sl, h, cb) for h in range(2) for cb in range(4)]
                    if nxt is not None:
                        Lx(nxt, sl)
                    bgs = [nbank(4, 8) for _ in range(2)]
                    bus = [nbank(4, 8) for _ in range(2)]
                    for h in range(2):
                        xkh = [("xT", sl, h, cb) for cb in range(4)]
                        for c in range(16):
                            PE_mm(psb[bgs[h]][:, :], xT[sl][:, c, h * P:(h + 1) * P], wgb[sl][:, c, :], c == 0, c == 15,
                                  ["wgb%d" % sl] + xkh, [pk(bgs[h])], inc=(c == 15))
                    if nxt is not None:
                        Lwg(nxt, sl)
                    for h in range(2):
                        xkh = [("xT", sl, h, cb) for cb in range(4)]
                        for c in range(16):
                            PE_mm(psb[bus[h]][:, :], xT[sl][:, c, h * P:(h + 1) * P], wub[sl][:, c, :], c == 0, c == 15,
                                  ["wub%d" % sl] + xkh, [pk(bus[h])], inc=(c == 15))
                    if nxt is not None:
                        Lwu(nxt, sl)
                    for h in range(2):
                        bg, bu = bgs[h], bus[h]
                        s2 = h
                        A_act(sgm[s2][:], psb[bg][:, :], AF.Silu, rk=[pk(bg)], wk=["sgm%d" % s2])
                        V_tt(sgm[s2][:], sgm[s2][:], psb[bu][:, :], ALU.mult, rk=["sgm%d" % s2, pk(bu)], wk=["sgm%d" % s2])
                        bt = nbank(0, 4)
                        for fc in range(4):
                            PE_T(psb[bt][:, fc * P:(fc + 1) * P], sgm[s2][:, fc::4], P, ["sgm%d" % s2, "ident"], [pk(bt)], inc=(fc == 3))
                        evac_copy(hidb[sl][:, :, h * P:(h + 1) * P], psb[bt][:, :].rearrange("p (f s) -> p f s", f=4), bt,
                                  [("hidb", sl, fc_, h) for fc_ in range(4)])
                    for h in range(2):
                        ys = ycnt_[0] % 2
                        ycnt_[0] += 1
                        for db in range(4):
                            bk = nbank(0, 4)
                            for fc in range(4):
                                PE_mm(psb[bk][:, :], hidb[sl][:, fc, h * P:(h + 1) * P], wdb[sl][:, fc, db * 512:(db + 1) * 512],
                                      fc == 0, fc == 3, ["wdb%d" % sl, ("hidb", sl, fc, h)], [pk(bk)], inc=(fc == 3))
                            evac_copy(ysb[ys][:, db * 512:(db + 1) * 512], psb[bk][:, :], bk, [("ysb", ys, db)])
                        r0 = j * TS + h * P
                        if h == 1 and nxt is not None:
                            Lwd(nxt, sl)
                        S.idma(yd, yix[:, 2 * j + h:2 * j + h + 1], ysb[ys][:], None, reads=[("ysb", ys, db_) for db_ in range(4)],
                               sem="yw%d" % ys, bounds=NS - 1)


                NH = 32
                order = []
                for k in range(NH):
                    order.append((k, k % 2))
                    if k < NT - NH:
                        order.append((NT - 1 - k, k % 2))
                Lt(0, 0); Lt(1, 1)
                for i_, (j_, sl) in enumerate(order):
                    nx = [jj for (jj, s_) in order[i_ + 1:] if s_ == sl]
                    Ct(j_, sl, nx[0] if nx else None)
                S.barrier()
            gate(108)
            with ExitStack() as es3:
                xa = [sb(es3, "xa%d" % i, [P, D]) for i in range(2)]
                y1 = [sb(es3, "y1_%d" % i, [P, D]) for i in range(2)]
                y2 = [sb(es3, "y2_%d" % i, [P, D]) for i in range(2)]
                for tt in range(16):
                    sl = tt % 2
                    r0 = tt * P
                    S.dma("sp", xa[sl][:], x2d[r0:r0 + P, :], writes=["xa%d" % sl], sem="xa%d" % sl)
                    S.idma(y1[sl][:], None, yd, s1i[:, tt:tt + 1], reads=[], writes=["y1_%d" % sl], sem="y1_%d" % sl)
                    S.idma(y2[sl][:], None, yd, s2i[:, tt:tt + 1], reads=[], writes=["y2_%d" % sl], sem="y2_%d" % sl)
                    V_stt(xa[sl][:], y1[sl][:], comb_g1[:, tt:tt + 1], xa[sl][:], ALU.mult, ALU.add,
                          rk=["y1_%d" % sl, "xa%d" % sl], wk=["xa%d" % sl])
                    V_stt(xa[sl][:], y2[sl][:], comb_g2[:, tt:tt + 1], xa[sl][:], ALU.mult, ALU.add,
                          rk=["y2_%d" % sl, "xa%d" % sl], wk=["xa%d" % sl])
                    S.dma("sp", out[r0:r0 + P, :], xa[sl][:], reads=["xa%d" % sl], sem="outd%d" % sl)
                S.barrier()

        S.barrier()
        import os
        if os.environ.get("KDEBUG"):
            print("sched counts", S.cnt, {k: v[1] for k, v in S.dsem.items()})
    return nc


_INPUT_LAYOUT = {
    "x": None,
}


def _prep_inputs(inputs, b):
    g = lambda k: np.ascontiguousarray(np.asarray(inputs[k], dtype=np.float32))
    m = {
        "x": g("x")[b],
        "attn_norm_g": g("attn_norm_g")[0].reshape(16, 128),
        "w_in": g("w_in")[0],
        "lambda_re": g("lambda_re")[0].reshape(32, 128),
        "lambda_im": g("lambda_im")[0].reshape(32, 128),
        "log_dt": g("log_dt")[0].reshape(32, 2),
        "ssm_b_re": g("ssm_b_re")[0].reshape(32, 2048),
        "ssm_b_im": g("ssm_b_im")[0].reshape(32, 2048),
        "ssm_c_re": g("ssm_c_re")[0].reshape(32, 2048),
        "ssm_c_im": g("ssm_c_im")[0].reshape(32, 2048),
        "ssm_d": g("ssm_d")[0].reshape(8, 128),
        "w_glu": g("w_glu")[0],
        "q_norm_g": g("q_norm_g")[0].reshape(1, 128),
        "k_norm_g": g("k_norm_g")[0].reshape(1, 128),
        "w_branch_ssm": g("w_branch_ssm")[0],
        "w_branch_att": g("w_branch_att")[0],
        "w_out": g("w_out")[0],
        "ffn_norm_g": g("ffn_norm_g")[0].reshape(16, 128),
        "ffn_norm_g_row": g("ffn_norm_g")[0].reshape(1, D),
        "router_group_w": g("router_group_w")[0],
        "router_group_b": g("router_group_b")[0].reshape(1, 4),
        "router_expert_w": g("router_expert_w")[0],
        "router_expert_b": g("router_expert_b")[0].reshape(1, 32),
        "expert_w_gate": g("expert_w_gate")[0],
        "expert_w_up": g("expert_w_up")[0],
        "expert_w_down": g("expert_w_down")[0],
    }
    return m


def kernel(**inputs):
    nc = build()
    shared = _prep_inputs(inputs, 0)
    xs = np.asarray(inputs["x"], dtype=np.float32)
    in_maps = []
    for b in range(8):
        m = dict(shared)
        m["x"] = np.ascontiguousarray(xs[b])
        in_maps.append(m)
    res = run_bass_kernel_spmd(nc, in_maps, core_ids=list(range(8)))
    return np.stack([np.asarray(r["out"], dtype=np.float32) for r in res.results], axis=0)
```
